# Optimizing a Trainium2 kernel written in Bass

```python
import math
import jax, jax.numpy as jnp
from jax import lax
import numpy as np

D_MODEL = 1024
BATCH = 2
SEQ = 8192
DEPTH = 1

GRID_W = 64
CTX_LEN = 256
GLA_HEADS = 4
GLA_DK = 64
GLA_DV = 128
GLA_RANK = 16
GLA_NORMALIZER = 16.0
GLA_CHUNK = 64
ATT_HEADS = 8
ATT_KV_HEADS = 2
ATT_DH = 64
WINDOW = 128
ATT_BLOCK = 128
ROPE_THETA = 10000.0
PEER_HEADS = 8
N_KEYS = 128
N_EXPERTS = N_KEYS * N_KEYS
PEER_DQ = 128
PEER_TOPK = 16
PEER_BLOCK = 128
GLA_WIDTH = GLA_HEADS * GLA_DV
ATT_WIDTH = ATT_HEADS * ATT_DH
MIX_WIDTH = GLA_WIDTH + ATT_WIDTH
N_MOD = 6
LN_EPS = 1e-5
NEG_INF = -1e30
SPLIT_SIZES = (GLA_HEADS * GLA_DK, GLA_HEADS * GLA_DK, GLA_WIDTH, GLA_WIDTH, GLA_RANK, GLA_RANK,
               ATT_WIDTH, ATT_KV_HEADS * ATT_DH, ATT_KV_HEADS * ATT_DH)
SPLIT_POINTS = tuple(sum(SPLIT_SIZES[:i + 1]) for i in range(len(SPLIT_SIZES) - 1))
IN_WIDTH = sum(SPLIT_SIZES)

kernel_name = "hybrid_gla_swa_peer_dit_block"


def _layernorm(x, g, b):
    xf = x.astype(jnp.float32)
    mu = jnp.mean(xf, axis=-1, keepdims=True)
    var = jnp.mean(jnp.square(xf - mu), axis=-1, keepdims=True)
    y = (xf - mu) * lax.rsqrt(var + LN_EPS) * g.astype(jnp.float32) + b.astype(jnp.float32)
    return y.astype(x.dtype)


def _axial_rope(n_tokens):
    rows = n_tokens // GRID_W
    row = jnp.repeat(jnp.arange(rows, dtype=jnp.float32), GRID_W)
    col = jnp.tile(jnp.arange(GRID_W, dtype=jnp.float32), rows)
    n_freq = ATT_DH // 4
    inv = ROPE_THETA ** (-jnp.arange(n_freq, dtype=jnp.float32) / n_freq)
    ang = jnp.stack([row[:, None] * inv, col[:, None] * inv], axis=1)
    return jnp.cos(ang), jnp.sin(ang)


def _apply_rope(x, cos, sin):
    B, T, H, _ = x.shape
    xr = x.astype(jnp.float32).reshape(B, T, H, 2, 2, ATT_DH // 4)
    x1, x2 = xr[..., 0, :], xr[..., 1, :]
    cs, sn = cos[None, :, None], sin[None, :, None]
    out = jnp.stack([x1 * cs - x2 * sn, x2 * cs + x1 * sn], axis=-2)
    return out.reshape(B, T, H, ATT_DH).astype(x.dtype)


def _gla_prepare(parts, w_gate2_f, b_gate_f, w_gate2_b, b_gate_b):
    q, k, v, r, zf, zb = parts[:6]
    B, T = q.shape[:2]

    def heads(a, d):
        return a.reshape(B, T, GLA_HEADS, d).transpose(0, 2, 1, 3).astype(jnp.float32)

    def decay(z, w2, b):
        return heads(jax.nn.log_sigmoid((z @ w2 + b).astype(jnp.float32)) / GLA_NORMALIZER, GLA_DK)

    return (heads(q, GLA_DK) * (GLA_DK ** -0.5), heads(k, GLA_DK), heads(v, GLA_DV),
            decay(zf, w_gate2_f, b_gate_f), decay(zb, w_gate2_b, b_gate_b), r)


def _gla_chunked(q, k, v, g, s0):
    B, H, T, dk = q.shape
    dv = v.shape[-1]
    n = T // GLA_CHUNK

    def to_chunks(a):
        return a.reshape(B, H, n, GLA_CHUNK, a.shape[-1]).transpose(2, 0, 1, 3, 4)

    tril = jnp.tril(jnp.ones((GLA_CHUNK, GLA_CHUNK), dtype=bool))

    def step(S, inp):
        qi, ki, vi, gi = inp
        b = jnp.cumsum(gi, axis=-2)
        qe = qi * jnp.exp(b)
        ke = ki * jnp.exp(-b)
        a = jnp.where(tril, jnp.einsum('bhcd,bhsd->bhcs', qe, ke), 0.0)
        o = jnp.einsum('bhcs,bhsv->bhcv', a, vi) + jnp.einsum('bhcd,bhdv->bhcv', qe, S)
        b_last = b[..., -1:, :]
        kd = ki * jnp.exp(b_last - b)
        S_new = jnp.exp(b_last)[..., 0, :, None] * S + jnp.einsum('bhsd,bhsv->bhdv', kd, vi)
        return S_new, o

    S, o = lax.scan(step, s0, (to_chunks(q), to_chunks(k), to_chunks(v), to_chunks(g)))
    return o.transpose(1, 2, 0, 3, 4).reshape(B, H, T, dv), S


def _gla_bidirectional(q, k, v, g_f, g_b, s0_f, s0_b):
    o_f, s_f = _gla_chunked(q, k, v, g_f, s0_f)
    flip = lambda a: jnp.flip(a, axis=2)
    o_b, s_b = _gla_chunked(flip(q), flip(k), flip(v), flip(g_b), s0_b)
    return o_f + flip(o_b), s_f, s_b


def _gla_output(o, r, gla_norm_g):
    B, H, T, dv = o.shape
    o = o * lax.rsqrt(jnp.mean(jnp.square(o), axis=-1, keepdims=True) + LN_EPS) * gla_norm_g.astype(jnp.float32)
    o = o.transpose(0, 2, 1, 3).reshape(B, T, H * dv)
    return o.astype(r.dtype) * jax.nn.silu(r)


def _window_attention(q, k, v, kc, vc, sink):
    B, T, H, dh = q.shape
    L = kc.shape[1]
    KV = ATT_KV_HEADS
    G = H // KV
    BLK = ATT_BLOCK
    nb = T // BLK
    scale = dh ** -0.5
    qb = q.reshape(B, nb, BLK, KV, G, dh)

    def band(a):
        ap = jnp.pad(a, ((0, 0), (BLK, BLK), (0, 0), (0, 0)))
        return jnp.concatenate([ap[:, o:o + T].reshape(B, nb, BLK, KV, dh) for o in (0, BLK, 2 * BLK)], axis=2)

    kb, vb = band(k), band(v)
    qi = jnp.arange(BLK)[:, None]
    sj = jnp.arange(3 * BLK)[None, :]
    key_pos = jnp.arange(nb)[:, None, None] * BLK - BLK + sj[None]
    valid = (jnp.abs(sj - BLK - qi)[None] <= WINDOW) & (key_pos >= 0) & (key_pos < T)
    s_loc = jnp.einsum('bnqkgd,bnskd->bnkgqs', qb, kb).astype(jnp.float32) * scale
    s_loc = jnp.where(valid[None, :, None, None], s_loc, NEG_INF)
    s_ctx = jnp.einsum('bnqkgd,blkd->bnkgql', qb, kc).astype(jnp.float32) * scale
    s_sink = jnp.broadcast_to(sink.astype(jnp.float32).reshape(KV, G)[None, None, :, :, None, None],
                              s_loc.shape[:-1] + (1,))
    p = jax.nn.softmax(jnp.concatenate([s_loc, s_ctx, s_sink], axis=-1), axis=-1)
    p_loc = p[..., :3 * BLK].astype(v.dtype)
    p_ctx = p[..., 3 * BLK:3 * BLK + L].astype(v.dtype)
    o = jnp.einsum('bnkgqs,bnskd->bnqkgd', p_loc, vb) + jnp.einsum('bnkgql,blkd->bnqkgd', p_ctx, vc)
    return o.reshape(B, T, H * dh)


def _ctx_attention(qc, kc, vc, sink):
    B, L, H, dh = qc.shape
    KV = ATT_KV_HEADS
    G = H // KV
    qg = qc.reshape(B, L, KV, G, dh)
    s = jnp.einsum('blkgd,bmkd->bkglm', qg, kc).astype(jnp.float32) * (dh ** -0.5)
    s_sink = jnp.broadcast_to(sink.astype(jnp.float32).reshape(KV, G)[None, :, :, None, None], s.shape[:-1] + (1,))
    p = jax.nn.softmax(jnp.concatenate([s, s_sink], axis=-1), axis=-1)[..., :L]
    o = jnp.einsum('bkglm,bmkd->blkgd', p.astype(vc.dtype), vc)
    return o.reshape(B, L, H * dh)


def _mixer(h, hc, w_in, w_gate2_f, b_gate_f, w_gate2_b, b_gate_b, gla_norm_g, attn_sink, w_out,
           cos, sin, with_ctx_out):
    B, T, _ = h.shape
    L = hc.shape[1]
    lat = jnp.split(h @ w_in, SPLIT_POINTS, axis=-1)
    con = jnp.split(hc @ w_in, SPLIT_POINTS, axis=-1)
    ql, kl, vl, gfl, gbl, rl = _gla_prepare(lat, w_gate2_f, b_gate_f, w_gate2_b, b_gate_b)
    qc, kc, vc, gfc, gbc, rc = _gla_prepare(con, w_gate2_f, b_gate_f, w_gate2_b, b_gate_b)
    zeros = jnp.zeros((B, GLA_HEADS, GLA_DK, GLA_DV), jnp.float32)
    o_c, s_f, s_b = _gla_bidirectional(qc, kc, vc, gfc, gbc, zeros, zeros)
    o_l, _, _ = _gla_bidirectional(ql, kl, vl, gfl, gbl, s_f, s_b)
    gla_lat = _gla_output(o_l, rl, gla_norm_g)
    aq = _apply_rope(lat[6].reshape(B, T, ATT_HEADS, ATT_DH), cos, sin)
    ak = _apply_rope(lat[7].reshape(B, T, ATT_KV_HEADS, ATT_DH), cos, sin)
    av = lat[8].reshape(B, T, ATT_KV_HEADS, ATT_DH)
    cq = con[6].reshape(B, L, ATT_HEADS, ATT_DH)
    ck = con[7].reshape(B, L, ATT_KV_HEADS, ATT_DH)
    cv = con[8].reshape(B, L, ATT_KV_HEADS, ATT_DH)
    att_lat = _window_attention(aq, ak, av, ck, cv, attn_sink)
    y_lat = jnp.concatenate([gla_lat, att_lat], axis=-1) @ w_out
    y_ctx = None
    if with_ctx_out:
        gla_ctx = _gla_output(o_c, rc, gla_norm_g)
        att_ctx = _ctx_attention(cq, ck, cv, attn_sink)
        y_ctx = jnp.concatenate([gla_ctx, att_ctx], axis=-1) @ w_out
    return y_lat, y_ctx


def _peer(h, peer_wq, peer_subkeys, peer_u, peer_v):
    B, T, D = h.shape
    xb = h.reshape(-1, PEER_BLOCK, D)

    def block(xt):
        P = xt.shape[0]
        qh = (xt @ peer_wq).reshape(P, PEER_HEADS, 2, PEER_DQ // 2)
        s = jnp.einsum('phad,hand->phan', qh, peer_subkeys).astype(jnp.float32)
        s1, i1 = lax.top_k(s[:, :, 0], PEER_TOPK)
        s2, i2 = lax.top_k(s[:, :, 1], PEER_TOPK)
        cand = (s1[..., :, None] + s2[..., None, :]).reshape(P, PEER_HEADS, PEER_TOPK * PEER_TOPK)
        cidx = (i1[..., :, None] * N_KEYS + i2[..., None, :]).reshape(P, PEER_HEADS, PEER_TOPK * PEER_TOPK)
        top_s, pos = lax.top_k(cand, PEER_TOPK)
        eidx = jnp.take_along_axis(cidx, pos, axis=-1)
        gate = jax.nn.softmax(top_s, axis=-1)
        u = peer_u[eidx]
        v = peer_v[eidx]
        act = jax.nn.gelu(jnp.einsum('pd,phkd->phk', xt, u).astype(jnp.float32), approximate=False)
        return jnp.einsum('phk,phkd->pd', (gate * act).astype(v.dtype), v)

    return lax.map(block, xb).reshape(B, T, D)


def setup_inputs(seed: int = 0) -> dict:
    key = jax.random.key(seed)
    ks = jax.random.split(key, 24)
    D = D_MODEL
    beta = (8.0 * DEPTH) ** -0.25
    nrm = lambda k, shape: jax.random.normal(k, shape, jnp.float32)
    col_scale = jnp.ones((IN_WIDTH,), jnp.float32)
    col_scale = col_scale.at[SPLIT_POINTS[1]:SPLIT_POINTS[2]].set(beta)
    col_scale = col_scale.at[SPLIT_POINTS[7]:].set(beta)
    return {
        "x": nrm(ks[0], (BATCH, SEQ, D)),
        "c": nrm(ks[1], (BATCH, D)),
        "ctx": nrm(ks[2], (BATCH, CTX_LEN, D)),
        "c_ctx": nrm(ks[3], (D,)),
        "w_ada": nrm(ks[4], (DEPTH, D, N_MOD * D)) * D ** -0.5,
        "b_ada": nrm(ks[5], (DEPTH, N_MOD * D)) * 0.02,
        "w_in": nrm(ks[6], (DEPTH, D, IN_WIDTH)) * D ** -0.5 * col_scale,
        "w_gate2_f": nrm(ks[7], (DEPTH, GLA_RANK, GLA_HEADS * GLA_DK)) * GLA_RANK ** -0.5,
        "b_gate_f": nrm(ks[8], (DEPTH, GLA_HEADS * GLA_DK)) * 0.02,
        "w_gate2_b": nrm(ks[9], (DEPTH, GLA_RANK, GLA_HEADS * GLA_DK)) * GLA_RANK ** -0.5,
        "b_gate_b": nrm(ks[10], (DEPTH, GLA_HEADS * GLA_DK)) * 0.02,
        "gla_norm_g": 1.0 + 0.01 * nrm(ks[11], (DEPTH, GLA_DV)),
        "attn_sink": nrm(ks[12], (DEPTH, ATT_HEADS)),
        "w_out": nrm(ks[13], (DEPTH, MIX_WIDTH, D)) * MIX_WIDTH ** -0.5 * beta,
        "ln1_g": 1.0 + 0.01 * nrm(ks[14], (DEPTH, D)),
        "ln1_b": 0.01 * nrm(ks[15], (DEPTH, D)),
        "peer_wq": nrm(ks[16], (DEPTH, D, PEER_HEADS * PEER_DQ)) * D ** -0.5,
        "peer_subkeys": nrm(ks[17], (DEPTH, PEER_HEADS, 2, N_KEYS, PEER_DQ // 2)) * (PEER_DQ // 2) ** -0.5,
        "peer_u": nrm(ks[18], (DEPTH, N_EXPERTS, D)) * D ** -0.5,
        "peer_v": nrm(ks[19], (DEPTH, N_EXPERTS, D)) * 0.5 * beta,
        "ln2_g": 1.0 + 0.01 * nrm(ks[20], (DEPTH, D)),
        "ln2_b": 0.01 * nrm(ks[21], (DEPTH, D)),
    }


def reference(x, c, ctx, c_ctx, w_ada, b_ada, w_in, w_gate2_f, b_gate_f, w_gate2_b, b_gate_b,
              gla_norm_g, attn_sink, w_out, ln1_g, ln1_b, peer_wq, peer_subkeys, peer_u, peer_v,
              ln2_g, ln2_b):
    B, T, D = x.shape
    alpha = (2.0 * DEPTH) ** 0.25
    cos, sin = _axial_rope(T)
    silu_c = jax.nn.silu(c)
    silu_cc = jax.nn.silu(c_ctx)
    for i in range(DEPTH):
        last = i == DEPTH - 1
        mod = (silu_c @ w_ada[i] + b_ada[i]).reshape(B, N_MOD, 1, D)
        modc = (silu_cc @ w_ada[i] + b_ada[i]).reshape(N_MOD, D)
        sh1, sc1, g1, sh2, sc2, g2 = [mod[:, j] for j in range(N_MOD)]
        csh1, csc1, cg1, csh2, csc2, cg2 = [modc[j] for j in range(N_MOD)]
        h = x * (1.0 + sc1) + sh1
        hc = ctx * (1.0 + csc1) + csh1
        y, yc = _mixer(h, hc, w_in[i], w_gate2_f[i], b_gate_f[i], w_gate2_b[i], b_gate_b[i],
                       gla_norm_g[i], attn_sink[i], w_out[i], cos, sin, not last)
        x = _layernorm(alpha * x + g1 * y, ln1_g[i], ln1_b[i])
        x = _layernorm(alpha * x + g2 * _peer(x * (1.0 + sc2) + sh2, peer_wq[i], peer_subkeys[i],
                                              peer_u[i], peer_v[i]), ln2_g[i], ln2_b[i])
        if not last:
            ctx = _layernorm(alpha * ctx + cg1 * yc, ln1_g[i], ln1_b[i])
            ctx = _layernorm(alpha * ctx + cg2 * _peer(ctx * (1.0 + csc2) + csh2, peer_wq[i], peer_subkeys[i],
                                                       peer_u[i], peer_v[i]), ln2_g[i], ln2_b[i])
    return x
```

```python
import os
import numpy as np
from contextlib import ExitStack
import concourse.bass as bass
import concourse.mybir as mybir
from concourse.bass_utils import run_bass_kernel_spmd

F32 = mybir.dt.float32
BF16 = mybir.dt.bfloat16
U32 = mybir.dt.uint32
AF = mybir.ActivationFunctionType
ALU = mybir.AluOpType
AX = mybir.AxisListType

SAME_ENGINE_SYNC = True
NPRE = 48
LN_EPS = 1e-5
ALPHA = 2.0 ** 0.25
NG = 12
LAG = 3


class Res:
    __slots__ = ("w", "rs")

    def __init__(self):
        self.w = None
        self.rs = {}


class Buf:
    def __init__(self, t):
        self.t = t
        self.r = Res()


class Sched:
    def __init__(self, nc, stack):
        self.nc = nc
        self.stack = stack
        self.sems = {}
        self.count = {}
        self.seen = {k: {} for k in ("pe", "dve", "act", "pool", "sp")}
        self.streams = {k: [] for k in ("pe", "dve", "act", "pool", "sp")}
        for k in ("pe", "dve", "act", "pool"):
            self.sems[k] = stack.enter_context(nc.semaphore("sem_" + k))
            self.count[k] = 0
        self.nslots = 0

    def dma_slot(self, name=""):
        self.nslots += 1
        key = "d%d%s" % (self.nslots, name)
        self.sems[key] = self.stack.enter_context(self.nc.semaphore("s_" + key))
        self.count[key] = 0
        return key

    def _waits(self, q, reads, writes, same_ok):
        deps = {}
        for b in reads:
            r = b.r
            if r.w is not None:
                k, c = r.w
                deps[k] = max(deps.get(k, 0), c)
        for b in writes:
            w = b.r
            if w.w is not None:
                k, c = w.w
                deps[k] = max(deps.get(k, 0), c)
            for k, c in w.rs.items():
                deps[k] = max(deps.get(k, 0), c)
        out = []
        for k, c in deps.items():
            if k == q and not same_ok:
                continue
            if self.seen[q].get(k, 0) >= c:
                continue
            self.seen[q][k] = c
            out.append((k, c))
        return out

    def op(self, q, fn, r=(), w=()):
        same_ok = SAME_ENGINE_SYNC and q != "pe"
        waits = self._waits(q, r, w, same_ok)
        self.count[q] += 1
        c = self.count[q]
        sems = self.sems
        st = self.streams[q]
        for k, v in waits:
            st.append(lambda e, k=k, v=v: e.wait_ge(sems[k], v))
        st.append(lambda e, fn=fn: fn(e).then_inc(sems[q], 1))
        for b in r:
            b.r.rs[q] = c
        for b in w:
            b.r.w = (q, c)
            b.r.rs = {}

    def dma(self, q, slot, fn, r=(), w=()):
        waits = self._waits(q, r, w, True)
        prev = self.count[slot]
        if prev > 0 and self.seen[q].get(slot, 0) < prev:
            self.seen[q][slot] = prev
            waits.append((slot, prev))
        self.count[slot] += 16
        c = self.count[slot]
        sems = self.sems
        st = self.streams[q]
        for k, v in waits:
            st.append(lambda e, k=k, v=v: e.wait_ge(sems[k], v))
        st.append(lambda e, fn=fn: fn(e).then_inc(sems[slot], 16))
        for b in r:
            b.r.rs[slot] = c
        for b in w:
            b.r.w = (slot, c)
            b.r.rs = {}

    def wait_all(self, q):
        sems = self.sems
        for k in list(self.count.keys()):
            c = self.count[k]
            if c == 0 or k == q or self.seen[q].get(k, 0) >= c:
                continue
            self.seen[q][k] = c
            self.streams[q].append(lambda e, k=k, c=c: e.wait_ge(sems[k], c))

    def barrier(self):
        for q in ("pe", "dve", "act", "pool", "sp"):
            self.wait_all(q)

    def run(self):
        streams = self.streams
        with self.nc.Block() as block:
            @block.tensor
            def _(e):
                for f in streams["pe"]:
                    f(e)

            @block.vector
            def _(e):
                for f in streams["dve"]:
                    f(e)

            @block.scalar
            def _(e):
                for f in streams["act"]:
                    f(e)

            @block.gpsimd
            def _(e):
                for f in streams["pool"]:
                    f(e)

            @block.sync
            def _(e):
                for f in streams["sp"]:
                    f(e)


def build_nc(mode="full"):
    nc = bass.Bass("TRN2", target_bir_lowering=False)
    D = lambda name, shape, dt=F32, kind="ExternalInput": nc.dram_tensor(name, shape, dt, kind=kind).ap()
    d_xpf = D("xpre_f", [NPRE * 128, 1024]); d_xpb = D("xpre_b", [NPRE * 128, 1024])
    d_xown = D("xown", [18 * 128, 1024]); d_ctx = D("ctx", [256, 1024])
    d_flags = D("flags", [128, 100]); d_c2T = D("c2T", [128, 8, 2])
    d_wada = D("w_ada", [1024, 6144]); d_bada = D("b_ada", [1, 6144])
    d_win = D("w_in", [1024, 2336]); d_w2 = D("w2", [16, 512]); d_bg = D("bg", [1, 512])
    d_gng = D("gng", [128, 128]); d_sink = D("sink", [128, 8])
    d_wout = D("w_out", [1024, 1024])
    d_ln1g = D("ln1g", [128, 1024]); d_ln1b = D("ln1b", [128, 1024])
    d_ln2g = D("ln2g", [128, 1024]); d_ln2b = D("ln2b", [128, 1024])
    d_wq = D("peer_wq", [1024, 1024]); d_skT = D("skT", [128, 8, 128])
    d_puv = D("peer_uv", [16384, 2048])
    d_puv16 = D("puv16", [16384, 2048], BF16, kind="Internal")
    d_cos = D("cosT", [18 * 128, 32]); d_sin = D("sinT", [18 * 128, 32])
    d_ident = D("ident", [128, 128]); d_tri = D("tri", [128, 4, 128]); d_ci = D("ci", [128, 2])
    d_amask = D("amask", [128, 2, 128]); d_gmask = D("gmask", [128, 2, 64]); d_iota = D("iota16", [128, 16])
    d_out = D("out", [2048, 1024], kind="ExternalOutput")
    if mode == "B":
        d_x1 = D("x1s", [2048, 1024])
    else:
        d_x1 = D("x1s", [2048, 1024], kind="ExternalOutput" if mode == "A" else "Internal")

    with ExitStack() as top:
        S = Sched(nc, top)
        OP = S.op

        def sbuf(st, name, shape, dt=F32):
            return Buf(st.enter_context(nc.sbuf_tensor("s_" + name, shape, dt)))

        banks = [Buf(top.enter_context(nc.psum_tensor("pb%d" % i, [128, 512], F32))) for i in range(6)]
        accP = [Buf(top.enter_context(nc.psum_tensor("pacc%d" % i, [128, 512], F32))) for i in range(2)]
        bank_i = [0]

        def bank():
            b = banks[bank_i[0] % 6]
            bank_i[0] += 1
            return b

        ld = [S.dma_slot("ld%d" % i) for i in range(2)]
        cst = S.dma_slot("cst")
        sts = S.dma_slot("st")
        csl = S.dma_slot("cs"); snl = S.dma_slot("sn")

        puvB = [Buf(None) for _ in range(16)]
        cvs = [S.dma_slot("cv%d" % i) for i in range(4)]

        def convert_tables():
            for ci_ in range(16):
                S.dma("pool", cvs[ci_ % 4], lambda e, ci_=ci_: e.dma_start(out=d_puv16[ci_ * 1024:(ci_ + 1) * 1024, :],
                                                                         in_=d_puv[ci_ * 1024:(ci_ + 1) * 1024, :]), w=[puvB[ci_]])
        ident = sbuf(top, "ident", [128, 128])
        S.dma("sp", cst, lambda e: e.dma_start(out=ident.t[:], in_=d_ident), w=[ident])
        flags = sbuf(top, "flags", [128, 100])
        S.dma("sp", cst, lambda e: e.dma_start(out=flags.t[:], in_=d_flags), w=[flags])
        eps_t = sbuf(top, "eps_t", [128, 1])
        OP("dve", lambda e: e.memset(eps_t.t[:], LN_EPS), w=[eps_t])
        modT = sbuf(top, "modT", [128, 48, 2])
        sc1p = sbuf(top, "sc1p", [128, 8, 2])
        g1bc = sbuf(top, "g1bc", [128, 1024])
        d_modbc = D("modbc", [3, 128, 1024], kind="Internal")
        xt_i = [0]

        def transpose_to(src, nchunks, dst_fn, width=128, rows=128):
            for c0 in range(0, nchunks, 4):
                pb = bank()
                n = min(4, nchunks - c0)
                for c in range(c0, c0 + n):
                    OP("pe", lambda e, c=c, pb=pb, c0=c0: e.transpose(
                        out=pb.t[0:width, (c - c0) * 128:(c - c0) * 128 + rows],
                        in_=src.t[0:rows, c * width:(c + 1) * width], identity=ident.t[0:rows, 0:rows]),
                       r=[src, ident], w=[pb])
                dst_fn(c0, n, pb)

        if mode != "B":
            with ExitStack() as p0:
                c2T = sbuf(p0, "c2T", [128, 8, 2]); sc2 = sbuf(p0, "sc2", [128, 8, 2])
                S.dma("sp", cst, lambda e: e.dma_start(out=c2T.t[:], in_=d_c2T), w=[c2T])
                OP("act", lambda e: e.activation(out=sc2.t[:], in_=c2T.t[:], func=AF.Silu), r=[c2T], w=[sc2])
                modrow = sbuf(p0, "modrow", [2, 6144])
                brow = sbuf(p0, "brow", [128, 6144]); ones2 = sbuf(p0, "ones2", [128, 2])
                OP("pool", lambda e: e.memset(brow.t[:], 0.0), w=[brow])
                S.dma("sp", cst, lambda e: e.dma_start(out=brow.t[0:1, :], in_=d_bada), w=[brow])
                OP("dve", lambda e: e.memset(ones2.t[:], 0.0), w=[ones2])
                OP("dve", lambda e: e.memset(ones2.t[0:1, :], 1.0), w=[ones2])
                S.barrier()
                wst = [sbuf(p0, "wst%d" % i, [128, 3072]) for i in range(2)]
                wada_v = d_wada.rearrange("(k p) n -> k p n", p=128)
                for half in range(2):
                    pbs = [bank() for _ in range(6)]
                    for j in range(6):
                        col = half * 3072 + j * 512
                        OP("pe", lambda e, pbj=pbs[j], col=col: e.matmul(pbj.t[0:2, :], lhsT=ones2.t[:], rhs=brow.t[:, col:col + 512],
                                                                        start=True, stop=False), r=[ones2, brow], w=[pbs[j]])
                    for kc in range(8):
                        ws = wst[kc % 2]
                        S.dma("sp", ld[kc % 2], lambda e, ws=ws, kc=kc, half=half: e.dma_start(
                            out=ws.t[:], in_=wada_v[kc, :, half * 3072:(half + 1) * 3072]), w=[ws])
                        for j in range(6):
                            OP("pe", lambda e, j=j, ws=ws, kc=kc, pbj=pbs[j]: e.matmul(
                                pbj.t[0:2, :], lhsT=sc2.t[:, kc, :], rhs=ws.t[:, j * 512:(j + 1) * 512],
                                start=False, stop=(kc == 7)), r=[sc2, ws], w=[pbs[j]])
                    for j in range(6):
                        col = half * 3072 + j * 512
                        OP("act", lambda e, pbj=pbs[j], col=col: e.activation(out=modrow.t[:, col:col + 512], in_=pbj.t[0:2, :],
                                                                       func=AF.Identity), r=[pbs[j]], w=[modrow])
                for c0 in range(0, 48, 4):
                    pb = bank()
                    for c in range(c0, c0 + 4):
                        OP("pe", lambda e, c=c, pb=pb, c0=c0: e.transpose(
                            out=pb.t[:, (c - c0) * 2:(c - c0) * 2 + 2], in_=modrow.t[0:2, c * 128:(c + 1) * 128],
                            identity=ident.t[0:2, 0:2]), r=[modrow, ident], w=[pb])
                    OP("dve", lambda e, pb=pb, c0=c0: e.tensor_copy(
                        out=modT.t[:, c0:c0 + 4, :], in_=pb.t[:, 0:8].rearrange("p (a b) -> p a b", a=4)), r=[pb], w=[modT])
                OP("dve", lambda e: e.tensor_scalar(out=sc1p.t[:], in0=modT.t[:, 8:16, :], scalar1=1.0, scalar2=None, op0=ALU.add),
                   r=[modT], w=[sc1p])
                sel = sbuf(p0, "sel", [2, 128])
                OP("dve", lambda e: e.memset(sel.t[:], 0.0), w=[sel])
                OP("dve", lambda e: e.memset(sel.t[0:1, :], 1.0), w=[sel])
                bct = sbuf(p0, "bct", [128, 1024])
                for dst, j, add1, di in ((g1bc, 2, False, None), (bct, 3, False, 0), (bct, 4, True, 1), (bct, 5, False, 2)):
                    for hf in range(2):
                        pb = bank()
                        col = j * 1024 + hf * 512
                        OP("pe", lambda e, pb=pb, col=col: e.matmul(pb.t[:], lhsT=sel.t[:], rhs=modrow.t[:, col:col + 512],
                                                                    start=True, stop=True), r=[sel, modrow], w=[pb])
                        if add1:
                            OP("dve", lambda e, pb=pb, dst=dst, hf=hf: e.tensor_scalar(
                                out=dst.t[:, hf * 512:(hf + 1) * 512], in0=pb.t[:], scalar1=1.0, scalar2=None, op0=ALU.add),
                               r=[pb], w=[dst])
                        else:
                            OP("act", lambda e, pb=pb, dst=dst, hf=hf: e.activation(
                                out=dst.t[:, hf * 512:(hf + 1) * 512], in_=pb.t[:], func=AF.Identity), r=[pb], w=[dst])
                    if di is not None:
                        S.dma("sp", sts, lambda e, di=di: e.dma_start(out=d_modbc[di], in_=bct.t[:]), r=[bct])
                S.barrier()

        if mode != "B":
            with ExitStack() as pa:
                xt = [sbuf(pa, "xt%d" % i, [128, 1024]) for i in range(2)]
                win = sbuf(pa, "win", [128, 8, 2336], BF16)
                wout = sbuf(pa, "wout", [128, 8, 1024], BF16)
                with ExitStack() as pw:
                    wst = [sbuf(pw, "wcst%d" % i, [128, 2336]) for i in range(2)]
                    win_v = d_win.rearrange("(k p) n -> k p n", p=128)
                    wout_v = d_wout.rearrange("(k p) n -> k p n", p=128)
                    for kc in range(8):
                        ws = wst[kc % 2]
                        S.dma("sp", ld[kc % 2], lambda e, ws=ws, kc=kc: e.dma_start(out=ws.t[:], in_=win_v[kc]), w=[ws])
                        OP("pool", lambda e, ws=ws, kc=kc: e.tensor_copy(out=win.t[:, kc, :], in_=ws.t[:]), r=[ws], w=[win])
                    for kc in range(8):
                        ws = wst[kc % 2]
                        S.dma("sp", ld[kc % 2], lambda e, ws=ws, kc=kc: e.dma_start(out=ws.t[:, 0:1024], in_=wout_v[kc]), w=[ws])
                        OP("pool", lambda e, ws=ws, kc=kc: e.tensor_copy(out=wout.t[:, kc, :], in_=ws.t[:, 0:1024]), r=[ws], w=[wout])
                    S.barrier()
                w2 = sbuf(pa, "w2", [128, 512])
                OP("pool", lambda e: e.memset(w2.t[:], 0.0), w=[w2])
                gng = sbuf(pa, "gng", [128, 128]); esink = sbuf(pa, "esink", [128, 8])
                ln1g = sbuf(pa, "ln1g", [128, 1024]); ln1b = sbuf(pa, "ln1b", [128, 1024])
                tri = sbuf(pa, "tri", [128, 4, 128]); ci = sbuf(pa, "ci", [128, 2])
                amask = sbuf(pa, "amask", [128, 2, 128]); gmask = sbuf(pa, "gmask", [128, 2, 64])
                amask_e = sbuf(pa, "amask_e", [128, 2, 128])
                S.dma("sp", cst, lambda e: e.dma_start(out=w2.t[0:16, :], in_=d_w2), w=[w2])
                S.dma("sp", cst, lambda e: e.dma_start(out=w2.t[16:17, :], in_=d_bg), w=[w2])
                for b_, d_ in ((gng, d_gng), (esink, d_sink), (ln1g, d_ln1g), (ln1b, d_ln1b),
                               (tri, d_tri), (ci, d_ci), (amask, d_amask), (gmask, d_gmask)):
                    S.dma("sp", cst, lambda e, b_=b_, d_=d_: e.dma_start(out=b_.t[:], in_=d_), w=[b_])
                S.barrier()
                OP("act", lambda e: e.activation(out=esink.t[:], in_=esink.t[:], func=AF.Exp), r=[esink], w=[esink])
                for m in range(2):
                    OP("dve", lambda e, m=m: e.tensor_scalar(out=amask_e.t[:, m, :], in0=amask.t[:, m, :],
                                                             scalar1=flags.t[:, 96 + m:97 + m], scalar2=None, op0=ALU.mult),
                       r=[amask, flags], w=[amask_e])
                sbst = sbuf(pa, "sbst", [128, 32, 256])
                kT_all = sbuf(pa, "kT_all", [64, 18, 256]); v_all = sbuf(pa, "v_all", [128, 18, 130])
                kTc = sbuf(pa, "kTc", [64, 2, 256]); vc = sbuf(pa, "vc", [128, 2, 130])
                OP("pool", lambda e: e.memset(v_all.t[:], 1.0), w=[v_all])
                OP("pool", lambda e: e.memset(vc.t[:], 1.0), w=[vc])
                hT = sbuf(pa, "hT", [128, 8, 128], BF16)
                qk = sbuf(pa, "qk", [128, 512]); vsb = sbuf(pa, "vsb", [128, 512])
                zT = [sbuf(pa, "zT", [128, 128])] * 2
                OP("dve", lambda e: e.memset(zT[0].t[:], 0.0), w=[zT[0]])
                OP("dve", lambda e: e.memset(zT[0].t[0:17, :], 1.0), w=[zT[0]])
                sp_ = [sbuf(pa, "sp", [128, 256])] * 2
                et = sbuf(pa, "et", [128, 256])
                Eb = [sbuf(pa, "Eb", [128, 256])] * 2
                Ei = [sbuf(pa, "Ei", [128, 256])] * 2
                Er = [sbuf(pa, "Er", [128, 256])] * 2
                qe = [sbuf(pa, "qe", [128, 256])] * 2
                ke = [sbuf(pa, "ke", [128, 256])] * 2
                kd = [sbuf(pa, "kd", [128, 256])] * 2
                Tz = [sbuf(pa, "Tz%d" % i, [128, 4, 128]) for i in range(2)]
                keT = [sbuf(pa, "keT%d" % i, [128, 2, 128]) for i in range(2)]
                ATz = sbuf(pa, "ATz", [128, 2, 4, 2, 64])
                for b_ in (Tz[0], Tz[1], ATz):
                    OP("pool", lambda e, b_=b_: e.memset(b_.t[:], 0.0), w=[b_])
                asb = [sbuf(pa, "asb", [128, 2, 2])] * 2
                Sst = {0: [sbuf(pa, "Sf%d" % i, [128, 2, 128]) for i in range(2)],
                       1: [sbuf(pa, "Sb%d" % i, [128, 2, 128]) for i in range(2)]}
                Scur = {0: 0, 1: 0}
                Stmp = sbuf(pa, "Stmp", [128, 2, 128])
                for dd in range(2):
                    OP("dve", lambda e, dd=dd: e.memset(Sst[dd][0].t[:], 0.0), w=[Sst[dd][0]])
                rsb = sbuf(pa, "rsb", [128, 512])
                ss = sbuf(pa, "ss", [128, 4]); rstd4 = sbuf(pa, "rstd4", [128, 4])
                on = sbuf(pa, "on", [128, 512]); junk = on
                aqs = on; qrot = sbuf(pa, "qrot", [128, 512]); rt = rsb
                akv = sbuf(pa, "akv", [128, 256]); krot = sbuf(pa, "krot", [128, 128])
                cs = sbuf(pa, "cs", [128, 32]); sn = sbuf(pa, "sn", [128, 32])
                qTs = sbuf(pa, "qTs", [64, 8, 128])
                Pall = sbuf(pa, "Pall", [128, 5, 512])
                den = sbuf(pa, "den", [128, 4]); cat = sbuf(pa, "cat", [128, 1024])
                catT = hT
                r1 = cat; x1o = cat
                stats = sbuf(pa, "stats", [128, 12]); mv = sbuf(pa, "mv", [128, 2]); rs1 = sbuf(pa, "rs1", [128, 1])

                def front(src_ap, row):
                    xb = xt[xt_i[0] % 2]
                    slot = ld[xt_i[0] % 2]
                    xt_i[0] += 1
                    S.dma("sp", slot, lambda e: e.dma_start(out=xb.t[:], in_=src_ap), w=[xb])

                    def evac(c0, n, pb):
                        for c in range(c0, c0 + n):
                            OP("act", lambda e, c=c, pb=pb, c0=c0: e.activation(
                                out=hT.t[:, c, :], in_=pb.t[:, (c - c0) * 128:(c - c0 + 1) * 128], func=AF.Identity,
                                scale=sc1p.t[:, c, row:row + 1], bias=modT.t[:, c, row:row + 1]),
                               r=[pb, sc1p, modT], w=[hT])
                    transpose_to(xb, 8, evac)
                    return xb

                def inproj(col0, ncols, pb, pcol=0):
                    for kc in range(8):
                        OP("pe", lambda e, kc=kc: e.matmul(pb.t[:, pcol:pcol + ncols], lhsT=hT.t[:, kc, :],
                                                           rhs=win.t[:, kc, col0:col0 + ncols], start=(kc == 0), stop=(kc == 7)),
                           r=[hT, win], w=[pb])

                def gates(dd, flagcol=None):
                    pz = bank()
                    for kc in range(8):
                        OP("pe", lambda e, kc=kc: e.matmul(pz.t[0:16, 0:128], lhsT=win.t[:, kc, 2304 + 16 * dd:2320 + 16 * dd],
                                                           rhs=hT.t[:, kc, :], start=(kc == 0), stop=(kc == 7)), r=[hT, win], w=[pz])
                    OP("act", lambda e: e.activation(out=zT[dd].t[0:16, :], in_=pz.t[0:16, 0:128], func=AF.Identity), r=[pz], w=[zT[dd]])
                    pg = bank()
                    OP("pe", lambda e: e.matmul(pg.t[:, 0:256], lhsT=zT[dd].t[:], rhs=w2.t[:, dd * 256:(dd + 1) * 256],
                                                start=True, stop=True), r=[zT[dd], w2], w=[pg])
                    OP("act", lambda e: e.activation(out=et.t[:], in_=pg.t[:, 0:256], func=AF.Exp, scale=-1.0), r=[pg], w=[et])
                    OP("act", lambda e: e.activation(out=sp_[dd].t[:], in_=et.t[:], func=AF.Ln, bias=1.0), r=[et], w=[sp_[dd]])
                    if flagcol is not None:
                        OP("dve", lambda e: e.tensor_scalar(out=sp_[dd].t[:], in0=sp_[dd].t[:], scalar1=flags.t[:, flagcol:flagcol + 1],
                                                            scalar2=None, op0=ALU.mult), r=[sp_[dd], flags], w=[sp_[dd]])

                def decay_k(dd, ksrc, flagcol=None):
                    pr = bank()
                    OP("pe", lambda e: e.matmul(pr.t[:, 0:256], lhsT=tri.t[:, 1 + 2 * dd, :], rhs=sp_[dd].t[:], start=True, stop=True),
                       r=[tri, sp_[dd]], w=[pr])
                    OP("act", lambda e: e.activation(out=Er[dd].t[:], in_=pr.t[:, 0:256], func=AF.Exp), r=[pr], w=[Er[dd]])
                    if flagcol is None:
                        OP("dve", lambda e: e.tensor_tensor(out=kd[dd].t[:], in0=ksrc.t[:, 256:512], in1=Er[dd].t[:], op=ALU.mult),
                           r=[ksrc, Er[dd]], w=[kd[dd]])
                    else:
                        OP("dve", lambda e: e.scalar_tensor_tensor(out=kd[dd].t[:], in0=ksrc.t[:, 256:512],
                                                                   scalar=flags.t[:, flagcol:flagcol + 1], in1=Er[dd].t[:],
                                                                   op0=ALU.mult, op1=ALU.mult), r=[ksrc, Er[dd], flags], w=[kd[dd]])
                    pa_ = bank()
                    for hp in range(2):
                        OP("pe", lambda e, hp=hp: e.matmul(pa_.t[:, hp * 2:hp * 2 + 2], lhsT=sp_[dd].t[:, hp * 128:(hp + 1) * 128],
                                                           rhs=ci.t[:], start=True, stop=True), r=[sp_[dd], ci], w=[pa_])
                    OP("act", lambda e: e.activation(out=asb[dd].t[:], in_=pa_.t[:, 0:4].rearrange("p (a b) -> p a b", a=2),
                                                     func=AF.Exp), r=[pa_], w=[asb[dd]])

                def state_update(dd, c, vsrc):
                    pp = bank()
                    for h in range(4):
                        hp, par = h // 2, h % 2
                        OP("pe", lambda e, h=h, hp=hp, par=par: e.matmul(
                            pp.t[par * 64:(par + 1) * 64, hp * 128:(hp + 1) * 128],
                            lhsT=kd[dd].t[c * 64:(c + 1) * 64, h * 64:(h + 1) * 64],
                            rhs=vsrc.t[c * 64:(c + 1) * 64, h * 128:(h + 1) * 128], start=True, stop=True),
                           r=[kd[dd], vsrc], w=[pp])
                    so = Sst[dd][Scur[dd]]
                    sn_ = Sst[dd][1 - Scur[dd]]
                    OP("dve", lambda e: e.tensor_tensor(out=Stmp.t[:], in0=so.t[:],
                                                        in1=asb[dd].t[:, :, c:c + 1].broadcast_to([128, 2, 128]), op=ALU.mult),
                       r=[so, asb[dd]], w=[Stmp])
                    OP("dve", lambda e: e.tensor_tensor(out=sn_.t[:], in0=Stmp.t[:],
                                                        in1=pp.t[:, 0:256].rearrange("p (a b) -> p a b", a=2), op=ALU.add),
                       r=[Stmp, pp], w=[sn_])
                    Scur[dd] = 1 - Scur[dd]

                def state_tile(src_ap, row, dd, flagcol=None, store=None):
                    front(src_ap, row)
                    pk = bank()
                    inproj(256, 256, pk, 256)
                    pv = bank()
                    inproj(512, 512, pv)
                    OP("act", lambda e: e.activation(out=vsb.t[:], in_=pv.t[:], func=AF.Identity), r=[pv], w=[vsb])
                    gates(dd, flagcol)
                    decay_k(dd, pk, flagcol)
                    for c in ((0, 1) if dd == 0 else (1, 0)):
                        if store is not None:
                            cur = Sst[dd][Scur[dd]]
                            OP("pool", lambda e, c=c, cur=cur: e.tensor_copy(out=sbst.t[:, store * 2 + c, :],
                                                                             in_=cur.t[:].rearrange("p a b -> p (a b)")),
                               r=[cur], w=[sbst])
                        state_update(dd, c, vsb)

                def rope(src, dst, H, tmp):
                    v5 = lambda b: b.t[:, 0:H * 64].rearrange("p (h a f d) -> p h a f d", h=H, a=2, f=2, d=16)
                    cb = cs.t[:].rearrange("p (a d) -> p a d", a=2).unsqueeze(1).broadcast_to([128, H, 2, 16])
                    sb_ = sn.t[:].rearrange("p (a d) -> p a d", a=2).unsqueeze(1).broadcast_to([128, H, 2, 16])
                    x1, x2 = v5(src)[:, :, :, 0, :], v5(src)[:, :, :, 1, :]
                    o1, o2 = v5(dst)[:, :, :, 0, :], v5(dst)[:, :, :, 1, :]
                    t1, t2 = v5(tmp)[:, :, :, 0, :], v5(tmp)[:, :, :, 1, :]
                    OP("pool", lambda e: e.tensor_tensor(out=o1, in0=x1, in1=cb, op=ALU.mult), r=[src, cs], w=[dst])
                    OP("pool", lambda e: e.tensor_tensor(out=t1, in0=x2, in1=sb_, op=ALU.mult), r=[src, sn], w=[tmp])
                    OP("pool", lambda e: e.tensor_tensor(out=o1, in0=o1, in1=t1, op=ALU.subtract), r=[dst, tmp], w=[dst])
                    OP("pool", lambda e: e.tensor_tensor(out=o2, in0=x2, in1=cb, op=ALU.mult), r=[src, cs], w=[dst])
                    OP("pool", lambda e: e.tensor_tensor(out=t2, in0=x1, in1=sb_, op=ALU.mult), r=[src, sn], w=[tmp])
                    OP("pool", lambda e: e.tensor_tensor(out=o2, in0=o2, in1=t2, op=ALU.add), r=[dst, tmp], w=[dst])

                def kv_tile(src_ap, row, j, is_ctx):
                    front(src_ap, row)
                    pb = bank()
                    inproj(2048, 256, pb)
                    OP("act", lambda e: e.activation(out=akv.t[:], in_=pb.t[:, 0:256], func=AF.Identity), r=[pb], w=[akv])
                    if is_ctx:
                        ksrc, kdst, vdst = akv, kTc, vc
                    else:
                        S.dma("sp", csl, lambda e: e.dma_start(out=cs.t[:], in_=d_cos[j * 128:(j + 1) * 128, :]), w=[cs])
                        S.dma("sp", snl, lambda e: e.dma_start(out=sn.t[:], in_=d_sin[j * 128:(j + 1) * 128, :]), w=[sn])
                        rope(akv, krot, 2, rt)
                        ksrc, kdst, vdst = krot, kT_all, v_all
                    OP("pool", lambda e: e.tensor_copy(
                        out=vdst.t[:, j, :].rearrange("p (g d) -> p g d", g=2)[:, :, 0:64],
                        in_=akv.t[:, 128:256].rearrange("p (g d) -> p g d", g=2)), r=[akv], w=[vdst])

                    def evac(c0, n, pb2):
                        OP("act", lambda e: e.activation(out=kdst.t[:, j, :], in_=pb2.t[0:64, 0:256], func=AF.Identity),
                           r=[pb2], w=[kdst])
                    transpose_to(ksrc, 2, evac, width=64)

                def own_tile(i):
                    j = i + 1
                    xb = front(d_xown[j * 128:(j + 1) * 128, :], 0)
                    pqk = bank(); inproj(0, 512, pqk)
                    OP("act", lambda e: e.activation(out=qk.t[:], in_=pqk.t[:], func=AF.Identity), r=[pqk], w=[qk])
                    pv = bank(); inproj(512, 512, pv)
                    OP("act", lambda e: e.activation(out=vsb.t[:], in_=pv.t[:], func=AF.Identity), r=[pv], w=[vsb])
                    pr_ = bank(); inproj(1024, 512, pr_)
                    OP("act", lambda e: e.activation(out=rsb.t[:], in_=pr_.t[:], func=AF.Silu), r=[pr_], w=[rsb])
                    for dd in range(2):
                        gates(dd)
                        pbn = bank()
                        OP("pe", lambda e, dd=dd, pbn=pbn: e.matmul(pbn.t[:, 0:256], lhsT=tri.t[:, 2 * dd, :], rhs=sp_[dd].t[:],
                                                                    start=True, stop=True), r=[tri, sp_[dd]], w=[pbn])
                        OP("act", lambda e, dd=dd, pbn=pbn: e.activation(out=Eb[dd].t[:], in_=pbn.t[:, 0:256], func=AF.Exp),
                           r=[pbn], w=[Eb[dd]])
                        OP("act", lambda e, dd=dd, pbn=pbn: e.activation(out=Ei[dd].t[:], in_=pbn.t[:, 0:256], func=AF.Exp, scale=-1.0),
                           r=[pbn], w=[Ei[dd]])
                        OP("dve", lambda e, dd=dd: e.scalar_tensor_tensor(out=qe[dd].t[:], in0=qk.t[:, 0:256], scalar=0.125, in1=Eb[dd].t[:],
                                                                          op0=ALU.mult, op1=ALU.mult), r=[qk, Eb[dd]], w=[qe[dd]])
                        OP("dve", lambda e, dd=dd: e.tensor_tensor(out=ke[dd].t[:], in0=qk.t[:, 256:512], in1=Ei[dd].t[:], op=ALU.mult),
                           r=[qk, Ei[dd]], w=[ke[dd]])
                        if dd == 0:
                            decay_k(dd, qk)
                        pT = bank()
                        for idx, srcb in enumerate((qe[dd], qe[dd], ke[dd], ke[dd])):
                            hp = idx % 2
                            OP("pe", lambda e, idx=idx, hp=hp, srcb=srcb, pT=pT: e.transpose(
                                out=pT.t[:, idx * 128:(idx + 1) * 128], in_=srcb.t[:, hp * 128:(hp + 1) * 128], identity=ident.t[:]),
                               r=[srcb, ident], w=[pT])
                        OP("act", lambda e, dd=dd, pT=pT: e.activation(out=keT[dd].t[:].rearrange("p a b -> p (a b)"), in_=pT.t[:, 256:512],
                                                                       func=AF.Identity), r=[pT], w=[keT[dd]])
                        for par in range(2):
                            OP("act", lambda e, dd=dd, pT=pT, par=par: e.activation(
                                out=Tz[dd].t[par * 64:(par + 1) * 64, par::2, :],
                                in_=pT.t[par * 64:(par + 1) * 64, 0:256].rearrange("p (a b) -> p a b", a=2), func=AF.Identity),
                               r=[pT], w=[Tz[dd]])
                    pAT = bank()
                    for dd in range(2):
                        for c in range(2):
                            for h in range(4):
                                hp = h // 2
                                OP("pe", lambda e, dd=dd, c=c, h=h, hp=hp: e.matmul(
                                    pAT.t[c * 64:(c + 1) * 64, (dd * 4 + h) * 64:(dd * 4 + h + 1) * 64],
                                    lhsT=keT[dd].t[:, hp, c * 64:(c + 1) * 64],
                                    rhs=Tz[dd].t[:, h, c * 64:(c + 1) * 64], start=True, stop=True),
                                   r=[keT[dd], Tz[dd]], w=[pAT])
                    for c in range(2):
                        OP("dve", lambda e, c=c: e.tensor_tensor(
                            out=ATz.t[c * 64:(c + 1) * 64, :, :, c, :],
                            in0=pAT.t[c * 64:(c + 1) * 64, :].rearrange("p (d h c) -> p d h c", d=2, h=4),
                            in1=gmask.t[c * 64:(c + 1) * 64, :, :].unsqueeze(2).broadcast_to([64, 2, 4, 64]), op=ALU.mult),
                           r=[pAT, gmask], w=[ATz])
                    po = bank()
                    for c in range(2):
                        sf = Sst[0][Scur[0]]
                        for h in range(4):
                            hp = h // 2
                            outp = po.t[c * 64:(c + 1) * 64, h * 128:(h + 1) * 128]
                            vv = vsb.t[:, h * 128:(h + 1) * 128]
                            OP("pe", lambda e, c=c, h=h, outp=outp, vv=vv: e.matmul(
                                outp, lhsT=ATz.t[:, 0, h, c, :], rhs=vv, start=True, stop=False), r=[ATz, vsb], w=[po])
                            OP("pe", lambda e, c=c, h=h, hp=hp, outp=outp, sf=sf: e.matmul(
                                outp, lhsT=Tz[0].t[:, h, c * 64:(c + 1) * 64], rhs=sf.t[:, hp, :], start=False, stop=False),
                               r=[Tz[0], sf], w=[po])
                            OP("pe", lambda e, c=c, h=h, outp=outp, vv=vv: e.matmul(
                                outp, lhsT=ATz.t[:, 1, h, c, :], rhs=vv, start=False, stop=False), r=[ATz, vsb], w=[po])
                            OP("pe", lambda e, c=c, h=h, hp=hp, outp=outp: e.matmul(
                                outp, lhsT=Tz[1].t[:, h, c * 64:(c + 1) * 64],
                                rhs=sbst.t[:, i * 2 + c, hp * 128:(hp + 1) * 128], start=False, stop=True), r=[Tz[1], sbst], w=[po])
                        state_update(0, c, vsb)
                    for h in range(4):
                        OP("act", lambda e, h=h: e.activation(out=junk.t[:, h * 128:(h + 1) * 128], in_=po.t[:, h * 128:(h + 1) * 128],
                                                              func=AF.Square, accum_out=ss.t[:, h:h + 1]), r=[po], w=[junk, ss])
                    OP("dve", lambda e: e.tensor_scalar(out=rstd4.t[:], in0=ss.t[:], scalar1=1.0 / 128, scalar2=eps_t.t[:, 0:1],
                                                        op0=ALU.mult, op1=ALU.add), r=[ss, eps_t], w=[rstd4])
                    OP("act", lambda e: e.activation(out=rstd4.t[:], in_=rstd4.t[:], func=AF.Sqrt), r=[rstd4], w=[rstd4])
                    OP("dve", lambda e: e.reciprocal(out=rstd4.t[:], in_=rstd4.t[:]), r=[rstd4], w=[rstd4])
                    on3 = on.t[:].rearrange("p (h d) -> p h d", h=4)
                    OP("dve", lambda e: e.tensor_tensor(out=on3, in0=po.t[:].rearrange("p (h d) -> p h d", h=4),
                                                        in1=rstd4.t[:].unsqueeze(2).broadcast_to([128, 4, 128]), op=ALU.mult),
                       r=[po, rstd4], w=[on])
                    OP("pool", lambda e: e.tensor_tensor(out=on3, in0=on3, in1=gng.t[:].unsqueeze(1).broadcast_to([128, 4, 128]),
                                                         op=ALU.mult), r=[on, gng], w=[on])
                    OP("pool", lambda e: e.tensor_tensor(out=cat.t[:, 0:512], in0=on.t[:], in1=rsb.t[:], op=ALU.mult),
                       r=[on, rsb], w=[cat])
                    paq = bank(); inproj(1536, 512, paq)
                    OP("act", lambda e: e.activation(out=aqs.t[:], in_=paq.t[:], func=AF.Identity), r=[paq], w=[aqs])
                    S.dma("sp", csl, lambda e: e.dma_start(out=cs.t[:], in_=d_cos[j * 128:(j + 1) * 128, :]), w=[cs])
                    S.dma("sp", snl, lambda e: e.dma_start(out=sn.t[:], in_=d_sin[j * 128:(j + 1) * 128, :]), w=[sn])
                    rope(aqs, qrot, 8, rt)

                    def evq(c0, n, pb2):
                        OP("act", lambda e: e.activation(out=qTs.t[:, c0:c0 + n, :].rearrange("p a b -> p (a b)"),
                                                         in_=pb2.t[0:64, 0:n * 128], func=AF.Identity, scale=0.125), r=[pb2], w=[qTs])
                    transpose_to(qrot, 8, evq, width=64)
                    def att_group(g):
                        kts = [(kT_all, v_all, j - 1, amask_e if i == 0 else amask, 0), (kT_all, v_all, j, None, 0),
                               (kT_all, v_all, j + 1, amask_e if i == 15 else amask, 1), (kTc, vc, 0, None, 0), (kTc, vc, 1, None, 0)]
                        for n_, (kb, vb, jj, mk, mi) in enumerate(kts):
                            pst = bank()
                            OP("pe", lambda e, kb=kb, jj=jj, pst=pst: e.matmul(
                                pst.t[:], lhsT=kb.t[:, jj, g * 128:(g + 1) * 128],
                                rhs=qTs.t[:, 4 * g:4 * g + 4, :].rearrange("p a b -> p (a b)"), start=True, stop=True),
                               r=[kb, qTs], w=[pst])
                            OP("act", lambda e, n_=n_, pst=pst: e.activation(out=Pall.t[:, n_, :], in_=pst.t[:], func=AF.Exp),
                               r=[pst], w=[Pall])
                            if mk is not None:
                                OP("dve", lambda e, n_=n_, mk=mk, mi=mi: e.tensor_tensor(
                                    out=Pall.t[:, n_, :].rearrange("p (h q) -> p h q", h=4),
                                    in0=Pall.t[:, n_, :].rearrange("p (h q) -> p h q", h=4),
                                    in1=mk.t[:, mi:mi + 1, :].broadcast_to([128, 4, 128]), op=ALU.mult), r=[Pall, mk], w=[Pall])
                        pO = bank()
                        for hh in range(4):
                            for n_, (kb, vb, jj, mk, mi) in enumerate(kts):
                                OP("pe", lambda e, hh=hh, n_=n_, vb=vb, jj=jj: e.matmul(
                                    pO.t[:, hh * 65:(hh + 1) * 65], lhsT=Pall.t[:, n_, hh * 128:(hh + 1) * 128],
                                    rhs=vb.t[:, jj, g * 65:(g + 1) * 65], start=(n_ == 0), stop=(n_ == 4)), r=[Pall, vb], w=[pO])
                        pO3 = pO.t[:, 0:260].rearrange("p (h d) -> p h d", h=4)
                        OP("dve", lambda e, pO3=pO3: e.tensor_tensor(out=den.t[:].unsqueeze(2), in0=pO3[:, :, 64:65],
                                                                     in1=esink.t[:, 4 * g:4 * g + 4].unsqueeze(2), op=ALU.add),
                           r=[pO, esink], w=[den])
                        OP("dve", lambda e: e.reciprocal(out=den.t[:], in_=den.t[:]), r=[den], w=[den])
                        OP("dve", lambda e, pO3=pO3: e.tensor_tensor(
                            out=cat.t[:, 512 + g * 256:512 + (g + 1) * 256].rearrange("p (h d) -> p h d", h=4),
                            in0=pO3[:, :, 0:64], in1=den.t[:].unsqueeze(2).broadcast_to([128, 4, 64]), op=ALU.mult),
                           r=[pO, den], w=[cat])
                    for g_ in range(2):
                        att_group(g_)
                    if os.environ.get("K_DBG") == "cat":
                        S.dma("pool", sts, lambda e: e.dma_start(out=d_x1[i * 128:(i + 1) * 128, :], in_=cat.t[:]), r=[cat])
                        return
                    def evc(c0, n, pb2):
                        OP("act", lambda e: e.activation(out=catT.t[:, c0:c0 + n, :].rearrange("p a b -> p (a b)"),
                                                         in_=pb2.t[:, 0:n * 128], func=AF.Identity), r=[pb2], w=[catT])
                    transpose_to(cat, 8, evc)
                    for hf in range(2):
                        py = bank()
                        for kc in range(8):
                            OP("pe", lambda e, kc=kc, py=py, hf=hf: e.matmul(py.t[:], lhsT=catT.t[:, kc, :],
                                                                      rhs=wout.t[:, kc, hf * 512:(hf + 1) * 512],
                                                                      start=(kc == 0), stop=(kc == 7)), r=[catT, wout], w=[py])
                        OP("dve", lambda e, py=py, hf=hf: e.tensor_tensor(out=r1.t[:, hf * 512:(hf + 1) * 512], in0=py.t[:],
                                                                   in1=g1bc.t[:, hf * 512:(hf + 1) * 512], op=ALU.mult),
                           r=[py, g1bc], w=[r1])
                    OP("dve", lambda e: e.scalar_tensor_tensor(out=r1.t[:], in0=xb.t[:], scalar=ALPHA, in1=r1.t[:],
                                                               op0=ALU.mult, op1=ALU.add), r=[xb, r1], w=[r1])
                    layernorm(r1, x1o, ln1g, ln1b, stats, mv, rs1)
                    S.dma("pool", sts, lambda e: e.dma_start(out=d_x1[i * 128:(i + 1) * 128, :], in_=x1o.t[:]), r=[x1o])

                def layernorm(src, dst, gb, bb, stats, mv, rs1):
                    for hf in range(2):
                        OP("dve", lambda e, hf=hf: e.bn_stats(out=stats.t[:, hf * 6:(hf + 1) * 6], in_=src.t[:, hf * 512:(hf + 1) * 512]),
                           r=[src], w=[stats])
                    OP("dve", lambda e: e.bn_aggr(out=mv.t[:], in_=stats.t[:]), r=[stats], w=[mv])
                    OP("dve", lambda e: e.tensor_scalar(out=rs1.t[:], in0=mv.t[:, 1:2], scalar1=eps_t.t[:, 0:1], scalar2=None, op0=ALU.add),
                       r=[mv, eps_t], w=[rs1])
                    OP("act", lambda e: e.activation(out=rs1.t[:], in_=rs1.t[:], func=AF.Sqrt), r=[rs1], w=[rs1])
                    OP("dve", lambda e: e.reciprocal(out=rs1.t[:], in_=rs1.t[:]), r=[rs1], w=[rs1])
                    OP("dve", lambda e: e.tensor_scalar(out=dst.t[:], in0=src.t[:], scalar1=mv.t[:, 0:1], scalar2=rs1.t[:, 0:1],
                                                        op0=ALU.subtract, op1=ALU.mult), r=[src, mv, rs1], w=[dst])
                    OP("pool", lambda e: e.tensor_tensor(out=dst.t[:], in0=dst.t[:], in1=gb.t[:], op=ALU.mult), r=[dst, gb], w=[dst])
                    OP("pool", lambda e: e.tensor_tensor(out=dst.t[:], in0=dst.t[:], in1=bb.t[:], op=ALU.add), r=[dst, bb], w=[dst])

                for t in range(2):
                    state_tile(d_ctx[t * 128:(t + 1) * 128, :], 1, 0)
                for t in (1, 0):
                    state_tile(d_ctx[t * 128:(t + 1) * 128, :], 1, 1)
                for t in range(2):
                    kv_tile(d_ctx[t * 128:(t + 1) * 128, :], 1, t, True)
                convert_tables()
                for t in range(NPRE):
                    state_tile(d_xpf[t * 128:(t + 1) * 128, :], 0, 0, flagcol=t)
                for t in range(NPRE):
                    state_tile(d_xpb[t * 128:(t + 1) * 128, :], 0, 1, flagcol=48 + t)
                for i in range(15, -1, -1):
                    state_tile(d_xown[(i + 1) * 128:(i + 2) * 128, :], 0, 1, store=i)
                for j in range(18):
                    kv_tile(d_xown[j * 128:(j + 1) * 128, :], 0, j, False)
                for i in range(16):
                    own_tile(i)
                S.barrier()

        if mode != "A":
            with ExitStack() as pbs_:
                wq = sbuf(pbs_, "wq", [128, 8, 1024]); skT = sbuf(pbs_, "skT", [128, 8, 128])
                ln2g = sbuf(pbs_, "ln2g", [128, 1024]); ln2b = sbuf(pbs_, "ln2b", [128, 1024])
                iota = sbuf(pbs_, "iota", [128, 16])
                S.dma("sp", cst, lambda e: e.dma_start(out=wq.t[:], in_=d_wq.rearrange("(k p) n -> p k n", p=128)), w=[wq])
                for b_, d_ in ((skT, d_skT), (ln2g, d_ln2g), (ln2b, d_ln2b), (iota, d_iota)):
                    S.dma("sp", cst, lambda e, b_=b_, d_=d_: e.dma_start(out=b_.t[:], in_=d_), w=[b_])
                S.barrier()
                if mode == "B":
                    convert_tables()
                identb = sbuf(pbs_, "identb", [128, 128], BF16)
                OP("dve", lambda e: e.tensor_copy(out=identb.t[:], in_=ident.t[:]), r=[ident], w=[identb])
                dg = [sbuf(pbs_, "dg%d" % i, [128, 128], BF16) for i in range(4)]
                sc2bc = sbuf(pbs_, "sc2bc", [128, 1024]); sh2bc = sbuf(pbs_, "sh2bc", [128, 1024]); g2bc = sbuf(pbs_, "g2bc", [128, 1024])
                if mode == "B":
                    OP("dve", lambda e: e.memset(sc2bc.t[:], 1.0), w=[sc2bc])
                    OP("dve", lambda e: e.memset(sh2bc.t[:], 0.0), w=[sh2bc])
                    OP("dve", lambda e: e.memset(g2bc.t[:], 1.0), w=[g2bc])
                else:
                    for b_, di in ((sh2bc, 0), (sc2bc, 1), (g2bc, 2)):
                        S.dma("sp", cst, lambda e, b_=b_, di=di: e.dma_start(out=b_.t[:], in_=d_modbc[di]), w=[b_])
                    S.barrier()
                x1t = [sbuf(pbs_, "x1t%d" % i, [128, 1024]) for i in range(3)]
                h2 = [sbuf(pbs_, "h2_%d" % i, [128, 1024]) for i in range(2)]
                h2T = sbuf(pbs_, "h2T", [128, 8, 128]); qTp = sbuf(pbs_, "qTp", [128, 8, 128])
                ssb = sbuf(pbs_, "ssb", [128, 16, 128]); sw = sbuf(pbs_, "sw", [128, 128])
                m16 = sbuf(pbs_, "m16", [128, 16, 16]); ix16 = sbuf(pbs_, "ix16", [128, 16, 16], U32)
                ixf = sbuf(pbs_, "ixf", [128, 16, 16])
                cand = sbuf(pbs_, "cand", [128, 8, 256]); cw = sbuf(pbs_, "cw", [128, 256])
                tsv = sbuf(pbs_, "tsv", [128, 8, 16]); pos = sbuf(pbs_, "pos", [128, 8, 16], U32)
                posf = sbuf(pbs_, "posf", [128, 8, 16])
                lohi = sbuf(pbs_, "lohi", [128, 2, 16])
                paf = sbuf(pbs_, "paf", [128, 8, 16]); pbf = sbuf(pbs_, "pbf", [128, 8, 16])
                oh = sbuf(pbs_, "oh", [128, 8, 16, 16])
                i1s = sbuf(pbs_, "i1s", [128, 8, 16]); i2s = sbuf(pbs_, "i2s", [128, 8, 16])
                eidf = sbuf(pbs_, "eidf", [128, 128])
                eidx = [sbuf(pbs_, "eidx%d" % i, [128, 128], U32) for i in range(3)]
                gate = [sbuf(pbs_, "gate%d" % i, [128, 8, 16]) for i in range(2)]
                gsum = sbuf(pbs_, "gsum", [128, 8])
                dots = [sbuf(pbs_, "dots%d" % i, [128, 128]) for i in range(2)]
                coef = [sbuf(pbs_, "coef%d" % i, [128, 128]) for i in range(2)]
                gb = [sbuf(pbs_, "gb%d" % i, [128, 2048], BF16) for i in range(NG)]
                gs = [S.dma_slot("g%d" % i) for i in range(NG)]
                prod = [sbuf(pbs_, "prod%d" % i, [128, 1024], BF16) for i in range(3)]
                h2b = [sbuf(pbs_, "h2b%d" % i, [128, 1024], BF16) for i in range(2)]
                acc = [sbuf(pbs_, "acc%d" % i, [128, 1024]) for i in range(2)]
                stats2 = sbuf(pbs_, "stats2", [128, 12]); mv2 = sbuf(pbs_, "mv2", [128, 2]); rs2 = sbuf(pbs_, "rs2", [128, 1])
                x1ld = [S.dma_slot("x1l%d" % i) for i in range(3)]

                def layernorm2(src, dst):
                    for hf in range(2):
                        OP("dve", lambda e, hf=hf: e.bn_stats(out=stats2.t[:, hf * 6:(hf + 1) * 6], in_=src.t[:, hf * 512:(hf + 1) * 512]),
                           r=[src], w=[stats2])
                    OP("dve", lambda e: e.bn_aggr(out=mv2.t[:], in_=stats2.t[:]), r=[stats2], w=[mv2])
                    OP("dve", lambda e: e.tensor_scalar(out=rs2.t[:], in0=mv2.t[:, 1:2], scalar1=eps_t.t[:, 0:1], scalar2=None, op0=ALU.add),
                       r=[mv2, eps_t], w=[rs2])
                    OP("act", lambda e: e.activation(out=rs2.t[:], in_=rs2.t[:], func=AF.Sqrt), r=[rs2], w=[rs2])
                    OP("dve", lambda e: e.reciprocal(out=rs2.t[:], in_=rs2.t[:]), r=[rs2], w=[rs2])
                    OP("dve", lambda e: e.tensor_scalar(out=dst.t[:], in0=src.t[:], scalar1=mv2.t[:, 0:1], scalar2=rs2.t[:, 0:1],
                                                        op0=ALU.subtract, op1=ALU.mult), r=[src, mv2, rs2], w=[dst])
                    OP("dve", lambda e: e.tensor_tensor(out=dst.t[:], in0=dst.t[:], in1=ln2g.t[:], op=ALU.mult), r=[dst, ln2g], w=[dst])
                    OP("dve", lambda e: e.tensor_tensor(out=dst.t[:], in0=dst.t[:], in1=ln2b.t[:], op=ALU.add), r=[dst, ln2b], w=[dst])

                def top16(src3, n, work, mdst, idst):
                    (sa, sbuf_), (wa, wbuf), (ma, mbuf), (ia, ibuf) = src3, work, mdst, idst
                    OP("dve", lambda e: e.max(out=ma[:, 0:8], in_=sa), r=[sbuf_], w=[mbuf])
                    OP("dve", lambda e: e.max_index(out=ia[:, 0:8], in_max=ma[:, 0:8], in_values=sa), r=[sbuf_, mbuf], w=[ibuf])
                    OP("dve", lambda e: e.match_replace(out=wa, in_to_replace=ma[:, 0:8], in_values=sa, imm_value=-1e30),
                       r=[sbuf_, mbuf], w=[wbuf])
                    OP("dve", lambda e: e.max(out=ma[:, 8:16], in_=wa), r=[wbuf], w=[mbuf])
                    OP("dve", lambda e: e.max_index(out=ia[:, 8:16], in_max=ma[:, 8:16], in_values=wa), r=[wbuf, mbuf], w=[ibuf])

                def route(i):
                    xs = x1t[i % 3]
                    S.dma("sp", x1ld[i % 3], lambda e: e.dma_start(out=xs.t[:], in_=d_x1[i * 128:(i + 1) * 128, :]), w=[xs])
                    hh = h2[i % 2]
                    OP("dve", lambda e: e.tensor_tensor(out=hh.t[:], in0=xs.t[:], in1=sc2bc.t[:], op=ALU.mult), r=[xs, sc2bc], w=[hh])
                    OP("dve", lambda e: e.tensor_tensor(out=hh.t[:], in0=hh.t[:], in1=sh2bc.t[:], op=ALU.add), r=[hh, sh2bc], w=[hh])
                    OP("act", lambda e: e.activation(out=h2b[i % 2].t[:], in_=hh.t[:], func=AF.Identity), r=[hh], w=[h2b[i % 2]])

                    if KSTOP < 2: return
                    def ev(c0, n, pb2):
                        OP("act", lambda e: e.activation(out=h2T.t[:, c0:c0 + n, :].rearrange("p a b -> p (a b)"),
                                                         in_=pb2.t[:, 0:n * 128], func=AF.Identity), r=[pb2], w=[h2T])
                    yield
                    transpose_to(hh, 8, ev)
                    yield
                    if KSTOP < 2.3: return
                    for j0 in range(0, 8, 4):
                        pq = bank()
                        for jj in range(j0, j0 + 4):
                            for kc in range(8):
                                OP("pe", lambda e, jj=jj, kc=kc, pq=pq, j0=j0: e.matmul(
                                    pq.t[:, (jj - j0) * 128:(jj - j0 + 1) * 128], lhsT=wq.t[:, kc, jj * 128:(jj + 1) * 128],
                                    rhs=h2T.t[:, kc, :], start=(kc == 0), stop=(kc == 7)), r=[wq, h2T], w=[pq])
                        OP("act", lambda e, pq=pq, j0=j0: e.activation(out=qTp.t[:, j0:j0 + 4, :].rearrange("p a b -> p (a b)"),
                                                                       in_=pq.t[:], func=AF.Identity), r=[pq], w=[qTp])
                    if KSTOP < 2.6: return
                    for h0 in range(0, 8, 4):
                        for a in range(2):
                            psc = bank()
                            for h in range(h0, h0 + 4):
                                OP("pe", lambda e, h=h, a=a, psc=psc, h0=h0: e.matmul(
                                    psc.t[:, (h - h0) * 128:(h - h0 + 1) * 128], lhsT=qTp.t[a * 64:(a + 1) * 64, h, :],
                                    rhs=skT.t[a * 64:(a + 1) * 64, h, :], start=True, stop=True), r=[qTp, skT], w=[psc])
                            OP("act", lambda e, psc=psc, h0=h0, a=a: e.activation(
                                out=ssb.t[:, 2 * h0 + a:2 * h0 + 8:2, :], in_=psc.t[:].rearrange("p (a b) -> p a b", a=4),
                                func=AF.Identity), r=[psc], w=[ssb])
                    if KSTOP < 3: return
                    for q_ in range(16):
                        top16((ssb.t[:, q_, :], ssb), 128, (sw.t[:], sw), (m16.t[:, q_, :], m16), (ix16.t[:, q_, :], ix16))
                        yield
                    if KSTOP < 4: return
                    OP("dve", lambda e: e.tensor_copy(out=ixf.t[:], in_=ix16.t[:]), r=[ix16], w=[ixf])
                    m4 = m16.t[:].rearrange("p (h a) k -> p h a k", a=2)
                    OP("dve", lambda e: e.tensor_tensor(
                        out=cand.t[:].rearrange("p h (a b) -> p h a b", a=16),
                        in0=m4[:, :, 0, :].unsqueeze(3).broadcast_to([128, 8, 16, 16]),
                        in1=m4[:, :, 1, :].unsqueeze(2).broadcast_to([128, 8, 16, 16]), op=ALU.add), r=[m16], w=[cand])
                    for h in range(8):
                        top16((cand.t[:, h, :], cand), 256, (cw.t[:], cw), (tsv.t[:, h, :], tsv), (pos.t[:, h, :], pos))
                        yield
                    gt = gate[i % 2]
                    OP("dve", lambda e: e.tensor_tensor(out=gt.t[:], in0=tsv.t[:], in1=tsv.t[:, :, 0:1].broadcast_to([128, 8, 16]),
                                                        op=ALU.subtract), r=[tsv], w=[gt])
                    OP("act", lambda e: e.activation(out=gt.t[:], in_=gt.t[:], func=AF.Exp), r=[gt], w=[gt])
                    OP("dve", lambda e: e.tensor_reduce(out=gsum.t[:], in_=gt.t[:], axis=AX.X, op=ALU.add), r=[gt], w=[gsum])
                    OP("dve", lambda e: e.reciprocal(out=gsum.t[:], in_=gsum.t[:]), r=[gsum], w=[gsum])
                    OP("dve", lambda e: e.tensor_tensor(out=gt.t[:], in0=gt.t[:], in1=gsum.t[:].unsqueeze(2).broadcast_to([128, 8, 16]),
                                                        op=ALU.mult), r=[gt, gsum], w=[gt])
                    if KSTOP < 5: return
                    yield
                    OP("dve", lambda e: e.tensor_copy(out=posf.t[:], in_=pos.t[:]), r=[pos], w=[posf])
                    oh2 = Buf(None); oh2.r = ssb.r
                    oh2.t = ssb.t[:].rearrange("p a b -> p (a b)").rearrange("p (h j c) -> p h j c", h=8, j=16)
                    ix4 = ixf.t[:].rearrange("p (h a) k -> p h a k", a=2)
                    bc4 = lambda ap2: ap2.unsqueeze(1).unsqueeze(1).broadcast_to([128, 8, 16, 16])
                    pf4 = posf.t[:].unsqueeze(3).broadcast_to([128, 8, 16, 16])
                    OP("dve", lambda e: e.tensor_tensor(out=oh.t[:], in0=pf4, in1=bc4(lohi.t[:, 0, :]), op=ALU.is_ge), r=[posf, lohi], w=[oh])
                    OP("dve", lambda e: e.tensor_tensor(out=oh2.t[:], in0=pf4, in1=bc4(lohi.t[:, 1, :]), op=ALU.is_ge), r=[posf, lohi], w=[oh2])
                    OP("dve", lambda e: e.tensor_tensor(out=oh.t[:], in0=oh.t[:], in1=oh2.t[:], op=ALU.subtract), r=[oh, oh2], w=[oh])
                    OP("dve", lambda e: e.tensor_tensor(out=oh2.t[:], in0=oh.t[:], in1=bc4(lohi.t[:, 0, :]), op=ALU.mult), r=[oh, lohi], w=[oh2])
                    yield
                    OP("dve", lambda e: e.tensor_reduce(out=paf.t[:], in_=oh2.t[:], axis=AX.X, op=ALU.add), r=[oh2], w=[paf])
                    OP("dve", lambda e: e.tensor_tensor(out=oh.t[:], in0=oh.t[:],
                                                        in1=ix4[:, :, 0, :].unsqueeze(2).broadcast_to([128, 8, 16, 16]), op=ALU.mult),
                       r=[oh, ixf], w=[oh])
                    OP("dve", lambda e: e.tensor_reduce(out=i1s.t[:], in_=oh.t[:], axis=AX.X, op=ALU.add), r=[oh], w=[i1s])
                    yield
                    OP("dve", lambda e: e.tensor_tensor(out=pbf.t[:], in0=posf.t[:], in1=paf.t[:], op=ALU.subtract), r=[posf, paf], w=[pbf])
                    OP("dve", lambda e: e.tensor_tensor(out=oh.t[:], in0=bc4(iota.t[:]),
                                                        in1=pbf.t[:].unsqueeze(3).broadcast_to([128, 8, 16, 16]), op=ALU.is_equal),
                       r=[iota, pbf], w=[oh])
                    OP("dve", lambda e: e.tensor_tensor(out=oh.t[:], in0=oh.t[:],
                                                        in1=ix4[:, :, 1, :].unsqueeze(2).broadcast_to([128, 8, 16, 16]), op=ALU.mult),
                       r=[oh, ixf], w=[oh])
                    OP("dve", lambda e: e.tensor_reduce(out=i2s.t[:], in_=oh.t[:], axis=AX.X, op=ALU.add), r=[oh], w=[i2s])
                    OP("dve", lambda e: e.scalar_tensor_tensor(out=eidf.t[:].rearrange("p (h k) -> p h k", h=8), in0=i1s.t[:], scalar=128.0,
                                                               in1=i2s.t[:], op0=ALU.mult, op1=ALU.add), r=[i1s, i2s], w=[eidf])
                    OP("dve", lambda e: e.tensor_copy(out=eidx[i % 3].t[:], in_=eidf.t[:]), r=[eidf], w=[eidx[i % 3]])

                gi = [0]

                class V:
                    def __init__(self, t):
                        self.t = t
                        self.r = Res()
                dcol = [[V(dots[p_].t) for _ in range(128)] for p_ in range(2)]
                ccol = [[V(coef[p_].t) for _ in range(128)] for p_ in range(2)]

                gk = {}

                def slot_u(i, s):
                    k = gi[0] % NG
                    gi[0] += 1
                    gk[(i, s)] = k
                    S.dma("pool", gs[k], lambda e: e.indirect_dma_start(
                        out=gb[k].t[:], out_offset=None, in_=d_puv16,
                        in_offset=bass.IndirectOffsetOnAxis(ap=eidx[i % 3].t[:, s:s + 1], axis=0)), r=[eidx[i % 3]] + puvB, w=[gb[k]])
                    pr = prod[s % 3]
                    dc, cc = dcol[i % 2][s], ccol[i % 2][s]
                    OP("dve", lambda e: e.tensor_tensor(out=pr.t[:], in0=gb[k].t[:, 0:1024], in1=h2b[i % 2].t[:], op=ALU.mult),
                       r=[gb[k], h2b[i % 2]], w=[pr])
                    OP("act", lambda e: e.activation(out=pr.t[:], in_=pr.t[:], func=AF.Identity, accum_out=dc.t[:, s:s + 1]),
                       r=[pr], w=[pr, dc])
                    OP("act", lambda e: e.activation(out=cc.t[:, s:s + 1], in_=dc.t[:, s:s + 1], func=AF.Gelu), r=[dc], w=[cc])

                def slot_v(i, s):
                    k = gk.pop((i, s))
                    cc = ccol[i % 2][s]
                    dgk = dg[s % 4]
                    OP("dve", lambda e: e.tensor_scalar(out=dgk.t[:], in0=identb.t[:], scalar1=cc.t[:, s:s + 1],
                                                        scalar2=gate[i % 2].t[:, s // 16, s % 16:s % 16 + 1], op0=ALU.mult, op1=ALU.mult),
                       r=[identb, cc, gate[i % 2]], w=[dgk])
                    for hf in range(2):
                        OP("pe", lambda e, hf=hf: e.matmul(accP[hf].t[:], lhsT=dgk.t[:], rhs=gb[k].t[:, 1024 + hf * 512:1536 + hf * 512],
                                                           start=(s == 0), stop=(s == 127)), r=[dgk, gb[k]], w=[accP[hf]])

                def finish_v(i):
                    r2 = acc[i % 2]; yo = acc[i % 2]
                    for hf in range(2):
                        OP("dve", lambda e, hf=hf: e.tensor_tensor(out=r2.t[:, hf * 512:(hf + 1) * 512], in0=accP[hf].t[:],
                                                                   in1=g2bc.t[:, hf * 512:(hf + 1) * 512], op=ALU.mult),
                           r=[accP[hf], g2bc], w=[r2])
                    OP("dve", lambda e: e.scalar_tensor_tensor(out=r2.t[:], in0=x1t[i % 3].t[:], scalar=ALPHA, in1=r2.t[:],
                                                               op0=ALU.mult, op1=ALU.add), r=[x1t[i % 3], r2], w=[r2])
                    layernorm2(r2, yo)
                    S.dma("sp", sts, lambda e: e.dma_start(out=d_out[i * 128:(i + 1) * 128, :], in_=yo.t[:]), r=[yo])

                OP("dve", lambda e: e.tensor_scalar(out=lohi.t[:, 0, :], in0=iota.t[:], scalar1=16.0, scalar2=None, op0=ALU.mult), r=[iota], w=[lohi])
                OP("dve", lambda e: e.tensor_scalar(out=lohi.t[:, 1, :], in0=iota.t[:], scalar1=16.0, scalar2=16.0, op0=ALU.mult, op1=ALU.add),
                   r=[iota], w=[lohi])
                NT = int(os.environ.get('K_NT', '16'))
                KSTOP = float(os.environ.get('K_STOP', '9'))
                for _ in route(0):
                    pass
                for i in range(NT):
                    gen = route(i + 1) if i + 1 < NT else iter(())
                    for s in range(128 + LAG if KSTOP >= 6 else 0):
                        if s < 128:
                            slot_u(i, s)
                        if s >= LAG:
                            slot_v(i, s - LAG)
                        next(gen, None)
                    for _ in gen:
                        pass
                    if KSTOP >= 7:
                        finish_v(i)
                S.barrier()
        S.barrier()
        S.run()
    return nc


def _consts():
    s = np.arange(128)[:, None]
    t = np.arange(128)[None, :]
    same = (s // 64) == (t // 64)
    g = -1.0 / 16.0
    tri = np.stack([(same & (s <= t)), (same & (s > t)), (same & (s >= t)), (same & (s < t))], axis=1).astype(np.float32) * g
    ci = np.stack([(np.arange(128) // 64 == c) for c in range(2)], axis=1).astype(np.float32) * g
    amask = np.stack([(s >= t), (s <= t)], axis=1).astype(np.float32)
    sc = (np.arange(128) % 64)[:, None]
    cc = np.arange(64)[None, :]
    gmask = np.stack([(sc <= cc), (sc >= cc)], axis=1).astype(np.float32)
    iota = np.broadcast_to(np.arange(16, dtype=np.float32), (128, 16)).copy()
    return dict(ident=np.eye(128, dtype=np.float32), tri=np.ascontiguousarray(tri), ci=np.ascontiguousarray(ci),
                amask=np.ascontiguousarray(amask), gmask=np.ascontiguousarray(gmask), iota16=iota)


def _rope_tables():
    rows = 8192 // 64
    row = np.repeat(np.arange(rows, dtype=np.float32), 64)
    col = np.tile(np.arange(64, dtype=np.float32), rows)
    inv = (np.float32(10000.0) ** (-np.arange(16, dtype=np.float32) / np.float32(16))).astype(np.float32)
    ang = np.stack([row[:, None] * inv, col[:, None] * inv], axis=1).astype(np.float32)
    return np.cos(ang).reshape(8192, 32).astype(np.float32), np.sin(ang).reshape(8192, 32).astype(np.float32)


def make_in_maps(x, c, ctx, c_ctx, w_ada, b_ada, w_in, w_gate2_f, b_gate_f, w_gate2_b, b_gate_b, gla_norm_g, attn_sink,
                 w_out, ln1_g, ln1_b, peer_wq, peer_subkeys, peer_u, peer_v, ln2_g, ln2_b):
    f = lambda a: np.ascontiguousarray(np.asarray(a, dtype=np.float32))
    bc = lambda v, n: np.ascontiguousarray(np.broadcast_to(np.asarray(v, np.float32).reshape(1, -1), (128, n)))
    x = f(x); ctx = f(ctx)
    wi = f(w_in[0])
    wperm = np.concatenate([wi[:, 0:256], wi[:, 256:512], wi[:, 512:1024], wi[:, 1024:1536], wi[:, 1568:2080],
                            wi[:, 2080:2208], wi[:, 2208:2336], wi[:, 1536:1552], wi[:, 1552:1568]], axis=1)
    cosT, sinT = _rope_tables()
    common = dict(
        w_ada=f(w_ada[0]), b_ada=f(b_ada[0]).reshape(1, -1), w_in=np.ascontiguousarray(wperm),
        w2=np.ascontiguousarray(np.concatenate([f(w_gate2_f[0]), f(w_gate2_b[0])], axis=1)),
        bg=np.ascontiguousarray(np.concatenate([f(b_gate_f[0]), f(b_gate_b[0])]).reshape(1, -1)),
        gng=bc(gla_norm_g[0], 128), sink=bc(attn_sink[0], 8), w_out=f(w_out[0]),
        ln1g=bc(ln1_g[0], 1024), ln1b=bc(ln1_b[0], 1024), ln2g=bc(ln2_g[0], 1024), ln2b=bc(ln2_b[0], 1024),
        peer_wq=f(peer_wq[0]),
        skT=np.ascontiguousarray(np.transpose(f(peer_subkeys[0]), (1, 3, 0, 2)).reshape(128, 8, 128)),
        peer_uv=np.ascontiguousarray(np.concatenate([f(peer_u[0]), f(peer_v[0])], axis=1)), **_consts())
    maps = []
    zt = np.zeros((128, 1024), np.float32)
    for core in range(8):
        b, s = core // 4, core % 4
        xb = x[b].reshape(64, 128, 1024)
        npf = 16 * s
        xpf = np.zeros((NPRE, 128, 1024), np.float32)
        if npf:
            xpf[NPRE - npf:] = xb[0:npf]
        npb = 16 * (3 - s)
        xpb = np.zeros((NPRE, 128, 1024), np.float32)
        if npb:
            xpb[NPRE - npb:] = xb[63:16 * (s + 1) - 1:-1]
        flags = np.zeros((128, 100), np.float32)
        flags[:, NPRE - npf:NPRE] = 1.0
        flags[:, 48 + NPRE - npb:48 + NPRE] = 1.0
        t0 = 16 * s
        own = np.zeros((18, 128, 1024), np.float32)
        own[1:17] = xb[t0:t0 + 16]
        cos_o = np.zeros((18, 128, 32), np.float32); sin_o = np.zeros((18, 128, 32), np.float32)
        cos_o[1:17] = cosT.reshape(64, 128, 32)[t0:t0 + 16]; sin_o[1:17] = sinT.reshape(64, 128, 32)[t0:t0 + 16]
        if t0 > 0:
            own[0] = xb[t0 - 1]; flags[:, 96] = 1.0
            cos_o[0] = cosT.reshape(64, 128, 32)[t0 - 1]; sin_o[0] = sinT.reshape(64, 128, 32)[t0 - 1]
        if t0 + 16 < 64:
            own[17] = xb[t0 + 16]; flags[:, 97] = 1.0
            cos_o[17] = cosT.reshape(64, 128, 32)[t0 + 16]; sin_o[17] = sinT.reshape(64, 128, 32)[t0 + 16]
        c2 = np.stack([f(c)[b], f(c_ctx)], axis=0)
        c2T = np.ascontiguousarray(c2.reshape(2, 8, 128).transpose(2, 1, 0))
        m = dict(common)
        m.update(xpre_f=xpf.reshape(-1, 1024), xpre_b=xpb.reshape(-1, 1024), xown=own.reshape(-1, 1024), ctx=ctx[b],
                 flags=flags, c2T=c2T, cosT=cos_o.reshape(-1, 32), sinT=sin_o.reshape(-1, 32))
        maps.append(m)
    return maps


_NC_CACHE = {}


def kernel(**inputs):
    if "full" not in _NC_CACHE:
        _NC_CACHE["full"] = build_nc("full")
    nc = _NC_CACHE["full"]
    maps = make_in_maps(**inputs)
    res = run_bass_kernel_spmd(nc, maps, core_ids=list(range(8)))
    out = np.zeros((2, 8192, 1024), np.float32)
    for core in range(8):
        b, s = core // 4, core % 4
        out[b, s * 2048:(s + 1) * 2048] = np.asarray(res.results[core]["out"], np.float32)
    return out
```

```python
import os
import numpy as np
from contextlib import ExitStack
import concourse.bass as bass
import concourse.mybir as mybir
from concourse.bass_utils import run_bass_kernel_spmd

F32 = mybir.dt.float32
BF16 = mybir.dt.bfloat16
U32 = mybir.dt.uint32
AF = mybir.ActivationFunctionType
ALU = mybir.AluOpType
AX = mybir.AxisListType

SAME_ENGINE_SYNC = True
NPRE = 48
LN_EPS = 1e-5
ALPHA = 2.0 ** 0.25
NG = 12
LAG = 3


class Res:
    __slots__ = ("w", "rs")

    def __init__(self):
        self.w = None
        self.rs = {}


class Buf:
    def __init__(self, t):
        self.t = t
        self.r = Res()


class Sched:
    def __init__(self, nc, stack):
        self.nc = nc
        self.stack = stack
        self.sems = {}
        self.count = {}
        self.seen = {k: {} for k in ("pe", "dve", "act", "pool", "sp")}
        self.streams = {k: [] for k in ("pe", "dve", "act", "pool", "sp")}
        for k in ("pe", "dve", "act", "pool"):
            self.sems[k] = stack.enter_context(nc.semaphore("sem_" + k))
            self.count[k] = 0
        self.nslots = 0

    def dma_slot(self, name=""):
        self.nslots += 1
        key = "d%d%s" % (self.nslots, name)
        self.sems[key] = self.stack.enter_context(self.nc.semaphore("s_" + key))
        self.count[key] = 0
        return key

    def _waits(self, q, reads, writes, same_ok):
        deps = {}
        for b in reads:
            r = b.r
            if r.w is not None:
                k, c = r.w
                deps[k] = max(deps.get(k, 0), c)
        for b in writes:
            w = b.r
            if w.w is not None:
                k, c = w.w
                deps[k] = max(deps.get(k, 0), c)
            for k, c in w.rs.items():
                deps[k] = max(deps.get(k, 0), c)
        out = []
        for k, c in deps.items():
            if k == q and not same_ok:
                continue
            if self.seen[q].get(k, 0) >= c:
                continue
            self.seen[q][k] = c
            out.append((k, c))
        return out

    def op(self, q, fn, r=(), w=()):
        same_ok = SAME_ENGINE_SYNC and q != "pe"
        waits = self._waits(q, r, w, same_ok)
        self.count[q] += 1
        c = self.count[q]
        sems = self.sems
        st = self.streams[q]
        for k, v in waits:
            st.append(lambda e, k=k, v=v: e.wait_ge(sems[k], v))
        st.append(lambda e, fn=fn: fn(e).then_inc(sems[q], 1))
        for b in r:
            b.r.rs[q] = c
        for b in w:
            b.r.w = (q, c)
            b.r.rs = {}

    def dma(self, q, slot, fn, r=(), w=()):
        waits = self._waits(q, r, w, True)
        prev = self.count[slot]
        if prev > 0 and self.seen[q].get(slot, 0) < prev:
            self.seen[q][slot] = prev
            waits.append((slot, prev))
        self.count[slot] += 16
        c = self.count[slot]
        sems = self.sems
        st = self.streams[q]
        for k, v in waits:
            st.append(lambda e, k=k, v=v: e.wait_ge(sems[k], v))
        st.append(lambda e, fn=fn: fn(e).then_inc(sems[slot], 16))
        for b in r:
            b.r.rs[slot] = c
        for b in w:
            b.r.w = (slot, c)
            b.r.rs = {}

    def wait_all(self, q):
        sems = self.sems
        for k in list(self.count.keys()):
            c = self.count[k]
            if c == 0 or k == q or self.seen[q].get(k, 0) >= c:
                continue
            self.seen[q][k] = c
            self.streams[q].append(lambda e, k=k, c=c: e.wait_ge(sems[k], c))

    def barrier(self):
        for q in ("pe", "dve", "act", "pool", "sp"):
            self.wait_all(q)

    def run(self):
        streams = self.streams
        with self.nc.Block() as block:
            @block.tensor
            def _(e):
                for f in streams["pe"]:
                    f(e)

            @block.vector
            def _(e):
                for f in streams["dve"]:
                    f(e)

            @block.scalar
            def _(e):
                for f in streams["act"]:
                    f(e)

            @block.gpsimd
            def _(e):
                for f in streams["pool"]:
                    f(e)

            @block.sync
            def _(e):
                for f in streams["sp"]:
                    f(e)


def build_nc(mode="full"):
    nc = bass.Bass("TRN2", target_bir_lowering=False)
    D = lambda name, shape, dt=F32, kind="ExternalInput": nc.dram_tensor(name, shape, dt, kind=kind).ap()
    d_xpf = D("xpre_f", [NPRE * 128, 1024]); d_xpb = D("xpre_b", [NPRE * 128, 1024])
    d_xown = D("xown", [18 * 128, 1024]); d_ctx = D("ctx", [256, 1024])
    d_flags = D("flags", [128, 100]); d_c2T = D("c2T", [128, 8, 2])
    d_wada = D("w_ada", [1024, 6144]); d_bada = D("b_ada", [1, 6144])
    d_win = D("w_in", [1024, 2336]); d_w2 = D("w2", [16, 512]); d_bg = D("bg", [1, 512])
    d_gng = D("gng", [128, 128]); d_sink = D("sink", [128, 8])
    d_wout = D("w_out", [1024, 1024])
    d_ln1g = D("ln1g", [128, 1024]); d_ln1b = D("ln1b", [128, 1024])
    d_ln2g = D("ln2g", [128, 1024]); d_ln2b = D("ln2b", [128, 1024])
    d_wq = D("peer_wq", [1024, 1024]); d_skT = D("skT", [128, 8, 128])
    d_puv = D("peer_uv", [16384, 2048])
    d_puv16 = D("puv16", [16384, 2048], BF16, kind="Internal")
    d_cos = D("cosT", [18 * 128, 32]); d_sin = D("sinT", [18 * 128, 32])
    d_ident = D("ident", [128, 128]); d_tri = D("tri", [128, 4, 128]); d_ci = D("ci", [128, 2])
    d_amask = D("amask", [128, 2, 128]); d_gmask = D("gmask", [128, 2, 64]); d_iota = D("iota16", [128, 16])
    d_out = D("out", [2048, 1024], kind="ExternalOutput")
    if mode == "B":
        d_x1 = D("x1s", [2048, 1024])
    else:
        d_x1 = D("x1s", [2048, 1024], kind="ExternalOutput" if mode == "A" else "Internal")

    with ExitStack() as top:
        S = Sched(nc, top)
        OP = S.op

        def sbuf(st, name, shape, dt=F32):
            return Buf(st.enter_context(nc.sbuf_tensor("s_" + name, shape, dt)))

        banks = [Buf(top.enter_context(nc.psum_tensor("pb%d" % i, [128, 512], F32))) for i in range(6)]
        accP = [Buf(top.enter_context(nc.psum_tensor("pacc%d" % i, [128, 512], F32))) for i in range(2)]
        bank_i = [0]

        def bank():
            b = banks[bank_i[0] % 6]
            bank_i[0] += 1
            return b

        ld = [S.dma_slot("ld%d" % i) for i in range(2)]
        cst = S.dma_slot("cst")
        sts = S.dma_slot("st")
        csl = S.dma_slot("cs"); snl = S.dma_slot("sn")

        puvB = [Buf(None) for _ in range(16)]
        cvs = [S.dma_slot("cv%d" % i) for i in range(4)]

        def convert_tables():
            for ci_ in range(16):
                S.dma("pool", cvs[ci_ % 4], lambda e, ci_=ci_: e.dma_start(out=d_puv16[ci_ * 1024:(ci_ + 1) * 1024, :],
                                                                         in_=d_puv[ci_ * 1024:(ci_ + 1) * 1024, :]), w=[puvB[ci_]])
        ident = sbuf(top, "ident", [128, 128])
        S.dma("sp", cst, lambda e: e.dma_start(out=ident.t[:], in_=d_ident), w=[ident])
        flags = sbuf(top, "flags", [128, 100])
        S.dma("sp", cst, lambda e: e.dma_start(out=flags.t[:], in_=d_flags), w=[flags])
        eps_t = sbuf(top, "eps_t", [128, 1])
        OP("dve", lambda e: e.memset(eps_t.t[:], LN_EPS), w=[eps_t])
        modT = sbuf(top, "modT", [128, 48, 2])
        sc1p = sbuf(top, "sc1p", [128, 8, 2])
        g1bc = sbuf(top, "g1bc", [128, 1024])
        d_modbc = D("modbc", [3, 128, 1024], kind="Internal")
        xt_i = [0]

        def transpose_to(src, nchunks, dst_fn, width=128, rows=128):
            for c0 in range(0, nchunks, 4):
                pb = bank()
                n = min(4, nchunks - c0)
                for c in range(c0, c0 + n):
                    OP("pe", lambda e, c=c, pb=pb, c0=c0: e.transpose(
                        out=pb.t[0:width, (c - c0) * 128:(c - c0) * 128 + rows],
                        in_=src.t[0:rows, c * width:(c + 1) * width], identity=ident.t[0:rows, 0:rows]),
                       r=[src, ident], w=[pb])
                dst_fn(c0, n, pb)

        if mode != "B":
            with ExitStack() as p0:
                c2T = sbuf(p0, "c2T", [128, 8, 2]); sc2 = sbuf(p0, "sc2", [128, 8, 2])
                S.dma("sp", cst, lambda e: e.dma_start(out=c2T.t[:], in_=d_c2T), w=[c2T])
                OP("act", lambda e: e.activation(out=sc2.t[:], in_=c2T.t[:], func=AF.Silu), r=[c2T], w=[sc2])
                modrow = sbuf(p0, "modrow", [2, 6144])
                brow = sbuf(p0, "brow", [128, 6144]); ones2 = sbuf(p0, "ones2", [128, 2])
                OP("pool", lambda e: e.memset(brow.t[:], 0.0), w=[brow])
                S.dma("sp", cst, lambda e: e.dma_start(out=brow.t[0:1, :], in_=d_bada), w=[brow])
                OP("dve", lambda e: e.memset(ones2.t[:], 0.0), w=[ones2])
                OP("dve", lambda e: e.memset(ones2.t[0:1, :], 1.0), w=[ones2])
                S.barrier()
                wst = [sbuf(p0, "wst%d" % i, [128, 3072]) for i in range(2)]
                wada_v = d_wada.rearrange("(k p) n -> k p n", p=128)
                for half in range(2):
                    pbs = [bank() for _ in range(6)]
                    for j in range(6):
                        col = half * 3072 + j * 512
                        OP("pe", lambda e, pbj=pbs[j], col=col: e.matmul(pbj.t[0:2, :], lhsT=ones2.t[:], rhs=brow.t[:, col:col + 512],
                                                                        start=True, stop=False), r=[ones2, brow], w=[pbs[j]])
                    for kc in range(8):
                        ws = wst[kc % 2]
                        S.dma("sp", ld[kc % 2], lambda e, ws=ws, kc=kc, half=half: e.dma_start(
                            out=ws.t[:], in_=wada_v[kc, :, half * 3072:(half + 1) * 3072]), w=[ws])
                        for j in range(6):
                            OP("pe", lambda e, j=j, ws=ws, kc=kc, pbj=pbs[j]: e.matmul(
                                pbj.t[0:2, :], lhsT=sc2.t[:, kc, :], rhs=ws.t[:, j * 512:(j + 1) * 512],
                                start=False, stop=(kc == 7)), r=[sc2, ws], w=[pbs[j]])
                    for j in range(6):
                        col = half * 3072 + j * 512
                        OP("act", lambda e, pbj=pbs[j], col=col: e.activation(out=modrow.t[:, col:col + 512], in_=pbj.t[0:2, :],
                                                                       func=AF.Identity), r=[pbs[j]], w=[modrow])
                for c0 in range(0, 48, 4):
                    pb = bank()
                    for c in range(c0, c0 + 4):
                        OP("pe", lambda e, c=c, pb=pb, c0=c0: e.transpose(
                            out=pb.t[:, (c - c0) * 2:(c - c0) * 2 + 2], in_=modrow.t[0:2, c * 128:(c + 1) * 128],
                            identity=ident.t[0:2, 0:2]), r=[modrow, ident], w=[pb])
                    OP("dve", lambda e, pb=pb, c0=c0: e.tensor_copy(
                        out=modT.t[:, c0:c0 + 4, :], in_=pb.t[:, 0:8].rearrange("p (a b) -> p a b", a=4)), r=[pb], w=[modT])
                OP("dve", lambda e: e.tensor_scalar(out=sc1p.t[:], in0=modT.t[:, 8:16, :], scalar1=1.0, scalar2=None, op0=ALU.add),
                   r=[modT], w=[sc1p])
                sel = sbuf(p0, "sel", [2, 128])
                OP("dve", lambda e: e.memset(sel.t[:], 0.0), w=[sel])
                OP("dve", lambda e: e.memset(sel.t[0:1, :], 1.0), w=[sel])
                bct = sbuf(p0, "bct", [128, 1024])
                for dst, j, add1, di in ((g1bc, 2, False, None), (bct, 3, False, 0), (bct, 4, True, 1), (bct, 5, False, 2)):
                    for hf in range(2):
                        pb = bank()
                        col = j * 1024 + hf * 512
                        OP("pe", lambda e, pb=pb, col=col: e.matmul(pb.t[:], lhsT=sel.t[:], rhs=modrow.t[:, col:col + 512],
                                                                    start=True, stop=True), r=[sel, modrow], w=[pb])
                        if add1:
                            OP("dve", lambda e, pb=pb, dst=dst, hf=hf: e.tensor_scalar(
                                out=dst.t[:, hf * 512:(hf + 1) * 512], in0=pb.t[:], scalar1=1.0, scalar2=None, op0=ALU.add),
                               r=[pb], w=[dst])
                        else:
                            OP("act", lambda e, pb=pb, dst=dst, hf=hf: e.activation(
                                out=dst.t[:, hf * 512:(hf + 1) * 512], in_=pb.t[:], func=AF.Identity), r=[pb], w=[dst])
                    if di is not None:
                        S.dma("sp", sts, lambda e, di=di: e.dma_start(out=d_modbc[di], in_=bct.t[:]), r=[bct])
                S.barrier()

        if mode != "B":
            with ExitStack() as pa:
                xt = [sbuf(pa, "xt%d" % i, [128, 1024]) for i in range(2)]
                win = sbuf(pa, "win", [128, 8, 2336], BF16)
                wout = sbuf(pa, "wout", [128, 8, 1024], BF16)
                with ExitStack() as pw:
                    wst = [sbuf(pw, "wcst%d" % i, [128, 2336]) for i in range(2)]
                    win_v = d_win.rearrange("(k p) n -> k p n", p=128)
                    wout_v = d_wout.rearrange("(k p) n -> k p n", p=128)
                    for kc in range(8):
                        ws = wst[kc % 2]
                        S.dma("sp", ld[kc % 2], lambda e, ws=ws, kc=kc: e.dma_start(out=ws.t[:], in_=win_v[kc]), w=[ws])
                        OP("pool", lambda e, ws=ws, kc=kc: e.tensor_copy(out=win.t[:, kc, :], in_=ws.t[:]), r=[ws], w=[win])
                    for kc in range(8):
                        ws = wst[kc % 2]
                        S.dma("sp", ld[kc % 2], lambda e, ws=ws, kc=kc: e.dma_start(out=ws.t[:, 0:1024], in_=wout_v[kc]), w=[ws])
                        OP("pool", lambda e, ws=ws, kc=kc: e.tensor_copy(out=wout.t[:, kc, :], in_=ws.t[:, 0:1024]), r=[ws], w=[wout])
                    S.barrier()
                w2 = sbuf(pa, "w2", [128, 512])
                OP("pool", lambda e: e.memset(w2.t[:], 0.0), w=[w2])
                gng = sbuf(pa, "gng", [128, 128]); esink = sbuf(pa, "esink", [128, 8])
                ln1g = sbuf(pa, "ln1g", [128, 1024]); ln1b = sbuf(pa, "ln1b", [128, 1024])
                tri = sbuf(pa, "tri", [128, 4, 128]); ci = sbuf(pa, "ci", [128, 2])
                amask = sbuf(pa, "amask", [128, 2, 128]); gmask = sbuf(pa, "gmask", [128, 2, 64])
                amask_e = sbuf(pa, "amask_e", [128, 2, 128])
                S.dma("sp", cst, lambda e: e.dma_start(out=w2.t[0:16, :], in_=d_w2), w=[w2])
                S.dma("sp", cst, lambda e: e.dma_start(out=w2.t[16:17, :], in_=d_bg), w=[w2])
                for b_, d_ in ((gng, d_gng), (esink, d_sink), (ln1g, d_ln1g), (ln1b, d_ln1b),
                               (tri, d_tri), (ci, d_ci), (amask, d_amask), (gmask, d_gmask)):
                    S.dma("sp", cst, lambda e, b_=b_, d_=d_: e.dma_start(out=b_.t[:], in_=d_), w=[b_])
                S.barrier()
                OP("act", lambda e: e.activation(out=esink.t[:], in_=esink.t[:], func=AF.Exp), r=[esink], w=[esink])
                for m in range(2):
                    OP("dve", lambda e, m=m: e.tensor_scalar(out=amask_e.t[:, m, :], in0=amask.t[:, m, :],
                                                             scalar1=flags.t[:, 96 + m:97 + m], scalar2=None, op0=ALU.mult),
                       r=[amask, flags], w=[amask_e])
                sbst = sbuf(pa, "sbst", [128, 32, 256])
                kT_all = sbuf(pa, "kT_all", [64, 18, 256]); v_all = sbuf(pa, "v_all", [128, 18, 130])
                kTc = sbuf(pa, "kTc", [64, 2, 256]); vc = sbuf(pa, "vc", [128, 2, 130])
                OP("pool", lambda e: e.memset(v_all.t[:], 1.0), w=[v_all])
                OP("pool", lambda e: e.memset(vc.t[:], 1.0), w=[vc])
                hT2 = [sbuf(pa, "hT%d" % i, [128, 8, 128], BF16) for i in range(2)]
                vsb2 = [sbuf(pa, "vsb%d" % i, [128, 512]) for i in range(2)]
                curb = {"hT": hT2[0], "vsb": vsb2[0]}
                qk = sbuf(pa, "qk", [128, 512])
                zT = [sbuf(pa, "zT", [128, 128])] * 2
                OP("dve", lambda e: e.memset(zT[0].t[:], 0.0), w=[zT[0]])
                OP("dve", lambda e: e.memset(zT[0].t[0:17, :], 1.0), w=[zT[0]])
                sp_ = [sbuf(pa, "sp", [128, 256])] * 2
                et = sbuf(pa, "et", [128, 256])
                Eb = [sbuf(pa, "Eb", [128, 256])] * 2
                Ei = [sbuf(pa, "Ei", [128, 256])] * 2
                Er = [sbuf(pa, "Er", [128, 256])] * 2
                qe = [sbuf(pa, "qe", [128, 256])] * 2
                ke = [sbuf(pa, "ke", [128, 256])] * 2
                kd = [sbuf(pa, "kd", [128, 256])] * 2
                Tz = [sbuf(pa, "Tz%d" % i, [128, 4, 128]) for i in range(2)]
                keT = [sbuf(pa, "keT%d" % i, [128, 2, 128]) for i in range(2)]
                ATz = sbuf(pa, "ATz", [128, 2, 4, 2, 64])
                for b_ in (Tz[0], Tz[1], ATz):
                    OP("pool", lambda e, b_=b_: e.memset(b_.t[:], 0.0), w=[b_])
                asb = [sbuf(pa, "asb", [128, 2, 2])] * 2
                Sst = {0: [sbuf(pa, "Sf%d" % i, [128, 2, 128]) for i in range(2)],
                       1: [sbuf(pa, "Sb%d" % i, [128, 2, 128]) for i in range(2)]}
                Scur = {0: 0, 1: 0}
                Stmp = sbuf(pa, "Stmp", [128, 2, 128])
                for dd in range(2):
                    OP("dve", lambda e, dd=dd: e.memset(Sst[dd][0].t[:], 0.0), w=[Sst[dd][0]])
                rsb = sbuf(pa, "rsb", [128, 512])
                ss = sbuf(pa, "ss", [128, 4]); rstd4 = sbuf(pa, "rstd4", [128, 4])
                on = sbuf(pa, "on", [128, 512]); junk = on
                aqs = on; qrot = sbuf(pa, "qrot", [128, 512]); rt = rsb
                akv = sbuf(pa, "akv", [128, 256]); krot = sbuf(pa, "krot", [128, 128])
                cs = sbuf(pa, "cs", [128, 32]); sn = sbuf(pa, "sn", [128, 32])
                qTs = sbuf(pa, "qTs", [64, 8, 128])
                Pall = sbuf(pa, "Pall", [128, 5, 512])
                den = sbuf(pa, "den", [128, 4]); cat = sbuf(pa, "cat", [128, 1024])
                r1 = cat; x1o = cat
                stats = sbuf(pa, "stats", [128, 12]); mv = sbuf(pa, "mv", [128, 2]); rs1 = sbuf(pa, "rs1", [128, 1])

                def front(src_ap, row):
                    xb = xt[xt_i[0] % 2]
                    slot = ld[xt_i[0] % 2]
                    curb["hT"] = hT2[xt_i[0] % 2]; curb["vsb"] = vsb2[xt_i[0] % 2]
                    hT = curb["hT"]
                    xt_i[0] += 1
                    S.dma("sp", slot, lambda e: e.dma_start(out=xb.t[:], in_=src_ap), w=[xb])

                    def evac(c0, n, pb):
                        for c in range(c0, c0 + n):
                            OP("act", lambda e, c=c, pb=pb, c0=c0: e.activation(
                                out=hT.t[:, c, :], in_=pb.t[:, (c - c0) * 128:(c - c0 + 1) * 128], func=AF.Identity,
                                scale=sc1p.t[:, c, row:row + 1], bias=modT.t[:, c, row:row + 1]),
                               r=[pb, sc1p, modT], w=[hT])
                    transpose_to(xb, 8, evac)
                    return xb

                def inproj(col0, ncols, pb, pcol=0):
                    hT = curb["hT"]
                    for kc in range(8):
                        OP("pe", lambda e, kc=kc: e.matmul(pb.t[:, pcol:pcol + ncols], lhsT=hT.t[:, kc, :],
                                                           rhs=win.t[:, kc, col0:col0 + ncols], start=(kc == 0), stop=(kc == 7)),
                           r=[hT, win], w=[pb])

                def gates(dd, flagcol=None):
                    hT = curb["hT"]
                    pz = bank()
                    for kc in range(8):
                        OP("pe", lambda e, kc=kc: e.matmul(pz.t[0:16, 0:128], lhsT=win.t[:, kc, 2304 + 16 * dd:2320 + 16 * dd],
                                                           rhs=hT.t[:, kc, :], start=(kc == 0), stop=(kc == 7)), r=[hT, win], w=[pz])
                    OP("act", lambda e: e.activation(out=zT[dd].t[0:16, :], in_=pz.t[0:16, 0:128], func=AF.Identity), r=[pz], w=[zT[dd]])
                    pg = bank()
                    OP("pe", lambda e: e.matmul(pg.t[:, 0:256], lhsT=zT[dd].t[:], rhs=w2.t[:, dd * 256:(dd + 1) * 256],
                                                start=True, stop=True), r=[zT[dd], w2], w=[pg])
                    OP("act", lambda e: e.activation(out=et.t[:], in_=pg.t[:, 0:256], func=AF.Exp, scale=-1.0), r=[pg], w=[et])
                    OP("act", lambda e: e.activation(out=sp_[dd].t[:], in_=et.t[:], func=AF.Ln, bias=1.0), r=[et], w=[sp_[dd]])
                    if flagcol is not None:
                        OP("dve", lambda e: e.tensor_scalar(out=sp_[dd].t[:], in0=sp_[dd].t[:], scalar1=flags.t[:, flagcol:flagcol + 1],
                                                            scalar2=None, op0=ALU.mult), r=[sp_[dd], flags], w=[sp_[dd]])

                def decay_k(dd, ksrc, flagcol=None):
                    pr = bank()
                    OP("pe", lambda e: e.matmul(pr.t[:, 0:256], lhsT=tri.t[:, 1 + 2 * dd, :], rhs=sp_[dd].t[:], start=True, stop=True),
                       r=[tri, sp_[dd]], w=[pr])
                    OP("act", lambda e: e.activation(out=Er[dd].t[:], in_=pr.t[:, 0:256], func=AF.Exp), r=[pr], w=[Er[dd]])
                    if flagcol is None:
                        OP("dve", lambda e: e.tensor_tensor(out=kd[dd].t[:], in0=ksrc.t[:, 256:512], in1=Er[dd].t[:], op=ALU.mult),
                           r=[ksrc, Er[dd]], w=[kd[dd]])
                    else:
                        OP("dve", lambda e: e.scalar_tensor_tensor(out=kd[dd].t[:], in0=ksrc.t[:, 256:512],
                                                                   scalar=flags.t[:, flagcol:flagcol + 1], in1=Er[dd].t[:],
                                                                   op0=ALU.mult, op1=ALU.mult), r=[ksrc, Er[dd], flags], w=[kd[dd]])
                    pa_ = bank()
                    for hp in range(2):
                        OP("pe", lambda e, hp=hp: e.matmul(pa_.t[:, hp * 2:hp * 2 + 2], lhsT=sp_[dd].t[:, hp * 128:(hp + 1) * 128],
                                                           rhs=ci.t[:], start=True, stop=True), r=[sp_[dd], ci], w=[pa_])
                    OP("act", lambda e: e.activation(out=asb[dd].t[:], in_=pa_.t[:, 0:4].rearrange("p (a b) -> p a b", a=2),
                                                     func=AF.Exp), r=[pa_], w=[asb[dd]])

                def state_update(dd, c, vsrc):
                    pp = bank()
                    for h in range(4):
                        hp, par = h // 2, h % 2
                        OP("pe", lambda e, h=h, hp=hp, par=par: e.matmul(
                            pp.t[par * 64:(par + 1) * 64, hp * 128:(hp + 1) * 128],
                            lhsT=kd[dd].t[c * 64:(c + 1) * 64, h * 64:(h + 1) * 64],
                            rhs=vsrc.t[c * 64:(c + 1) * 64, h * 128:(h + 1) * 128], start=True, stop=True),
                           r=[kd[dd], vsrc], w=[pp])
                    so = Sst[dd][Scur[dd]]
                    sn_ = Sst[dd][1 - Scur[dd]]
                    OP("dve", lambda e: e.tensor_tensor(out=Stmp.t[:], in0=so.t[:],
                                                        in1=asb[dd].t[:, :, c:c + 1].broadcast_to([128, 2, 128]), op=ALU.mult),
                       r=[so, asb[dd]], w=[Stmp])
                    OP("dve", lambda e: e.tensor_tensor(out=sn_.t[:], in0=Stmp.t[:],
                                                        in1=pp.t[:, 0:256].rearrange("p (a b) -> p a b", a=2), op=ALU.add),
                       r=[Stmp, pp], w=[sn_])
                    Scur[dd] = 1 - Scur[dd]

                def state_tile(src_ap, row, dd, flagcol=None, store=None):
                    front(src_ap, row)
                    vsb = curb["vsb"]
                    pk = bank()
                    inproj(256, 256, pk, 256)
                    pv = bank()
                    inproj(512, 512, pv)
                    OP("act", lambda e: e.activation(out=vsb.t[:], in_=pv.t[:], func=AF.Identity), r=[pv], w=[vsb])
                    gates(dd, flagcol)
                    decay_k(dd, pk, flagcol)
                    for c in ((0, 1) if dd == 0 else (1, 0)):
                        if store is not None:
                            cur = Sst[dd][Scur[dd]]
                            OP("pool", lambda e, c=c, cur=cur: e.tensor_copy(out=sbst.t[:, store * 2 + c, :],
                                                                             in_=cur.t[:].rearrange("p a b -> p (a b)")),
                               r=[cur], w=[sbst])
                        state_update(dd, c, vsb)

                def rope(src, dst, H, tmp):
                    v5 = lambda b: b.t[:, 0:H * 64].rearrange("p (h a f d) -> p h a f d", h=H, a=2, f=2, d=16)
                    cb = cs.t[:].rearrange("p (a d) -> p a d", a=2).unsqueeze(1).broadcast_to([128, H, 2, 16])
                    sb_ = sn.t[:].rearrange("p (a d) -> p a d", a=2).unsqueeze(1).broadcast_to([128, H, 2, 16])
                    x1, x2 = v5(src)[:, :, :, 0, :], v5(src)[:, :, :, 1, :]
                    o1, o2 = v5(dst)[:, :, :, 0, :], v5(dst)[:, :, :, 1, :]
                    t1, t2 = v5(tmp)[:, :, :, 0, :], v5(tmp)[:, :, :, 1, :]
                    OP("pool", lambda e: e.tensor_tensor(out=o1, in0=x1, in1=cb, op=ALU.mult), r=[src, cs], w=[dst])
                    OP("pool", lambda e: e.tensor_tensor(out=t1, in0=x2, in1=sb_, op=ALU.mult), r=[src, sn], w=[tmp])
                    OP("pool", lambda e: e.tensor_tensor(out=o1, in0=o1, in1=t1, op=ALU.subtract), r=[dst, tmp], w=[dst])
                    OP("pool", lambda e: e.tensor_tensor(out=o2, in0=x2, in1=cb, op=ALU.mult), r=[src, cs], w=[dst])
                    OP("pool", lambda e: e.tensor_tensor(out=t2, in0=x1, in1=sb_, op=ALU.mult), r=[src, sn], w=[tmp])
                    OP("pool", lambda e: e.tensor_tensor(out=o2, in0=o2, in1=t2, op=ALU.add), r=[dst, tmp], w=[dst])

                def kv_tile(src_ap, row, j, is_ctx):
                    front(src_ap, row)
                    pb = bank()
                    inproj(2048, 256, pb)
                    OP("act", lambda e: e.activation(out=akv.t[:], in_=pb.t[:, 0:256], func=AF.Identity), r=[pb], w=[akv])
                    if is_ctx:
                        ksrc, kdst, vdst = akv, kTc, vc
                    else:
                        S.dma("sp", csl, lambda e: e.dma_start(out=cs.t[:], in_=d_cos[j * 128:(j + 1) * 128, :]), w=[cs])
                        S.dma("sp", snl, lambda e: e.dma_start(out=sn.t[:], in_=d_sin[j * 128:(j + 1) * 128, :]), w=[sn])
                        rope(akv, krot, 2, rt)
                        ksrc, kdst, vdst = krot, kT_all, v_all
                    OP("pool", lambda e: e.tensor_copy(
                        out=vdst.t[:, j, :].rearrange("p (g d) -> p g d", g=2)[:, :, 0:64],
                        in_=akv.t[:, 128:256].rearrange("p (g d) -> p g d", g=2)), r=[akv], w=[vdst])

                    def evac(c0, n, pb2):
                        OP("act", lambda e: e.activation(out=kdst.t[:, j, :], in_=pb2.t[0:64, 0:256], func=AF.Identity),
                           r=[pb2], w=[kdst])
                    transpose_to(ksrc, 2, evac, width=64)

                def own_tile(i):
                    j = i + 1
                    xb = front(d_xown[j * 128:(j + 1) * 128, :], 0)
                    vsb = curb["vsb"]; catT = curb["hT"]
                    pqk = bank(); inproj(0, 512, pqk)
                    OP("act", lambda e: e.activation(out=qk.t[:], in_=pqk.t[:], func=AF.Identity), r=[pqk], w=[qk])
                    pv = bank(); inproj(512, 512, pv)
                    OP("act", lambda e: e.activation(out=vsb.t[:], in_=pv.t[:], func=AF.Identity), r=[pv], w=[vsb])
                    pr_ = bank(); inproj(1024, 512, pr_)
                    OP("act", lambda e: e.activation(out=rsb.t[:], in_=pr_.t[:], func=AF.Silu), r=[pr_], w=[rsb])
                    for dd in range(2):
                        gates(dd)
                        pbn = bank()
                        OP("pe", lambda e, dd=dd, pbn=pbn: e.matmul(pbn.t[:, 0:256], lhsT=tri.t[:, 2 * dd, :], rhs=sp_[dd].t[:],
                                                                    start=True, stop=True), r=[tri, sp_[dd]], w=[pbn])
                        OP("act", lambda e, dd=dd, pbn=pbn: e.activation(out=Eb[dd].t[:], in_=pbn.t[:, 0:256], func=AF.Exp),
                           r=[pbn], w=[Eb[dd]])
                        OP("act", lambda e, dd=dd, pbn=pbn: e.activation(out=Ei[dd].t[:], in_=pbn.t[:, 0:256], func=AF.Exp, scale=-1.0),
                           r=[pbn], w=[Ei[dd]])
                        OP("dve", lambda e, dd=dd: e.scalar_tensor_tensor(out=qe[dd].t[:], in0=qk.t[:, 0:256], scalar=0.125, in1=Eb[dd].t[:],
                                                                          op0=ALU.mult, op1=ALU.mult), r=[qk, Eb[dd]], w=[qe[dd]])
                        OP("dve", lambda e, dd=dd: e.tensor_tensor(out=ke[dd].t[:], in0=qk.t[:, 256:512], in1=Ei[dd].t[:], op=ALU.mult),
                           r=[qk, Ei[dd]], w=[ke[dd]])
                        if dd == 0:
                            decay_k(dd, qk)
                        pT = bank()
                        for idx, srcb in enumerate((qe[dd], qe[dd], ke[dd], ke[dd])):
                            hp = idx % 2
                            OP("pe", lambda e, idx=idx, hp=hp, srcb=srcb, pT=pT: e.transpose(
                                out=pT.t[:, idx * 128:(idx + 1) * 128], in_=srcb.t[:, hp * 128:(hp + 1) * 128], identity=ident.t[:]),
                               r=[srcb, ident], w=[pT])
                        OP("act", lambda e, dd=dd, pT=pT: e.activation(out=keT[dd].t[:].rearrange("p a b -> p (a b)"), in_=pT.t[:, 256:512],
                                                                       func=AF.Identity), r=[pT], w=[keT[dd]])
                        for par in range(2):
                            OP("act", lambda e, dd=dd, pT=pT, par=par: e.activation(
                                out=Tz[dd].t[par * 64:(par + 1) * 64, par::2, :],
                                in_=pT.t[par * 64:(par + 1) * 64, 0:256].rearrange("p (a b) -> p a b", a=2), func=AF.Identity),
                               r=[pT], w=[Tz[dd]])
                    pAT = bank()
                    for dd in range(2):
                        for c in range(2):
                            for h in range(4):
                                hp = h // 2
                                OP("pe", lambda e, dd=dd, c=c, h=h, hp=hp: e.matmul(
                                    pAT.t[c * 64:(c + 1) * 64, (dd * 4 + h) * 64:(dd * 4 + h + 1) * 64],
                                    lhsT=keT[dd].t[:, hp, c * 64:(c + 1) * 64],
                                    rhs=Tz[dd].t[:, h, c * 64:(c + 1) * 64], start=True, stop=True),
                                   r=[keT[dd], Tz[dd]], w=[pAT])
                    for c in range(2):
                        OP("dve", lambda e, c=c: e.tensor_tensor(
                            out=ATz.t[c * 64:(c + 1) * 64, :, :, c, :],
                            in0=pAT.t[c * 64:(c + 1) * 64, :].rearrange("p (d h c) -> p d h c", d=2, h=4),
                            in1=gmask.t[c * 64:(c + 1) * 64, :, :].unsqueeze(2).broadcast_to([64, 2, 4, 64]), op=ALU.mult),
                           r=[pAT, gmask], w=[ATz])
                    po = bank()
                    for c in range(2):
                        sf = Sst[0][Scur[0]]
                        for h in range(4):
                            hp = h // 2
                            outp = po.t[c * 64:(c + 1) * 64, h * 128:(h + 1) * 128]
                            vv = vsb.t[:, h * 128:(h + 1) * 128]
                            OP("pe", lambda e, c=c, h=h, outp=outp, vv=vv: e.matmul(
                                outp, lhsT=ATz.t[:, 0, h, c, :], rhs=vv, start=True, stop=False), r=[ATz, vsb], w=[po])
                            OP("pe", lambda e, c=c, h=h, hp=hp, outp=outp, sf=sf: e.matmul(
                                outp, lhsT=Tz[0].t[:, h, c * 64:(c + 1) * 64], rhs=sf.t[:, hp, :], start=False, stop=False),
                               r=[Tz[0], sf], w=[po])
                            OP("pe", lambda e, c=c, h=h, outp=outp, vv=vv: e.matmul(
                                outp, lhsT=ATz.t[:, 1, h, c, :], rhs=vv, start=False, stop=False), r=[ATz, vsb], w=[po])
                            OP("pe", lambda e, c=c, h=h, hp=hp, outp=outp: e.matmul(
                                outp, lhsT=Tz[1].t[:, h, c * 64:(c + 1) * 64],
                                rhs=sbst.t[:, i * 2 + c, hp * 128:(hp + 1) * 128], start=False, stop=True), r=[Tz[1], sbst], w=[po])
                        state_update(0, c, vsb)
                    for h in range(4):
                        OP("act", lambda e, h=h: e.activation(out=junk.t[:, h * 128:(h + 1) * 128], in_=po.t[:, h * 128:(h + 1) * 128],
                                                              func=AF.Square, accum_out=ss.t[:, h:h + 1]), r=[po], w=[junk, ss])
                    OP("dve", lambda e: e.tensor_scalar(out=rstd4.t[:], in0=ss.t[:], scalar1=1.0 / 128, scalar2=eps_t.t[:, 0:1],
                                                        op0=ALU.mult, op1=ALU.add), r=[ss, eps_t], w=[rstd4])
                    OP("act", lambda e: e.activation(out=rstd4.t[:], in_=rstd4.t[:], func=AF.Sqrt), r=[rstd4], w=[rstd4])
                    OP("dve", lambda e: e.reciprocal(out=rstd4.t[:], in_=rstd4.t[:]), r=[rstd4], w=[rstd4])
                    on3 = on.t[:].rearrange("p (h d) -> p h d", h=4)
                    OP("dve", lambda e: e.tensor_tensor(out=on3, in0=po.t[:].rearrange("p (h d) -> p h d", h=4),
                                                        in1=rstd4.t[:].unsqueeze(2).broadcast_to([128, 4, 128]), op=ALU.mult),
                       r=[po, rstd4], w=[on])
                    OP("pool", lambda e: e.tensor_tensor(out=on3, in0=on3, in1=gng.t[:].unsqueeze(1).broadcast_to([128, 4, 128]),
                                                         op=ALU.mult), r=[on, gng], w=[on])
                    OP("pool", lambda e: e.tensor_tensor(out=cat.t[:, 0:512], in0=on.t[:], in1=rsb.t[:], op=ALU.mult),
                       r=[on, rsb], w=[cat])
                    paq = bank(); inproj(1536, 512, paq)
                    OP("act", lambda e: e.activation(out=aqs.t[:], in_=paq.t[:], func=AF.Identity), r=[paq], w=[aqs])
                    S.dma("sp", csl, lambda e: e.dma_start(out=cs.t[:], in_=d_cos[j * 128:(j + 1) * 128, :]), w=[cs])
                    S.dma("sp", snl, lambda e: e.dma_start(out=sn.t[:], in_=d_sin[j * 128:(j + 1) * 128, :]), w=[sn])
                    rope(aqs, qrot, 8, rt)

                    def evq(c0, n, pb2):
                        OP("act", lambda e: e.activation(out=qTs.t[:, c0:c0 + n, :].rearrange("p a b -> p (a b)"),
                                                         in_=pb2.t[0:64, 0:n * 128], func=AF.Identity, scale=0.125), r=[pb2], w=[qTs])
                    transpose_to(qrot, 8, evq, width=64)
                    def att_group(g):
                        kts = [(kT_all, v_all, j - 1, amask_e if i == 0 else amask, 0), (kT_all, v_all, j, None, 0),
                               (kT_all, v_all, j + 1, amask_e if i == 15 else amask, 1), (kTc, vc, 0, None, 0), (kTc, vc, 1, None, 0)]
                        for n_, (kb, vb, jj, mk, mi) in enumerate(kts):
                            pst = bank()
                            OP("pe", lambda e, kb=kb, jj=jj, pst=pst: e.matmul(
                                pst.t[:], lhsT=kb.t[:, jj, g * 128:(g + 1) * 128],
                                rhs=qTs.t[:, 4 * g:4 * g + 4, :].rearrange("p a b -> p (a b)"), start=True, stop=True),
                               r=[kb, qTs], w=[pst])
                            OP("act", lambda e, n_=n_, pst=pst: e.activation(out=Pall.t[:, n_, :], in_=pst.t[:], func=AF.Exp),
                               r=[pst], w=[Pall])
                            if mk is not None:
                                OP("dve", lambda e, n_=n_, mk=mk, mi=mi: e.tensor_tensor(
                                    out=Pall.t[:, n_, :].rearrange("p (h q) -> p h q", h=4),
                                    in0=Pall.t[:, n_, :].rearrange("p (h q) -> p h q", h=4),
                                    in1=mk.t[:, mi:mi + 1, :].broadcast_to([128, 4, 128]), op=ALU.mult), r=[Pall, mk], w=[Pall])
                        pO = bank()
                        for hh in range(4):
                            for n_, (kb, vb, jj, mk, mi) in enumerate(kts):
                                OP("pe", lambda e, hh=hh, n_=n_, vb=vb, jj=jj: e.matmul(
                                    pO.t[:, hh * 65:(hh + 1) * 65], lhsT=Pall.t[:, n_, hh * 128:(hh + 1) * 128],
                                    rhs=vb.t[:, jj, g * 65:(g + 1) * 65], start=(n_ == 0), stop=(n_ == 4)), r=[Pall, vb], w=[pO])
                        pO3 = pO.t[:, 0:260].rearrange("p (h d) -> p h d", h=4)
                        OP("dve", lambda e, pO3=pO3: e.tensor_tensor(out=den.t[:].unsqueeze(2), in0=pO3[:, :, 64:65],
                                                                     in1=esink.t[:, 4 * g:4 * g + 4].unsqueeze(2), op=ALU.add),
                           r=[pO, esink], w=[den])
                        OP("dve", lambda e: e.reciprocal(out=den.t[:], in_=den.t[:]), r=[den], w=[den])
                        OP("dve", lambda e, pO3=pO3: e.tensor_tensor(
                            out=cat.t[:, 512 + g * 256:512 + (g + 1) * 256].rearrange("p (h d) -> p h d", h=4),
                            in0=pO3[:, :, 0:64], in1=den.t[:].unsqueeze(2).broadcast_to([128, 4, 64]), op=ALU.mult),
                           r=[pO, den], w=[cat])
                    for g_ in range(2):
                        att_group(g_)
                    if os.environ.get("K_DBG") == "cat":
                        S.dma("pool", sts, lambda e: e.dma_start(out=d_x1[i * 128:(i + 1) * 128, :], in_=cat.t[:]), r=[cat])
                        return
                    def evc(c0, n, pb2):
                        OP("act", lambda e: e.activation(out=catT.t[:, c0:c0 + n, :].rearrange("p a b -> p (a b)"),
                                                         in_=pb2.t[:, 0:n * 128], func=AF.Identity), r=[pb2], w=[catT])
                    transpose_to(cat, 8, evc)
                    for hf in range(2):
                        py = bank()
                        for kc in range(8):
                            OP("pe", lambda e, kc=kc, py=py, hf=hf: e.matmul(py.t[:], lhsT=catT.t[:, kc, :],
                                                                      rhs=wout.t[:, kc, hf * 512:(hf + 1) * 512],
                                                                      start=(kc == 0), stop=(kc == 7)), r=[catT, wout], w=[py])
                        OP("dve", lambda e, py=py, hf=hf: e.tensor_tensor(out=r1.t[:, hf * 512:(hf + 1) * 512], in0=py.t[:],
                                                                   in1=g1bc.t[:, hf * 512:(hf + 1) * 512], op=ALU.mult),
                           r=[py, g1bc], w=[r1])
                    OP("dve", lambda e: e.scalar_tensor_tensor(out=r1.t[:], in0=xb.t[:], scalar=ALPHA, in1=r1.t[:],
                                                               op0=ALU.mult, op1=ALU.add), r=[xb, r1], w=[r1])
                    layernorm(r1, x1o, ln1g, ln1b, stats, mv, rs1)
                    S.dma("pool", sts, lambda e: e.dma_start(out=d_x1[i * 128:(i + 1) * 128, :], in_=x1o.t[:]), r=[x1o])

                def layernorm(src, dst, gb, bb, stats, mv, rs1):
                    for hf in range(2):
                        OP("dve", lambda e, hf=hf: e.bn_stats(out=stats.t[:, hf * 6:(hf + 1) * 6], in_=src.t[:, hf * 512:(hf + 1) * 512]),
                           r=[src], w=[stats])
                    OP("dve", lambda e: e.bn_aggr(out=mv.t[:], in_=stats.t[:]), r=[stats], w=[mv])
                    OP("dve", lambda e: e.tensor_scalar(out=rs1.t[:], in0=mv.t[:, 1:2], scalar1=eps_t.t[:, 0:1], scalar2=None, op0=ALU.add),
                       r=[mv, eps_t], w=[rs1])
                    OP("act", lambda e: e.activation(out=rs1.t[:], in_=rs1.t[:], func=AF.Sqrt), r=[rs1], w=[rs1])
                    OP("dve", lambda e: e.reciprocal(out=rs1.t[:], in_=rs1.t[:]), r=[rs1], w=[rs1])
                    OP("dve", lambda e: e.tensor_scalar(out=dst.t[:], in0=src.t[:], scalar1=mv.t[:, 0:1], scalar2=rs1.t[:, 0:1],
                                                        op0=ALU.subtract, op1=ALU.mult), r=[src, mv, rs1], w=[dst])
                    OP("pool", lambda e: e.tensor_tensor(out=dst.t[:], in0=dst.t[:], in1=gb.t[:], op=ALU.mult), r=[dst, gb], w=[dst])
                    OP("pool", lambda e: e.tensor_tensor(out=dst.t[:], in0=dst.t[:], in1=bb.t[:], op=ALU.add), r=[dst, bb], w=[dst])

                for t in range(2):
                    state_tile(d_ctx[t * 128:(t + 1) * 128, :], 1, 0)
                for t in (1, 0):
                    state_tile(d_ctx[t * 128:(t + 1) * 128, :], 1, 1)
                for t in range(2):
                    kv_tile(d_ctx[t * 128:(t + 1) * 128, :], 1, t, True)
                convert_tables()
                for t in range(NPRE):
                    state_tile(d_xpf[t * 128:(t + 1) * 128, :], 0, 0, flagcol=t)
                for t in range(NPRE):
                    state_tile(d_xpb[t * 128:(t + 1) * 128, :], 0, 1, flagcol=48 + t)
                for i in range(15, -1, -1):
                    state_tile(d_xown[(i + 1) * 128:(i + 2) * 128, :], 0, 1, store=i)
                for j in range(18):
                    kv_tile(d_xown[j * 128:(j + 1) * 128, :], 0, j, False)
                for i in range(16):
                    own_tile(i)
                S.barrier()

        if mode != "A":
            with ExitStack() as pbs_:
                wq = sbuf(pbs_, "wq", [128, 8, 1024]); skT = sbuf(pbs_, "skT", [128, 8, 128])
                ln2g = sbuf(pbs_, "ln2g", [128, 1024]); ln2b = sbuf(pbs_, "ln2b", [128, 1024])
                iota = sbuf(pbs_, "iota", [128, 16])
                S.dma("sp", cst, lambda e: e.dma_start(out=wq.t[:], in_=d_wq.rearrange("(k p) n -> p k n", p=128)), w=[wq])
                for b_, d_ in ((skT, d_skT), (ln2g, d_ln2g), (ln2b, d_ln2b), (iota, d_iota)):
                    S.dma("sp", cst, lambda e, b_=b_, d_=d_: e.dma_start(out=b_.t[:], in_=d_), w=[b_])
                S.barrier()
                if mode == "B":
                    convert_tables()
                identb = sbuf(pbs_, "identb", [128, 128], BF16)
                OP("dve", lambda e: e.tensor_copy(out=identb.t[:], in_=ident.t[:]), r=[ident], w=[identb])
                dg = [sbuf(pbs_, "dg%d" % i, [128, 128], BF16) for i in range(4)]
                sc2bc = sbuf(pbs_, "sc2bc", [128, 1024]); sh2bc = sbuf(pbs_, "sh2bc", [128, 1024]); g2bc = sbuf(pbs_, "g2bc", [128, 1024])
                if mode == "B":
                    OP("dve", lambda e: e.memset(sc2bc.t[:], 1.0), w=[sc2bc])
                    OP("dve", lambda e: e.memset(sh2bc.t[:], 0.0), w=[sh2bc])
                    OP("dve", lambda e: e.memset(g2bc.t[:], 1.0), w=[g2bc])
                else:
                    for b_, di in ((sh2bc, 0), (sc2bc, 1), (g2bc, 2)):
                        S.dma("sp", cst, lambda e, b_=b_, di=di: e.dma_start(out=b_.t[:], in_=d_modbc[di]), w=[b_])
                    S.barrier()
                x1t = [sbuf(pbs_, "x1t%d" % i, [128, 1024]) for i in range(3)]
                h2 = [sbuf(pbs_, "h2_%d" % i, [128, 1024]) for i in range(2)]
                h2T = sbuf(pbs_, "h2T", [128, 8, 128]); qTp = sbuf(pbs_, "qTp", [128, 8, 128])
                ssb = sbuf(pbs_, "ssb", [128, 16, 128]); sw = sbuf(pbs_, "sw", [128, 128])
                m16 = sbuf(pbs_, "m16", [128, 16, 16]); ix16 = sbuf(pbs_, "ix16", [128, 16, 16], U32)
                ixf = sbuf(pbs_, "ixf", [128, 16, 16])
                cand = sbuf(pbs_, "cand", [128, 8, 256]); cw = sbuf(pbs_, "cw", [128, 256])
                tsv = sbuf(pbs_, "tsv", [128, 8, 16]); pos = sbuf(pbs_, "pos", [128, 8, 16], U32)
                posf = sbuf(pbs_, "posf", [128, 8, 16])
                lohi = sbuf(pbs_, "lohi", [128, 2, 16])
                paf = sbuf(pbs_, "paf", [128, 8, 16]); pbf = sbuf(pbs_, "pbf", [128, 8, 16])
                oh = sbuf(pbs_, "oh", [128, 8, 16, 16])
                i1s = sbuf(pbs_, "i1s", [128, 8, 16]); i2s = sbuf(pbs_, "i2s", [128, 8, 16])
                eidf = sbuf(pbs_, "eidf", [128, 128])
                eidx = [sbuf(pbs_, "eidx%d" % i, [128, 128], U32) for i in range(3)]
                gate = [sbuf(pbs_, "gate%d" % i, [128, 8, 16]) for i in range(2)]
                gsum = sbuf(pbs_, "gsum", [128, 8])
                dots = [sbuf(pbs_, "dots%d" % i, [128, 128]) for i in range(2)]
                coef = [sbuf(pbs_, "coef%d" % i, [128, 128]) for i in range(2)]
                gb = [sbuf(pbs_, "gb%d" % i, [128, 2048], BF16) for i in range(NG)]
                gs = [S.dma_slot("g%d" % i) for i in range(NG)]
                prod = [sbuf(pbs_, "prod%d" % i, [128, 1024], BF16) for i in range(3)]
                h2b = [sbuf(pbs_, "h2b%d" % i, [128, 1024], BF16) for i in range(2)]
                acc = [sbuf(pbs_, "acc%d" % i, [128, 1024]) for i in range(2)]
                stats2 = sbuf(pbs_, "stats2", [128, 12]); mv2 = sbuf(pbs_, "mv2", [128, 2]); rs2 = sbuf(pbs_, "rs2", [128, 1])
                x1ld = [S.dma_slot("x1l%d" % i) for i in range(3)]

                def layernorm2(src, dst):
                    for hf in range(2):
                        OP("dve", lambda e, hf=hf: e.bn_stats(out=stats2.t[:, hf * 6:(hf + 1) * 6], in_=src.t[:, hf * 512:(hf + 1) * 512]),
                           r=[src], w=[stats2])
                    OP("dve", lambda e: e.bn_aggr(out=mv2.t[:], in_=stats2.t[:]), r=[stats2], w=[mv2])
                    OP("dve", lambda e: e.tensor_scalar(out=rs2.t[:], in0=mv2.t[:, 1:2], scalar1=eps_t.t[:, 0:1], scalar2=None, op0=ALU.add),
                       r=[mv2, eps_t], w=[rs2])
                    OP("act", lambda e: e.activation(out=rs2.t[:], in_=rs2.t[:], func=AF.Sqrt), r=[rs2], w=[rs2])
                    OP("dve", lambda e: e.reciprocal(out=rs2.t[:], in_=rs2.t[:]), r=[rs2], w=[rs2])
                    OP("dve", lambda e: e.tensor_scalar(out=dst.t[:], in0=src.t[:], scalar1=mv2.t[:, 0:1], scalar2=rs2.t[:, 0:1],
                                                        op0=ALU.subtract, op1=ALU.mult), r=[src, mv2, rs2], w=[dst])
                    OP("dve", lambda e: e.tensor_tensor(out=dst.t[:], in0=dst.t[:], in1=ln2g.t[:], op=ALU.mult), r=[dst, ln2g], w=[dst])
                    OP("dve", lambda e: e.tensor_tensor(out=dst.t[:], in0=dst.t[:], in1=ln2b.t[:], op=ALU.add), r=[dst, ln2b], w=[dst])

                def top16(src3, n, work, mdst, idst):
                    (sa, sbuf_), (wa, wbuf), (ma, mbuf), (ia, ibuf) = src3, work, mdst, idst
                    OP("dve", lambda e: e.max(out=ma[:, 0:8], in_=sa), r=[sbuf_], w=[mbuf])
                    OP("dve", lambda e: e.max_index(out=ia[:, 0:8], in_max=ma[:, 0:8], in_values=sa), r=[sbuf_, mbuf], w=[ibuf])
                    OP("dve", lambda e: e.match_replace(out=wa, in_to_replace=ma[:, 0:8], in_values=sa, imm_value=-1e30),
                       r=[sbuf_, mbuf], w=[wbuf])
                    OP("dve", lambda e: e.max(out=ma[:, 8:16], in_=wa), r=[wbuf], w=[mbuf])
                    OP("dve", lambda e: e.max_index(out=ia[:, 8:16], in_max=ma[:, 8:16], in_values=wa), r=[wbuf, mbuf], w=[ibuf])

                def route(i):
                    xs = x1t[i % 3]
                    S.dma("sp", x1ld[i % 3], lambda e: e.dma_start(out=xs.t[:], in_=d_x1[i * 128:(i + 1) * 128, :]), w=[xs])
                    hh = h2[i % 2]
                    OP("dve", lambda e: e.tensor_tensor(out=hh.t[:], in0=xs.t[:], in1=sc2bc.t[:], op=ALU.mult), r=[xs, sc2bc], w=[hh])
                    OP("dve", lambda e: e.tensor_tensor(out=hh.t[:], in0=hh.t[:], in1=sh2bc.t[:], op=ALU.add), r=[hh, sh2bc], w=[hh])
                    OP("act", lambda e: e.activation(out=h2b[i % 2].t[:], in_=hh.t[:], func=AF.Identity), r=[hh], w=[h2b[i % 2]])

                    if KSTOP < 2: return
                    def ev(c0, n, pb2):
                        OP("act", lambda e: e.activation(out=h2T.t[:, c0:c0 + n, :].rearrange("p a b -> p (a b)"),
                                                         in_=pb2.t[:, 0:n * 128], func=AF.Identity), r=[pb2], w=[h2T])
                    yield
                    transpose_to(hh, 8, ev)
                    yield
                    if KSTOP < 2.3: return
                    for j0 in range(0, 8, 4):
                        pq = bank()
                        for jj in range(j0, j0 + 4):
                            for kc in range(8):
                                OP("pe", lambda e, jj=jj, kc=kc, pq=pq, j0=j0: e.matmul(
                                    pq.t[:, (jj - j0) * 128:(jj - j0 + 1) * 128], lhsT=wq.t[:, kc, jj * 128:(jj + 1) * 128],
                                    rhs=h2T.t[:, kc, :], start=(kc == 0), stop=(kc == 7)), r=[wq, h2T], w=[pq])
                        OP("act", lambda e, pq=pq, j0=j0: e.activation(out=qTp.t[:, j0:j0 + 4, :].rearrange("p a b -> p (a b)"),
                                                                       in_=pq.t[:], func=AF.Identity), r=[pq], w=[qTp])
                    if KSTOP < 2.6: return
                    for h0 in range(0, 8, 4):
                        for a in range(2):
                            psc = bank()
                            for h in range(h0, h0 + 4):
                                OP("pe", lambda e, h=h, a=a, psc=psc, h0=h0: e.matmul(
                                    psc.t[:, (h - h0) * 128:(h - h0 + 1) * 128], lhsT=qTp.t[a * 64:(a + 1) * 64, h, :],
                                    rhs=skT.t[a * 64:(a + 1) * 64, h, :], start=True, stop=True), r=[qTp, skT], w=[psc])
                            OP("act", lambda e, psc=psc, h0=h0, a=a: e.activation(
                                out=ssb.t[:, 2 * h0 + a:2 * h0 + 8:2, :], in_=psc.t[:].rearrange("p (a b) -> p a b", a=4),
                                func=AF.Identity), r=[psc], w=[ssb])
                    if KSTOP < 3: return
                    for q_ in range(16):
                        top16((ssb.t[:, q_, :], ssb), 128, (sw.t[:], sw), (m16.t[:, q_, :], m16), (ix16.t[:, q_, :], ix16))
                        yield
                    if KSTOP < 4: return
                    OP("dve", lambda e: e.tensor_copy(out=ixf.t[:], in_=ix16.t[:]), r=[ix16], w=[ixf])
                    m4 = m16.t[:].rearrange("p (h a) k -> p h a k", a=2)
                    OP("dve", lambda e: e.tensor_tensor(
                        out=cand.t[:].rearrange("p h (a b) -> p h a b", a=16),
                        in0=m4[:, :, 0, :].unsqueeze(3).broadcast_to([128, 8, 16, 16]),
                        in1=m4[:, :, 1, :].unsqueeze(2).broadcast_to([128, 8, 16, 16]), op=ALU.add), r=[m16], w=[cand])
                    for h in range(8):
                        top16((cand.t[:, h, :], cand), 256, (cw.t[:], cw), (tsv.t[:, h, :], tsv), (pos.t[:, h, :], pos))
                        yield
                    gt = gate[i % 2]
                    OP("dve", lambda e: e.tensor_tensor(out=gt.t[:], in0=tsv.t[:], in1=tsv.t[:, :, 0:1].broadcast_to([128, 8, 16]),
                                                        op=ALU.subtract), r=[tsv], w=[gt])
                    OP("act", lambda e: e.activation(out=gt.t[:], in_=gt.t[:], func=AF.Exp), r=[gt], w=[gt])
                    OP("dve", lambda e: e.tensor_reduce(out=gsum.t[:], in_=gt.t[:], axis=AX.X, op=ALU.add), r=[gt], w=[gsum])
                    OP("dve", lambda e: e.reciprocal(out=gsum.t[:], in_=gsum.t[:]), r=[gsum], w=[gsum])
                    OP("dve", lambda e: e.tensor_tensor(out=gt.t[:], in0=gt.t[:], in1=gsum.t[:].unsqueeze(2).broadcast_to([128, 8, 16]),
                                                        op=ALU.mult), r=[gt, gsum], w=[gt])
                    if KSTOP < 5: return
                    yield
                    OP("dve", lambda e: e.tensor_copy(out=posf.t[:], in_=pos.t[:]), r=[pos], w=[posf])
                    oh2 = Buf(None); oh2.r = ssb.r
                    oh2.t = ssb.t[:].rearrange("p a b -> p (a b)").rearrange("p (h j c) -> p h j c", h=8, j=16)
                    ix4 = ixf.t[:].rearrange("p (h a) k -> p h a k", a=2)
                    bc4 = lambda ap2: ap2.unsqueeze(1).unsqueeze(1).broadcast_to([128, 8, 16, 16])
                    pf4 = posf.t[:].unsqueeze(3).broadcast_to([128, 8, 16, 16])
                    OP("dve", lambda e: e.tensor_tensor(out=oh.t[:], in0=pf4, in1=bc4(lohi.t[:, 0, :]), op=ALU.is_ge), r=[posf, lohi], w=[oh])
                    OP("dve", lambda e: e.tensor_tensor(out=oh2.t[:], in0=pf4, in1=bc4(lohi.t[:, 1, :]), op=ALU.is_ge), r=[posf, lohi], w=[oh2])
                    OP("dve", lambda e: e.tensor_tensor(out=oh.t[:], in0=oh.t[:], in1=oh2.t[:], op=ALU.subtract), r=[oh, oh2], w=[oh])
                    OP("dve", lambda e: e.tensor_tensor(out=oh2.t[:], in0=oh.t[:], in1=bc4(lohi.t[:, 0, :]), op=ALU.mult), r=[oh, lohi], w=[oh2])
                    yield
                    OP("dve", lambda e: e.tensor_reduce(out=paf.t[:], in_=oh2.t[:], axis=AX.X, op=ALU.add), r=[oh2], w=[paf])
                    OP("dve", lambda e: e.tensor_tensor(out=oh.t[:], in0=oh.t[:],
                                                        in1=ix4[:, :, 0, :].unsqueeze(2).broadcast_to([128, 8, 16, 16]), op=ALU.mult),
                       r=[oh, ixf], w=[oh])
                    OP("dve", lambda e: e.tensor_reduce(out=i1s.t[:], in_=oh.t[:], axis=AX.X, op=ALU.add), r=[oh], w=[i1s])
                    yield
                    OP("dve", lambda e: e.tensor_tensor(out=pbf.t[:], in0=posf.t[:], in1=paf.t[:], op=ALU.subtract), r=[posf, paf], w=[pbf])
                    OP("dve", lambda e: e.tensor_tensor(out=oh.t[:], in0=bc4(iota.t[:]),
                                                        in1=pbf.t[:].unsqueeze(3).broadcast_to([128, 8, 16, 16]), op=ALU.is_equal),
                       r=[iota, pbf], w=[oh])
                    OP("dve", lambda e: e.tensor_tensor(out=oh.t[:], in0=oh.t[:],
                                                        in1=ix4[:, :, 1, :].unsqueeze(2).broadcast_to([128, 8, 16, 16]), op=ALU.mult),
                       r=[oh, ixf], w=[oh])
                    OP("dve", lambda e: e.tensor_reduce(out=i2s.t[:], in_=oh.t[:], axis=AX.X, op=ALU.add), r=[oh], w=[i2s])
                    OP("dve", lambda e: e.scalar_tensor_tensor(out=eidf.t[:].rearrange("p (h k) -> p h k", h=8), in0=i1s.t[:], scalar=128.0,
                                                               in1=i2s.t[:], op0=ALU.mult, op1=ALU.add), r=[i1s, i2s], w=[eidf])
                    OP("dve", lambda e: e.tensor_copy(out=eidx[i % 3].t[:], in_=eidf.t[:]), r=[eidf], w=[eidx[i % 3]])

                gi = [0]

                class V:
                    def __init__(self, t):
                        self.t = t
                        self.r = Res()
                dcol = [[V(dots[p_].t) for _ in range(128)] for p_ in range(2)]
                ccol = [[V(coef[p_].t) for _ in range(128)] for p_ in range(2)]

                gk = {}

                def slot_u(i, s):
                    k = gi[0] % NG
                    gi[0] += 1
                    gk[(i, s)] = k
                    S.dma("pool", gs[k], lambda e: e.indirect_dma_start(
                        out=gb[k].t[:], out_offset=None, in_=d_puv16,
                        in_offset=bass.IndirectOffsetOnAxis(ap=eidx[i % 3].t[:, s:s + 1], axis=0)), r=[eidx[i % 3]] + puvB, w=[gb[k]])
                    pr = prod[s % 3]
                    dc, cc = dcol[i % 2][s], ccol[i % 2][s]
                    OP("dve", lambda e: e.tensor_tensor(out=pr.t[:], in0=gb[k].t[:, 0:1024], in1=h2b[i % 2].t[:], op=ALU.mult),
                       r=[gb[k], h2b[i % 2]], w=[pr])
                    OP("act", lambda e: e.activation(out=pr.t[:], in_=pr.t[:], func=AF.Identity, accum_out=dc.t[:, s:s + 1]),
                       r=[pr], w=[pr, dc])
                    OP("act", lambda e: e.activation(out=cc.t[:, s:s + 1], in_=dc.t[:, s:s + 1], func=AF.Gelu), r=[dc], w=[cc])

                def slot_v(i, s):
                    k = gk.pop((i, s))
                    cc = ccol[i % 2][s]
                    dgk = dg[s % 4]
                    OP("dve", lambda e: e.tensor_scalar(out=dgk.t[:], in0=identb.t[:], scalar1=cc.t[:, s:s + 1],
                                                        scalar2=gate[i % 2].t[:, s // 16, s % 16:s % 16 + 1], op0=ALU.mult, op1=ALU.mult),
                       r=[identb, cc, gate[i % 2]], w=[dgk])
                    for hf in range(2):
                        OP("pe", lambda e, hf=hf: e.matmul(accP[hf].t[:], lhsT=dgk.t[:], rhs=gb[k].t[:, 1024 + hf * 512:1536 + hf * 512],
                                                           start=(s == 0), stop=(s == 127)), r=[dgk, gb[k]], w=[accP[hf]])

                def finish_v(i):
                    r2 = acc[i % 2]; yo = acc[i % 2]
                    for hf in range(2):
                        OP("dve", lambda e, hf=hf: e.tensor_tensor(out=r2.t[:, hf * 512:(hf + 1) * 512], in0=accP[hf].t[:],
                                                                   in1=g2bc.t[:, hf * 512:(hf + 1) * 512], op=ALU.mult),
                           r=[accP[hf], g2bc], w=[r2])
                    OP("dve", lambda e: e.scalar_tensor_tensor(out=r2.t[:], in0=x1t[i % 3].t[:], scalar=ALPHA, in1=r2.t[:],
                                                               op0=ALU.mult, op1=ALU.add), r=[x1t[i % 3], r2], w=[r2])
                    layernorm2(r2, yo)
                    S.dma("sp", sts, lambda e: e.dma_start(out=d_out[i * 128:(i + 1) * 128, :], in_=yo.t[:]), r=[yo])

                OP("dve", lambda e: e.tensor_scalar(out=lohi.t[:, 0, :], in0=iota.t[:], scalar1=16.0, scalar2=None, op0=ALU.mult), r=[iota], w=[lohi])
                OP("dve", lambda e: e.tensor_scalar(out=lohi.t[:, 1, :], in0=iota.t[:], scalar1=16.0, scalar2=16.0, op0=ALU.mult, op1=ALU.add),
                   r=[iota], w=[lohi])
                NT = int(os.environ.get('K_NT', '16'))
                KSTOP = float(os.environ.get('K_STOP', '9'))
                for _ in route(0):
                    pass
                for i in range(NT):
                    gen = route(i + 1) if i + 1 < NT else iter(())
                    for s in range(128 + LAG if KSTOP >= 6 else 0):
                        if s < 128:
                            slot_u(i, s)
                        if s >= LAG:
                            slot_v(i, s - LAG)
                        next(gen, None)
                    for _ in gen:
                        pass
                    if KSTOP >= 7:
                        finish_v(i)
                S.barrier()
        S.barrier()
        S.run()
    return nc


def _consts():
    s = np.arange(128)[:, None]
    t = np.arange(128)[None, :]
    same = (s // 64) == (t // 64)
    g = -1.0 / 16.0
    tri = np.stack([(same & (s <= t)), (same & (s > t)), (same & (s >= t)), (same & (s < t))], axis=1).astype(np.float32) * g
    ci = np.stack([(np.arange(128) // 64 == c) for c in range(2)], axis=1).astype(np.float32) * g
    amask = np.stack([(s >= t), (s <= t)], axis=1).astype(np.float32)
    sc = (np.arange(128) % 64)[:, None]
    cc = np.arange(64)[None, :]
    gmask = np.stack([(sc <= cc), (sc >= cc)], axis=1).astype(np.float32)
    iota = np.broadcast_to(np.arange(16, dtype=np.float32), (128, 16)).copy()
    return dict(ident=np.eye(128, dtype=np.float32), tri=np.ascontiguousarray(tri), ci=np.ascontiguousarray(ci),
                amask=np.ascontiguousarray(amask), gmask=np.ascontiguousarray(gmask), iota16=iota)


def _rope_tables():
    rows = 8192 // 64
    row = np.repeat(np.arange(rows, dtype=np.float32), 64)
    col = np.tile(np.arange(64, dtype=np.float32), rows)
    inv = (np.float32(10000.0) ** (-np.arange(16, dtype=np.float32) / np.float32(16))).astype(np.float32)
    ang = np.stack([row[:, None] * inv, col[:, None] * inv], axis=1).astype(np.float32)
    return np.cos(ang).reshape(8192, 32).astype(np.float32), np.sin(ang).reshape(8192, 32).astype(np.float32)


def make_in_maps(x, c, ctx, c_ctx, w_ada, b_ada, w_in, w_gate2_f, b_gate_f, w_gate2_b, b_gate_b, gla_norm_g, attn_sink,
                 w_out, ln1_g, ln1_b, peer_wq, peer_subkeys, peer_u, peer_v, ln2_g, ln2_b):
    f = lambda a: np.ascontiguousarray(np.asarray(a, dtype=np.float32))
    bc = lambda v, n: np.ascontiguousarray(np.broadcast_to(np.asarray(v, np.float32).reshape(1, -1), (128, n)))
    x = f(x); ctx = f(ctx)
    wi = f(w_in[0])
    wperm = np.concatenate([wi[:, 0:256], wi[:, 256:512], wi[:, 512:1024], wi[:, 1024:1536], wi[:, 1568:2080],
                            wi[:, 2080:2208], wi[:, 2208:2336], wi[:, 1536:1552], wi[:, 1552:1568]], axis=1)
    cosT, sinT = _rope_tables()
    common = dict(
        w_ada=f(w_ada[0]), b_ada=f(b_ada[0]).reshape(1, -1), w_in=np.ascontiguousarray(wperm),
        w2=np.ascontiguousarray(np.concatenate([f(w_gate2_f[0]), f(w_gate2_b[0])], axis=1)),
        bg=np.ascontiguousarray(np.concatenate([f(b_gate_f[0]), f(b_gate_b[0])]).reshape(1, -1)),
        gng=bc(gla_norm_g[0], 128), sink=bc(attn_sink[0], 8), w_out=f(w_out[0]),
        ln1g=bc(ln1_g[0], 1024), ln1b=bc(ln1_b[0], 1024), ln2g=bc(ln2_g[0], 1024), ln2b=bc(ln2_b[0], 1024),
        peer_wq=f(peer_wq[0]),
        skT=np.ascontiguousarray(np.transpose(f(peer_subkeys[0]), (1, 3, 0, 2)).reshape(128, 8, 128)),
        peer_uv=np.ascontiguousarray(np.concatenate([f(peer_u[0]), f(peer_v[0])], axis=1)), **_consts())
    maps = []
    zt = np.zeros((128, 1024), np.float32)
    for core in range(8):
        b, s = core // 4, core % 4
        xb = x[b].reshape(64, 128, 1024)
        npf = 16 * s
        xpf = np.zeros((NPRE, 128, 1024), np.float32)
        if npf:
            xpf[NPRE - npf:] = xb[0:npf]
        npb = 16 * (3 - s)
        xpb = np.zeros((NPRE, 128, 1024), np.float32)
        if npb:
            xpb[NPRE - npb:] = xb[63:16 * (s + 1) - 1:-1]
        flags = np.zeros((128, 100), np.float32)
        flags[:, NPRE - npf:NPRE] = 1.0
        flags[:, 48 + NPRE - npb:48 + NPRE] = 1.0
        t0 = 16 * s
        own = np.zeros((18, 128, 1024), np.float32)
        own[1:17] = xb[t0:t0 + 16]
        cos_o = np.zeros((18, 128, 32), np.float32); sin_o = np.zeros((18, 128, 32), np.float32)
        cos_o[1:17] = cosT.reshape(64, 128, 32)[t0:t0 + 16]; sin_o[1:17] = sinT.reshape(64, 128, 32)[t0:t0 + 16]
        if t0 > 0:
            own[0] = xb[t0 - 1]; flags[:, 96] = 1.0
            cos_o[0] = cosT.reshape(64, 128, 32)[t0 - 1]; sin_o[0] = sinT.reshape(64, 128, 32)[t0 - 1]
        if t0 + 16 < 64:
            own[17] = xb[t0 + 16]; flags[:, 97] = 1.0
            cos_o[17] = cosT.reshape(64, 128, 32)[t0 + 16]; sin_o[17] = sinT.reshape(64, 128, 32)[t0 + 16]
        c2 = np.stack([f(c)[b], f(c_ctx)], axis=0)
        c2T = np.ascontiguousarray(c2.reshape(2, 8, 128).transpose(2, 1, 0))
        m = dict(common)
        m.update(xpre_f=xpf.reshape(-1, 1024), xpre_b=xpb.reshape(-1, 1024), xown=own.reshape(-1, 1024), ctx=ctx[b],
                 flags=flags, c2T=c2T, cosT=cos_o.reshape(-1, 32), sinT=sin_o.reshape(-1, 32))
        maps.append(m)
    return maps


_NC_CACHE = {}


def kernel(**inputs):
    if "full" not in _NC_CACHE:
        _NC_CACHE["full"] = build_nc("full")
    nc = _NC_CACHE["full"]
    maps = make_in_maps(**inputs)
    res = run_bass_kernel_spmd(nc, maps, core_ids=list(range(8)))
    out = np.zeros((2, 8192, 1024), np.float32)
    for core in range(8):
        b, s = core // 4, core % 4
        out[b, s * 2048:(s + 1) * 2048] = np.asarray(res.results[core]["out"], np.float32)
    return out
```

```python
import os
import numpy as np
from contextlib import ExitStack
import concourse.bass as bass
import concourse.mybir as mybir
from concourse.bass_utils import run_bass_kernel_spmd

F32 = mybir.dt.float32
BF16 = mybir.dt.bfloat16
U32 = mybir.dt.uint32
AF = mybir.ActivationFunctionType
ALU = mybir.AluOpType
AX = mybir.AxisListType

SAME_ENGINE_SYNC = True
NPRE = 48
LN_EPS = 1e-5
ALPHA = 2.0 ** 0.25
NG = 12
LAG = 3


class Res:
    __slots__ = ("w", "rs")

    def __init__(self):
        self.w = None
        self.rs = {}


class Buf:
    def __init__(self, t):
        self.t = t
        self.r = Res()


class Sched:
    def __init__(self, nc, stack):
        self.nc = nc
        self.stack = stack
        self.sems = {}
        self.count = {}
        self.seen = {k: {} for k in ("pe", "dve", "act", "pool", "sp")}
        self.streams = {k: [] for k in ("pe", "dve", "act", "pool", "sp")}
        for k in ("pe", "dve", "act", "pool"):
            self.sems[k] = stack.enter_context(nc.semaphore("sem_" + k))
            self.count[k] = 0
        self.nslots = 0

    def dma_slot(self, name=""):
        self.nslots += 1
        key = "d%d%s" % (self.nslots, name)
        self.sems[key] = self.stack.enter_context(self.nc.semaphore("s_" + key))
        self.count[key] = 0
        return key

    def _waits(self, q, reads, writes, same_ok):
        deps = {}
        for b in reads:
            r = b.r
            if r.w is not None:
                k, c = r.w
                deps[k] = max(deps.get(k, 0), c)
        for b in writes:
            w = b.r
            if w.w is not None:
                k, c = w.w
                deps[k] = max(deps.get(k, 0), c)
            for k, c in w.rs.items():
                deps[k] = max(deps.get(k, 0), c)
        out = []
        for k, c in deps.items():
            if k == q and not same_ok:
                continue
            if self.seen[q].get(k, 0) >= c:
                continue
            self.seen[q][k] = c
            out.append((k, c))
        return out

    def op(self, q, fn, r=(), w=()):
        same_ok = SAME_ENGINE_SYNC and q != "pe"
        waits = self._waits(q, r, w, same_ok)
        self.count[q] += 1
        c = self.count[q]
        sems = self.sems
        st = self.streams[q]
        for k, v in waits:
            st.append(lambda e, k=k, v=v: e.wait_ge(sems[k], v))
        st.append(lambda e, fn=fn: fn(e).then_inc(sems[q], 1))
        for b in r:
            b.r.rs[q] = c
        for b in w:
            b.r.w = (q, c)
            b.r.rs = {}

    def dma(self, q, slot, fn, r=(), w=()):
        waits = self._waits(q, r, w, True)
        prev = self.count[slot]
        if prev > 0 and self.seen[q].get(slot, 0) < prev:
            self.seen[q][slot] = prev
            waits.append((slot, prev))
        self.count[slot] += 16
        c = self.count[slot]
        sems = self.sems
        st = self.streams[q]
        for k, v in waits:
            st.append(lambda e, k=k, v=v: e.wait_ge(sems[k], v))
        st.append(lambda e, fn=fn: fn(e).then_inc(sems[slot], 16))
        for b in r:
            b.r.rs[slot] = c
        for b in w:
            b.r.w = (slot, c)
            b.r.rs = {}

    def wait_all(self, q):
        sems = self.sems
        for k in list(self.count.keys()):
            c = self.count[k]
            if c == 0 or k == q or self.seen[q].get(k, 0) >= c:
                continue
            self.seen[q][k] = c
            self.streams[q].append(lambda e, k=k, c=c: e.wait_ge(sems[k], c))

    def barrier(self):
        for q in ("pe", "dve", "act", "pool", "sp"):
            self.wait_all(q)

    def run(self):
        streams = self.streams
        with self.nc.Block() as block:
            @block.tensor
            def _(e):
                for f in streams["pe"]:
                    f(e)

            @block.vector
            def _(e):
                for f in streams["dve"]:
                    f(e)

            @block.scalar
            def _(e):
                for f in streams["act"]:
                    f(e)

            @block.gpsimd
            def _(e):
                for f in streams["pool"]:
                    f(e)

            @block.sync
            def _(e):
                for f in streams["sp"]:
                    f(e)


def build_nc(mode="full"):
    nc = bass.Bass("TRN2", target_bir_lowering=False)
    D = lambda name, shape, dt=F32, kind="ExternalInput": nc.dram_tensor(name, shape, dt, kind=kind).ap()
    d_xpf = D("xpre_f", [NPRE * 128, 1024]); d_xpb = D("xpre_b", [NPRE * 128, 1024])
    d_xown = D("xown", [18 * 128, 1024]); d_ctx = D("ctx", [256, 1024])
    d_flags = D("flags", [128, 100]); d_c2T = D("c2T", [128, 8, 2])
    d_wada = D("w_ada", [1024, 6144]); d_bada = D("b_ada", [1, 6144])
    d_win = D("w_in", [1024, 2336]); d_w2 = D("w2", [16, 512]); d_bg = D("bg", [1, 512])
    d_gng = D("gng", [128, 128]); d_sink = D("sink", [128, 8])
    d_wout = D("w_out", [1024, 1024])
    d_ln1g = D("ln1g", [128, 1024]); d_ln1b = D("ln1b", [128, 1024])
    d_ln2g = D("ln2g", [128, 1024]); d_ln2b = D("ln2b", [128, 1024])
    d_wq = D("peer_wq", [1024, 1024]); d_skT = D("skT", [128, 8, 128])
    d_puv = D("peer_uv", [16384, 2048])
    d_puv16 = D("puv16", [16384, 2048], BF16, kind="Internal")
    d_cos = D("cosT", [18 * 128, 32]); d_sin = D("sinT", [18 * 128, 32])
    d_ident = D("ident", [128, 128]); d_tri = D("tri", [128, 4, 128]); d_ci = D("ci", [128, 2])
    d_amask = D("amask", [128, 2, 128]); d_gmask = D("gmask", [128, 2, 64]); d_iota = D("iota16", [128, 16])
    d_out = D("out", [2048, 1024], kind="ExternalOutput")
    if mode == "B":
        d_x1 = D("x1s", [2048, 1024])
    else:
        d_x1 = D("x1s", [2048, 1024], kind="ExternalOutput" if mode == "A" else "Internal")

    with ExitStack() as top:
        S = Sched(nc, top)
        OP = S.op

        def sbuf(st, name, shape, dt=F32):
            return Buf(st.enter_context(nc.sbuf_tensor("s_" + name, shape, dt)))

        banks = [Buf(top.enter_context(nc.psum_tensor("pb%d" % i, [128, 512], F32))) for i in range(6)]
        accP = [Buf(top.enter_context(nc.psum_tensor("pacc%d" % i, [128, 512], F32))) for i in range(2)]
        bank_i = [0]

        def bank():
            b = banks[bank_i[0] % 6]
            bank_i[0] += 1
            return b

        ld = [S.dma_slot("ld%d" % i) for i in range(2)]
        cst = S.dma_slot("cst")
        sts = S.dma_slot("st")
        csl = S.dma_slot("cs"); snl = S.dma_slot("sn")

        puvB = [Buf(None) for _ in range(16)]
        cvs = [S.dma_slot("cv%d" % i) for i in range(4)]

        def convert_tables():
            for ci_ in range(16):
                S.dma("pool", cvs[ci_ % 4], lambda e, ci_=ci_: e.dma_start(out=d_puv16[ci_ * 1024:(ci_ + 1) * 1024, :],
                                                                         in_=d_puv[ci_ * 1024:(ci_ + 1) * 1024, :]), w=[puvB[ci_]])
        ident = sbuf(top, "ident", [128, 128])
        S.dma("sp", cst, lambda e: e.dma_start(out=ident.t[:], in_=d_ident), w=[ident])
        flags = sbuf(top, "flags", [128, 100])
        S.dma("sp", cst, lambda e: e.dma_start(out=flags.t[:], in_=d_flags), w=[flags])
        eps_t = sbuf(top, "eps_t", [128, 1])
        OP("dve", lambda e: e.memset(eps_t.t[:], LN_EPS), w=[eps_t])
        modT = sbuf(top, "modT", [128, 48, 2])
        sc1p = sbuf(top, "sc1p", [128, 8, 2])
        g1bc = sbuf(top, "g1bc", [128, 1024])
        d_modbc = D("modbc", [3, 128, 1024], kind="Internal")
        xt_i = [0]

        def transpose_to(src, nchunks, dst_fn, width=128, rows=128):
            for c0 in range(0, nchunks, 4):
                pb = bank()
                n = min(4, nchunks - c0)
                for c in range(c0, c0 + n):
                    OP("pe", lambda e, c=c, pb=pb, c0=c0: e.transpose(
                        out=pb.t[0:width, (c - c0) * 128:(c - c0) * 128 + rows],
                        in_=src.t[0:rows, c * width:(c + 1) * width], identity=ident.t[0:rows, 0:rows]),
                       r=[src, ident], w=[pb])
                dst_fn(c0, n, pb)

        if mode != "B":
            with ExitStack() as p0:
                c2T = sbuf(p0, "c2T", [128, 8, 2]); sc2 = sbuf(p0, "sc2", [128, 8, 2])
                S.dma("sp", cst, lambda e: e.dma_start(out=c2T.t[:], in_=d_c2T), w=[c2T])
                OP("act", lambda e: e.activation(out=sc2.t[:], in_=c2T.t[:], func=AF.Silu), r=[c2T], w=[sc2])
                modrow = sbuf(p0, "modrow", [2, 6144])
                brow = sbuf(p0, "brow", [128, 6144]); ones2 = sbuf(p0, "ones2", [128, 2])
                OP("pool", lambda e: e.memset(brow.t[:], 0.0), w=[brow])
                S.dma("sp", cst, lambda e: e.dma_start(out=brow.t[0:1, :], in_=d_bada), w=[brow])
                OP("dve", lambda e: e.memset(ones2.t[:], 0.0), w=[ones2])
                OP("dve", lambda e: e.memset(ones2.t[0:1, :], 1.0), w=[ones2])
                S.barrier()
                wst = [sbuf(p0, "wst%d" % i, [128, 3072]) for i in range(2)]
                wada_v = d_wada.rearrange("(k p) n -> k p n", p=128)
                for half in range(2):
                    pbs = [bank() for _ in range(6)]
                    for j in range(6):
                        col = half * 3072 + j * 512
                        OP("pe", lambda e, pbj=pbs[j], col=col: e.matmul(pbj.t[0:2, :], lhsT=ones2.t[:], rhs=brow.t[:, col:col + 512],
                                                                        start=True, stop=False), r=[ones2, brow], w=[pbs[j]])
                    for kc in range(8):
                        ws = wst[kc % 2]
                        S.dma("sp", ld[kc % 2], lambda e, ws=ws, kc=kc, half=half: e.dma_start(
                            out=ws.t[:], in_=wada_v[kc, :, half * 3072:(half + 1) * 3072]), w=[ws])
                        for j in range(6):
                            OP("pe", lambda e, j=j, ws=ws, kc=kc, pbj=pbs[j]: e.matmul(
                                pbj.t[0:2, :], lhsT=sc2.t[:, kc, :], rhs=ws.t[:, j * 512:(j + 1) * 512],
                                start=False, stop=(kc == 7)), r=[sc2, ws], w=[pbs[j]])
                    for j in range(6):
                        col = half * 3072 + j * 512
                        OP("act", lambda e, pbj=pbs[j], col=col: e.activation(out=modrow.t[:, col:col + 512], in_=pbj.t[0:2, :],
                                                                       func=AF.Identity), r=[pbs[j]], w=[modrow])
                for c0 in range(0, 48, 4):
                    pb = bank()
                    for c in range(c0, c0 + 4):
                        OP("pe", lambda e, c=c, pb=pb, c0=c0: e.transpose(
                            out=pb.t[:, (c - c0) * 2:(c - c0) * 2 + 2], in_=modrow.t[0:2, c * 128:(c + 1) * 128],
                            identity=ident.t[0:2, 0:2]), r=[modrow, ident], w=[pb])
                    OP("dve", lambda e, pb=pb, c0=c0: e.tensor_copy(
                        out=modT.t[:, c0:c0 + 4, :], in_=pb.t[:, 0:8].rearrange("p (a b) -> p a b", a=4)), r=[pb], w=[modT])
                OP("dve", lambda e: e.tensor_scalar(out=sc1p.t[:], in0=modT.t[:, 8:16, :], scalar1=1.0, scalar2=None, op0=ALU.add),
                   r=[modT], w=[sc1p])
                sel = sbuf(p0, "sel", [2, 128])
                OP("dve", lambda e: e.memset(sel.t[:], 0.0), w=[sel])
                OP("dve", lambda e: e.memset(sel.t[0:1, :], 1.0), w=[sel])
                bct = sbuf(p0, "bct", [128, 1024])
                for dst, j, add1, di in ((g1bc, 2, False, None), (bct, 3, False, 0), (bct, 4, True, 1), (bct, 5, False, 2)):
                    for hf in range(2):
                        pb = bank()
                        col = j * 1024 + hf * 512
                        OP("pe", lambda e, pb=pb, col=col: e.matmul(pb.t[:], lhsT=sel.t[:], rhs=modrow.t[:, col:col + 512],
                                                                    start=True, stop=True), r=[sel, modrow], w=[pb])
                        if add1:
                            OP("dve", lambda e, pb=pb, dst=dst, hf=hf: e.tensor_scalar(
                                out=dst.t[:, hf * 512:(hf + 1) * 512], in0=pb.t[:], scalar1=1.0, scalar2=None, op0=ALU.add),
                               r=[pb], w=[dst])
                        else:
                            OP("act", lambda e, pb=pb, dst=dst, hf=hf: e.activation(
                                out=dst.t[:, hf * 512:(hf + 1) * 512], in_=pb.t[:], func=AF.Identity), r=[pb], w=[dst])
                    if di is not None:
                        S.dma("sp", sts, lambda e, di=di: e.dma_start(out=d_modbc[di], in_=bct.t[:]), r=[bct])
                S.barrier()

        if mode != "B":
            with ExitStack() as pa:
                xt = [sbuf(pa, "xt%d" % i, [128, 1024]) for i in range(2)]
                win = sbuf(pa, "win", [128, 8, 2336], BF16)
                wout = sbuf(pa, "wout", [128, 8, 1024], BF16)
                with ExitStack() as pw:
                    wst = [sbuf(pw, "wcst%d" % i, [128, 2336]) for i in range(2)]
                    win_v = d_win.rearrange("(k p) n -> k p n", p=128)
                    wout_v = d_wout.rearrange("(k p) n -> k p n", p=128)
                    for kc in range(8):
                        ws = wst[kc % 2]
                        S.dma("sp", ld[kc % 2], lambda e, ws=ws, kc=kc: e.dma_start(out=ws.t[:], in_=win_v[kc]), w=[ws])
                        OP("pool", lambda e, ws=ws, kc=kc: e.tensor_copy(out=win.t[:, kc, :], in_=ws.t[:]), r=[ws], w=[win])
                    for kc in range(8):
                        ws = wst[kc % 2]
                        S.dma("sp", ld[kc % 2], lambda e, ws=ws, kc=kc: e.dma_start(out=ws.t[:, 0:1024], in_=wout_v[kc]), w=[ws])
                        OP("pool", lambda e, ws=ws, kc=kc: e.tensor_copy(out=wout.t[:, kc, :], in_=ws.t[:, 0:1024]), r=[ws], w=[wout])
                    S.barrier()
                w2 = sbuf(pa, "w2", [128, 512])
                OP("pool", lambda e: e.memset(w2.t[:], 0.0), w=[w2])
                gng = sbuf(pa, "gng", [128, 128]); esink = sbuf(pa, "esink", [128, 8])
                ln1g = sbuf(pa, "ln1g", [128, 1024]); ln1b = sbuf(pa, "ln1b", [128, 1024])
                tri = sbuf(pa, "tri", [128, 4, 128]); ci = sbuf(pa, "ci", [128, 2])
                amask = sbuf(pa, "amask", [128, 2, 128]); gmask = sbuf(pa, "gmask", [128, 2, 64])
                amask_e = sbuf(pa, "amask_e", [128, 2, 128])
                S.dma("sp", cst, lambda e: e.dma_start(out=w2.t[0:16, :], in_=d_w2), w=[w2])
                S.dma("sp", cst, lambda e: e.dma_start(out=w2.t[16:17, :], in_=d_bg), w=[w2])
                for b_, d_ in ((gng, d_gng), (esink, d_sink), (ln1g, d_ln1g), (ln1b, d_ln1b),
                               (tri, d_tri), (ci, d_ci), (amask, d_amask), (gmask, d_gmask)):
                    S.dma("sp", cst, lambda e, b_=b_, d_=d_: e.dma_start(out=b_.t[:], in_=d_), w=[b_])
                S.barrier()
                OP("act", lambda e: e.activation(out=esink.t[:], in_=esink.t[:], func=AF.Exp), r=[esink], w=[esink])
                for m in range(2):
                    OP("dve", lambda e, m=m: e.tensor_scalar(out=amask_e.t[:, m, :], in0=amask.t[:, m, :],
                                                             scalar1=flags.t[:, 96 + m:97 + m], scalar2=None, op0=ALU.mult),
                       r=[amask, flags], w=[amask_e])
                sbst = sbuf(pa, "sbst", [128, 32, 256])
                kT_all = sbuf(pa, "kT_all", [64, 18, 256]); v_all = sbuf(pa, "v_all", [128, 18, 130])
                kTc = sbuf(pa, "kTc", [64, 2, 256]); vc = sbuf(pa, "vc", [128, 2, 130])
                OP("pool", lambda e: e.memset(v_all.t[:], 1.0), w=[v_all])
                OP("pool", lambda e: e.memset(vc.t[:], 1.0), w=[vc])
                hT2 = [sbuf(pa, "hT%d" % i, [128, 8, 128], BF16) for i in range(2)]
                vsb2 = [sbuf(pa, "vsb%d" % i, [128, 512]) for i in range(2)]
                curb = {"hT": hT2[0], "vsb": vsb2[0]}
                qk = sbuf(pa, "qk", [128, 512])
                zT = [sbuf(pa, "zT", [128, 128])] * 2
                OP("dve", lambda e: e.memset(zT[0].t[:], 0.0), w=[zT[0]])
                OP("dve", lambda e: e.memset(zT[0].t[0:17, :], 1.0), w=[zT[0]])
                sp_ = [sbuf(pa, "sp", [128, 256])] * 2
                et = sbuf(pa, "et", [128, 256])
                Eb = [sbuf(pa, "Eb", [128, 256])] * 2
                Ei = [sbuf(pa, "Ei", [128, 256])] * 2
                Er = [sbuf(pa, "Er", [128, 256])] * 2
                qe = [sbuf(pa, "qe", [128, 256])] * 2
                ke = [sbuf(pa, "ke", [128, 256])] * 2
                kd = [sbuf(pa, "kd", [128, 256])] * 2
                Tz = [sbuf(pa, "Tz%d" % i, [128, 4, 128]) for i in range(2)]
                keT = [sbuf(pa, "keT%d" % i, [128, 2, 128]) for i in range(2)]
                ATz = sbuf(pa, "ATz", [128, 2, 4, 2, 64])
                for b_ in (Tz[0], Tz[1], ATz):
                    OP("pool", lambda e, b_=b_: e.memset(b_.t[:], 0.0), w=[b_])
                asb = [sbuf(pa, "asb", [128, 2, 2])] * 2
                Sst = {0: [sbuf(pa, "Sf%d" % i, [128, 2, 128]) for i in range(2)],
                       1: [sbuf(pa, "Sb%d" % i, [128, 2, 128]) for i in range(2)]}
                Scur = {0: 0, 1: 0}
                Stmp = sbuf(pa, "Stmp", [128, 2, 128])
                for dd in range(2):
                    OP("dve", lambda e, dd=dd: e.memset(Sst[dd][0].t[:], 0.0), w=[Sst[dd][0]])
                rsb = sbuf(pa, "rsb", [128, 512])
                ss = sbuf(pa, "ss", [128, 4]); rstd4 = sbuf(pa, "rstd4", [128, 4])
                on = sbuf(pa, "on", [128, 512]); junk = on
                aqs = on; qrot = sbuf(pa, "qrot", [128, 512]); rt = rsb
                akv = sbuf(pa, "akv", [128, 256]); krot = sbuf(pa, "krot", [128, 128])
                cs = sbuf(pa, "cs", [128, 32]); sn = sbuf(pa, "sn", [128, 32])
                qTs = sbuf(pa, "qTs", [64, 8, 128])
                Pall = sbuf(pa, "Pall", [128, 5, 512])
                den = sbuf(pa, "den", [128, 4]); cat = sbuf(pa, "cat", [128, 1024])
                r1 = cat; x1o = cat
                stats = sbuf(pa, "stats", [128, 12]); mv = sbuf(pa, "mv", [128, 2]); rs1 = sbuf(pa, "rs1", [128, 1])

                def front(src_ap, row):
                    xb = xt[xt_i[0] % 2]
                    slot = ld[xt_i[0] % 2]
                    curb["hT"] = hT2[xt_i[0] % 2]; curb["vsb"] = vsb2[xt_i[0] % 2]
                    hT = curb["hT"]
                    xt_i[0] += 1
                    S.dma("sp", slot, lambda e: e.dma_start(out=xb.t[:], in_=src_ap), w=[xb])

                    def evac(c0, n, pb):
                        for c in range(c0, c0 + n):
                            if c % 2 == 0:
                                OP("act", lambda e, c=c, pb=pb, c0=c0: e.activation(
                                    out=hT.t[:, c, :], in_=pb.t[:, (c - c0) * 128:(c - c0 + 1) * 128], func=AF.Identity,
                                    scale=sc1p.t[:, c, row:row + 1], bias=modT.t[:, c, row:row + 1]),
                                   r=[pb, sc1p, modT], w=[hT])
                            else:
                                OP("dve", lambda e, c=c, pb=pb, c0=c0: e.tensor_scalar(
                                    out=hT.t[:, c, :], in0=pb.t[:, (c - c0) * 128:(c - c0 + 1) * 128],
                                    scalar1=sc1p.t[:, c, row:row + 1], scalar2=modT.t[:, c, row:row + 1], op0=ALU.mult, op1=ALU.add),
                                   r=[pb, sc1p, modT], w=[hT])
                    transpose_to(xb, 8, evac)
                    return xb

                def inproj(col0, ncols, pb, pcol=0):
                    hT = curb["hT"]
                    for kc in range(8):
                        OP("pe", lambda e, kc=kc: e.matmul(pb.t[:, pcol:pcol + ncols], lhsT=hT.t[:, kc, :],
                                                           rhs=win.t[:, kc, col0:col0 + ncols], start=(kc == 0), stop=(kc == 7)),
                           r=[hT, win], w=[pb])

                def gates(dd, flagcol=None):
                    hT = curb["hT"]
                    pz = bank()
                    for kc in range(8):
                        OP("pe", lambda e, kc=kc: e.matmul(pz.t[0:16, 0:128], lhsT=win.t[:, kc, 2304 + 16 * dd:2320 + 16 * dd],
                                                           rhs=hT.t[:, kc, :], start=(kc == 0), stop=(kc == 7)), r=[hT, win], w=[pz])
                    OP("act", lambda e: e.activation(out=zT[dd].t[0:16, :], in_=pz.t[0:16, 0:128], func=AF.Identity), r=[pz], w=[zT[dd]])
                    pg = bank()
                    OP("pe", lambda e: e.matmul(pg.t[:, 0:256], lhsT=zT[dd].t[:], rhs=w2.t[:, dd * 256:(dd + 1) * 256],
                                                start=True, stop=True), r=[zT[dd], w2], w=[pg])
                    OP("act", lambda e: e.activation(out=et.t[:], in_=pg.t[:, 0:256], func=AF.Exp, scale=-1.0), r=[pg], w=[et])
                    OP("act", lambda e: e.activation(out=sp_[dd].t[:], in_=et.t[:], func=AF.Ln, bias=1.0), r=[et], w=[sp_[dd]])
                    if flagcol is not None:
                        OP("dve", lambda e: e.tensor_scalar(out=sp_[dd].t[:], in0=sp_[dd].t[:], scalar1=flags.t[:, flagcol:flagcol + 1],
                                                            scalar2=None, op0=ALU.mult), r=[sp_[dd], flags], w=[sp_[dd]])

                def decay_k(dd, ksrc, flagcol=None):
                    pr = bank()
                    OP("pe", lambda e: e.matmul(pr.t[:, 0:256], lhsT=tri.t[:, 1 + 2 * dd, :], rhs=sp_[dd].t[:], start=True, stop=True),
                       r=[tri, sp_[dd]], w=[pr])
                    OP("act", lambda e: e.activation(out=Er[dd].t[:], in_=pr.t[:, 0:256], func=AF.Exp), r=[pr], w=[Er[dd]])
                    if flagcol is None:
                        OP("dve", lambda e: e.tensor_tensor(out=kd[dd].t[:], in0=ksrc.t[:, 256:512], in1=Er[dd].t[:], op=ALU.mult),
                           r=[ksrc, Er[dd]], w=[kd[dd]])
                    else:
                        OP("dve", lambda e: e.scalar_tensor_tensor(out=kd[dd].t[:], in0=ksrc.t[:, 256:512],
                                                                   scalar=flags.t[:, flagcol:flagcol + 1], in1=Er[dd].t[:],
                                                                   op0=ALU.mult, op1=ALU.mult), r=[ksrc, Er[dd], flags], w=[kd[dd]])
                    pa_ = bank()
                    for hp in range(2):
                        OP("pe", lambda e, hp=hp: e.matmul(pa_.t[:, hp * 2:hp * 2 + 2], lhsT=sp_[dd].t[:, hp * 128:(hp + 1) * 128],
                                                           rhs=ci.t[:], start=True, stop=True), r=[sp_[dd], ci], w=[pa_])
                    OP("act", lambda e: e.activation(out=asb[dd].t[:], in_=pa_.t[:, 0:4].rearrange("p (a b) -> p a b", a=2),
                                                     func=AF.Exp), r=[pa_], w=[asb[dd]])

                def state_update(dd, c, vsrc):
                    pp = bank()
                    for h in range(4):
                        hp, par = h // 2, h % 2
                        OP("pe", lambda e, h=h, hp=hp, par=par: e.matmul(
                            pp.t[par * 64:(par + 1) * 64, hp * 128:(hp + 1) * 128],
                            lhsT=kd[dd].t[c * 64:(c + 1) * 64, h * 64:(h + 1) * 64],
                            rhs=vsrc.t[c * 64:(c + 1) * 64, h * 128:(h + 1) * 128], start=True, stop=True),
                           r=[kd[dd], vsrc], w=[pp])
                    so = Sst[dd][Scur[dd]]
                    sn_ = Sst[dd][1 - Scur[dd]]
                    OP("dve", lambda e: e.tensor_tensor(out=Stmp.t[:], in0=so.t[:],
                                                        in1=asb[dd].t[:, :, c:c + 1].broadcast_to([128, 2, 128]), op=ALU.mult),
                       r=[so, asb[dd]], w=[Stmp])
                    OP("dve", lambda e: e.tensor_tensor(out=sn_.t[:], in0=Stmp.t[:],
                                                        in1=pp.t[:, 0:256].rearrange("p (a b) -> p a b", a=2), op=ALU.add),
                       r=[Stmp, pp], w=[sn_])
                    Scur[dd] = 1 - Scur[dd]

                def state_tile(src_ap, row, dd, flagcol=None, store=None):
                    front(src_ap, row)
                    vsb = curb["vsb"]
                    pk = bank()
                    inproj(256, 256, pk, 256)
                    pv = bank()
                    inproj(512, 512, pv)
                    OP("act", lambda e: e.activation(out=vsb.t[:], in_=pv.t[:], func=AF.Identity), r=[pv], w=[vsb])
                    gates(dd, flagcol)
                    decay_k(dd, pk, flagcol)
                    for c in ((0, 1) if dd == 0 else (1, 0)):
                        if store is not None:
                            cur = Sst[dd][Scur[dd]]
                            OP("pool", lambda e, c=c, cur=cur: e.tensor_copy(out=sbst.t[:, store * 2 + c, :],
                                                                             in_=cur.t[:].rearrange("p a b -> p (a b)")),
                               r=[cur], w=[sbst])
                        state_update(dd, c, vsb)

                ksb2 = [sbuf(pa, "ksb%d" % i, [128, 512]) for i in range(2)]
                zT2 = [sbuf(pa, "zTp%d" % i, [128, 128]) for i in range(2)]
                for z_ in zT2:
                    OP("dve", lambda e, z_=z_: e.memset(z_.t[:], 0.0), w=[z_])
                    OP("dve", lambda e, z_=z_: e.memset(z_.t[0:17, :], 1.0), w=[z_])
                st_i = [0]

                def state_A(src_ap, row, dd):
                    front(src_ap, row)
                    hT = curb["hT"]
                    p_ = st_i[0] % 2
                    st_i[0] += 1
                    vsb, ksb, zTb = vsb2[p_], ksb2[p_], zT2[p_]
                    pk = bank()
                    inproj(256, 256, pk, 256)
                    OP("dve", lambda e: e.tensor_copy(out=ksb.t[:, 256:512], in_=pk.t[:, 256:512]), r=[pk], w=[ksb])
                    pv = bank()
                    inproj(512, 512, pv)
                    OP("act", lambda e: e.activation(out=vsb.t[:], in_=pv.t[:], func=AF.Identity), r=[pv], w=[vsb])
                    pz = bank()
                    for kc in range(8):
                        OP("pe", lambda e, kc=kc: e.matmul(pz.t[0:16, 0:128], lhsT=win.t[:, kc, 2304 + 16 * dd:2320 + 16 * dd],
                                                           rhs=hT.t[:, kc, :], start=(kc == 0), stop=(kc == 7)), r=[hT, win], w=[pz])
                    OP("act", lambda e: e.activation(out=zTb.t[0:16, :], in_=pz.t[0:16, 0:128], func=AF.Identity), r=[pz], w=[zTb])
                    return (vsb, ksb, zTb)

                def state_B(ctx_, dd, flagcol=None, store=None):
                    vsb, ksb, zTb = ctx_
                    pg = bank()
                    OP("pe", lambda e: e.matmul(pg.t[:, 0:256], lhsT=zTb.t[:], rhs=w2.t[:, dd * 256:(dd + 1) * 256],
                                                start=True, stop=True), r=[zTb, w2], w=[pg])
                    OP("act", lambda e: e.activation(out=et.t[:], in_=pg.t[:, 0:256], func=AF.Exp, scale=-1.0), r=[pg], w=[et])
                    OP("act", lambda e: e.activation(out=sp_[dd].t[:], in_=et.t[:], func=AF.Ln, bias=1.0), r=[et], w=[sp_[dd]])
                    if flagcol is not None:
                        OP("dve", lambda e: e.tensor_scalar(out=sp_[dd].t[:], in0=sp_[dd].t[:], scalar1=flags.t[:, flagcol:flagcol + 1],
                                                            scalar2=None, op0=ALU.mult), r=[sp_[dd], flags], w=[sp_[dd]])
                    decay_k(dd, ksb, flagcol)
                    for c in ((0, 1) if dd == 0 else (1, 0)):
                        if store is not None:
                            cur = Sst[dd][Scur[dd]]
                            OP("pool", lambda e, c=c, cur=cur: e.tensor_copy(out=sbst.t[:, store * 2 + c, :],
                                                                             in_=cur.t[:].rearrange("p a b -> p (a b)")),
                               r=[cur], w=[sbst])
                        state_update(dd, c, vsb)

                def run_state_jobs(jobs):
                    prev = None
                    for (src_ap, row, dd, flagcol, store) in jobs:
                        ctx_ = state_A(src_ap, row, dd)
                        if prev is not None:
                            state_B(*prev)
                        prev = (ctx_, dd, flagcol, store)
                    if prev is not None:
                        state_B(*prev)

                def rope(src, dst, H, tmp):
                    v5 = lambda b: b.t[:, 0:H * 64].rearrange("p (h a f d) -> p h a f d", h=H, a=2, f=2, d=16)
                    cb = cs.t[:].rearrange("p (a d) -> p a d", a=2).unsqueeze(1).broadcast_to([128, H, 2, 16])
                    sb_ = sn.t[:].rearrange("p (a d) -> p a d", a=2).unsqueeze(1).broadcast_to([128, H, 2, 16])
                    x1, x2 = v5(src)[:, :, :, 0, :], v5(src)[:, :, :, 1, :]
                    o1, o2 = v5(dst)[:, :, :, 0, :], v5(dst)[:, :, :, 1, :]
                    t1, t2 = v5(tmp)[:, :, :, 0, :], v5(tmp)[:, :, :, 1, :]
                    OP("pool", lambda e: e.tensor_tensor(out=o1, in0=x1, in1=cb, op=ALU.mult), r=[src, cs], w=[dst])
                    OP("pool", lambda e: e.tensor_tensor(out=t1, in0=x2, in1=sb_, op=ALU.mult), r=[src, sn], w=[tmp])
                    OP("pool", lambda e: e.tensor_tensor(out=o1, in0=o1, in1=t1, op=ALU.subtract), r=[dst, tmp], w=[dst])
                    OP("pool", lambda e: e.tensor_tensor(out=o2, in0=x2, in1=cb, op=ALU.mult), r=[src, cs], w=[dst])
                    OP("pool", lambda e: e.tensor_tensor(out=t2, in0=x1, in1=sb_, op=ALU.mult), r=[src, sn], w=[tmp])
                    OP("pool", lambda e: e.tensor_tensor(out=o2, in0=o2, in1=t2, op=ALU.add), r=[dst, tmp], w=[dst])

                def kv_tile(src_ap, row, j, is_ctx):
                    front(src_ap, row)
                    pb = bank()
                    inproj(2048, 256, pb)
                    OP("act", lambda e: e.activation(out=akv.t[:], in_=pb.t[:, 0:256], func=AF.Identity), r=[pb], w=[akv])
                    if is_ctx:
                        ksrc, kdst, vdst = akv, kTc, vc
                    else:
                        S.dma("sp", csl, lambda e: e.dma_start(out=cs.t[:], in_=d_cos[j * 128:(j + 1) * 128, :]), w=[cs])
                        S.dma("sp", snl, lambda e: e.dma_start(out=sn.t[:], in_=d_sin[j * 128:(j + 1) * 128, :]), w=[sn])
                        rope(akv, krot, 2, rt)
                        ksrc, kdst, vdst = krot, kT_all, v_all
                    OP("pool", lambda e: e.tensor_copy(
                        out=vdst.t[:, j, :].rearrange("p (g d) -> p g d", g=2)[:, :, 0:64],
                        in_=akv.t[:, 128:256].rearrange("p (g d) -> p g d", g=2)), r=[akv], w=[vdst])

                    def evac(c0, n, pb2):
                        OP("act", lambda e: e.activation(out=kdst.t[:, j, :], in_=pb2.t[0:64, 0:256], func=AF.Identity),
                           r=[pb2], w=[kdst])
                    transpose_to(ksrc, 2, evac, width=64)

                def own_tile(i):
                    j = i + 1
                    xb = front(d_xown[j * 128:(j + 1) * 128, :], 0)
                    vsb = curb["vsb"]; catT = curb["hT"]
                    pqk = bank(); inproj(0, 512, pqk)
                    OP("act", lambda e: e.activation(out=qk.t[:], in_=pqk.t[:], func=AF.Identity), r=[pqk], w=[qk])
                    pv = bank(); inproj(512, 512, pv)
                    OP("act", lambda e: e.activation(out=vsb.t[:], in_=pv.t[:], func=AF.Identity), r=[pv], w=[vsb])
                    pr_ = bank(); inproj(1024, 512, pr_)
                    OP("act", lambda e: e.activation(out=rsb.t[:], in_=pr_.t[:], func=AF.Silu), r=[pr_], w=[rsb])
                    for dd in range(2):
                        gates(dd)
                        pbn = bank()
                        OP("pe", lambda e, dd=dd, pbn=pbn: e.matmul(pbn.t[:, 0:256], lhsT=tri.t[:, 2 * dd, :], rhs=sp_[dd].t[:],
                                                                    start=True, stop=True), r=[tri, sp_[dd]], w=[pbn])
                        OP("act", lambda e, dd=dd, pbn=pbn: e.activation(out=Eb[dd].t[:], in_=pbn.t[:, 0:256], func=AF.Exp),
                           r=[pbn], w=[Eb[dd]])
                        OP("act", lambda e, dd=dd, pbn=pbn: e.activation(out=Ei[dd].t[:], in_=pbn.t[:, 0:256], func=AF.Exp, scale=-1.0),
                           r=[pbn], w=[Ei[dd]])
                        OP("dve", lambda e, dd=dd: e.scalar_tensor_tensor(out=qe[dd].t[:], in0=qk.t[:, 0:256], scalar=0.125, in1=Eb[dd].t[:],
                                                                          op0=ALU.mult, op1=ALU.mult), r=[qk, Eb[dd]], w=[qe[dd]])
                        OP("dve", lambda e, dd=dd: e.tensor_tensor(out=ke[dd].t[:], in0=qk.t[:, 256:512], in1=Ei[dd].t[:], op=ALU.mult),
                           r=[qk, Ei[dd]], w=[ke[dd]])
                        if dd == 0:
                            decay_k(dd, qk)
                        pT = bank()
                        for idx, srcb in enumerate((qe[dd], qe[dd], ke[dd], ke[dd])):
                            hp = idx % 2
                            OP("pe", lambda e, idx=idx, hp=hp, srcb=srcb, pT=pT: e.transpose(
                                out=pT.t[:, idx * 128:(idx + 1) * 128], in_=srcb.t[:, hp * 128:(hp + 1) * 128], identity=ident.t[:]),
                               r=[srcb, ident], w=[pT])
                        OP("act", lambda e, dd=dd, pT=pT: e.activation(out=keT[dd].t[:].rearrange("p a b -> p (a b)"), in_=pT.t[:, 256:512],
                                                                       func=AF.Identity), r=[pT], w=[keT[dd]])
                        for par in range(2):
                            OP("act", lambda e, dd=dd, pT=pT, par=par: e.activation(
                                out=Tz[dd].t[par * 64:(par + 1) * 64, par::2, :],
                                in_=pT.t[par * 64:(par + 1) * 64, 0:256].rearrange("p (a b) -> p a b", a=2), func=AF.Identity),
                               r=[pT], w=[Tz[dd]])
                    pAT = bank()
                    for dd in range(2):
                        for c in range(2):
                            for h in range(4):
                                hp = h // 2
                                OP("pe", lambda e, dd=dd, c=c, h=h, hp=hp: e.matmul(
                                    pAT.t[c * 64:(c + 1) * 64, (dd * 4 + h) * 64:(dd * 4 + h + 1) * 64],
                                    lhsT=keT[dd].t[:, hp, c * 64:(c + 1) * 64],
                                    rhs=Tz[dd].t[:, h, c * 64:(c + 1) * 64], start=True, stop=True),
                                   r=[keT[dd], Tz[dd]], w=[pAT])
                    for c in range(2):
                        OP("dve", lambda e, c=c: e.tensor_tensor(
                            out=ATz.t[c * 64:(c + 1) * 64, :, :, c, :],
                            in0=pAT.t[c * 64:(c + 1) * 64, :].rearrange("p (d h c) -> p d h c", d=2, h=4),
                            in1=gmask.t[c * 64:(c + 1) * 64, :, :].unsqueeze(2).broadcast_to([64, 2, 4, 64]), op=ALU.mult),
                           r=[pAT, gmask], w=[ATz])
                    po = bank()
                    for c in range(2):
                        sf = Sst[0][Scur[0]]
                        for h in range(4):
                            hp = h // 2
                            outp = po.t[c * 64:(c + 1) * 64, h * 128:(h + 1) * 128]
                            vv = vsb.t[:, h * 128:(h + 1) * 128]
                            OP("pe", lambda e, c=c, h=h, outp=outp, vv=vv: e.matmul(
                                outp, lhsT=ATz.t[:, 0, h, c, :], rhs=vv, start=True, stop=False), r=[ATz, vsb], w=[po])
                            OP("pe", lambda e, c=c, h=h, hp=hp, outp=outp, sf=sf: e.matmul(
                                outp, lhsT=Tz[0].t[:, h, c * 64:(c + 1) * 64], rhs=sf.t[:, hp, :], start=False, stop=False),
                               r=[Tz[0], sf], w=[po])
                            OP("pe", lambda e, c=c, h=h, outp=outp, vv=vv: e.matmul(
                                outp, lhsT=ATz.t[:, 1, h, c, :], rhs=vv, start=False, stop=False), r=[ATz, vsb], w=[po])
                            OP("pe", lambda e, c=c, h=h, hp=hp, outp=outp: e.matmul(
                                outp, lhsT=Tz[1].t[:, h, c * 64:(c + 1) * 64],
                                rhs=sbst.t[:, i * 2 + c, hp * 128:(hp + 1) * 128], start=False, stop=True), r=[Tz[1], sbst], w=[po])
                        state_update(0, c, vsb)
                    for h in range(4):
                        OP("act", lambda e, h=h: e.activation(out=junk.t[:, h * 128:(h + 1) * 128], in_=po.t[:, h * 128:(h + 1) * 128],
                                                              func=AF.Square, accum_out=ss.t[:, h:h + 1]), r=[po], w=[junk, ss])
                    OP("dve", lambda e: e.tensor_scalar(out=rstd4.t[:], in0=ss.t[:], scalar1=1.0 / 128, scalar2=eps_t.t[:, 0:1],
                                                        op0=ALU.mult, op1=ALU.add), r=[ss, eps_t], w=[rstd4])
                    OP("act", lambda e: e.activation(out=rstd4.t[:], in_=rstd4.t[:], func=AF.Sqrt), r=[rstd4], w=[rstd4])
                    OP("dve", lambda e: e.reciprocal(out=rstd4.t[:], in_=rstd4.t[:]), r=[rstd4], w=[rstd4])
                    on3 = on.t[:].rearrange("p (h d) -> p h d", h=4)
                    OP("dve", lambda e: e.tensor_tensor(out=on3, in0=po.t[:].rearrange("p (h d) -> p h d", h=4),
                                                        in1=rstd4.t[:].unsqueeze(2).broadcast_to([128, 4, 128]), op=ALU.mult),
                       r=[po, rstd4], w=[on])
                    OP("pool", lambda e: e.tensor_tensor(out=on3, in0=on3, in1=gng.t[:].unsqueeze(1).broadcast_to([128, 4, 128]),
                                                         op=ALU.mult), r=[on, gng], w=[on])
                    OP("pool", lambda e: e.tensor_tensor(out=cat.t[:, 0:512], in0=on.t[:], in1=rsb.t[:], op=ALU.mult),
                       r=[on, rsb], w=[cat])
                    paq = bank(); inproj(1536, 512, paq)
                    OP("act", lambda e: e.activation(out=aqs.t[:], in_=paq.t[:], func=AF.Identity), r=[paq], w=[aqs])
                    S.dma("sp", csl, lambda e: e.dma_start(out=cs.t[:], in_=d_cos[j * 128:(j + 1) * 128, :]), w=[cs])
                    S.dma("sp", snl, lambda e: e.dma_start(out=sn.t[:], in_=d_sin[j * 128:(j + 1) * 128, :]), w=[sn])
                    rope(aqs, qrot, 8, rt)

                    def evq(c0, n, pb2):
                        OP("act", lambda e: e.activation(out=qTs.t[:, c0:c0 + n, :].rearrange("p a b -> p (a b)"),
                                                         in_=pb2.t[0:64, 0:n * 128], func=AF.Identity, scale=0.125), r=[pb2], w=[qTs])
                    transpose_to(qrot, 8, evq, width=64)
                    def att_group(g):
                        kts = [(kT_all, v_all, j - 1, amask_e if i == 0 else amask, 0), (kT_all, v_all, j, None, 0),
                               (kT_all, v_all, j + 1, amask_e if i == 15 else amask, 1), (kTc, vc, 0, None, 0), (kTc, vc, 1, None, 0)]
                        for n_, (kb, vb, jj, mk, mi) in enumerate(kts):
                            pst = bank()
                            OP("pe", lambda e, kb=kb, jj=jj, pst=pst: e.matmul(
                                pst.t[:], lhsT=kb.t[:, jj, g * 128:(g + 1) * 128],
                                rhs=qTs.t[:, 4 * g:4 * g + 4, :].rearrange("p a b -> p (a b)"), start=True, stop=True),
                               r=[kb, qTs], w=[pst])
                            OP("act", lambda e, n_=n_, pst=pst: e.activation(out=Pall.t[:, n_, :], in_=pst.t[:], func=AF.Exp),
                               r=[pst], w=[Pall])
                            if mk is not None:
                                OP("dve", lambda e, n_=n_, mk=mk, mi=mi: e.tensor_tensor(
                                    out=Pall.t[:, n_, :].rearrange("p (h q) -> p h q", h=4),
                                    in0=Pall.t[:, n_, :].rearrange("p (h q) -> p h q", h=4),
                                    in1=mk.t[:, mi:mi + 1, :].broadcast_to([128, 4, 128]), op=ALU.mult), r=[Pall, mk], w=[Pall])
                        pO = bank()
                        for hh in range(4):
                            for n_, (kb, vb, jj, mk, mi) in enumerate(kts):
                                OP("pe", lambda e, hh=hh, n_=n_, vb=vb, jj=jj: e.matmul(
                                    pO.t[:, hh * 65:(hh + 1) * 65], lhsT=Pall.t[:, n_, hh * 128:(hh + 1) * 128],
                                    rhs=vb.t[:, jj, g * 65:(g + 1) * 65], start=(n_ == 0), stop=(n_ == 4)), r=[Pall, vb], w=[pO])
                        pO3 = pO.t[:, 0:260].rearrange("p (h d) -> p h d", h=4)
                        OP("dve", lambda e, pO3=pO3: e.tensor_tensor(out=den.t[:].unsqueeze(2), in0=pO3[:, :, 64:65],
                                                                     in1=esink.t[:, 4 * g:4 * g + 4].unsqueeze(2), op=ALU.add),
                           r=[pO, esink], w=[den])
                        OP("dve", lambda e: e.reciprocal(out=den.t[:], in_=den.t[:]), r=[den], w=[den])
                        OP("dve", lambda e, pO3=pO3: e.tensor_tensor(
                            out=cat.t[:, 512 + g * 256:512 + (g + 1) * 256].rearrange("p (h d) -> p h d", h=4),
                            in0=pO3[:, :, 0:64], in1=den.t[:].unsqueeze(2).broadcast_to([128, 4, 64]), op=ALU.mult),
                           r=[pO, den], w=[cat])
                    for g_ in range(2):
                        att_group(g_)
                    if os.environ.get("K_DBG") == "cat":
                        S.dma("pool", sts, lambda e: e.dma_start(out=d_x1[i * 128:(i + 1) * 128, :], in_=cat.t[:]), r=[cat])
                        return
                    def evc(c0, n, pb2):
                        OP("act", lambda e: e.activation(out=catT.t[:, c0:c0 + n, :].rearrange("p a b -> p (a b)"),
                                                         in_=pb2.t[:, 0:n * 128], func=AF.Identity), r=[pb2], w=[catT])
                    transpose_to(cat, 8, evc)
                    for hf in range(2):
                        py = bank()
                        for kc in range(8):
                            OP("pe", lambda e, kc=kc, py=py, hf=hf: e.matmul(py.t[:], lhsT=catT.t[:, kc, :],
                                                                      rhs=wout.t[:, kc, hf * 512:(hf + 1) * 512],
                                                                      start=(kc == 0), stop=(kc == 7)), r=[catT, wout], w=[py])
                        OP("dve", lambda e, py=py, hf=hf: e.tensor_tensor(out=r1.t[:, hf * 512:(hf + 1) * 512], in0=py.t[:],
                                                                   in1=g1bc.t[:, hf * 512:(hf + 1) * 512], op=ALU.mult),
                           r=[py, g1bc], w=[r1])
                    OP("dve", lambda e: e.scalar_tensor_tensor(out=r1.t[:], in0=xb.t[:], scalar=ALPHA, in1=r1.t[:],
                                                               op0=ALU.mult, op1=ALU.add), r=[xb, r1], w=[r1])
                    layernorm(r1, x1o, ln1g, ln1b, stats, mv, rs1)
                    S.dma("pool", sts, lambda e: e.dma_start(out=d_x1[i * 128:(i + 1) * 128, :], in_=x1o.t[:]), r=[x1o])

                def layernorm(src, dst, gb, bb, stats, mv, rs1):
                    for hf in range(2):
                        OP("dve", lambda e, hf=hf: e.bn_stats(out=stats.t[:, hf * 6:(hf + 1) * 6], in_=src.t[:, hf * 512:(hf + 1) * 512]),
                           r=[src], w=[stats])
                    OP("dve", lambda e: e.bn_aggr(out=mv.t[:], in_=stats.t[:]), r=[stats], w=[mv])
                    OP("dve", lambda e: e.tensor_scalar(out=rs1.t[:], in0=mv.t[:, 1:2], scalar1=eps_t.t[:, 0:1], scalar2=None, op0=ALU.add),
                       r=[mv, eps_t], w=[rs1])
                    OP("act", lambda e: e.activation(out=rs1.t[:], in_=rs1.t[:], func=AF.Sqrt), r=[rs1], w=[rs1])
                    OP("dve", lambda e: e.reciprocal(out=rs1.t[:], in_=rs1.t[:]), r=[rs1], w=[rs1])
                    OP("dve", lambda e: e.tensor_scalar(out=dst.t[:], in0=src.t[:], scalar1=mv.t[:, 0:1], scalar2=rs1.t[:, 0:1],
                                                        op0=ALU.subtract, op1=ALU.mult), r=[src, mv, rs1], w=[dst])
                    OP("pool", lambda e: e.tensor_tensor(out=dst.t[:], in0=dst.t[:], in1=gb.t[:], op=ALU.mult), r=[dst, gb], w=[dst])
                    OP("pool", lambda e: e.tensor_tensor(out=dst.t[:], in0=dst.t[:], in1=bb.t[:], op=ALU.add), r=[dst, bb], w=[dst])

                for t in range(2):
                    kv_tile(d_ctx[t * 128:(t + 1) * 128, :], 1, t, True)
                convert_tables()
                jobs = []
                for t in range(2):
                    jobs.append((d_ctx[t * 128:(t + 1) * 128, :], 1, 0, None, None))
                for t in (1, 0):
                    jobs.append((d_ctx[t * 128:(t + 1) * 128, :], 1, 1, None, None))
                for t in range(NPRE):
                    jobs.append((d_xpf[t * 128:(t + 1) * 128, :], 0, 0, t, None))
                for t in range(NPRE):
                    jobs.append((d_xpb[t * 128:(t + 1) * 128, :], 0, 1, 48 + t, None))
                for i in range(15, -1, -1):
                    jobs.append((d_xown[(i + 1) * 128:(i + 2) * 128, :], 0, 1, None, i))
                run_state_jobs(jobs)
                for j in range(18):
                    kv_tile(d_xown[j * 128:(j + 1) * 128, :], 0, j, False)
                for i in range(16):
                    own_tile(i)
                S.barrier()

        if mode != "A":
            with ExitStack() as pbs_:
                wq = sbuf(pbs_, "wq", [128, 8, 1024]); skT = sbuf(pbs_, "skT", [128, 8, 128])
                ln2g = sbuf(pbs_, "ln2g", [128, 1024]); ln2b = sbuf(pbs_, "ln2b", [128, 1024])
                iota = sbuf(pbs_, "iota", [128, 16])
                S.dma("sp", cst, lambda e: e.dma_start(out=wq.t[:], in_=d_wq.rearrange("(k p) n -> p k n", p=128)), w=[wq])
                for b_, d_ in ((skT, d_skT), (ln2g, d_ln2g), (ln2b, d_ln2b), (iota, d_iota)):
                    S.dma("sp", cst, lambda e, b_=b_, d_=d_: e.dma_start(out=b_.t[:], in_=d_), w=[b_])
                S.barrier()
                if mode == "B":
                    convert_tables()
                identb = sbuf(pbs_, "identb", [128, 128], BF16)
                OP("dve", lambda e: e.tensor_copy(out=identb.t[:], in_=ident.t[:]), r=[ident], w=[identb])
                dg = [sbuf(pbs_, "dg%d" % i, [128, 128], BF16) for i in range(4)]
                sc2bc = sbuf(pbs_, "sc2bc", [128, 1024]); sh2bc = sbuf(pbs_, "sh2bc", [128, 1024]); g2bc = sbuf(pbs_, "g2bc", [128, 1024])
                if mode == "B":
                    OP("dve", lambda e: e.memset(sc2bc.t[:], 1.0), w=[sc2bc])
                    OP("dve", lambda e: e.memset(sh2bc.t[:], 0.0), w=[sh2bc])
                    OP("dve", lambda e: e.memset(g2bc.t[:], 1.0), w=[g2bc])
                else:
                    for b_, di in ((sh2bc, 0), (sc2bc, 1), (g2bc, 2)):
                        S.dma("sp", cst, lambda e, b_=b_, di=di: e.dma_start(out=b_.t[:], in_=d_modbc[di]), w=[b_])
                    S.barrier()
                x1t = [sbuf(pbs_, "x1t%d" % i, [128, 1024]) for i in range(3)]
                h2 = [sbuf(pbs_, "h2_%d" % i, [128, 1024]) for i in range(2)]
                h2T = sbuf(pbs_, "h2T", [128, 8, 128]); qTp = sbuf(pbs_, "qTp", [128, 8, 128])
                ssb = sbuf(pbs_, "ssb", [128, 16, 128]); sw = sbuf(pbs_, "sw", [128, 128])
                m16 = sbuf(pbs_, "m16", [128, 16, 16]); ix16 = sbuf(pbs_, "ix16", [128, 16, 16], U32)
                ixf = sbuf(pbs_, "ixf", [128, 16, 16])
                cand = sbuf(pbs_, "cand", [128, 8, 256]); cw = sbuf(pbs_, "cw", [128, 256])
                tsv = sbuf(pbs_, "tsv", [128, 8, 16]); pos = sbuf(pbs_, "pos", [128, 8, 16], U32)
                posf = sbuf(pbs_, "posf", [128, 8, 16])
                lohi = sbuf(pbs_, "lohi", [128, 2, 16])
                paf = sbuf(pbs_, "paf", [128, 8, 16]); pbf = sbuf(pbs_, "pbf", [128, 8, 16])
                oh = sbuf(pbs_, "oh", [128, 8, 16, 16])
                i1s = sbuf(pbs_, "i1s", [128, 8, 16]); i2s = sbuf(pbs_, "i2s", [128, 8, 16])
                eidf = sbuf(pbs_, "eidf", [128, 128])
                eidx = [sbuf(pbs_, "eidx%d" % i, [128, 128], U32) for i in range(3)]
                gate = [sbuf(pbs_, "gate%d" % i, [128, 8, 16]) for i in range(2)]
                gsum = sbuf(pbs_, "gsum", [128, 8])
                dots = [sbuf(pbs_, "dots%d" % i, [128, 128]) for i in range(2)]
                coef = [sbuf(pbs_, "coef%d" % i, [128, 128]) for i in range(2)]
                gb = [sbuf(pbs_, "gb%d" % i, [128, 2048], BF16) for i in range(NG)]
                gs = [S.dma_slot("g%d" % i) for i in range(NG)]
                prod = [sbuf(pbs_, "prod%d" % i, [128, 1024], BF16) for i in range(3)]
                h2b = [sbuf(pbs_, "h2b%d" % i, [128, 1024], BF16) for i in range(2)]
                acc = [sbuf(pbs_, "acc%d" % i, [128, 1024]) for i in range(2)]
                stats2 = sbuf(pbs_, "stats2", [128, 12]); mv2 = sbuf(pbs_, "mv2", [128, 2]); rs2 = sbuf(pbs_, "rs2", [128, 1])
                x1ld = [S.dma_slot("x1l%d" % i) for i in range(3)]

                def layernorm2(src, dst):
                    for hf in range(2):
                        OP("dve", lambda e, hf=hf: e.bn_stats(out=stats2.t[:, hf * 6:(hf + 1) * 6], in_=src.t[:, hf * 512:(hf + 1) * 512]),
                           r=[src], w=[stats2])
                    OP("dve", lambda e: e.bn_aggr(out=mv2.t[:], in_=stats2.t[:]), r=[stats2], w=[mv2])
                    OP("dve", lambda e: e.tensor_scalar(out=rs2.t[:], in0=mv2.t[:, 1:2], scalar1=eps_t.t[:, 0:1], scalar2=None, op0=ALU.add),
                       r=[mv2, eps_t], w=[rs2])
                    OP("act", lambda e: e.activation(out=rs2.t[:], in_=rs2.t[:], func=AF.Sqrt), r=[rs2], w=[rs2])
                    OP("dve", lambda e: e.reciprocal(out=rs2.t[:], in_=rs2.t[:]), r=[rs2], w=[rs2])
                    OP("dve", lambda e: e.tensor_scalar(out=dst.t[:], in0=src.t[:], scalar1=mv2.t[:, 0:1], scalar2=rs2.t[:, 0:1],
                                                        op0=ALU.subtract, op1=ALU.mult), r=[src, mv2, rs2], w=[dst])
                    OP("dve", lambda e: e.tensor_tensor(out=dst.t[:], in0=dst.t[:], in1=ln2g.t[:], op=ALU.mult), r=[dst, ln2g], w=[dst])
                    OP("dve", lambda e: e.tensor_tensor(out=dst.t[:], in0=dst.t[:], in1=ln2b.t[:], op=ALU.add), r=[dst, ln2b], w=[dst])

                def top16(src3, n, work, mdst, idst):
                    (sa, sbuf_), (wa, wbuf), (ma, mbuf), (ia, ibuf) = src3, work, mdst, idst
                    OP("dve", lambda e: e.max(out=ma[:, 0:8], in_=sa), r=[sbuf_], w=[mbuf])
                    OP("dve", lambda e: e.max_index(out=ia[:, 0:8], in_max=ma[:, 0:8], in_values=sa), r=[sbuf_, mbuf], w=[ibuf])
                    OP("dve", lambda e: e.match_replace(out=wa, in_to_replace=ma[:, 0:8], in_values=sa, imm_value=-1e30),
                       r=[sbuf_, mbuf], w=[wbuf])
                    OP("dve", lambda e: e.max(out=ma[:, 8:16], in_=wa), r=[wbuf], w=[mbuf])
                    OP("dve", lambda e: e.max_index(out=ia[:, 8:16], in_max=ma[:, 8:16], in_values=wa), r=[wbuf, mbuf], w=[ibuf])

                def route(i):
                    xs = x1t[i % 3]
                    S.dma("sp", x1ld[i % 3], lambda e: e.dma_start(out=xs.t[:], in_=d_x1[i * 128:(i + 1) * 128, :]), w=[xs])
                    hh = h2[i % 2]
                    OP("dve", lambda e: e.tensor_tensor(out=hh.t[:], in0=xs.t[:], in1=sc2bc.t[:], op=ALU.mult), r=[xs, sc2bc], w=[hh])
                    OP("dve", lambda e: e.tensor_tensor(out=hh.t[:], in0=hh.t[:], in1=sh2bc.t[:], op=ALU.add), r=[hh, sh2bc], w=[hh])
                    OP("act", lambda e: e.activation(out=h2b[i % 2].t[:], in_=hh.t[:], func=AF.Identity), r=[hh], w=[h2b[i % 2]])

                    if KSTOP < 2: return
                    def ev(c0, n, pb2):
                        OP("act", lambda e: e.activation(out=h2T.t[:, c0:c0 + n, :].rearrange("p a b -> p (a b)"),
                                                         in_=pb2.t[:, 0:n * 128], func=AF.Identity), r=[pb2], w=[h2T])
                    yield
                    transpose_to(hh, 8, ev)
                    yield
                    if KSTOP < 2.3: return
                    for j0 in range(0, 8, 4):
                        pq = bank()
                        for jj in range(j0, j0 + 4):
                            for kc in range(8):
                                OP("pe", lambda e, jj=jj, kc=kc, pq=pq, j0=j0: e.matmul(
                                    pq.t[:, (jj - j0) * 128:(jj - j0 + 1) * 128], lhsT=wq.t[:, kc, jj * 128:(jj + 1) * 128],
                                    rhs=h2T.t[:, kc, :], start=(kc == 0), stop=(kc == 7)), r=[wq, h2T], w=[pq])
                        OP("act", lambda e, pq=pq, j0=j0: e.activation(out=qTp.t[:, j0:j0 + 4, :].rearrange("p a b -> p (a b)"),
                                                                       in_=pq.t[:], func=AF.Identity), r=[pq], w=[qTp])
                    if KSTOP < 2.6: return
                    for h0 in range(0, 8, 4):
                        for a in range(2):
                            psc = bank()
                            for h in range(h0, h0 + 4):
                                OP("pe", lambda e, h=h, a=a, psc=psc, h0=h0: e.matmul(
                                    psc.t[:, (h - h0) * 128:(h - h0 + 1) * 128], lhsT=qTp.t[a * 64:(a + 1) * 64, h, :],
                                    rhs=skT.t[a * 64:(a + 1) * 64, h, :], start=True, stop=True), r=[qTp, skT], w=[psc])
                            OP("act", lambda e, psc=psc, h0=h0, a=a: e.activation(
                                out=ssb.t[:, 2 * h0 + a:2 * h0 + 8:2, :], in_=psc.t[:].rearrange("p (a b) -> p a b", a=4),
                                func=AF.Identity), r=[psc], w=[ssb])
                    if KSTOP < 3: return
                    for q_ in range(16):
                        top16((ssb.t[:, q_, :], ssb), 128, (sw.t[:], sw), (m16.t[:, q_, :], m16), (ix16.t[:, q_, :], ix16))
                        yield
                    if KSTOP < 4: return
                    OP("dve", lambda e: e.tensor_copy(out=ixf.t[:], in_=ix16.t[:]), r=[ix16], w=[ixf])
                    m4 = m16.t[:].rearrange("p (h a) k -> p h a k", a=2)
                    OP("dve", lambda e: e.tensor_tensor(
                        out=cand.t[:].rearrange("p h (a b) -> p h a b", a=16),
                        in0=m4[:, :, 0, :].unsqueeze(3).broadcast_to([128, 8, 16, 16]),
                        in1=m4[:, :, 1, :].unsqueeze(2).broadcast_to([128, 8, 16, 16]), op=ALU.add), r=[m16], w=[cand])
                    for h in range(8):
                        top16((cand.t[:, h, :], cand), 256, (cw.t[:], cw), (tsv.t[:, h, :], tsv), (pos.t[:, h, :], pos))
                        yield
                    gt = gate[i % 2]
                    OP("dve", lambda e: e.tensor_tensor(out=gt.t[:], in0=tsv.t[:], in1=tsv.t[:, :, 0:1].broadcast_to([128, 8, 16]),
                                                        op=ALU.subtract), r=[tsv], w=[gt])
                    OP("act", lambda e: e.activation(out=gt.t[:], in_=gt.t[:], func=AF.Exp), r=[gt], w=[gt])
                    OP("dve", lambda e: e.tensor_reduce(out=gsum.t[:], in_=gt.t[:], axis=AX.X, op=ALU.add), r=[gt], w=[gsum])
                    OP("dve", lambda e: e.reciprocal(out=gsum.t[:], in_=gsum.t[:]), r=[gsum], w=[gsum])
                    OP("dve", lambda e: e.tensor_tensor(out=gt.t[:], in0=gt.t[:], in1=gsum.t[:].unsqueeze(2).broadcast_to([128, 8, 16]),
                                                        op=ALU.mult), r=[gt, gsum], w=[gt])
                    if KSTOP < 5: return
                    yield
                    OP("dve", lambda e: e.tensor_copy(out=posf.t[:], in_=pos.t[:]), r=[pos], w=[posf])
                    oh2 = Buf(None); oh2.r = ssb.r
                    oh2.t = ssb.t[:].rearrange("p a b -> p (a b)").rearrange("p (h j c) -> p h j c", h=8, j=16)
                    ix4 = ixf.t[:].rearrange("p (h a) k -> p h a k", a=2)
                    bc4 = lambda ap2: ap2.unsqueeze(1).unsqueeze(1).broadcast_to([128, 8, 16, 16])
                    pf4 = posf.t[:].unsqueeze(3).broadcast_to([128, 8, 16, 16])
                    OP("dve", lambda e: e.tensor_tensor(out=oh.t[:], in0=pf4, in1=bc4(lohi.t[:, 0, :]), op=ALU.is_ge), r=[posf, lohi], w=[oh])
                    OP("dve", lambda e: e.tensor_tensor(out=oh2.t[:], in0=pf4, in1=bc4(lohi.t[:, 1, :]), op=ALU.is_ge), r=[posf, lohi], w=[oh2])
                    OP("dve", lambda e: e.tensor_tensor(out=oh.t[:], in0=oh.t[:], in1=oh2.t[:], op=ALU.subtract), r=[oh, oh2], w=[oh])
                    OP("dve", lambda e: e.tensor_tensor(out=oh2.t[:], in0=oh.t[:], in1=bc4(lohi.t[:, 0, :]), op=ALU.mult), r=[oh, lohi], w=[oh2])
                    yield
                    OP("dve", lambda e: e.tensor_reduce(out=paf.t[:], in_=oh2.t[:], axis=AX.X, op=ALU.add), r=[oh2], w=[paf])
                    OP("dve", lambda e: e.tensor_tensor(out=oh.t[:], in0=oh.t[:],
                                                        in1=ix4[:, :, 0, :].unsqueeze(2).broadcast_to([128, 8, 16, 16]), op=ALU.mult),
                       r=[oh, ixf], w=[oh])
                    OP("dve", lambda e: e.tensor_reduce(out=i1s.t[:], in_=oh.t[:], axis=AX.X, op=ALU.add), r=[oh], w=[i1s])
                    yield
                    OP("dve", lambda e: e.tensor_tensor(out=pbf.t[:], in0=posf.t[:], in1=paf.t[:], op=ALU.subtract), r=[posf, paf], w=[pbf])
                    OP("dve", lambda e: e.tensor_tensor(out=oh.t[:], in0=bc4(iota.t[:]),
                                                        in1=pbf.t[:].unsqueeze(3).broadcast_to([128, 8, 16, 16]), op=ALU.is_equal),
                       r=[iota, pbf], w=[oh])
                    OP("dve", lambda e: e.tensor_tensor(out=oh.t[:], in0=oh.t[:],
                                                        in1=ix4[:, :, 1, :].unsqueeze(2).broadcast_to([128, 8, 16, 16]), op=ALU.mult),
                       r=[oh, ixf], w=[oh])
                    OP("dve", lambda e: e.tensor_reduce(out=i2s.t[:], in_=oh.t[:], axis=AX.X, op=ALU.add), r=[oh], w=[i2s])
                    OP("dve", lambda e: e.scalar_tensor_tensor(out=eidf.t[:].rearrange("p (h k) -> p h k", h=8), in0=i1s.t[:], scalar=128.0,
                                                               in1=i2s.t[:], op0=ALU.mult, op1=ALU.add), r=[i1s, i2s], w=[eidf])
                    OP("dve", lambda e: e.tensor_copy(out=eidx[i % 3].t[:], in_=eidf.t[:]), r=[eidf], w=[eidx[i % 3]])

                gi = [0]

                class V:
                    def __init__(self, t):
                        self.t = t
                        self.r = Res()
                dcol = [[V(dots[p_].t) for _ in range(128)] for p_ in range(2)]
                ccol = [[V(coef[p_].t) for _ in range(128)] for p_ in range(2)]

                gk = {}

                def slot_u(i, s):
                    k = gi[0] % NG
                    gi[0] += 1
                    gk[(i, s)] = k
                    S.dma("pool", gs[k], lambda e: e.indirect_dma_start(
                        out=gb[k].t[:], out_offset=None, in_=d_puv16,
                        in_offset=bass.IndirectOffsetOnAxis(ap=eidx[i % 3].t[:, s:s + 1], axis=0)), r=[eidx[i % 3]] + puvB, w=[gb[k]])
                    pr = prod[s % 3]
                    dc, cc = dcol[i % 2][s], ccol[i % 2][s]
                    OP("dve", lambda e: e.tensor_tensor(out=pr.t[:], in0=gb[k].t[:, 0:1024], in1=h2b[i % 2].t[:], op=ALU.mult),
                       r=[gb[k], h2b[i % 2]], w=[pr])
                    OP("act", lambda e: e.activation(out=pr.t[:], in_=pr.t[:], func=AF.Identity, accum_out=dc.t[:, s:s + 1]),
                       r=[pr], w=[pr, dc])
                    OP("act", lambda e: e.activation(out=cc.t[:, s:s + 1], in_=dc.t[:, s:s + 1], func=AF.Gelu), r=[dc], w=[cc])

                def slot_v(i, s):
                    k = gk.pop((i, s))
                    cc = ccol[i % 2][s]
                    dgk = dg[s % 4]
                    OP("dve", lambda e: e.tensor_scalar(out=dgk.t[:], in0=identb.t[:], scalar1=cc.t[:, s:s + 1],
                                                        scalar2=gate[i % 2].t[:, s // 16, s % 16:s % 16 + 1], op0=ALU.mult, op1=ALU.mult),
                       r=[identb, cc, gate[i % 2]], w=[dgk])
                    for hf in range(2):
                        OP("pe", lambda e, hf=hf: e.matmul(accP[hf].t[:], lhsT=dgk.t[:], rhs=gb[k].t[:, 1024 + hf * 512:1536 + hf * 512],
                                                           start=(s == 0), stop=(s == 127)), r=[dgk, gb[k]], w=[accP[hf]])

                def finish_v(i):
                    r2 = acc[i % 2]; yo = acc[i % 2]
                    for hf in range(2):
                        OP("dve", lambda e, hf=hf: e.tensor_tensor(out=r2.t[:, hf * 512:(hf + 1) * 512], in0=accP[hf].t[:],
                                                                   in1=g2bc.t[:, hf * 512:(hf + 1) * 512], op=ALU.mult),
                           r=[accP[hf], g2bc], w=[r2])
                    OP("dve", lambda e: e.scalar_tensor_tensor(out=r2.t[:], in0=x1t[i % 3].t[:], scalar=ALPHA, in1=r2.t[:],
                                                               op0=ALU.mult, op1=ALU.add), r=[x1t[i % 3], r2], w=[r2])
                    layernorm2(r2, yo)
                    S.dma("sp", sts, lambda e: e.dma_start(out=d_out[i * 128:(i + 1) * 128, :], in_=yo.t[:]), r=[yo])

                OP("dve", lambda e: e.tensor_scalar(out=lohi.t[:, 0, :], in0=iota.t[:], scalar1=16.0, scalar2=None, op0=ALU.mult), r=[iota], w=[lohi])
                OP("dve", lambda e: e.tensor_scalar(out=lohi.t[:, 1, :], in0=iota.t[:], scalar1=16.0, scalar2=16.0, op0=ALU.mult, op1=ALU.add),
                   r=[iota], w=[lohi])
                NT = int(os.environ.get('K_NT', '16'))
                KSTOP = float(os.environ.get('K_STOP', '9'))
                for _ in route(0):
                    pass
                for i in range(NT):
                    gen = route(i + 1) if i + 1 < NT else iter(())
                    for s in range(128 + LAG if KSTOP >= 6 else 0):
                        if s < 128:
                            slot_u(i, s)
                        if s >= LAG:
                            slot_v(i, s - LAG)
                        next(gen, None)
                    for _ in gen:
                        pass
                    if KSTOP >= 7:
                        finish_v(i)
                S.barrier()
        S.barrier()
        S.run()
    return nc


def _consts():
    s = np.arange(128)[:, None]
    t = np.arange(128)[None, :]
    same = (s // 64) == (t // 64)
    g = -1.0 / 16.0
    tri = np.stack([(same & (s <= t)), (same & (s > t)), (same & (s >= t)), (same & (s < t))], axis=1).astype(np.float32) * g
    ci = np.stack([(np.arange(128) // 64 == c) for c in range(2)], axis=1).astype(np.float32) * g
    amask = np.stack([(s >= t), (s <= t)], axis=1).astype(np.float32)
    sc = (np.arange(128) % 64)[:, None]
    cc = np.arange(64)[None, :]
    gmask = np.stack([(sc <= cc), (sc >= cc)], axis=1).astype(np.float32)
    iota = np.broadcast_to(np.arange(16, dtype=np.float32), (128, 16)).copy()
    return dict(ident=np.eye(128, dtype=np.float32), tri=np.ascontiguousarray(tri), ci=np.ascontiguousarray(ci),
                amask=np.ascontiguousarray(amask), gmask=np.ascontiguousarray(gmask), iota16=iota)


def _rope_tables():
    rows = 8192 // 64
    row = np.repeat(np.arange(rows, dtype=np.float32), 64)
    col = np.tile(np.arange(64, dtype=np.float32), rows)
    inv = (np.float32(10000.0) ** (-np.arange(16, dtype=np.float32) / np.float32(16))).astype(np.float32)
    ang = np.stack([row[:, None] * inv, col[:, None] * inv], axis=1).astype(np.float32)
    return np.cos(ang).reshape(8192, 32).astype(np.float32), np.sin(ang).reshape(8192, 32).astype(np.float32)


def make_in_maps(x, c, ctx, c_ctx, w_ada, b_ada, w_in, w_gate2_f, b_gate_f, w_gate2_b, b_gate_b, gla_norm_g, attn_sink,
                 w_out, ln1_g, ln1_b, peer_wq, peer_subkeys, peer_u, peer_v, ln2_g, ln2_b):
    f = lambda a: np.ascontiguousarray(np.asarray(a, dtype=np.float32))
    bc = lambda v, n: np.ascontiguousarray(np.broadcast_to(np.asarray(v, np.float32).reshape(1, -1), (128, n)))
    x = f(x); ctx = f(ctx)
    wi = f(w_in[0])
    wperm = np.concatenate([wi[:, 0:256], wi[:, 256:512], wi[:, 512:1024], wi[:, 1024:1536], wi[:, 1568:2080],
                            wi[:, 2080:2208], wi[:, 2208:2336], wi[:, 1536:1552], wi[:, 1552:1568]], axis=1)
    cosT, sinT = _rope_tables()
    common = dict(
        w_ada=f(w_ada[0]), b_ada=f(b_ada[0]).reshape(1, -1), w_in=np.ascontiguousarray(wperm),
        w2=np.ascontiguousarray(np.concatenate([f(w_gate2_f[0]), f(w_gate2_b[0])], axis=1)),
        bg=np.ascontiguousarray(np.concatenate([f(b_gate_f[0]), f(b_gate_b[0])]).reshape(1, -1)),
        gng=bc(gla_norm_g[0], 128), sink=bc(attn_sink[0], 8), w_out=f(w_out[0]),
        ln1g=bc(ln1_g[0], 1024), ln1b=bc(ln1_b[0], 1024), ln2g=bc(ln2_g[0], 1024), ln2b=bc(ln2_b[0], 1024),
        peer_wq=f(peer_wq[0]),
        skT=np.ascontiguousarray(np.transpose(f(peer_subkeys[0]), (1, 3, 0, 2)).reshape(128, 8, 128)),
        peer_uv=np.ascontiguousarray(np.concatenate([f(peer_u[0]), f(peer_v[0])], axis=1)), **_consts())
    maps = []
    zt = np.zeros((128, 1024), np.float32)
    for core in range(8):
        b, s = core // 4, core % 4
        xb = x[b].reshape(64, 128, 1024)
        npf = 16 * s
        xpf = np.zeros((NPRE, 128, 1024), np.float32)
        if npf:
            xpf[NPRE - npf:] = xb[0:npf]
        npb = 16 * (3 - s)
        xpb = np.zeros((NPRE, 128, 1024), np.float32)
        if npb:
            xpb[NPRE - npb:] = xb[63:16 * (s + 1) - 1:-1]
        flags = np.zeros((128, 100), np.float32)
        flags[:, NPRE - npf:NPRE] = 1.0
        flags[:, 48 + NPRE - npb:48 + NPRE] = 1.0
        t0 = 16 * s
        own = np.zeros((18, 128, 1024), np.float32)
        own[1:17] = xb[t0:t0 + 16]
        cos_o = np.zeros((18, 128, 32), np.float32); sin_o = np.zeros((18, 128, 32), np.float32)
        cos_o[1:17] = cosT.reshape(64, 128, 32)[t0:t0 + 16]; sin_o[1:17] = sinT.reshape(64, 128, 32)[t0:t0 + 16]
        if t0 > 0:
            own[0] = xb[t0 - 1]; flags[:, 96] = 1.0
            cos_o[0] = cosT.reshape(64, 128, 32)[t0 - 1]; sin_o[0] = sinT.reshape(64, 128, 32)[t0 - 1]
        if t0 + 16 < 64:
            own[17] = xb[t0 + 16]; flags[:, 97] = 1.0
            cos_o[17] = cosT.reshape(64, 128, 32)[t0 + 16]; sin_o[17] = sinT.reshape(64, 128, 32)[t0 + 16]
        c2 = np.stack([f(c)[b], f(c_ctx)], axis=0)
        c2T = np.ascontiguousarray(c2.reshape(2, 8, 128).transpose(2, 1, 0))
        m = dict(common)
        m.update(xpre_f=xpf.reshape(-1, 1024), xpre_b=xpb.reshape(-1, 1024), xown=own.reshape(-1, 1024), ctx=ctx[b],
                 flags=flags, c2T=c2T, cosT=cos_o.reshape(-1, 32), sinT=sin_o.reshape(-1, 32))
        maps.append(m)
    return maps


_NC_CACHE = {}


def kernel(**inputs):
    if "full" not in _NC_CACHE:
        _NC_CACHE["full"] = build_nc("full")
    nc = _NC_CACHE["full"]
    maps = make_in_maps(**inputs)
    res = run_bass_kernel_spmd(nc, maps, core_ids=list(range(8)))
    out = np.zeros((2, 8192, 1024), np.float32)
    for core in range(8):
        b, s = core // 4, core % 4
        out[b, s * 2048:(s + 1) * 2048] = np.asarray(res.results[core]["out"], np.float32)
    return out
```

```python
import os
import numpy as np
from contextlib import ExitStack
import concourse.bass as bass
import concourse.mybir as mybir
from concourse.bass_utils import run_bass_kernel_spmd

F32 = mybir.dt.float32
BF16 = mybir.dt.bfloat16
U32 = mybir.dt.uint32
AF = mybir.ActivationFunctionType
ALU = mybir.AluOpType
AX = mybir.AxisListType

SAME_ENGINE_SYNC = True
NPRE = 48
LN_EPS = 1e-5
ALPHA = 2.0 ** 0.25
NG = 12
LAG = 3


class Res:
    __slots__ = ("w", "rs")

    def __init__(self):
        self.w = None
        self.rs = {}


class Buf:
    def __init__(self, t):
        self.t = t
        self.r = Res()


class Sched:
    def __init__(self, nc, stack):
        self.nc = nc
        self.stack = stack
        self.sems = {}
        self.count = {}
        self.seen = {k: {} for k in ("pe", "dve", "act", "pool", "sp")}
        self.streams = {k: [] for k in ("pe", "dve", "act", "pool", "sp")}
        for k in ("pe", "dve", "act", "pool"):
            self.sems[k] = stack.enter_context(nc.semaphore("sem_" + k))
            self.count[k] = 0
        self.nslots = 0

    def dma_slot(self, name=""):
        self.nslots += 1
        key = "d%d%s" % (self.nslots, name)
        self.sems[key] = self.stack.enter_context(self.nc.semaphore("s_" + key))
        self.count[key] = 0
        return key

    def _waits(self, q, reads, writes, same_ok):
        deps = {}
        for b in reads:
            r = b.r
            if r.w is not None:
                k, c = r.w
                deps[k] = max(deps.get(k, 0), c)
        for b in writes:
            w = b.r
            if w.w is not None:
                k, c = w.w
                deps[k] = max(deps.get(k, 0), c)
            for k, c in w.rs.items():
                deps[k] = max(deps.get(k, 0), c)
        out = []
        for k, c in deps.items():
            if k == q and not same_ok:
                continue
            if self.seen[q].get(k, 0) >= c:
                continue
            self.seen[q][k] = c
            out.append((k, c))
        return out

    def op(self, q, fn, r=(), w=()):
        same_ok = SAME_ENGINE_SYNC and q != "pe"
        waits = self._waits(q, r, w, same_ok)
        self.count[q] += 1
        c = self.count[q]
        sems = self.sems
        st = self.streams[q]
        for k, v in waits:
            st.append(lambda e, k=k, v=v: e.wait_ge(sems[k], v))
        st.append(lambda e, fn=fn: fn(e).then_inc(sems[q], 1))
        for b in r:
            b.r.rs[q] = c
        for b in w:
            b.r.w = (q, c)
            b.r.rs = {}

    def dma(self, q, slot, fn, r=(), w=()):
        waits = self._waits(q, r, w, True)
        prev = self.count[slot]
        if prev > 0 and self.seen[q].get(slot, 0) < prev:
            self.seen[q][slot] = prev
            waits.append((slot, prev))
        self.count[slot] += 16
        c = self.count[slot]
        sems = self.sems
        st = self.streams[q]
        for k, v in waits:
            st.append(lambda e, k=k, v=v: e.wait_ge(sems[k], v))
        st.append(lambda e, fn=fn: fn(e).then_inc(sems[slot], 16))
        for b in r:
            b.r.rs[slot] = c
        for b in w:
            b.r.w = (slot, c)
            b.r.rs = {}

    def wait_all(self, q):
        sems = self.sems
        for k in list(self.count.keys()):
            c = self.count[k]
            if c == 0 or k == q or self.seen[q].get(k, 0) >= c:
                continue
            self.seen[q][k] = c
            self.streams[q].append(lambda e, k=k, c=c: e.wait_ge(sems[k], c))

    def barrier(self):
        for q in ("pe", "dve", "act", "pool", "sp"):
            self.wait_all(q)

    def run(self):
        streams = self.streams
        with self.nc.Block() as block:
            @block.tensor
            def _(e):
                for f in streams["pe"]:
                    f(e)

            @block.vector
            def _(e):
                for f in streams["dve"]:
                    f(e)

            @block.scalar
            def _(e):
                for f in streams["act"]:
                    f(e)

            @block.gpsimd
            def _(e):
                for f in streams["pool"]:
                    f(e)

            @block.sync
            def _(e):
                for f in streams["sp"]:
                    f(e)


def build_nc(mode="full"):
    nc = bass.Bass("TRN2", target_bir_lowering=False)
    D = lambda name, shape, dt=F32, kind="ExternalInput": nc.dram_tensor(name, shape, dt, kind=kind).ap()
    d_xpf = D("xpre_f", [NPRE * 128, 1024]); d_xpb = D("xpre_b", [NPRE * 128, 1024])
    d_xown = D("xown", [18 * 128, 1024]); d_ctx = D("ctx", [256, 1024])
    d_flags = D("flags", [128, 100]); d_c2T = D("c2T", [128, 8, 2])
    d_wada = D("w_ada", [1024, 6144]); d_bada = D("b_ada", [1, 6144])
    d_win = D("w_in", [1024, 2336]); d_w2 = D("w2", [16, 512]); d_bg = D("bg", [1, 512])
    d_gng = D("gng", [128, 128]); d_sink = D("sink", [128, 8])
    d_wout = D("w_out", [1024, 1024])
    d_ln1g = D("ln1g", [128, 1024]); d_ln1b = D("ln1b", [128, 1024])
    d_ln2g = D("ln2g", [128, 1024]); d_ln2b = D("ln2b", [128, 1024])
    d_wq = D("peer_wq", [1024, 1024]); d_skT = D("skT", [128, 8, 128])
    d_puv = D("peer_uv", [16384, 2048])
    d_puv16 = D("puv16", [16384, 2048], BF16, kind="Internal")
    d_cos = D("cosT", [18 * 128, 32]); d_sin = D("sinT", [18 * 128, 32])
    d_ident = D("ident", [128, 128]); d_tri = D("tri", [128, 4, 128]); d_ci = D("ci", [128, 2])
    d_amask = D("amask", [128, 2, 128]); d_gmask = D("gmask", [128, 2, 64]); d_iota = D("iota16", [128, 16])
    d_out = D("out", [2048, 1024], kind="ExternalOutput")
    if mode == "B":
        d_x1 = D("x1s", [2048, 1024])
    else:
        d_x1 = D("x1s", [2048, 1024], kind="ExternalOutput" if mode == "A" else "Internal")

    with ExitStack() as top:
        S = Sched(nc, top)
        OP = S.op

        def sbuf(st, name, shape, dt=F32):
            return Buf(st.enter_context(nc.sbuf_tensor("s_" + name, shape, dt)))

        banks = [Buf(top.enter_context(nc.psum_tensor("pb%d" % i, [128, 512], F32))) for i in range(6)]
        accP = [Buf(top.enter_context(nc.psum_tensor("pacc%d" % i, [128, 512], F32))) for i in range(2)]
        bank_i = [0]

        def bank():
            b = banks[bank_i[0] % 6]
            bank_i[0] += 1
            return b

        ld = [S.dma_slot("ld%d" % i) for i in range(2)]
        cst = S.dma_slot("cst")
        sts = S.dma_slot("st")
        csl = S.dma_slot("cs"); snl = S.dma_slot("sn")

        puvB = [Buf(None) for _ in range(16)]
        cvs = [S.dma_slot("cv%d" % i) for i in range(4)]

        def convert_tables():
            for ci_ in range(16):
                S.dma("pool", cvs[ci_ % 4], lambda e, ci_=ci_: e.dma_start(out=d_puv16[ci_ * 1024:(ci_ + 1) * 1024, :],
                                                                         in_=d_puv[ci_ * 1024:(ci_ + 1) * 1024, :]), w=[puvB[ci_]])
        ident = sbuf(top, "ident", [128, 128])
        S.dma("sp", cst, lambda e: e.dma_start(out=ident.t[:], in_=d_ident), w=[ident])
        flags = sbuf(top, "flags", [128, 100])
        S.dma("sp", cst, lambda e: e.dma_start(out=flags.t[:], in_=d_flags), w=[flags])
        eps_t = sbuf(top, "eps_t", [128, 1])
        OP("dve", lambda e: e.memset(eps_t.t[:], LN_EPS), w=[eps_t])
        modT = sbuf(top, "modT", [128, 48, 2])
        sc1p = sbuf(top, "sc1p", [128, 8, 2])
        g1bc = sbuf(top, "g1bc", [128, 1024])
        d_modbc = D("modbc", [3, 128, 1024], kind="Internal")
        xt_i = [0]

        def transpose_to(src, nchunks, dst_fn, width=128, rows=128):
            for c0 in range(0, nchunks, 4):
                pb = bank()
                n = min(4, nchunks - c0)
                for c in range(c0, c0 + n):
                    OP("pe", lambda e, c=c, pb=pb, c0=c0: e.transpose(
                        out=pb.t[0:width, (c - c0) * 128:(c - c0) * 128 + rows],
                        in_=src.t[0:rows, c * width:(c + 1) * width], identity=ident.t[0:rows, 0:rows]),
                       r=[src, ident], w=[pb])
                dst_fn(c0, n, pb)

        if mode != "B":
            with ExitStack() as p0:
                c2T = sbuf(p0, "c2T", [128, 8, 2]); sc2 = sbuf(p0, "sc2", [128, 8, 2])
                S.dma("sp", cst, lambda e: e.dma_start(out=c2T.t[:], in_=d_c2T), w=[c2T])
                OP("act", lambda e: e.activation(out=sc2.t[:], in_=c2T.t[:], func=AF.Silu), r=[c2T], w=[sc2])
                modrow = sbuf(p0, "modrow", [2, 6144])
                brow = sbuf(p0, "brow", [128, 6144]); ones2 = sbuf(p0, "ones2", [128, 2])
                OP("pool", lambda e: e.memset(brow.t[:], 0.0), w=[brow])
                S.dma("sp", cst, lambda e: e.dma_start(out=brow.t[0:1, :], in_=d_bada), w=[brow])
                OP("dve", lambda e: e.memset(ones2.t[:], 0.0), w=[ones2])
                OP("dve", lambda e: e.memset(ones2.t[0:1, :], 1.0), w=[ones2])
                S.barrier()
                wst = [sbuf(p0, "wst%d" % i, [128, 3072]) for i in range(2)]
                wada_v = d_wada.rearrange("(k p) n -> k p n", p=128)
                for half in range(2):
                    pbs = [bank() for _ in range(6)]
                    for j in range(6):
                        col = half * 3072 + j * 512
                        OP("pe", lambda e, pbj=pbs[j], col=col: e.matmul(pbj.t[0:2, :], lhsT=ones2.t[:], rhs=brow.t[:, col:col + 512],
                                                                        start=True, stop=False), r=[ones2, brow], w=[pbs[j]])
                    for kc in range(8):
                        ws = wst[kc % 2]
                        S.dma("sp", ld[kc % 2], lambda e, ws=ws, kc=kc, half=half: e.dma_start(
                            out=ws.t[:], in_=wada_v[kc, :, half * 3072:(half + 1) * 3072]), w=[ws])
                        for j in range(6):
                            OP("pe", lambda e, j=j, ws=ws, kc=kc, pbj=pbs[j]: e.matmul(
                                pbj.t[0:2, :], lhsT=sc2.t[:, kc, :], rhs=ws.t[:, j * 512:(j + 1) * 512],
                                start=False, stop=(kc == 7)), r=[sc2, ws], w=[pbs[j]])
                    for j in range(6):
                        col = half * 3072 + j * 512
                        OP("act", lambda e, pbj=pbs[j], col=col: e.activation(out=modrow.t[:, col:col + 512], in_=pbj.t[0:2, :],
                                                                       func=AF.Identity), r=[pbs[j]], w=[modrow])
                for c0 in range(0, 48, 4):
                    pb = bank()
                    for c in range(c0, c0 + 4):
                        OP("pe", lambda e, c=c, pb=pb, c0=c0: e.transpose(
                            out=pb.t[:, (c - c0) * 2:(c - c0) * 2 + 2], in_=modrow.t[0:2, c * 128:(c + 1) * 128],
                            identity=ident.t[0:2, 0:2]), r=[modrow, ident], w=[pb])
                    OP("dve", lambda e, pb=pb, c0=c0: e.tensor_copy(
                        out=modT.t[:, c0:c0 + 4, :], in_=pb.t[:, 0:8].rearrange("p (a b) -> p a b", a=4)), r=[pb], w=[modT])
                OP("dve", lambda e: e.tensor_scalar(out=sc1p.t[:], in0=modT.t[:, 8:16, :], scalar1=1.0, scalar2=None, op0=ALU.add),
                   r=[modT], w=[sc1p])
                sel = sbuf(p0, "sel", [2, 128])
                OP("dve", lambda e: e.memset(sel.t[:], 0.0), w=[sel])
                OP("dve", lambda e: e.memset(sel.t[0:1, :], 1.0), w=[sel])
                bct = sbuf(p0, "bct", [128, 1024])
                for dst, j, add1, di in ((g1bc, 2, False, None), (bct, 3, False, 0), (bct, 4, True, 1), (bct, 5, False, 2)):
                    for hf in range(2):
                        pb = bank()
                        col = j * 1024 + hf * 512
                        OP("pe", lambda e, pb=pb, col=col: e.matmul(pb.t[:], lhsT=sel.t[:], rhs=modrow.t[:, col:col + 512],
                                                                    start=True, stop=True), r=[sel, modrow], w=[pb])
                        if add1:
                            OP("dve", lambda e, pb=pb, dst=dst, hf=hf: e.tensor_scalar(
                                out=dst.t[:, hf * 512:(hf + 1) * 512], in0=pb.t[:], scalar1=1.0, scalar2=None, op0=ALU.add),
                               r=[pb], w=[dst])
                        else:
                            OP("act", lambda e, pb=pb, dst=dst, hf=hf: e.activation(
                                out=dst.t[:, hf * 512:(hf + 1) * 512], in_=pb.t[:], func=AF.Identity), r=[pb], w=[dst])
                    if di is not None:
                        S.dma("sp", sts, lambda e, di=di: e.dma_start(out=d_modbc[di], in_=bct.t[:]), r=[bct])
                S.barrier()

        if mode != "B":
            with ExitStack() as pa:
                xt = [sbuf(pa, "xt%d" % i, [128, 1024]) for i in range(2)]
                win = sbuf(pa, "win", [128, 8, 2336], BF16)
                wout = sbuf(pa, "wout", [128, 8, 1024], BF16)
                with ExitStack() as pw:
                    wst = [sbuf(pw, "wcst%d" % i, [128, 2336]) for i in range(2)]
                    win_v = d_win.rearrange("(k p) n -> k p n", p=128)
                    wout_v = d_wout.rearrange("(k p) n -> k p n", p=128)
                    for kc in range(8):
                        ws = wst[kc % 2]
                        S.dma("sp", ld[kc % 2], lambda e, ws=ws, kc=kc: e.dma_start(out=ws.t[:], in_=win_v[kc]), w=[ws])
                        OP("pool", lambda e, ws=ws, kc=kc: e.tensor_copy(out=win.t[:, kc, :], in_=ws.t[:]), r=[ws], w=[win])
                    for kc in range(8):
                        ws = wst[kc % 2]
                        S.dma("sp", ld[kc % 2], lambda e, ws=ws, kc=kc: e.dma_start(out=ws.t[:, 0:1024], in_=wout_v[kc]), w=[ws])
                        OP("pool", lambda e, ws=ws, kc=kc: e.tensor_copy(out=wout.t[:, kc, :], in_=ws.t[:, 0:1024]), r=[ws], w=[wout])
                    S.barrier()
                w2 = sbuf(pa, "w2", [128, 512])
                OP("pool", lambda e: e.memset(w2.t[:], 0.0), w=[w2])
                gng = sbuf(pa, "gng", [128, 128]); esink = sbuf(pa, "esink", [128, 8])
                ln1g = sbuf(pa, "ln1g", [128, 1024]); ln1b = sbuf(pa, "ln1b", [128, 1024])
                tri = sbuf(pa, "tri", [128, 4, 128]); ci = sbuf(pa, "ci", [128, 2])
                amask = sbuf(pa, "amask", [128, 2, 128]); gmask = sbuf(pa, "gmask", [128, 2, 64])
                amask_e = sbuf(pa, "amask_e", [128, 2, 128])
                S.dma("sp", cst, lambda e: e.dma_start(out=w2.t[0:16, :], in_=d_w2), w=[w2])
                S.dma("sp", cst, lambda e: e.dma_start(out=w2.t[16:17, :], in_=d_bg), w=[w2])
                for b_, d_ in ((gng, d_gng), (esink, d_sink), (ln1g, d_ln1g), (ln1b, d_ln1b),
                               (tri, d_tri), (ci, d_ci), (amask, d_amask), (gmask, d_gmask)):
                    S.dma("sp", cst, lambda e, b_=b_, d_=d_: e.dma_start(out=b_.t[:], in_=d_), w=[b_])
                S.barrier()
                OP("act", lambda e: e.activation(out=esink.t[:], in_=esink.t[:], func=AF.Exp), r=[esink], w=[esink])
                for m in range(2):
                    OP("dve", lambda e, m=m: e.tensor_scalar(out=amask_e.t[:, m, :], in0=amask.t[:, m, :],
                                                             scalar1=flags.t[:, 96 + m:97 + m], scalar2=None, op0=ALU.mult),
                       r=[amask, flags], w=[amask_e])
                sbst = sbuf(pa, "sbst", [128, 32, 256])
                kT_all = sbuf(pa, "kT_all", [64, 18, 256]); v_all = sbuf(pa, "v_all", [128, 18, 130])
                kTc = sbuf(pa, "kTc", [64, 2, 256]); vc = sbuf(pa, "vc", [128, 2, 130])
                OP("pool", lambda e: e.memset(v_all.t[:], 1.0), w=[v_all])
                OP("pool", lambda e: e.memset(vc.t[:], 1.0), w=[vc])
                hT2 = [sbuf(pa, "hT%d" % i, [128, 8, 128], BF16) for i in range(2)]
                for h_ in hT2:
                    h_.c = []
                    for _ in range(8):
                        v_ = Buf(h_.t)
                        h_.c.append(v_)
                vsb2 = [sbuf(pa, "vsb%d" % i, [128, 512]) for i in range(2)]
                curb = {"hT": hT2[0], "vsb": vsb2[0]}
                qk = sbuf(pa, "qk", [128, 512])
                zT = [sbuf(pa, "zT", [128, 128])] * 2
                OP("dve", lambda e: e.memset(zT[0].t[:], 0.0), w=[zT[0]])
                OP("dve", lambda e: e.memset(zT[0].t[0:17, :], 1.0), w=[zT[0]])
                sp_ = [sbuf(pa, "sp", [128, 256])] * 2
                et = sbuf(pa, "et", [128, 256])
                Eb = [sbuf(pa, "Eb", [128, 256])] * 2
                Ei = [sbuf(pa, "Ei", [128, 256])] * 2
                Er = [sbuf(pa, "Er", [128, 256])] * 2
                qe = [sbuf(pa, "qe", [128, 256])] * 2
                ke = [sbuf(pa, "ke", [128, 256])] * 2
                kd = [sbuf(pa, "kd", [128, 256])] * 2
                Tz = [sbuf(pa, "Tz%d" % i, [128, 4, 128]) for i in range(2)]
                keT = [sbuf(pa, "keT%d" % i, [128, 2, 128]) for i in range(2)]
                ATz = sbuf(pa, "ATz", [128, 2, 4, 2, 64])
                for b_ in (Tz[0], Tz[1], ATz):
                    OP("pool", lambda e, b_=b_: e.memset(b_.t[:], 0.0), w=[b_])
                asb = [sbuf(pa, "asb", [128, 2, 2])] * 2
                Sst = {0: [sbuf(pa, "Sf%d" % i, [128, 2, 128]) for i in range(2)],
                       1: [sbuf(pa, "Sb%d" % i, [128, 2, 128]) for i in range(2)]}
                Scur = {0: 0, 1: 0}
                Stmp = sbuf(pa, "Stmp", [128, 2, 128])
                for dd in range(2):
                    OP("dve", lambda e, dd=dd: e.memset(Sst[dd][0].t[:], 0.0), w=[Sst[dd][0]])
                rsb = sbuf(pa, "rsb", [128, 512])
                ss = sbuf(pa, "ss", [128, 4]); rstd4 = sbuf(pa, "rstd4", [128, 4])
                on = sbuf(pa, "on", [128, 512]); junk = on
                aqs = on; qrot = sbuf(pa, "qrot", [128, 512]); rt = rsb
                akv = sbuf(pa, "akv", [128, 256]); krot = sbuf(pa, "krot", [128, 128])
                cs = sbuf(pa, "cs", [128, 32]); sn = sbuf(pa, "sn", [128, 32])
                qTs = sbuf(pa, "qTs", [64, 8, 128])
                Pall = sbuf(pa, "Pall", [128, 5, 512])
                den = sbuf(pa, "den", [128, 4]); cat = sbuf(pa, "cat", [128, 1024])
                r1 = cat; x1o = cat
                stats = sbuf(pa, "stats", [128, 12]); mv = sbuf(pa, "mv", [128, 2]); rs1 = sbuf(pa, "rs1", [128, 1])

                def front(src_ap, row):
                    xb = xt[xt_i[0] % 2]
                    slot = ld[xt_i[0] % 2]
                    curb["hT"] = hT2[xt_i[0] % 2]; curb["vsb"] = vsb2[xt_i[0] % 2]
                    hT = curb["hT"]
                    xt_i[0] += 1
                    S.dma("sp", slot, lambda e: e.dma_start(out=xb.t[:], in_=src_ap), w=[xb])

                    def evac(c0, n, pb):
                        for c in range(c0, c0 + n):
                            if c0 == 0:
                                OP("act", lambda e, c=c, pb=pb, c0=c0: e.activation(
                                    out=hT.t[:, c, :], in_=pb.t[:, (c - c0) * 128:(c - c0 + 1) * 128], func=AF.Identity,
                                    scale=sc1p.t[:, c, row:row + 1], bias=modT.t[:, c, row:row + 1]),
                                   r=[pb, sc1p, modT], w=[hT.c[c]])
                            else:
                                OP("dve", lambda e, c=c, pb=pb, c0=c0: e.tensor_scalar(
                                    out=hT.t[:, c, :], in0=pb.t[:, (c - c0) * 128:(c - c0 + 1) * 128],
                                    scalar1=sc1p.t[:, c, row:row + 1], scalar2=modT.t[:, c, row:row + 1], op0=ALU.mult, op1=ALU.add),
                                   r=[pb, sc1p, modT], w=[hT.c[c]])
                    transpose_to(xb, 8, evac)
                    return xb

                def inproj(col0, ncols, pb, pcol=0):
                    hT = curb["hT"]
                    for kc in range(8):
                        OP("pe", lambda e, kc=kc: e.matmul(pb.t[:, pcol:pcol + ncols], lhsT=hT.t[:, kc, :],
                                                           rhs=win.t[:, kc, col0:col0 + ncols], start=(kc == 0), stop=(kc == 7)),
                           r=[hT.c[kc], win], w=[pb])

                def gates(dd, flagcol=None):
                    hT = curb["hT"]
                    pz = bank()
                    for kc in range(8):
                        OP("pe", lambda e, kc=kc: e.matmul(pz.t[0:16, 0:128], lhsT=win.t[:, kc, 2304 + 16 * dd:2320 + 16 * dd],
                                                           rhs=hT.t[:, kc, :], start=(kc == 0), stop=(kc == 7)), r=[hT.c[kc], win], w=[pz])
                    OP("act", lambda e: e.activation(out=zT[dd].t[0:16, :], in_=pz.t[0:16, 0:128], func=AF.Identity), r=[pz], w=[zT[dd]])
                    pg = bank()
                    OP("pe", lambda e: e.matmul(pg.t[:, 0:256], lhsT=zT[dd].t[:], rhs=w2.t[:, dd * 256:(dd + 1) * 256],
                                                start=True, stop=True), r=[zT[dd], w2], w=[pg])
                    OP("act", lambda e: e.activation(out=et.t[:], in_=pg.t[:, 0:256], func=AF.Exp, scale=-1.0), r=[pg], w=[et])
                    OP("act", lambda e: e.activation(out=sp_[dd].t[:], in_=et.t[:], func=AF.Ln, bias=1.0), r=[et], w=[sp_[dd]])
                    if flagcol is not None:
                        OP("dve", lambda e: e.tensor_scalar(out=sp_[dd].t[:], in0=sp_[dd].t[:], scalar1=flags.t[:, flagcol:flagcol + 1],
                                                            scalar2=None, op0=ALU.mult), r=[sp_[dd], flags], w=[sp_[dd]])

                def decay_k(dd, ksrc, flagcol=None):
                    pr = bank()
                    OP("pe", lambda e: e.matmul(pr.t[:, 0:256], lhsT=tri.t[:, 1 + 2 * dd, :], rhs=sp_[dd].t[:], start=True, stop=True),
                       r=[tri, sp_[dd]], w=[pr])
                    OP("act", lambda e: e.activation(out=Er[dd].t[:], in_=pr.t[:, 0:256], func=AF.Exp), r=[pr], w=[Er[dd]])
                    if flagcol is None:
                        OP("dve", lambda e: e.tensor_tensor(out=kd[dd].t[:], in0=ksrc.t[:, 256:512], in1=Er[dd].t[:], op=ALU.mult),
                           r=[ksrc, Er[dd]], w=[kd[dd]])
                    else:
                        OP("dve", lambda e: e.scalar_tensor_tensor(out=kd[dd].t[:], in0=ksrc.t[:, 256:512],
                                                                   scalar=flags.t[:, flagcol:flagcol + 1], in1=Er[dd].t[:],
                                                                   op0=ALU.mult, op1=ALU.mult), r=[ksrc, Er[dd], flags], w=[kd[dd]])
                    pa_ = bank()
                    for hp in range(2):
                        OP("pe", lambda e, hp=hp: e.matmul(pa_.t[:, hp * 2:hp * 2 + 2], lhsT=sp_[dd].t[:, hp * 128:(hp + 1) * 128],
                                                           rhs=ci.t[:], start=True, stop=True), r=[sp_[dd], ci], w=[pa_])
                    OP("act", lambda e: e.activation(out=asb[dd].t[:], in_=pa_.t[:, 0:4].rearrange("p (a b) -> p a b", a=2),
                                                     func=AF.Exp), r=[pa_], w=[asb[dd]])

                def state_update(dd, c, vsrc):
                    pp = bank()
                    for h in range(4):
                        hp, par = h // 2, h % 2
                        OP("pe", lambda e, h=h, hp=hp, par=par: e.matmul(
                            pp.t[par * 64:(par + 1) * 64, hp * 128:(hp + 1) * 128],
                            lhsT=kd[dd].t[c * 64:(c + 1) * 64, h * 64:(h + 1) * 64],
                            rhs=vsrc.t[c * 64:(c + 1) * 64, h * 128:(h + 1) * 128], start=True, stop=True),
                           r=[kd[dd], vsrc], w=[pp])
                    so = Sst[dd][Scur[dd]]
                    sn_ = Sst[dd][1 - Scur[dd]]
                    OP("dve", lambda e: e.tensor_tensor(out=Stmp.t[:], in0=so.t[:],
                                                        in1=asb[dd].t[:, :, c:c + 1].broadcast_to([128, 2, 128]), op=ALU.mult),
                       r=[so, asb[dd]], w=[Stmp])
                    OP("dve", lambda e: e.tensor_tensor(out=sn_.t[:], in0=Stmp.t[:],
                                                        in1=pp.t[:, 0:256].rearrange("p (a b) -> p a b", a=2), op=ALU.add),
                       r=[Stmp, pp], w=[sn_])
                    Scur[dd] = 1 - Scur[dd]

                def state_tile(src_ap, row, dd, flagcol=None, store=None):
                    front(src_ap, row)
                    vsb = curb["vsb"]
                    pk = bank()
                    inproj(256, 256, pk, 256)
                    pv = bank()
                    inproj(512, 512, pv)
                    OP("act", lambda e: e.activation(out=vsb.t[:], in_=pv.t[:], func=AF.Identity), r=[pv], w=[vsb])
                    gates(dd, flagcol)
                    decay_k(dd, pk, flagcol)
                    for c in ((0, 1) if dd == 0 else (1, 0)):
                        if store is not None:
                            cur = Sst[dd][Scur[dd]]
                            OP("pool", lambda e, c=c, cur=cur: e.tensor_copy(out=sbst.t[:, store * 2 + c, :],
                                                                             in_=cur.t[:].rearrange("p a b -> p (a b)")),
                               r=[cur], w=[sbst])
                        state_update(dd, c, vsb)

                ksb2 = [sbuf(pa, "ksb%d" % i, [128, 512]) for i in range(2)]
                zT2 = [sbuf(pa, "zTp%d" % i, [128, 128]) for i in range(2)]
                for z_ in zT2:
                    OP("dve", lambda e, z_=z_: e.memset(z_.t[:], 0.0), w=[z_])
                    OP("dve", lambda e, z_=z_: e.memset(z_.t[0:17, :], 1.0), w=[z_])
                st_i = [0]

                def state_A1(n, src_ap, row):
                    front(src_ap, row)
                    hT = curb["hT"]
                    return dict(hT=hT, vsb=vsb2[n % 2], ksb=ksb2[n % 2], zTb=zT2[n % 2])

                def inproj_h(hT, col0, ncols, pb, pcol=0):
                    for kc in range(8):
                        OP("pe", lambda e, kc=kc: e.matmul(pb.t[:, pcol:pcol + ncols], lhsT=hT.t[:, kc, :],
                                                           rhs=win.t[:, kc, col0:col0 + ncols], start=(kc == 0), stop=(kc == 7)),
                           r=[hT.c[kc], win], w=[pb])

                def state_A2(cx):
                    pk = bank()
                    inproj_h(cx["hT"], 256, 256, pk, 256)
                    ksb = cx["ksb"]
                    OP("dve", lambda e: e.tensor_copy(out=ksb.t[:, 256:512], in_=pk.t[:, 256:512]), r=[pk], w=[ksb])

                def state_A3(cx):
                    pv = bank()
                    inproj_h(cx["hT"], 512, 512, pv)
                    vsb = cx["vsb"]
                    OP("act", lambda e: e.activation(out=vsb.t[:], in_=pv.t[:], func=AF.Identity), r=[pv], w=[vsb])

                def state_A4(cx, dd):
                    pz = bank()
                    hT, zTb = cx["hT"], cx["zTb"]
                    for kc in range(8):
                        OP("pe", lambda e, kc=kc: e.matmul(pz.t[0:16, 0:128], lhsT=win.t[:, kc, 2304 + 16 * dd:2320 + 16 * dd],
                                                           rhs=hT.t[:, kc, :], start=(kc == 0), stop=(kc == 7)), r=[hT.c[kc], win], w=[pz])
                    OP("act", lambda e: e.activation(out=zTb.t[0:16, :], in_=pz.t[0:16, 0:128], func=AF.Identity), r=[pz], w=[zTb])

                def state_B1(cx, dd, flagcol):
                    zTb = cx["zTb"]
                    pg = bank()
                    OP("pe", lambda e: e.matmul(pg.t[:, 0:256], lhsT=zTb.t[:], rhs=w2.t[:, dd * 256:(dd + 1) * 256],
                                                start=True, stop=True), r=[zTb, w2], w=[pg])
                    OP("act", lambda e: e.activation(out=et.t[:], in_=pg.t[:, 0:256], func=AF.Exp, scale=-1.0), r=[pg], w=[et])
                    OP("act", lambda e: e.activation(out=sp_[dd].t[:], in_=et.t[:], func=AF.Ln, bias=1.0), r=[et], w=[sp_[dd]])
                    if flagcol is not None:
                        OP("dve", lambda e: e.tensor_scalar(out=sp_[dd].t[:], in0=sp_[dd].t[:], scalar1=flags.t[:, flagcol:flagcol + 1],
                                                            scalar2=None, op0=ALU.mult), r=[sp_[dd], flags], w=[sp_[dd]])

                def state_B2(cx, dd, flagcol):
                    decay_k(dd, cx["ksb"], flagcol)

                def state_B3(cx, dd, store, c):
                    if store is not None:
                        cur = Sst[dd][Scur[dd]]
                        OP("pool", lambda e, c=c, cur=cur: e.tensor_copy(out=sbst.t[:, store * 2 + c, :],
                                                                         in_=cur.t[:].rearrange("p a b -> p (a b)")),
                           r=[cur], w=[sbst])
                    state_update(dd, c, cx["vsb"])

                def run_state_jobs(jobs):
                    N = len(jobs)
                    cxs = {}
                    for step in range(N + 2):
                        if step < N:
                            cxs[step] = state_A1(step, jobs[step][0], jobs[step][1])
                        m, b_ = step - 1, step - 2
                        hm = 0 <= m < N
                        hb = 0 <= b_ < N
                        ORD = int(os.environ.get("K_ORD", "1"))
                        if hb:
                            _, _, ddb, flb, stb = jobs[b_]
                            cs_ = (0, 1) if ddb == 0 else (1, 0)
                        if ORD == 0:
                            seq = ["A2", "A3", "A4", "B1", "B2", "B3", "B4"]
                        elif ORD == 1:
                            seq = ["B1", "A2", "B2", "A3", "B3", "B4", "A4"]
                        elif ORD == 2:
                            seq = ["B1", "A2", "B2", "A3", "A4", "B3", "B4"]
                        else:
                            seq = ["A2", "B1", "A3", "B2", "A4", "B3", "B4"]
                        for st_ in seq:
                            if st_[0] == "A" and hm:
                                if st_ == "A2": state_A2(cxs[m])
                                elif st_ == "A3": state_A3(cxs[m])
                                else: state_A4(cxs[m], jobs[m][2])
                            if st_[0] == "B" and hb:
                                if st_ == "B1": state_B1(cxs[b_], ddb, flb)
                                elif st_ == "B2": state_B2(cxs[b_], ddb, flb)
                                elif st_ == "B3": state_B3(cxs[b_], ddb, stb, cs_[0])
                                else: state_B3(cxs[b_], ddb, stb, cs_[1])
                        if hb:
                            del cxs[b_]

                def rope(src, dst, H, tmp):
                    v5 = lambda b: b.t[:, 0:H * 64].rearrange("p (h a f d) -> p h a f d", h=H, a=2, f=2, d=16)
                    cb = cs.t[:].rearrange("p (a d) -> p a d", a=2).unsqueeze(1).broadcast_to([128, H, 2, 16])
                    sb_ = sn.t[:].rearrange("p (a d) -> p a d", a=2).unsqueeze(1).broadcast_to([128, H, 2, 16])
                    x1, x2 = v5(src)[:, :, :, 0, :], v5(src)[:, :, :, 1, :]
                    o1, o2 = v5(dst)[:, :, :, 0, :], v5(dst)[:, :, :, 1, :]
                    t1, t2 = v5(tmp)[:, :, :, 0, :], v5(tmp)[:, :, :, 1, :]
                    OP("pool", lambda e: e.tensor_tensor(out=o1, in0=x1, in1=cb, op=ALU.mult), r=[src, cs], w=[dst])
                    OP("pool", lambda e: e.tensor_tensor(out=t1, in0=x2, in1=sb_, op=ALU.mult), r=[src, sn], w=[tmp])
                    OP("pool", lambda e: e.tensor_tensor(out=o1, in0=o1, in1=t1, op=ALU.subtract), r=[dst, tmp], w=[dst])
                    OP("pool", lambda e: e.tensor_tensor(out=o2, in0=x2, in1=cb, op=ALU.mult), r=[src, cs], w=[dst])
                    OP("pool", lambda e: e.tensor_tensor(out=t2, in0=x1, in1=sb_, op=ALU.mult), r=[src, sn], w=[tmp])
                    OP("pool", lambda e: e.tensor_tensor(out=o2, in0=o2, in1=t2, op=ALU.add), r=[dst, tmp], w=[dst])

                def kv_tile(src_ap, row, j, is_ctx):
                    front(src_ap, row)
                    pb = bank()
                    inproj(2048, 256, pb)
                    OP("act", lambda e: e.activation(out=akv.t[:], in_=pb.t[:, 0:256], func=AF.Identity), r=[pb], w=[akv])
                    if is_ctx:
                        ksrc, kdst, vdst = akv, kTc, vc
                    else:
                        S.dma("sp", csl, lambda e: e.dma_start(out=cs.t[:], in_=d_cos[j * 128:(j + 1) * 128, :]), w=[cs])
                        S.dma("sp", snl, lambda e: e.dma_start(out=sn.t[:], in_=d_sin[j * 128:(j + 1) * 128, :]), w=[sn])
                        rope(akv, krot, 2, rt)
                        ksrc, kdst, vdst = krot, kT_all, v_all
                    OP("pool", lambda e: e.tensor_copy(
                        out=vdst.t[:, j, :].rearrange("p (g d) -> p g d", g=2)[:, :, 0:64],
                        in_=akv.t[:, 128:256].rearrange("p (g d) -> p g d", g=2)), r=[akv], w=[vdst])

                    def evac(c0, n, pb2):
                        OP("act", lambda e: e.activation(out=kdst.t[:, j, :], in_=pb2.t[0:64, 0:256], func=AF.Identity),
                           r=[pb2], w=[kdst])
                    transpose_to(ksrc, 2, evac, width=64)

                def own_tile(i):
                    j = i + 1
                    xb = front(d_xown[j * 128:(j + 1) * 128, :], 0)
                    vsb = curb["vsb"]; catT = curb["hT"]
                    pqk = bank(); inproj(0, 512, pqk)
                    OP("act", lambda e: e.activation(out=qk.t[:], in_=pqk.t[:], func=AF.Identity), r=[pqk], w=[qk])
                    pv = bank(); inproj(512, 512, pv)
                    OP("act", lambda e: e.activation(out=vsb.t[:], in_=pv.t[:], func=AF.Identity), r=[pv], w=[vsb])
                    pr_ = bank(); inproj(1024, 512, pr_)
                    OP("act", lambda e: e.activation(out=rsb.t[:], in_=pr_.t[:], func=AF.Silu), r=[pr_], w=[rsb])
                    for dd in range(2):
                        gates(dd)
                        pbn = bank()
                        OP("pe", lambda e, dd=dd, pbn=pbn: e.matmul(pbn.t[:, 0:256], lhsT=tri.t[:, 2 * dd, :], rhs=sp_[dd].t[:],
                                                                    start=True, stop=True), r=[tri, sp_[dd]], w=[pbn])
                        OP("act", lambda e, dd=dd, pbn=pbn: e.activation(out=Eb[dd].t[:], in_=pbn.t[:, 0:256], func=AF.Exp),
                           r=[pbn], w=[Eb[dd]])
                        OP("act", lambda e, dd=dd, pbn=pbn: e.activation(out=Ei[dd].t[:], in_=pbn.t[:, 0:256], func=AF.Exp, scale=-1.0),
                           r=[pbn], w=[Ei[dd]])
                        OP("dve", lambda e, dd=dd: e.scalar_tensor_tensor(out=qe[dd].t[:], in0=qk.t[:, 0:256], scalar=0.125, in1=Eb[dd].t[:],
                                                                          op0=ALU.mult, op1=ALU.mult), r=[qk, Eb[dd]], w=[qe[dd]])
                        OP("dve", lambda e, dd=dd: e.tensor_tensor(out=ke[dd].t[:], in0=qk.t[:, 256:512], in1=Ei[dd].t[:], op=ALU.mult),
                           r=[qk, Ei[dd]], w=[ke[dd]])
                        if dd == 0:
                            decay_k(dd, qk)
                        pT = bank()
                        for idx, srcb in enumerate((qe[dd], qe[dd], ke[dd], ke[dd])):
                            hp = idx % 2
                            OP("pe", lambda e, idx=idx, hp=hp, srcb=srcb, pT=pT: e.transpose(
                                out=pT.t[:, idx * 128:(idx + 1) * 128], in_=srcb.t[:, hp * 128:(hp + 1) * 128], identity=ident.t[:]),
                               r=[srcb, ident], w=[pT])
                        OP("act", lambda e, dd=dd, pT=pT: e.activation(out=keT[dd].t[:].rearrange("p a b -> p (a b)"), in_=pT.t[:, 256:512],
                                                                       func=AF.Identity), r=[pT], w=[keT[dd]])
                        for par in range(2):
                            OP("act", lambda e, dd=dd, pT=pT, par=par: e.activation(
                                out=Tz[dd].t[par * 64:(par + 1) * 64, par::2, :],
                                in_=pT.t[par * 64:(par + 1) * 64, 0:256].rearrange("p (a b) -> p a b", a=2), func=AF.Identity),
                               r=[pT], w=[Tz[dd]])
                    pAT = bank()
                    for dd in range(2):
                        for c in range(2):
                            for h in range(4):
                                hp = h // 2
                                OP("pe", lambda e, dd=dd, c=c, h=h, hp=hp: e.matmul(
                                    pAT.t[c * 64:(c + 1) * 64, (dd * 4 + h) * 64:(dd * 4 + h + 1) * 64],
                                    lhsT=keT[dd].t[:, hp, c * 64:(c + 1) * 64],
                                    rhs=Tz[dd].t[:, h, c * 64:(c + 1) * 64], start=True, stop=True),
                                   r=[keT[dd], Tz[dd]], w=[pAT])
                    for c in range(2):
                        OP("dve", lambda e, c=c: e.tensor_tensor(
                            out=ATz.t[c * 64:(c + 1) * 64, :, :, c, :],
                            in0=pAT.t[c * 64:(c + 1) * 64, :].rearrange("p (d h c) -> p d h c", d=2, h=4),
                            in1=gmask.t[c * 64:(c + 1) * 64, :, :].unsqueeze(2).broadcast_to([64, 2, 4, 64]), op=ALU.mult),
                           r=[pAT, gmask], w=[ATz])
                    po = bank()
                    for c in range(2):
                        sf = Sst[0][Scur[0]]
                        for h in range(4):
                            hp = h // 2
                            outp = po.t[c * 64:(c + 1) * 64, h * 128:(h + 1) * 128]
                            vv = vsb.t[:, h * 128:(h + 1) * 128]
                            OP("pe", lambda e, c=c, h=h, outp=outp, vv=vv: e.matmul(
                                outp, lhsT=ATz.t[:, 0, h, c, :], rhs=vv, start=True, stop=False), r=[ATz, vsb], w=[po])
                            OP("pe", lambda e, c=c, h=h, hp=hp, outp=outp, sf=sf: e.matmul(
                                outp, lhsT=Tz[0].t[:, h, c * 64:(c + 1) * 64], rhs=sf.t[:, hp, :], start=False, stop=False),
                               r=[Tz[0], sf], w=[po])
                            OP("pe", lambda e, c=c, h=h, outp=outp, vv=vv: e.matmul(
                                outp, lhsT=ATz.t[:, 1, h, c, :], rhs=vv, start=False, stop=False), r=[ATz, vsb], w=[po])
                            OP("pe", lambda e, c=c, h=h, hp=hp, outp=outp: e.matmul(
                                outp, lhsT=Tz[1].t[:, h, c * 64:(c + 1) * 64],
                                rhs=sbst.t[:, i * 2 + c, hp * 128:(hp + 1) * 128], start=False, stop=True), r=[Tz[1], sbst], w=[po])
                        state_update(0, c, vsb)
                    for h in range(4):
                        OP("act", lambda e, h=h: e.activation(out=junk.t[:, h * 128:(h + 1) * 128], in_=po.t[:, h * 128:(h + 1) * 128],
                                                              func=AF.Square, accum_out=ss.t[:, h:h + 1]), r=[po], w=[junk, ss])
                    OP("dve", lambda e: e.tensor_scalar(out=rstd4.t[:], in0=ss.t[:], scalar1=1.0 / 128, scalar2=eps_t.t[:, 0:1],
                                                        op0=ALU.mult, op1=ALU.add), r=[ss, eps_t], w=[rstd4])
                    OP("act", lambda e: e.activation(out=rstd4.t[:], in_=rstd4.t[:], func=AF.Sqrt), r=[rstd4], w=[rstd4])
                    OP("dve", lambda e: e.reciprocal(out=rstd4.t[:], in_=rstd4.t[:]), r=[rstd4], w=[rstd4])
                    on3 = on.t[:].rearrange("p (h d) -> p h d", h=4)
                    OP("dve", lambda e: e.tensor_tensor(out=on3, in0=po.t[:].rearrange("p (h d) -> p h d", h=4),
                                                        in1=rstd4.t[:].unsqueeze(2).broadcast_to([128, 4, 128]), op=ALU.mult),
                       r=[po, rstd4], w=[on])
                    OP("pool", lambda e: e.tensor_tensor(out=on3, in0=on3, in1=gng.t[:].unsqueeze(1).broadcast_to([128, 4, 128]),
                                                         op=ALU.mult), r=[on, gng], w=[on])
                    OP("pool", lambda e: e.tensor_tensor(out=cat.t[:, 0:512], in0=on.t[:], in1=rsb.t[:], op=ALU.mult),
                       r=[on, rsb], w=[cat])
                    paq = bank(); inproj(1536, 512, paq)
                    OP("act", lambda e: e.activation(out=aqs.t[:], in_=paq.t[:], func=AF.Identity), r=[paq], w=[aqs])
                    S.dma("sp", csl, lambda e: e.dma_start(out=cs.t[:], in_=d_cos[j * 128:(j + 1) * 128, :]), w=[cs])
                    S.dma("sp", snl, lambda e: e.dma_start(out=sn.t[:], in_=d_sin[j * 128:(j + 1) * 128, :]), w=[sn])
                    rope(aqs, qrot, 8, rt)

                    def evq(c0, n, pb2):
                        OP("act", lambda e: e.activation(out=qTs.t[:, c0:c0 + n, :].rearrange("p a b -> p (a b)"),
                                                         in_=pb2.t[0:64, 0:n * 128], func=AF.Identity, scale=0.125), r=[pb2], w=[qTs])
                    transpose_to(qrot, 8, evq, width=64)
                    def att_group(g):
                        kts = [(kT_all, v_all, j - 1, amask_e if i == 0 else amask, 0), (kT_all, v_all, j, None, 0),
                               (kT_all, v_all, j + 1, amask_e if i == 15 else amask, 1), (kTc, vc, 0, None, 0), (kTc, vc, 1, None, 0)]
                        for n_, (kb, vb, jj, mk, mi) in enumerate(kts):
                            pst = bank()
                            OP("pe", lambda e, kb=kb, jj=jj, pst=pst: e.matmul(
                                pst.t[:], lhsT=kb.t[:, jj, g * 128:(g + 1) * 128],
                                rhs=qTs.t[:, 4 * g:4 * g + 4, :].rearrange("p a b -> p (a b)"), start=True, stop=True),
                               r=[kb, qTs], w=[pst])
                            OP("act", lambda e, n_=n_, pst=pst: e.activation(out=Pall.t[:, n_, :], in_=pst.t[:], func=AF.Exp),
                               r=[pst], w=[Pall])
                            if mk is not None:
                                OP("dve", lambda e, n_=n_, mk=mk, mi=mi: e.tensor_tensor(
                                    out=Pall.t[:, n_, :].rearrange("p (h q) -> p h q", h=4),
                                    in0=Pall.t[:, n_, :].rearrange("p (h q) -> p h q", h=4),
                                    in1=mk.t[:, mi:mi + 1, :].broadcast_to([128, 4, 128]), op=ALU.mult), r=[Pall, mk], w=[Pall])
                        pO = bank()
                        for hh in range(4):
                            for n_, (kb, vb, jj, mk, mi) in enumerate(kts):
                                OP("pe", lambda e, hh=hh, n_=n_, vb=vb, jj=jj: e.matmul(
                                    pO.t[:, hh * 65:(hh + 1) * 65], lhsT=Pall.t[:, n_, hh * 128:(hh + 1) * 128],
                                    rhs=vb.t[:, jj, g * 65:(g + 1) * 65], start=(n_ == 0), stop=(n_ == 4)), r=[Pall, vb], w=[pO])
                        pO3 = pO.t[:, 0:260].rearrange("p (h d) -> p h d", h=4)
                        OP("dve", lambda e, pO3=pO3: e.tensor_tensor(out=den.t[:].unsqueeze(2), in0=pO3[:, :, 64:65],
                                                                     in1=esink.t[:, 4 * g:4 * g + 4].unsqueeze(2), op=ALU.add),
                           r=[pO, esink], w=[den])
                        OP("dve", lambda e: e.reciprocal(out=den.t[:], in_=den.t[:]), r=[den], w=[den])
                        OP("dve", lambda e, pO3=pO3: e.tensor_tensor(
                            out=cat.t[:, 512 + g * 256:512 + (g + 1) * 256].rearrange("p (h d) -> p h d", h=4),
                            in0=pO3[:, :, 0:64], in1=den.t[:].unsqueeze(2).broadcast_to([128, 4, 64]), op=ALU.mult),
                           r=[pO, den], w=[cat])
                    for g_ in range(2):
                        att_group(g_)
                    if os.environ.get("K_DBG") == "cat":
                        S.dma("pool", sts, lambda e: e.dma_start(out=d_x1[i * 128:(i + 1) * 128, :], in_=cat.t[:]), r=[cat])
                        return
                    def evc(c0, n, pb2):
                        OP("act", lambda e: e.activation(out=catT.t[:, c0:c0 + n, :].rearrange("p a b -> p (a b)"),
                                                         in_=pb2.t[:, 0:n * 128], func=AF.Identity), r=[pb2], w=catT.c[c0:c0 + n])
                    transpose_to(cat, 8, evc)
                    for hf in range(2):
                        py = bank()
                        for kc in range(8):
                            OP("pe", lambda e, kc=kc, py=py, hf=hf: e.matmul(py.t[:], lhsT=catT.t[:, kc, :],
                                                                      rhs=wout.t[:, kc, hf * 512:(hf + 1) * 512],
                                                                      start=(kc == 0), stop=(kc == 7)), r=[catT.c[kc], wout], w=[py])
                        OP("dve", lambda e, py=py, hf=hf: e.tensor_tensor(out=r1.t[:, hf * 512:(hf + 1) * 512], in0=py.t[:],
                                                                   in1=g1bc.t[:, hf * 512:(hf + 1) * 512], op=ALU.mult),
                           r=[py, g1bc], w=[r1])
                    OP("dve", lambda e: e.scalar_tensor_tensor(out=r1.t[:], in0=xb.t[:], scalar=ALPHA, in1=r1.t[:],
                                                               op0=ALU.mult, op1=ALU.add), r=[xb, r1], w=[r1])
                    layernorm(r1, x1o, ln1g, ln1b, stats, mv, rs1)
                    S.dma("pool", sts, lambda e: e.dma_start(out=d_x1[i * 128:(i + 1) * 128, :], in_=x1o.t[:]), r=[x1o])

                def layernorm(src, dst, gb, bb, stats, mv, rs1):
                    for hf in range(2):
                        OP("dve", lambda e, hf=hf: e.bn_stats(out=stats.t[:, hf * 6:(hf + 1) * 6], in_=src.t[:, hf * 512:(hf + 1) * 512]),
                           r=[src], w=[stats])
                    OP("dve", lambda e: e.bn_aggr(out=mv.t[:], in_=stats.t[:]), r=[stats], w=[mv])
                    OP("dve", lambda e: e.tensor_scalar(out=rs1.t[:], in0=mv.t[:, 1:2], scalar1=eps_t.t[:, 0:1], scalar2=None, op0=ALU.add),
                       r=[mv, eps_t], w=[rs1])
                    OP("act", lambda e: e.activation(out=rs1.t[:], in_=rs1.t[:], func=AF.Sqrt), r=[rs1], w=[rs1])
                    OP("dve", lambda e: e.reciprocal(out=rs1.t[:], in_=rs1.t[:]), r=[rs1], w=[rs1])
                    OP("dve", lambda e: e.tensor_scalar(out=dst.t[:], in0=src.t[:], scalar1=mv.t[:, 0:1], scalar2=rs1.t[:, 0:1],
                                                        op0=ALU.subtract, op1=ALU.mult), r=[src, mv, rs1], w=[dst])
                    OP("pool", lambda e: e.tensor_tensor(out=dst.t[:], in0=dst.t[:], in1=gb.t[:], op=ALU.mult), r=[dst, gb], w=[dst])
                    OP("pool", lambda e: e.tensor_tensor(out=dst.t[:], in0=dst.t[:], in1=bb.t[:], op=ALU.add), r=[dst, bb], w=[dst])

                for t in range(2):
                    kv_tile(d_ctx[t * 128:(t + 1) * 128, :], 1, t, True)
                convert_tables()
                jobs = []
                for t in range(2):
                    jobs.append((d_ctx[t * 128:(t + 1) * 128, :], 1, 0, None, None))
                for t in (1, 0):
                    jobs.append((d_ctx[t * 128:(t + 1) * 128, :], 1, 1, None, None))
                for t in range(NPRE):
                    jobs.append((d_xpf[t * 128:(t + 1) * 128, :], 0, 0, t, None))
                for t in range(NPRE):
                    jobs.append((d_xpb[t * 128:(t + 1) * 128, :], 0, 1, 48 + t, None))
                for i in range(15, -1, -1):
                    jobs.append((d_xown[(i + 1) * 128:(i + 2) * 128, :], 0, 1, None, i))
                run_state_jobs(jobs)
                for j in range(18):
                    kv_tile(d_xown[j * 128:(j + 1) * 128, :], 0, j, False)
                for i in range(16):
                    own_tile(i)
                S.barrier()

        if mode != "A":
            with ExitStack() as pbs_:
                wq = sbuf(pbs_, "wq", [128, 8, 1024]); skT = sbuf(pbs_, "skT", [128, 8, 128])
                ln2g = sbuf(pbs_, "ln2g", [128, 1024]); ln2b = sbuf(pbs_, "ln2b", [128, 1024])
                iota = sbuf(pbs_, "iota", [128, 16])
                S.dma("sp", cst, lambda e: e.dma_start(out=wq.t[:], in_=d_wq.rearrange("(k p) n -> p k n", p=128)), w=[wq])
                for b_, d_ in ((skT, d_skT), (ln2g, d_ln2g), (ln2b, d_ln2b), (iota, d_iota)):
                    S.dma("sp", cst, lambda e, b_=b_, d_=d_: e.dma_start(out=b_.t[:], in_=d_), w=[b_])
                S.barrier()
                if mode == "B":
                    convert_tables()
                identb = sbuf(pbs_, "identb", [128, 128], BF16)
                OP("dve", lambda e: e.tensor_copy(out=identb.t[:], in_=ident.t[:]), r=[ident], w=[identb])
                dg = [sbuf(pbs_, "dg%d" % i, [128, 128], BF16) for i in range(4)]
                sc2bc = sbuf(pbs_, "sc2bc", [128, 1024]); sh2bc = sbuf(pbs_, "sh2bc", [128, 1024]); g2bc = sbuf(pbs_, "g2bc", [128, 1024])
                if mode == "B":
                    OP("dve", lambda e: e.memset(sc2bc.t[:], 1.0), w=[sc2bc])
                    OP("dve", lambda e: e.memset(sh2bc.t[:], 0.0), w=[sh2bc])
                    OP("dve", lambda e: e.memset(g2bc.t[:], 1.0), w=[g2bc])
                else:
                    for b_, di in ((sh2bc, 0), (sc2bc, 1), (g2bc, 2)):
                        S.dma("sp", cst, lambda e, b_=b_, di=di: e.dma_start(out=b_.t[:], in_=d_modbc[di]), w=[b_])
                    S.barrier()
                x1t = [sbuf(pbs_, "x1t%d" % i, [128, 1024]) for i in range(3)]
                h2 = [sbuf(pbs_, "h2_%d" % i, [128, 1024]) for i in range(2)]
                h2T = sbuf(pbs_, "h2T", [128, 8, 128]); qTp = sbuf(pbs_, "qTp", [128, 8, 128])
                ssb = sbuf(pbs_, "ssb", [128, 16, 128]); sw = sbuf(pbs_, "sw", [128, 128])
                m16 = sbuf(pbs_, "m16", [128, 16, 16]); ix16 = sbuf(pbs_, "ix16", [128, 16, 16], U32)
                ixf = sbuf(pbs_, "ixf", [128, 16, 16])
                cand = sbuf(pbs_, "cand", [128, 8, 256]); cw = sbuf(pbs_, "cw", [128, 256])
                tsv = sbuf(pbs_, "tsv", [128, 8, 16]); pos = sbuf(pbs_, "pos", [128, 8, 16], U32)
                posf = sbuf(pbs_, "posf", [128, 8, 16])
                lohi = sbuf(pbs_, "lohi", [128, 2, 16])
                paf = sbuf(pbs_, "paf", [128, 8, 16]); pbf = sbuf(pbs_, "pbf", [128, 8, 16])
                oh = sbuf(pbs_, "oh", [128, 8, 16, 16])
                i1s = sbuf(pbs_, "i1s", [128, 8, 16]); i2s = sbuf(pbs_, "i2s", [128, 8, 16])
                eidf = sbuf(pbs_, "eidf", [128, 128])
                eidx = [sbuf(pbs_, "eidx%d" % i, [128, 128], U32) for i in range(3)]
                gate = [sbuf(pbs_, "gate%d" % i, [128, 8, 16]) for i in range(2)]
                gsum = sbuf(pbs_, "gsum", [128, 8])
                dots = [sbuf(pbs_, "dots%d" % i, [128, 128]) for i in range(2)]
                coef = [sbuf(pbs_, "coef%d" % i, [128, 128]) for i in range(2)]
                gb = [sbuf(pbs_, "gb%d" % i, [128, 2048], BF16) for i in range(NG)]
                gs = [S.dma_slot("g%d" % i) for i in range(NG)]
                prod = [sbuf(pbs_, "prod%d" % i, [128, 1024], BF16) for i in range(3)]
                h2b = [sbuf(pbs_, "h2b%d" % i, [128, 1024], BF16) for i in range(2)]
                acc = [sbuf(pbs_, "acc%d" % i, [128, 1024]) for i in range(2)]
                stats2 = sbuf(pbs_, "stats2", [128, 12]); mv2 = sbuf(pbs_, "mv2", [128, 2]); rs2 = sbuf(pbs_, "rs2", [128, 1])
                x1ld = [S.dma_slot("x1l%d" % i) for i in range(3)]

                def layernorm2(src, dst):
                    for hf in range(2):
                        OP("dve", lambda e, hf=hf: e.bn_stats(out=stats2.t[:, hf * 6:(hf + 1) * 6], in_=src.t[:, hf * 512:(hf + 1) * 512]),
                           r=[src], w=[stats2])
                    OP("dve", lambda e: e.bn_aggr(out=mv2.t[:], in_=stats2.t[:]), r=[stats2], w=[mv2])
                    OP("dve", lambda e: e.tensor_scalar(out=rs2.t[:], in0=mv2.t[:, 1:2], scalar1=eps_t.t[:, 0:1], scalar2=None, op0=ALU.add),
                       r=[mv2, eps_t], w=[rs2])
                    OP("act", lambda e: e.activation(out=rs2.t[:], in_=rs2.t[:], func=AF.Sqrt), r=[rs2], w=[rs2])
                    OP("dve", lambda e: e.reciprocal(out=rs2.t[:], in_=rs2.t[:]), r=[rs2], w=[rs2])
                    OP("dve", lambda e: e.tensor_scalar(out=dst.t[:], in0=src.t[:], scalar1=mv2.t[:, 0:1], scalar2=rs2.t[:, 0:1],
                                                        op0=ALU.subtract, op1=ALU.mult), r=[src, mv2, rs2], w=[dst])
                    OP("dve", lambda e: e.tensor_tensor(out=dst.t[:], in0=dst.t[:], in1=ln2g.t[:], op=ALU.mult), r=[dst, ln2g], w=[dst])
                    OP("dve", lambda e: e.tensor_tensor(out=dst.t[:], in0=dst.t[:], in1=ln2b.t[:], op=ALU.add), r=[dst, ln2b], w=[dst])

                def top16(src3, n, work, mdst, idst):
                    (sa, sbuf_), (wa, wbuf), (ma, mbuf), (ia, ibuf) = src3, work, mdst, idst
                    OP("dve", lambda e: e.max(out=ma[:, 0:8], in_=sa), r=[sbuf_], w=[mbuf])
                    OP("dve", lambda e: e.max_index(out=ia[:, 0:8], in_max=ma[:, 0:8], in_values=sa), r=[sbuf_, mbuf], w=[ibuf])
                    OP("dve", lambda e: e.match_replace(out=wa, in_to_replace=ma[:, 0:8], in_values=sa, imm_value=-1e30),
                       r=[sbuf_, mbuf], w=[wbuf])
                    OP("dve", lambda e: e.max(out=ma[:, 8:16], in_=wa), r=[wbuf], w=[mbuf])
                    OP("dve", lambda e: e.max_index(out=ia[:, 8:16], in_max=ma[:, 8:16], in_values=wa), r=[wbuf, mbuf], w=[ibuf])

                def route(i):
                    xs = x1t[i % 3]
                    S.dma("sp", x1ld[i % 3], lambda e: e.dma_start(out=xs.t[:], in_=d_x1[i * 128:(i + 1) * 128, :]), w=[xs])
                    hh = h2[i % 2]
                    OP("dve", lambda e: e.tensor_tensor(out=hh.t[:], in0=xs.t[:], in1=sc2bc.t[:], op=ALU.mult), r=[xs, sc2bc], w=[hh])
                    OP("dve", lambda e: e.tensor_tensor(out=hh.t[:], in0=hh.t[:], in1=sh2bc.t[:], op=ALU.add), r=[hh, sh2bc], w=[hh])
                    OP("act", lambda e: e.activation(out=h2b[i % 2].t[:], in_=hh.t[:], func=AF.Identity), r=[hh], w=[h2b[i % 2]])

                    if KSTOP < 2: return
                    def ev(c0, n, pb2):
                        OP("act", lambda e: e.activation(out=h2T.t[:, c0:c0 + n, :].rearrange("p a b -> p (a b)"),
                                                         in_=pb2.t[:, 0:n * 128], func=AF.Identity), r=[pb2], w=[h2T])
                    yield
                    transpose_to(hh, 8, ev)
                    yield
                    if KSTOP < 2.3: return
                    for j0 in range(0, 8, 4):
                        pq = bank()
                        for jj in range(j0, j0 + 4):
                            for kc in range(8):
                                OP("pe", lambda e, jj=jj, kc=kc, pq=pq, j0=j0: e.matmul(
                                    pq.t[:, (jj - j0) * 128:(jj - j0 + 1) * 128], lhsT=wq.t[:, kc, jj * 128:(jj + 1) * 128],
                                    rhs=h2T.t[:, kc, :], start=(kc == 0), stop=(kc == 7)), r=[wq, h2T], w=[pq])
                        OP("act", lambda e, pq=pq, j0=j0: e.activation(out=qTp.t[:, j0:j0 + 4, :].rearrange("p a b -> p (a b)"),
                                                                       in_=pq.t[:], func=AF.Identity), r=[pq], w=[qTp])
                    if KSTOP < 2.6: return
                    for h0 in range(0, 8, 4):
                        for a in range(2):
                            psc = bank()
                            for h in range(h0, h0 + 4):
                                OP("pe", lambda e, h=h, a=a, psc=psc, h0=h0: e.matmul(
                                    psc.t[:, (h - h0) * 128:(h - h0 + 1) * 128], lhsT=qTp.t[a * 64:(a + 1) * 64, h, :],
                                    rhs=skT.t[a * 64:(a + 1) * 64, h, :], start=True, stop=True), r=[qTp, skT], w=[psc])
                            OP("act", lambda e, psc=psc, h0=h0, a=a: e.activation(
                                out=ssb.t[:, 2 * h0 + a:2 * h0 + 8:2, :], in_=psc.t[:].rearrange("p (a b) -> p a b", a=4),
                                func=AF.Identity), r=[psc], w=[ssb])
                    if KSTOP < 3: return
                    for q_ in range(16):
                        top16((ssb.t[:, q_, :], ssb), 128, (sw.t[:], sw), (m16.t[:, q_, :], m16), (ix16.t[:, q_, :], ix16))
                        yield
                    if KSTOP < 4: return
                    OP("dve", lambda e: e.tensor_copy(out=ixf.t[:], in_=ix16.t[:]), r=[ix16], w=[ixf])
                    m4 = m16.t[:].rearrange("p (h a) k -> p h a k", a=2)
                    OP("dve", lambda e: e.tensor_tensor(
                        out=cand.t[:].rearrange("p h (a b) -> p h a b", a=16),
                        in0=m4[:, :, 0, :].unsqueeze(3).broadcast_to([128, 8, 16, 16]),
                        in1=m4[:, :, 1, :].unsqueeze(2).broadcast_to([128, 8, 16, 16]), op=ALU.add), r=[m16], w=[cand])
                    for h in range(8):
                        top16((cand.t[:, h, :], cand), 256, (cw.t[:], cw), (tsv.t[:, h, :], tsv), (pos.t[:, h, :], pos))
                        yield
                    gt = gate[i % 2]
                    OP("dve", lambda e: e.tensor_tensor(out=gt.t[:], in0=tsv.t[:], in1=tsv.t[:, :, 0:1].broadcast_to([128, 8, 16]),
                                                        op=ALU.subtract), r=[tsv], w=[gt])
                    OP("act", lambda e: e.activation(out=gt.t[:], in_=gt.t[:], func=AF.Exp), r=[gt], w=[gt])
                    OP("dve", lambda e: e.tensor_reduce(out=gsum.t[:], in_=gt.t[:], axis=AX.X, op=ALU.add), r=[gt], w=[gsum])
                    OP("dve", lambda e: e.reciprocal(out=gsum.t[:], in_=gsum.t[:]), r=[gsum], w=[gsum])
                    OP("dve", lambda e: e.tensor_tensor(out=gt.t[:], in0=gt.t[:], in1=gsum.t[:].unsqueeze(2).broadcast_to([128, 8, 16]),
                                                        op=ALU.mult), r=[gt, gsum], w=[gt])
                    if KSTOP < 5: return
                    yield
                    OP("dve", lambda e: e.tensor_copy(out=posf.t[:], in_=pos.t[:]), r=[pos], w=[posf])
                    oh2 = Buf(None); oh2.r = ssb.r
                    oh2.t = ssb.t[:].rearrange("p a b -> p (a b)").rearrange("p (h j c) -> p h j c", h=8, j=16)
                    ix4 = ixf.t[:].rearrange("p (h a) k -> p h a k", a=2)
                    bc4 = lambda ap2: ap2.unsqueeze(1).unsqueeze(1).broadcast_to([128, 8, 16, 16])
                    pf4 = posf.t[:].unsqueeze(3).broadcast_to([128, 8, 16, 16])
                    OP("dve", lambda e: e.tensor_tensor(out=oh.t[:], in0=pf4, in1=bc4(lohi.t[:, 0, :]), op=ALU.is_ge), r=[posf, lohi], w=[oh])
                    OP("dve", lambda e: e.tensor_tensor(out=oh2.t[:], in0=pf4, in1=bc4(lohi.t[:, 1, :]), op=ALU.is_ge), r=[posf, lohi], w=[oh2])
                    OP("dve", lambda e: e.tensor_tensor(out=oh.t[:], in0=oh.t[:], in1=oh2.t[:], op=ALU.subtract), r=[oh, oh2], w=[oh])
                    OP("dve", lambda e: e.tensor_tensor(out=oh2.t[:], in0=oh.t[:], in1=bc4(lohi.t[:, 0, :]), op=ALU.mult), r=[oh, lohi], w=[oh2])
                    yield
                    OP("dve", lambda e: e.tensor_reduce(out=paf.t[:], in_=oh2.t[:], axis=AX.X, op=ALU.add), r=[oh2], w=[paf])
                    OP("dve", lambda e: e.tensor_tensor(out=oh.t[:], in0=oh.t[:],
                                                        in1=ix4[:, :, 0, :].unsqueeze(2).broadcast_to([128, 8, 16, 16]), op=ALU.mult),
                       r=[oh, ixf], w=[oh])
                    OP("dve", lambda e: e.tensor_reduce(out=i1s.t[:], in_=oh.t[:], axis=AX.X, op=ALU.add), r=[oh], w=[i1s])
                    yield
                    OP("dve", lambda e: e.tensor_tensor(out=pbf.t[:], in0=posf.t[:], in1=paf.t[:], op=ALU.subtract), r=[posf, paf], w=[pbf])
                    OP("dve", lambda e: e.tensor_tensor(out=oh.t[:], in0=bc4(iota.t[:]),
                                                        in1=pbf.t[:].unsqueeze(3).broadcast_to([128, 8, 16, 16]), op=ALU.is_equal),
                       r=[iota, pbf], w=[oh])
                    OP("dve", lambda e: e.tensor_tensor(out=oh.t[:], in0=oh.t[:],
                                                        in1=ix4[:, :, 1, :].unsqueeze(2).broadcast_to([128, 8, 16, 16]), op=ALU.mult),
                       r=[oh, ixf], w=[oh])
                    OP("dve", lambda e: e.tensor_reduce(out=i2s.t[:], in_=oh.t[:], axis=AX.X, op=ALU.add), r=[oh], w=[i2s])
                    OP("dve", lambda e: e.scalar_tensor_tensor(out=eidf.t[:].rearrange("p (h k) -> p h k", h=8), in0=i1s.t[:], scalar=128.0,
                                                               in1=i2s.t[:], op0=ALU.mult, op1=ALU.add), r=[i1s, i2s], w=[eidf])
                    OP("dve", lambda e: e.tensor_copy(out=eidx[i % 3].t[:], in_=eidf.t[:]), r=[eidf], w=[eidx[i % 3]])

                gi = [0]

                class V:
                    def __init__(self, t):
                        self.t = t
                        self.r = Res()
                dcol = [[V(dots[p_].t) for _ in range(128)] for p_ in range(2)]
                ccol = [[V(coef[p_].t) for _ in range(128)] for p_ in range(2)]

                gk = {}

                def slot_u(i, s):
                    k = gi[0] % NG
                    gi[0] += 1
                    gk[(i, s)] = k
                    S.dma("pool", gs[k], lambda e: e.indirect_dma_start(
                        out=gb[k].t[:], out_offset=None, in_=d_puv16,
                        in_offset=bass.IndirectOffsetOnAxis(ap=eidx[i % 3].t[:, s:s + 1], axis=0)), r=[eidx[i % 3]] + puvB, w=[gb[k]])
                    pr = prod[s % 3]
                    dc, cc = dcol[i % 2][s], ccol[i % 2][s]
                    OP("dve", lambda e: e.tensor_tensor(out=pr.t[:], in0=gb[k].t[:, 0:1024], in1=h2b[i % 2].t[:], op=ALU.mult),
                       r=[gb[k], h2b[i % 2]], w=[pr])
                    OP("act", lambda e: e.activation(out=pr.t[:], in_=pr.t[:], func=AF.Identity, accum_out=dc.t[:, s:s + 1]),
                       r=[pr], w=[pr, dc])
                    OP("act", lambda e: e.activation(out=cc.t[:, s:s + 1], in_=dc.t[:, s:s + 1], func=AF.Gelu), r=[dc], w=[cc])

                def slot_v(i, s):
                    k = gk.pop((i, s))
                    cc = ccol[i % 2][s]
                    dgk = dg[s % 4]
                    OP("dve", lambda e: e.tensor_scalar(out=dgk.t[:], in0=identb.t[:], scalar1=cc.t[:, s:s + 1],
                                                        scalar2=gate[i % 2].t[:, s // 16, s % 16:s % 16 + 1], op0=ALU.mult, op1=ALU.mult),
                       r=[identb, cc, gate[i % 2]], w=[dgk])
                    for hf in range(2):
                        OP("pe", lambda e, hf=hf: e.matmul(accP[hf].t[:], lhsT=dgk.t[:], rhs=gb[k].t[:, 1024 + hf * 512:1536 + hf * 512],
                                                           start=(s == 0), stop=(s == 127)), r=[dgk, gb[k]], w=[accP[hf]])

                def finish_v(i):
                    r2 = acc[i % 2]; yo = acc[i % 2]
                    for hf in range(2):
                        OP("dve", lambda e, hf=hf: e.tensor_tensor(out=r2.t[:, hf * 512:(hf + 1) * 512], in0=accP[hf].t[:],
                                                                   in1=g2bc.t[:, hf * 512:(hf + 1) * 512], op=ALU.mult),
                           r=[accP[hf], g2bc], w=[r2])
                    OP("dve", lambda e: e.scalar_tensor_tensor(out=r2.t[:], in0=x1t[i % 3].t[:], scalar=ALPHA, in1=r2.t[:],
                                                               op0=ALU.mult, op1=ALU.add), r=[x1t[i % 3], r2], w=[r2])
                    layernorm2(r2, yo)
                    S.dma("sp", sts, lambda e: e.dma_start(out=d_out[i * 128:(i + 1) * 128, :], in_=yo.t[:]), r=[yo])

                OP("dve", lambda e: e.tensor_scalar(out=lohi.t[:, 0, :], in0=iota.t[:], scalar1=16.0, scalar2=None, op0=ALU.mult), r=[iota], w=[lohi])
                OP("dve", lambda e: e.tensor_scalar(out=lohi.t[:, 1, :], in0=iota.t[:], scalar1=16.0, scalar2=16.0, op0=ALU.mult, op1=ALU.add),
                   r=[iota], w=[lohi])
                NT = int(os.environ.get('K_NT', '16'))
                KSTOP = float(os.environ.get('K_STOP', '9'))
                for _ in route(0):
                    pass
                for i in range(NT):
                    gen = route(i + 1) if i + 1 < NT else iter(())
                    for s in range(128 + LAG if KSTOP >= 6 else 0):
                        if s < 128:
                            slot_u(i, s)
                        if s >= LAG:
                            slot_v(i, s - LAG)
                        next(gen, None)
                    for _ in gen:
                        pass
                    if KSTOP >= 7:
                        finish_v(i)
                S.barrier()
        S.barrier()
        S.run()
    return nc


def _consts():
    s = np.arange(128)[:, None]
    t = np.arange(128)[None, :]
    same = (s // 64) == (t // 64)
    g = -1.0 / 16.0
    tri = np.stack([(same & (s <= t)), (same & (s > t)), (same & (s >= t)), (same & (s < t))], axis=1).astype(np.float32) * g
    ci = np.stack([(np.arange(128) // 64 == c) for c in range(2)], axis=1).astype(np.float32) * g
    amask = np.stack([(s >= t), (s <= t)], axis=1).astype(np.float32)
    sc = (np.arange(128) % 64)[:, None]
    cc = np.arange(64)[None, :]
    gmask = np.stack([(sc <= cc), (sc >= cc)], axis=1).astype(np.float32)
    iota = np.broadcast_to(np.arange(16, dtype=np.float32), (128, 16)).copy()
    return dict(ident=np.eye(128, dtype=np.float32), tri=np.ascontiguousarray(tri), ci=np.ascontiguousarray(ci),
                amask=np.ascontiguousarray(amask), gmask=np.ascontiguousarray(gmask), iota16=iota)


def _rope_tables():
    rows = 8192 // 64
    row = np.repeat(np.arange(rows, dtype=np.float32), 64)
    col = np.tile(np.arange(64, dtype=np.float32), rows)
    inv = (np.float32(10000.0) ** (-np.arange(16, dtype=np.float32) / np.float32(16))).astype(np.float32)
    ang = np.stack([row[:, None] * inv, col[:, None] * inv], axis=1).astype(np.float32)
    return np.cos(ang).reshape(8192, 32).astype(np.float32), np.sin(ang).reshape(8192, 32).astype(np.float32)


def make_in_maps(x, c, ctx, c_ctx, w_ada, b_ada, w_in, w_gate2_f, b_gate_f, w_gate2_b, b_gate_b, gla_norm_g, attn_sink,
                 w_out, ln1_g, ln1_b, peer_wq, peer_subkeys, peer_u, peer_v, ln2_g, ln2_b):
    f = lambda a: np.ascontiguousarray(np.asarray(a, dtype=np.float32))
    bc = lambda v, n: np.ascontiguousarray(np.broadcast_to(np.asarray(v, np.float32).reshape(1, -1), (128, n)))
    x = f(x); ctx = f(ctx)
    wi = f(w_in[0])
    wperm = np.concatenate([wi[:, 0:256], wi[:, 256:512], wi[:, 512:1024], wi[:, 1024:1536], wi[:, 1568:2080],
                            wi[:, 2080:2208], wi[:, 2208:2336], wi[:, 1536:1552], wi[:, 1552:1568]], axis=1)
    cosT, sinT = _rope_tables()
    common = dict(
        w_ada=f(w_ada[0]), b_ada=f(b_ada[0]).reshape(1, -1), w_in=np.ascontiguousarray(wperm),
        w2=np.ascontiguousarray(np.concatenate([f(w_gate2_f[0]), f(w_gate2_b[0])], axis=1)),
        bg=np.ascontiguousarray(np.concatenate([f(b_gate_f[0]), f(b_gate_b[0])]).reshape(1, -1)),
        gng=bc(gla_norm_g[0], 128), sink=bc(attn_sink[0], 8), w_out=f(w_out[0]),
        ln1g=bc(ln1_g[0], 1024), ln1b=bc(ln1_b[0], 1024), ln2g=bc(ln2_g[0], 1024), ln2b=bc(ln2_b[0], 1024),
        peer_wq=f(peer_wq[0]),
        skT=np.ascontiguousarray(np.transpose(f(peer_subkeys[0]), (1, 3, 0, 2)).reshape(128, 8, 128)),
        peer_uv=np.ascontiguousarray(np.concatenate([f(peer_u[0]), f(peer_v[0])], axis=1)), **_consts())
    maps = []
    zt = np.zeros((128, 1024), np.float32)
    for core in range(8):
        b, s = core // 4, core % 4
        xb = x[b].reshape(64, 128, 1024)
        npf = 16 * s
        xpf = np.zeros((NPRE, 128, 1024), np.float32)
        if npf:
            xpf[NPRE - npf:] = xb[0:npf]
        npb = 16 * (3 - s)
        xpb = np.zeros((NPRE, 128, 1024), np.float32)
        if npb:
            xpb[NPRE - npb:] = xb[63:16 * (s + 1) - 1:-1]
        flags = np.zeros((128, 100), np.float32)
        flags[:, NPRE - npf:NPRE] = 1.0
        flags[:, 48 + NPRE - npb:48 + NPRE] = 1.0
        t0 = 16 * s
        own = np.zeros((18, 128, 1024), np.float32)
        own[1:17] = xb[t0:t0 + 16]
        cos_o = np.zeros((18, 128, 32), np.float32); sin_o = np.zeros((18, 128, 32), np.float32)
        cos_o[1:17] = cosT.reshape(64, 128, 32)[t0:t0 + 16]; sin_o[1:17] = sinT.reshape(64, 128, 32)[t0:t0 + 16]
        if t0 > 0:
            own[0] = xb[t0 - 1]; flags[:, 96] = 1.0
            cos_o[0] = cosT.reshape(64, 128, 32)[t0 - 1]; sin_o[0] = sinT.reshape(64, 128, 32)[t0 - 1]
        if t0 + 16 < 64:
            own[17] = xb[t0 + 16]; flags[:, 97] = 1.0
            cos_o[17] = cosT.reshape(64, 128, 32)[t0 + 16]; sin_o[17] = sinT.reshape(64, 128, 32)[t0 + 16]
        c2 = np.stack([f(c)[b], f(c_ctx)], axis=0)
        c2T = np.ascontiguousarray(c2.reshape(2, 8, 128).transpose(2, 1, 0))
        m = dict(common)
        m.update(xpre_f=xpf.reshape(-1, 1024), xpre_b=xpb.reshape(-1, 1024), xown=own.reshape(-1, 1024), ctx=ctx[b],
                 flags=flags, c2T=c2T, cosT=cos_o.reshape(-1, 32), sinT=sin_o.reshape(-1, 32))
        maps.append(m)
    return maps


_NC_CACHE = {}


def kernel(**inputs):
    if "full" not in _NC_CACHE:
        _NC_CACHE["full"] = build_nc("full")
    nc = _NC_CACHE["full"]
    maps = make_in_maps(**inputs)
    res = run_bass_kernel_spmd(nc, maps, core_ids=list(range(8)))
    out = np.zeros((2, 8192, 1024), np.float32)
    for core in range(8):
        b, s = core // 4, core % 4
        out[b, s * 2048:(s + 1) * 2048] = np.asarray(res.results[core]["out"], np.float32)
    return out
```

```python
import os
import numpy as np
from contextlib import ExitStack
import concourse.bass as bass
import concourse.mybir as mybir
from concourse.bass_utils import run_bass_kernel_spmd

F32 = mybir.dt.float32
BF16 = mybir.dt.bfloat16
U32 = mybir.dt.uint32
AF = mybir.ActivationFunctionType
ALU = mybir.AluOpType
AX = mybir.AxisListType

SAME_ENGINE_SYNC = True
NPRE = 48
LN_EPS = 1e-5
ALPHA = 2.0 ** 0.25
NG = 12
LAG = 3


class Res:
    __slots__ = ("w", "rs")

    def __init__(self):
        self.w = None
        self.rs = {}


class Buf:
    def __init__(self, t):
        self.t = t
        self.r = Res()


class Sched:
    def __init__(self, nc, stack):
        self.nc = nc
        self.stack = stack
        self.sems = {}
        self.count = {}
        self.seen = {k: {} for k in ("pe", "dve", "act", "pool", "sp")}
        self.streams = {k: [] for k in ("pe", "dve", "act", "pool", "sp")}
        for k in ("pe", "dve", "act", "pool"):
            self.sems[k] = stack.enter_context(nc.semaphore("sem_" + k))
            self.count[k] = 0
        self.nslots = 0

    def dma_slot(self, name=""):
        self.nslots += 1
        key = "d%d%s" % (self.nslots, name)
        self.sems[key] = self.stack.enter_context(self.nc.semaphore("s_" + key))
        self.count[key] = 0
        return key

    def _waits(self, q, reads, writes, same_ok):
        deps = {}
        for b in reads:
            r = b.r
            if r.w is not None:
                k, c = r.w
                deps[k] = max(deps.get(k, 0), c)
        for b in writes:
            w = b.r
            if w.w is not None:
                k, c = w.w
                deps[k] = max(deps.get(k, 0), c)
            for k, c in w.rs.items():
                deps[k] = max(deps.get(k, 0), c)
        out = []
        for k, c in deps.items():
            if k == q and not same_ok:
                continue
            if self.seen[q].get(k, 0) >= c:
                continue
            self.seen[q][k] = c
            out.append((k, c))
        return out

    def op(self, q, fn, r=(), w=()):
        same_ok = SAME_ENGINE_SYNC and q != "pe"
        waits = self._waits(q, r, w, same_ok)
        self.count[q] += 1
        c = self.count[q]
        sems = self.sems
        st = self.streams[q]
        for k, v in waits:
            st.append(lambda e, k=k, v=v: e.wait_ge(sems[k], v))
        st.append(lambda e, fn=fn: fn(e).then_inc(sems[q], 1))
        for b in r:
            b.r.rs[q] = c
        for b in w:
            b.r.w = (q, c)
            b.r.rs = {}

    def dma(self, q, slot, fn, r=(), w=()):
        waits = self._waits(q, r, w, True)
        prev = self.count[slot]
        if prev > 0 and self.seen[q].get(slot, 0) < prev:
            self.seen[q][slot] = prev
            waits.append((slot, prev))
        self.count[slot] += 16
        c = self.count[slot]
        sems = self.sems
        st = self.streams[q]
        for k, v in waits:
            st.append(lambda e, k=k, v=v: e.wait_ge(sems[k], v))
        st.append(lambda e, fn=fn: fn(e).then_inc(sems[slot], 16))
        for b in r:
            b.r.rs[slot] = c
        for b in w:
            b.r.w = (slot, c)
            b.r.rs = {}

    def wait_all(self, q):
        sems = self.sems
        for k in list(self.count.keys()):
            c = self.count[k]
            if c == 0 or k == q or self.seen[q].get(k, 0) >= c:
                continue
            self.seen[q][k] = c
            self.streams[q].append(lambda e, k=k, c=c: e.wait_ge(sems[k], c))

    def barrier(self):
        for q in ("pe", "dve", "act", "pool", "sp"):
            self.wait_all(q)

    def run(self):
        streams = self.streams
        with self.nc.Block() as block:
            @block.tensor
            def _(e):
                for f in streams["pe"]:
                    f(e)

            @block.vector
            def _(e):
                for f in streams["dve"]:
                    f(e)

            @block.scalar
            def _(e):
                for f in streams["act"]:
                    f(e)

            @block.gpsimd
            def _(e):
                for f in streams["pool"]:
                    f(e)

            @block.sync
            def _(e):
                for f in streams["sp"]:
                    f(e)


def build_nc(mode="full"):
    nc = bass.Bass("TRN2", target_bir_lowering=False)
    D = lambda name, shape, dt=F32, kind="ExternalInput": nc.dram_tensor(name, shape, dt, kind=kind).ap()
    d_xpf = D("xpre_f", [NPRE * 128, 1024]); d_xpb = D("xpre_b", [NPRE * 128, 1024])
    d_xown = D("xown", [18 * 128, 1024]); d_ctx = D("ctx", [256, 1024])
    d_flags = D("flags", [128, 100]); d_c2T = D("c2T", [128, 8, 2])
    d_wada = D("w_ada", [1024, 6144]); d_bada = D("b_ada", [1, 6144])
    d_win = D("w_in", [1024, 2336]); d_w2 = D("w2", [16, 512]); d_bg = D("bg", [1, 512])
    d_gng = D("gng", [128, 128]); d_sink = D("sink", [128, 8])
    d_wout = D("w_out", [1024, 1024])
    d_ln1g = D("ln1g", [128, 1024]); d_ln1b = D("ln1b", [128, 1024])
    d_ln2g = D("ln2g", [128, 1024]); d_ln2b = D("ln2b", [128, 1024])
    d_wq = D("peer_wq", [1024, 1024]); d_skT = D("skT", [128, 8, 128])
    d_puv = D("peer_uv", [16384, 2048])
    d_puv16 = D("puv16", [16384, 2048], BF16, kind="Internal")
    d_cos = D("cosT", [18 * 128, 32]); d_sin = D("sinT", [18 * 128, 32])
    d_ident = D("ident", [128, 128]); d_tri = D("tri", [128, 4, 128]); d_ci = D("ci", [128, 2])
    d_amask = D("amask", [128, 2, 128]); d_gmask = D("gmask", [128, 2, 64]); d_iota = D("iota16", [128, 16])
    d_out = D("out", [2048, 1024], kind="ExternalOutput")
    if mode == "B":
        d_x1 = D("x1s", [2048, 1024])
    else:
        d_x1 = D("x1s", [2048, 1024], kind="ExternalOutput" if mode == "A" else "Internal")

    with ExitStack() as top:
        S = Sched(nc, top)
        OP = S.op

        def sbuf(st, name, shape, dt=F32):
            return Buf(st.enter_context(nc.sbuf_tensor("s_" + name, shape, dt)))

        banks = [Buf(top.enter_context(nc.psum_tensor("pb%d" % i, [128, 512], F32))) for i in range(6)]
        accP = [Buf(top.enter_context(nc.psum_tensor("pacc%d" % i, [128, 512], F32))) for i in range(2)]
        bank_i = [0]

        def bank():
            b = banks[bank_i[0] % 6]
            bank_i[0] += 1
            return b

        ld = [S.dma_slot("ld%d" % i) for i in range(2)]
        cst = S.dma_slot("cst")
        sts = S.dma_slot("st")
        csl = S.dma_slot("cs"); snl = S.dma_slot("sn")

        puvB = [Buf(None) for _ in range(16)]
        cvs = [S.dma_slot("cv%d" % i) for i in range(4)]

        def convert_tables():
            for ci_ in range(16):
                S.dma("pool", cvs[ci_ % 4], lambda e, ci_=ci_: e.dma_start(out=d_puv16[ci_ * 1024:(ci_ + 1) * 1024, :],
                                                                         in_=d_puv[ci_ * 1024:(ci_ + 1) * 1024, :]), w=[puvB[ci_]])
        ident = sbuf(top, "ident", [128, 128])
        S.dma("sp", cst, lambda e: e.dma_start(out=ident.t[:], in_=d_ident), w=[ident])
        flags = sbuf(top, "flags", [128, 100])
        S.dma("sp", cst, lambda e: e.dma_start(out=flags.t[:], in_=d_flags), w=[flags])
        eps_t = sbuf(top, "eps_t", [128, 1])
        OP("dve", lambda e: e.memset(eps_t.t[:], LN_EPS), w=[eps_t])
        modT = sbuf(top, "modT", [128, 48, 2])
        sc1p = sbuf(top, "sc1p", [128, 8, 2])
        g1bc = sbuf(top, "g1bc", [128, 1024])
        d_modbc = D("modbc", [3, 128, 1024], kind="Internal")
        xt_i = [0]

        def transpose_to(src, nchunks, dst_fn, width=128, rows=128):
            for c0 in range(0, nchunks, 4):
                pb = bank()
                n = min(4, nchunks - c0)
                for c in range(c0, c0 + n):
                    OP("pe", lambda e, c=c, pb=pb, c0=c0: e.transpose(
                        out=pb.t[0:width, (c - c0) * 128:(c - c0) * 128 + rows],
                        in_=src.t[0:rows, c * width:(c + 1) * width], identity=ident.t[0:rows, 0:rows]),
                       r=[src, ident], w=[pb])
                dst_fn(c0, n, pb)

        if mode != "B":
            with ExitStack() as p0:
                c2T = sbuf(p0, "c2T", [128, 8, 2]); sc2 = sbuf(p0, "sc2", [128, 8, 2])
                S.dma("sp", cst, lambda e: e.dma_start(out=c2T.t[:], in_=d_c2T), w=[c2T])
                OP("act", lambda e: e.activation(out=sc2.t[:], in_=c2T.t[:], func=AF.Silu), r=[c2T], w=[sc2])
                modrow = sbuf(p0, "modrow", [2, 6144])
                brow = sbuf(p0, "brow", [128, 6144]); ones2 = sbuf(p0, "ones2", [128, 2])
                OP("pool", lambda e: e.memset(brow.t[:], 0.0), w=[brow])
                S.dma("sp", cst, lambda e: e.dma_start(out=brow.t[0:1, :], in_=d_bada), w=[brow])
                OP("dve", lambda e: e.memset(ones2.t[:], 0.0), w=[ones2])
                OP("dve", lambda e: e.memset(ones2.t[0:1, :], 1.0), w=[ones2])
                S.barrier()
                wst = [sbuf(p0, "wst%d" % i, [128, 3072]) for i in range(2)]
                wada_v = d_wada.rearrange("(k p) n -> k p n", p=128)
                for half in range(2):
                    pbs = [bank() for _ in range(6)]
                    for j in range(6):
                        col = half * 3072 + j * 512
                        OP("pe", lambda e, pbj=pbs[j], col=col: e.matmul(pbj.t[0:2, :], lhsT=ones2.t[:], rhs=brow.t[:, col:col + 512],
                                                                        start=True, stop=False), r=[ones2, brow], w=[pbs[j]])
                    for kc in range(8):
                        ws = wst[kc % 2]
                        S.dma("sp", ld[kc % 2], lambda e, ws=ws, kc=kc, half=half: e.dma_start(
                            out=ws.t[:], in_=wada_v[kc, :, half * 3072:(half + 1) * 3072]), w=[ws])
                        for j in range(6):
                            OP("pe", lambda e, j=j, ws=ws, kc=kc, pbj=pbs[j]: e.matmul(
                                pbj.t[0:2, :], lhsT=sc2.t[:, kc, :], rhs=ws.t[:, j * 512:(j + 1) * 512],
                                start=False, stop=(kc == 7)), r=[sc2, ws], w=[pbs[j]])
                    for j in range(6):
                        col = half * 3072 + j * 512
                        OP("act", lambda e, pbj=pbs[j], col=col: e.activation(out=modrow.t[:, col:col + 512], in_=pbj.t[0:2, :],
                                                                       func=AF.Identity), r=[pbs[j]], w=[modrow])
                for c0 in range(0, 48, 4):
                    pb = bank()
                    for c in range(c0, c0 + 4):
                        OP("pe", lambda e, c=c, pb=pb, c0=c0: e.transpose(
                            out=pb.t[:, (c - c0) * 2:(c - c0) * 2 + 2], in_=modrow.t[0:2, c * 128:(c + 1) * 128],
                            identity=ident.t[0:2, 0:2]), r=[modrow, ident], w=[pb])
                    OP("dve", lambda e, pb=pb, c0=c0: e.tensor_copy(
                        out=modT.t[:, c0:c0 + 4, :], in_=pb.t[:, 0:8].rearrange("p (a b) -> p a b", a=4)), r=[pb], w=[modT])
                OP("dve", lambda e: e.tensor_scalar(out=sc1p.t[:], in0=modT.t[:, 8:16, :], scalar1=1.0, scalar2=None, op0=ALU.add),
                   r=[modT], w=[sc1p])
                sel = sbuf(p0, "sel", [2, 128])
                OP("dve", lambda e: e.memset(sel.t[:], 0.0), w=[sel])
                OP("dve", lambda e: e.memset(sel.t[0:1, :], 1.0), w=[sel])
                bct = sbuf(p0, "bct", [128, 1024])
                for dst, j, add1, di in ((g1bc, 2, False, None), (bct, 3, False, 0), (bct, 4, True, 1), (bct, 5, False, 2)):
                    for hf in range(2):
                        pb = bank()
                        col = j * 1024 + hf * 512
                        OP("pe", lambda e, pb=pb, col=col: e.matmul(pb.t[:], lhsT=sel.t[:], rhs=modrow.t[:, col:col + 512],
                                                                    start=True, stop=True), r=[sel, modrow], w=[pb])
                        if add1:
                            OP("dve", lambda e, pb=pb, dst=dst, hf=hf: e.tensor_scalar(
                                out=dst.t[:, hf * 512:(hf + 1) * 512], in0=pb.t[:], scalar1=1.0, scalar2=None, op0=ALU.add),
                               r=[pb], w=[dst])
                        else:
                            OP("act", lambda e, pb=pb, dst=dst, hf=hf: e.activation(
                                out=dst.t[:, hf * 512:(hf + 1) * 512], in_=pb.t[:], func=AF.Identity), r=[pb], w=[dst])
                    if di is not None:
                        S.dma("sp", sts, lambda e, di=di: e.dma_start(out=d_modbc[di], in_=bct.t[:]), r=[bct])
                S.barrier()

        if mode != "B":
            with ExitStack() as pa:
                xt = [sbuf(pa, "xt%d" % i, [128, 1024]) for i in range(2)]
                win = sbuf(pa, "win", [128, 8, 2336], BF16)
                wout = sbuf(pa, "wout", [128, 8, 1024], BF16)
                with ExitStack() as pw:
                    wst = [sbuf(pw, "wcst%d" % i, [128, 2336]) for i in range(2)]
                    win_v = d_win.rearrange("(k p) n -> k p n", p=128)
                    wout_v = d_wout.rearrange("(k p) n -> k p n", p=128)
                    for kc in range(8):
                        ws = wst[kc % 2]
                        S.dma("sp", ld[kc % 2], lambda e, ws=ws, kc=kc: e.dma_start(out=ws.t[:], in_=win_v[kc]), w=[ws])
                        OP("pool", lambda e, ws=ws, kc=kc: e.tensor_copy(out=win.t[:, kc, :], in_=ws.t[:]), r=[ws], w=[win])
                    for kc in range(8):
                        ws = wst[kc % 2]
                        S.dma("sp", ld[kc % 2], lambda e, ws=ws, kc=kc: e.dma_start(out=ws.t[:, 0:1024], in_=wout_v[kc]), w=[ws])
                        OP("pool", lambda e, ws=ws, kc=kc: e.tensor_copy(out=wout.t[:, kc, :], in_=ws.t[:, 0:1024]), r=[ws], w=[wout])
                    S.barrier()
                w2 = sbuf(pa, "w2", [128, 512])
                OP("pool", lambda e: e.memset(w2.t[:], 0.0), w=[w2])
                gng = sbuf(pa, "gng", [128, 128]); esink = sbuf(pa, "esink", [128, 8])
                ln1g = sbuf(pa, "ln1g", [128, 1024]); ln1b = sbuf(pa, "ln1b", [128, 1024])
                tri = sbuf(pa, "tri", [128, 4, 128]); ci = sbuf(pa, "ci", [128, 2])
                amask = sbuf(pa, "amask", [128, 2, 128]); gmask = sbuf(pa, "gmask", [128, 2, 64])
                amask_e = sbuf(pa, "amask_e", [128, 2, 128])
                S.dma("sp", cst, lambda e: e.dma_start(out=w2.t[0:16, :], in_=d_w2), w=[w2])
                S.dma("sp", cst, lambda e: e.dma_start(out=w2.t[16:17, :], in_=d_bg), w=[w2])
                for b_, d_ in ((gng, d_gng), (esink, d_sink), (ln1g, d_ln1g), (ln1b, d_ln1b),
                               (tri, d_tri), (ci, d_ci), (amask, d_amask), (gmask, d_gmask)):
                    S.dma("sp", cst, lambda e, b_=b_, d_=d_: e.dma_start(out=b_.t[:], in_=d_), w=[b_])
                S.barrier()
                OP("act", lambda e: e.activation(out=esink.t[:], in_=esink.t[:], func=AF.Exp), r=[esink], w=[esink])
                for m in range(2):
                    OP("dve", lambda e, m=m: e.tensor_scalar(out=amask_e.t[:, m, :], in0=amask.t[:, m, :],
                                                             scalar1=flags.t[:, 96 + m:97 + m], scalar2=None, op0=ALU.mult),
                       r=[amask, flags], w=[amask_e])
                sbst = sbuf(pa, "sbst", [128, 32, 256])
                kT_all = sbuf(pa, "kT_all", [64, 18, 256], BF16); v_all = sbuf(pa, "v_all", [128, 18, 130], BF16)
                kTc = sbuf(pa, "kTc", [64, 2, 256], BF16); vc = sbuf(pa, "vc", [128, 2, 130], BF16)
                OP("pool", lambda e: e.memset(v_all.t[:], 1.0), w=[v_all])
                OP("pool", lambda e: e.memset(vc.t[:], 1.0), w=[vc])
                hT2 = [sbuf(pa, "hT%d" % i, [128, 8, 128], BF16) for i in range(2)]
                for h_ in hT2:
                    h_.c = []
                    for _ in range(8):
                        v_ = Buf(h_.t)
                        h_.c.append(v_)
                vsb2 = [sbuf(pa, "vsb%d" % i, [128, 512]) for i in range(2)]
                curb = {"hT": hT2[0], "vsb": vsb2[0]}
                qk = sbuf(pa, "qk", [128, 512])
                zT = [sbuf(pa, "zT", [128, 128])] * 2
                OP("dve", lambda e: e.memset(zT[0].t[:], 0.0), w=[zT[0]])
                OP("dve", lambda e: e.memset(zT[0].t[0:17, :], 1.0), w=[zT[0]])
                sp_ = [sbuf(pa, "sp", [128, 256])] * 2
                et = sbuf(pa, "et", [128, 256])
                Eb = [sbuf(pa, "Eb", [128, 256])] * 2
                Ei = [sbuf(pa, "Ei", [128, 256])] * 2
                Er = [sbuf(pa, "Er", [128, 256])] * 2
                qe = [sbuf(pa, "qe", [128, 256])] * 2
                ke = [sbuf(pa, "ke", [128, 256])] * 2
                kd = [sbuf(pa, "kd", [128, 256])] * 2
                Tz = [sbuf(pa, "Tz%d" % i, [128, 4, 128]) for i in range(2)]
                keT = [sbuf(pa, "keT%d" % i, [128, 2, 128]) for i in range(2)]
                ATz = sbuf(pa, "ATz", [128, 2, 4, 2, 64])
                for b_ in (Tz[0], Tz[1], ATz):
                    OP("pool", lambda e, b_=b_: e.memset(b_.t[:], 0.0), w=[b_])
                asb = [sbuf(pa, "asb", [128, 2, 2])] * 2
                Sst = {0: [sbuf(pa, "Sf%d" % i, [128, 2, 128]) for i in range(2)],
                       1: [sbuf(pa, "Sb%d" % i, [128, 2, 128]) for i in range(2)]}
                Scur = {0: 0, 1: 0}
                Stmp = sbuf(pa, "Stmp", [128, 2, 128])
                for dd in range(2):
                    OP("dve", lambda e, dd=dd: e.memset(Sst[dd][0].t[:], 0.0), w=[Sst[dd][0]])
                rsb = sbuf(pa, "rsb", [128, 512])
                ss = sbuf(pa, "ss", [128, 4]); rstd4 = sbuf(pa, "rstd4", [128, 4])
                on = sbuf(pa, "on", [128, 512]); junk = on
                aqs = on; qrot = sbuf(pa, "qrot", [128, 512]); rt = rsb
                akv = sbuf(pa, "akv", [128, 256]); krot = sbuf(pa, "krot", [128, 128])
                cs = sbuf(pa, "cs", [128, 32]); sn = sbuf(pa, "sn", [128, 32])
                qTs = sbuf(pa, "qTs", [64, 8, 128], BF16)
                Pall = sbuf(pa, "Pall", [128, 5, 512], BF16)
                den = sbuf(pa, "den", [128, 4]); cat = sbuf(pa, "cat", [128, 1024])
                r1 = cat; x1o = cat
                stats = sbuf(pa, "stats", [128, 12]); mv = sbuf(pa, "mv", [128, 2]); rs1 = sbuf(pa, "rs1", [128, 1])

                def front(src_ap, row):
                    xb = xt[xt_i[0] % 2]
                    slot = ld[xt_i[0] % 2]
                    curb["hT"] = hT2[xt_i[0] % 2]; curb["vsb"] = vsb2[xt_i[0] % 2]
                    hT = curb["hT"]
                    xt_i[0] += 1
                    S.dma("sp", slot, lambda e: e.dma_start(out=xb.t[:], in_=src_ap), w=[xb])

                    def evac(c0, n, pb):
                        for c in range(c0, c0 + n):
                            if c0 == 0:
                                OP("act", lambda e, c=c, pb=pb, c0=c0: e.activation(
                                    out=hT.t[:, c, :], in_=pb.t[:, (c - c0) * 128:(c - c0 + 1) * 128], func=AF.Identity,
                                    scale=sc1p.t[:, c, row:row + 1], bias=modT.t[:, c, row:row + 1]),
                                   r=[pb, sc1p, modT], w=[hT.c[c]])
                            else:
                                OP("dve", lambda e, c=c, pb=pb, c0=c0: e.tensor_scalar(
                                    out=hT.t[:, c, :], in0=pb.t[:, (c - c0) * 128:(c - c0 + 1) * 128],
                                    scalar1=sc1p.t[:, c, row:row + 1], scalar2=modT.t[:, c, row:row + 1], op0=ALU.mult, op1=ALU.add),
                                   r=[pb, sc1p, modT], w=[hT.c[c]])
                    transpose_to(xb, 8, evac)
                    return xb

                def inproj(col0, ncols, pb, pcol=0):
                    hT = curb["hT"]
                    for kc in range(8):
                        OP("pe", lambda e, kc=kc: e.matmul(pb.t[:, pcol:pcol + ncols], lhsT=hT.t[:, kc, :],
                                                           rhs=win.t[:, kc, col0:col0 + ncols], start=(kc == 0), stop=(kc == 7)),
                           r=[hT.c[kc], win], w=[pb])

                def gates(dd, flagcol=None):
                    hT = curb["hT"]
                    pz = bank()
                    for kc in range(8):
                        OP("pe", lambda e, kc=kc: e.matmul(pz.t[0:16, 0:128], lhsT=win.t[:, kc, 2304 + 16 * dd:2320 + 16 * dd],
                                                           rhs=hT.t[:, kc, :], start=(kc == 0), stop=(kc == 7)), r=[hT.c[kc], win], w=[pz])
                    OP("act", lambda e: e.activation(out=zT[dd].t[0:16, :], in_=pz.t[0:16, 0:128], func=AF.Identity), r=[pz], w=[zT[dd]])
                    pg = bank()
                    OP("pe", lambda e: e.matmul(pg.t[:, 0:256], lhsT=zT[dd].t[:], rhs=w2.t[:, dd * 256:(dd + 1) * 256],
                                                start=True, stop=True), r=[zT[dd], w2], w=[pg])
                    OP("act", lambda e: e.activation(out=et.t[:], in_=pg.t[:, 0:256], func=AF.Exp, scale=-1.0), r=[pg], w=[et])
                    OP("act", lambda e: e.activation(out=sp_[dd].t[:], in_=et.t[:], func=AF.Ln, bias=1.0), r=[et], w=[sp_[dd]])
                    if flagcol is not None:
                        OP("dve", lambda e: e.tensor_scalar(out=sp_[dd].t[:], in0=sp_[dd].t[:], scalar1=flags.t[:, flagcol:flagcol + 1],
                                                            scalar2=None, op0=ALU.mult), r=[sp_[dd], flags], w=[sp_[dd]])

                def decay_k(dd, ksrc, flagcol=None):
                    pr = bank()
                    OP("pe", lambda e: e.matmul(pr.t[:, 0:256], lhsT=tri.t[:, 1 + 2 * dd, :], rhs=sp_[dd].t[:], start=True, stop=True),
                       r=[tri, sp_[dd]], w=[pr])
                    OP("act", lambda e: e.activation(out=Er[dd].t[:], in_=pr.t[:, 0:256], func=AF.Exp), r=[pr], w=[Er[dd]])
                    if flagcol is None:
                        OP("dve", lambda e: e.tensor_tensor(out=kd[dd].t[:], in0=ksrc.t[:, 256:512], in1=Er[dd].t[:], op=ALU.mult),
                           r=[ksrc, Er[dd]], w=[kd[dd]])
                    else:
                        OP("dve", lambda e: e.scalar_tensor_tensor(out=kd[dd].t[:], in0=ksrc.t[:, 256:512],
                                                                   scalar=flags.t[:, flagcol:flagcol + 1], in1=Er[dd].t[:],
                                                                   op0=ALU.mult, op1=ALU.mult), r=[ksrc, Er[dd], flags], w=[kd[dd]])
                    pa_ = bank()
                    for hp in range(2):
                        OP("pe", lambda e, hp=hp: e.matmul(pa_.t[:, hp * 2:hp * 2 + 2], lhsT=sp_[dd].t[:, hp * 128:(hp + 1) * 128],
                                                           rhs=ci.t[:], start=True, stop=True), r=[sp_[dd], ci], w=[pa_])
                    OP("act", lambda e: e.activation(out=asb[dd].t[:], in_=pa_.t[:, 0:4].rearrange("p (a b) -> p a b", a=2),
                                                     func=AF.Exp), r=[pa_], w=[asb[dd]])

                def state_update(dd, c, vsrc):
                    pp = bank()
                    for h in range(4):
                        hp, par = h // 2, h % 2
                        OP("pe", lambda e, h=h, hp=hp, par=par: e.matmul(
                            pp.t[par * 64:(par + 1) * 64, hp * 128:(hp + 1) * 128],
                            lhsT=kd[dd].t[c * 64:(c + 1) * 64, h * 64:(h + 1) * 64],
                            rhs=vsrc.t[c * 64:(c + 1) * 64, h * 128:(h + 1) * 128], start=True, stop=True),
                           r=[kd[dd], vsrc], w=[pp])
                    so = Sst[dd][Scur[dd]]
                    sn_ = Sst[dd][1 - Scur[dd]]
                    OP("dve", lambda e: e.tensor_tensor(out=Stmp.t[:], in0=so.t[:],
                                                        in1=asb[dd].t[:, :, c:c + 1].broadcast_to([128, 2, 128]), op=ALU.mult),
                       r=[so, asb[dd]], w=[Stmp])
                    OP("dve", lambda e: e.tensor_tensor(out=sn_.t[:], in0=Stmp.t[:],
                                                        in1=pp.t[:, 0:256].rearrange("p (a b) -> p a b", a=2), op=ALU.add),
                       r=[Stmp, pp], w=[sn_])
                    Scur[dd] = 1 - Scur[dd]

                def state_tile(src_ap, row, dd, flagcol=None, store=None):
                    front(src_ap, row)
                    vsb = curb["vsb"]
                    pk = bank()
                    inproj(256, 256, pk, 256)
                    pv = bank()
                    inproj(512, 512, pv)
                    OP("act", lambda e: e.activation(out=vsb.t[:], in_=pv.t[:], func=AF.Identity), r=[pv], w=[vsb])
                    gates(dd, flagcol)
                    decay_k(dd, pk, flagcol)
                    for c in ((0, 1) if dd == 0 else (1, 0)):
                        if store is not None:
                            cur = Sst[dd][Scur[dd]]
                            OP("pool", lambda e, c=c, cur=cur: e.tensor_copy(out=sbst.t[:, store * 2 + c, :],
                                                                             in_=cur.t[:].rearrange("p a b -> p (a b)")),
                               r=[cur], w=[sbst])
                        state_update(dd, c, vsb)

                ksb2 = [sbuf(pa, "ksb%d" % i, [128, 512]) for i in range(2)]
                zT2 = [sbuf(pa, "zTp%d" % i, [128, 128]) for i in range(2)]
                for z_ in zT2:
                    OP("dve", lambda e, z_=z_: e.memset(z_.t[:], 0.0), w=[z_])
                    OP("dve", lambda e, z_=z_: e.memset(z_.t[0:17, :], 1.0), w=[z_])
                st_i = [0]

                def state_A1(n, src_ap, row):
                    front(src_ap, row)
                    hT = curb["hT"]
                    return dict(hT=hT, vsb=vsb2[n % 2], ksb=ksb2[n % 2], zTb=zT2[n % 2])

                def inproj_h(hT, col0, ncols, pb, pcol=0):
                    for kc in range(8):
                        OP("pe", lambda e, kc=kc: e.matmul(pb.t[:, pcol:pcol + ncols], lhsT=hT.t[:, kc, :],
                                                           rhs=win.t[:, kc, col0:col0 + ncols], start=(kc == 0), stop=(kc == 7)),
                           r=[hT.c[kc], win], w=[pb])

                def state_A2(cx):
                    pk = bank()
                    inproj_h(cx["hT"], 256, 256, pk, 256)
                    ksb = cx["ksb"]
                    OP("dve", lambda e: e.tensor_copy(out=ksb.t[:, 256:512], in_=pk.t[:, 256:512]), r=[pk], w=[ksb])

                def state_A3(cx):
                    pv = bank()
                    inproj_h(cx["hT"], 512, 512, pv)
                    vsb = cx["vsb"]
                    OP("act", lambda e: e.activation(out=vsb.t[:], in_=pv.t[:], func=AF.Identity), r=[pv], w=[vsb])

                def state_A4(cx, dd):
                    pz = bank()
                    hT, zTb = cx["hT"], cx["zTb"]
                    for kc in range(8):
                        OP("pe", lambda e, kc=kc: e.matmul(pz.t[0:16, 0:128], lhsT=win.t[:, kc, 2304 + 16 * dd:2320 + 16 * dd],
                                                           rhs=hT.t[:, kc, :], start=(kc == 0), stop=(kc == 7)), r=[hT.c[kc], win], w=[pz])
                    OP("act", lambda e: e.activation(out=zTb.t[0:16, :], in_=pz.t[0:16, 0:128], func=AF.Identity), r=[pz], w=[zTb])

                def state_B1(cx, dd, flagcol):
                    zTb = cx["zTb"]
                    pg = bank()
                    OP("pe", lambda e: e.matmul(pg.t[:, 0:256], lhsT=zTb.t[:], rhs=w2.t[:, dd * 256:(dd + 1) * 256],
                                                start=True, stop=True), r=[zTb, w2], w=[pg])
                    OP("act", lambda e: e.activation(out=et.t[:], in_=pg.t[:, 0:256], func=AF.Exp, scale=-1.0), r=[pg], w=[et])
                    OP("act", lambda e: e.activation(out=sp_[dd].t[:], in_=et.t[:], func=AF.Ln, bias=1.0), r=[et], w=[sp_[dd]])
                    if flagcol is not None:
                        OP("dve", lambda e: e.tensor_scalar(out=sp_[dd].t[:], in0=sp_[dd].t[:], scalar1=flags.t[:, flagcol:flagcol + 1],
                                                            scalar2=None, op0=ALU.mult), r=[sp_[dd], flags], w=[sp_[dd]])

                def state_B2(cx, dd, flagcol):
                    decay_k(dd, cx["ksb"], flagcol)

                def state_B3(cx, dd, store, c):
                    if store is not None:
                        cur = Sst[dd][Scur[dd]]
                        OP("pool", lambda e, c=c, cur=cur: e.tensor_copy(out=sbst.t[:, store * 2 + c, :],
                                                                         in_=cur.t[:].rearrange("p a b -> p (a b)")),
                           r=[cur], w=[sbst])
                    state_update(dd, c, cx["vsb"])

                def run_state_jobs(jobs):
                    N = len(jobs)
                    cxs = {}
                    for step in range(N + 2):
                        if step < N:
                            cxs[step] = state_A1(step, jobs[step][0], jobs[step][1])
                        m, b_ = step - 1, step - 2
                        hm = 0 <= m < N
                        hb = 0 <= b_ < N
                        ORD = int(os.environ.get("K_ORD", "1"))
                        if hb:
                            _, _, ddb, flb, stb = jobs[b_]
                            cs_ = (0, 1) if ddb == 0 else (1, 0)
                        if ORD == 0:
                            seq = ["A2", "A3", "A4", "B1", "B2", "B3", "B4"]
                        elif ORD == 1:
                            seq = ["B1", "A2", "B2", "A3", "B3", "B4", "A4"]
                        elif ORD == 2:
                            seq = ["B1", "A2", "B2", "A3", "A4", "B3", "B4"]
                        else:
                            seq = ["A2", "B1", "A3", "B2", "A4", "B3", "B4"]
                        for st_ in seq:
                            if st_[0] == "A" and hm:
                                if st_ == "A2": state_A2(cxs[m])
                                elif st_ == "A3": state_A3(cxs[m])
                                else: state_A4(cxs[m], jobs[m][2])
                            if st_[0] == "B" and hb:
                                if st_ == "B1": state_B1(cxs[b_], ddb, flb)
                                elif st_ == "B2": state_B2(cxs[b_], ddb, flb)
                                elif st_ == "B3": state_B3(cxs[b_], ddb, stb, cs_[0])
                                else: state_B3(cxs[b_], ddb, stb, cs_[1])
                        if hb:
                            del cxs[b_]

                def rope(src, dst, H, tmp):
                    v5 = lambda b: b.t[:, 0:H * 64].rearrange("p (h a f d) -> p h a f d", h=H, a=2, f=2, d=16)
                    cb = cs.t[:].rearrange("p (a d) -> p a d", a=2).unsqueeze(1).broadcast_to([128, H, 2, 16])
                    sb_ = sn.t[:].rearrange("p (a d) -> p a d", a=2).unsqueeze(1).broadcast_to([128, H, 2, 16])
                    x1, x2 = v5(src)[:, :, :, 0, :], v5(src)[:, :, :, 1, :]
                    o1, o2 = v5(dst)[:, :, :, 0, :], v5(dst)[:, :, :, 1, :]
                    t1, t2 = v5(tmp)[:, :, :, 0, :], v5(tmp)[:, :, :, 1, :]
                    OP("pool", lambda e: e.tensor_tensor(out=o1, in0=x1, in1=cb, op=ALU.mult), r=[src, cs], w=[dst])
                    OP("pool", lambda e: e.tensor_tensor(out=t1, in0=x2, in1=sb_, op=ALU.mult), r=[src, sn], w=[tmp])
                    OP("pool", lambda e: e.tensor_tensor(out=o1, in0=o1, in1=t1, op=ALU.subtract), r=[dst, tmp], w=[dst])
                    OP("pool", lambda e: e.tensor_tensor(out=o2, in0=x2, in1=cb, op=ALU.mult), r=[src, cs], w=[dst])
                    OP("pool", lambda e: e.tensor_tensor(out=t2, in0=x1, in1=sb_, op=ALU.mult), r=[src, sn], w=[tmp])
                    OP("pool", lambda e: e.tensor_tensor(out=o2, in0=o2, in1=t2, op=ALU.add), r=[dst, tmp], w=[dst])

                def kv_tile(src_ap, row, j, is_ctx):
                    front(src_ap, row)
                    pb = bank()
                    inproj(2048, 256, pb)
                    OP("act", lambda e: e.activation(out=akv.t[:], in_=pb.t[:, 0:256], func=AF.Identity), r=[pb], w=[akv])
                    if is_ctx:
                        ksrc, kdst, vdst = akv, kTc, vc
                    else:
                        S.dma("sp", csl, lambda e: e.dma_start(out=cs.t[:], in_=d_cos[j * 128:(j + 1) * 128, :]), w=[cs])
                        S.dma("sp", snl, lambda e: e.dma_start(out=sn.t[:], in_=d_sin[j * 128:(j + 1) * 128, :]), w=[sn])
                        rope(akv, krot, 2, rt)
                        ksrc, kdst, vdst = krot, kT_all, v_all
                    OP("pool", lambda e: e.tensor_copy(
                        out=vdst.t[:, j, :].rearrange("p (g d) -> p g d", g=2)[:, :, 0:64],
                        in_=akv.t[:, 128:256].rearrange("p (g d) -> p g d", g=2)), r=[akv], w=[vdst])

                    def evac(c0, n, pb2):
                        OP("act", lambda e: e.activation(out=kdst.t[:, j, :], in_=pb2.t[0:64, 0:256], func=AF.Identity),
                           r=[pb2], w=[kdst])
                    transpose_to(ksrc, 2, evac, width=64)

                def own_tile(i):
                    j = i + 1
                    xb = front(d_xown[j * 128:(j + 1) * 128, :], 0)
                    vsb = curb["vsb"]; catT = curb["hT"]
                    pqk = bank(); inproj(0, 512, pqk)
                    OP("act", lambda e: e.activation(out=qk.t[:], in_=pqk.t[:], func=AF.Identity), r=[pqk], w=[qk])
                    pv = bank(); inproj(512, 512, pv)
                    OP("act", lambda e: e.activation(out=vsb.t[:], in_=pv.t[:], func=AF.Identity), r=[pv], w=[vsb])
                    pr_ = bank(); inproj(1024, 512, pr_)
                    OP("act", lambda e: e.activation(out=rsb.t[:], in_=pr_.t[:], func=AF.Silu), r=[pr_], w=[rsb])
                    for dd in range(2):
                        gates(dd)
                        pbn = bank()
                        OP("pe", lambda e, dd=dd, pbn=pbn: e.matmul(pbn.t[:, 0:256], lhsT=tri.t[:, 2 * dd, :], rhs=sp_[dd].t[:],
                                                                    start=True, stop=True), r=[tri, sp_[dd]], w=[pbn])
                        OP("act", lambda e, dd=dd, pbn=pbn: e.activation(out=Eb[dd].t[:], in_=pbn.t[:, 0:256], func=AF.Exp),
                           r=[pbn], w=[Eb[dd]])
                        OP("act", lambda e, dd=dd, pbn=pbn: e.activation(out=Ei[dd].t[:], in_=pbn.t[:, 0:256], func=AF.Exp, scale=-1.0),
                           r=[pbn], w=[Ei[dd]])
                        OP("dve", lambda e, dd=dd: e.scalar_tensor_tensor(out=qe[dd].t[:], in0=qk.t[:, 0:256], scalar=0.125, in1=Eb[dd].t[:],
                                                                          op0=ALU.mult, op1=ALU.mult), r=[qk, Eb[dd]], w=[qe[dd]])
                        OP("dve", lambda e, dd=dd: e.tensor_tensor(out=ke[dd].t[:], in0=qk.t[:, 256:512], in1=Ei[dd].t[:], op=ALU.mult),
                           r=[qk, Ei[dd]], w=[ke[dd]])
                        if dd == 0:
                            decay_k(dd, qk)
                        pT = bank()
                        for idx, srcb in enumerate((qe[dd], qe[dd], ke[dd], ke[dd])):
                            hp = idx % 2
                            OP("pe", lambda e, idx=idx, hp=hp, srcb=srcb, pT=pT: e.transpose(
                                out=pT.t[:, idx * 128:(idx + 1) * 128], in_=srcb.t[:, hp * 128:(hp + 1) * 128], identity=ident.t[:]),
                               r=[srcb, ident], w=[pT])
                        OP("act", lambda e, dd=dd, pT=pT: e.activation(out=keT[dd].t[:].rearrange("p a b -> p (a b)"), in_=pT.t[:, 256:512],
                                                                       func=AF.Identity), r=[pT], w=[keT[dd]])
                        for par in range(2):
                            OP("act", lambda e, dd=dd, pT=pT, par=par: e.activation(
                                out=Tz[dd].t[par * 64:(par + 1) * 64, par::2, :],
                                in_=pT.t[par * 64:(par + 1) * 64, 0:256].rearrange("p (a b) -> p a b", a=2), func=AF.Identity),
                               r=[pT], w=[Tz[dd]])
                    pAT = bank()
                    for dd in range(2):
                        for c in range(2):
                            for h in range(4):
                                hp = h // 2
                                OP("pe", lambda e, dd=dd, c=c, h=h, hp=hp: e.matmul(
                                    pAT.t[c * 64:(c + 1) * 64, (dd * 4 + h) * 64:(dd * 4 + h + 1) * 64],
                                    lhsT=keT[dd].t[:, hp, c * 64:(c + 1) * 64],
                                    rhs=Tz[dd].t[:, h, c * 64:(c + 1) * 64], start=True, stop=True),
                                   r=[keT[dd], Tz[dd]], w=[pAT])
                    for c in range(2):
                        OP("dve", lambda e, c=c: e.tensor_tensor(
                            out=ATz.t[c * 64:(c + 1) * 64, :, :, c, :],
                            in0=pAT.t[c * 64:(c + 1) * 64, :].rearrange("p (d h c) -> p d h c", d=2, h=4),
                            in1=gmask.t[c * 64:(c + 1) * 64, :, :].unsqueeze(2).broadcast_to([64, 2, 4, 64]), op=ALU.mult),
                           r=[pAT, gmask], w=[ATz])
                    po = bank()
                    for c in range(2):
                        sf = Sst[0][Scur[0]]
                        for h in range(4):
                            hp = h // 2
                            outp = po.t[c * 64:(c + 1) * 64, h * 128:(h + 1) * 128]
                            vv = vsb.t[:, h * 128:(h + 1) * 128]
                            OP("pe", lambda e, c=c, h=h, outp=outp, vv=vv: e.matmul(
                                outp, lhsT=ATz.t[:, 0, h, c, :], rhs=vv, start=True, stop=False), r=[ATz, vsb], w=[po])
                            OP("pe", lambda e, c=c, h=h, hp=hp, outp=outp, sf=sf: e.matmul(
                                outp, lhsT=Tz[0].t[:, h, c * 64:(c + 1) * 64], rhs=sf.t[:, hp, :], start=False, stop=False),
                               r=[Tz[0], sf], w=[po])
                            OP("pe", lambda e, c=c, h=h, outp=outp, vv=vv: e.matmul(
                                outp, lhsT=ATz.t[:, 1, h, c, :], rhs=vv, start=False, stop=False), r=[ATz, vsb], w=[po])
                            OP("pe", lambda e, c=c, h=h, hp=hp, outp=outp: e.matmul(
                                outp, lhsT=Tz[1].t[:, h, c * 64:(c + 1) * 64],
                                rhs=sbst.t[:, i * 2 + c, hp * 128:(hp + 1) * 128], start=False, stop=True), r=[Tz[1], sbst], w=[po])
                        state_update(0, c, vsb)
                    for h in range(4):
                        OP("act", lambda e, h=h: e.activation(out=junk.t[:, h * 128:(h + 1) * 128], in_=po.t[:, h * 128:(h + 1) * 128],
                                                              func=AF.Square, accum_out=ss.t[:, h:h + 1]), r=[po], w=[junk, ss])
                    OP("dve", lambda e: e.tensor_scalar(out=rstd4.t[:], in0=ss.t[:], scalar1=1.0 / 128, scalar2=eps_t.t[:, 0:1],
                                                        op0=ALU.mult, op1=ALU.add), r=[ss, eps_t], w=[rstd4])
                    OP("act", lambda e: e.activation(out=rstd4.t[:], in_=rstd4.t[:], func=AF.Sqrt), r=[rstd4], w=[rstd4])
                    OP("dve", lambda e: e.reciprocal(out=rstd4.t[:], in_=rstd4.t[:]), r=[rstd4], w=[rstd4])
                    on3 = on.t[:].rearrange("p (h d) -> p h d", h=4)
                    OP("dve", lambda e: e.tensor_tensor(out=on3, in0=po.t[:].rearrange("p (h d) -> p h d", h=4),
                                                        in1=rstd4.t[:].unsqueeze(2).broadcast_to([128, 4, 128]), op=ALU.mult),
                       r=[po, rstd4], w=[on])
                    OP("pool", lambda e: e.tensor_tensor(out=on3, in0=on3, in1=gng.t[:].unsqueeze(1).broadcast_to([128, 4, 128]),
                                                         op=ALU.mult), r=[on, gng], w=[on])
                    OP("pool", lambda e: e.tensor_tensor(out=cat.t[:, 0:512], in0=on.t[:], in1=rsb.t[:], op=ALU.mult),
                       r=[on, rsb], w=[cat])
                    paq = bank(); inproj(1536, 512, paq)
                    OP("act", lambda e: e.activation(out=aqs.t[:], in_=paq.t[:], func=AF.Identity), r=[paq], w=[aqs])
                    S.dma("sp", csl, lambda e: e.dma_start(out=cs.t[:], in_=d_cos[j * 128:(j + 1) * 128, :]), w=[cs])
                    S.dma("sp", snl, lambda e: e.dma_start(out=sn.t[:], in_=d_sin[j * 128:(j + 1) * 128, :]), w=[sn])
                    rope(aqs, qrot, 8, rt)

                    def evq(c0, n, pb2):
                        OP("act", lambda e: e.activation(out=qTs.t[:, c0:c0 + n, :].rearrange("p a b -> p (a b)"),
                                                         in_=pb2.t[0:64, 0:n * 128], func=AF.Identity, scale=0.125), r=[pb2], w=[qTs])
                    transpose_to(qrot, 8, evq, width=64)
                    def att_group(g):
                        kts = [(kT_all, v_all, j - 1, amask_e if i == 0 else amask, 0), (kT_all, v_all, j, None, 0),
                               (kT_all, v_all, j + 1, amask_e if i == 15 else amask, 1), (kTc, vc, 0, None, 0), (kTc, vc, 1, None, 0)]
                        for n_, (kb, vb, jj, mk, mi) in enumerate(kts):
                            pst = bank()
                            OP("pe", lambda e, kb=kb, jj=jj, pst=pst: e.matmul(
                                pst.t[:], lhsT=kb.t[:, jj, g * 128:(g + 1) * 128],
                                rhs=qTs.t[:, 4 * g:4 * g + 4, :].rearrange("p a b -> p (a b)"), start=True, stop=True),
                               r=[kb, qTs], w=[pst])
                            OP("act", lambda e, n_=n_, pst=pst: e.activation(out=Pall.t[:, n_, :], in_=pst.t[:], func=AF.Exp),
                               r=[pst], w=[Pall])
                            if mk is not None:
                                OP("dve", lambda e, n_=n_, mk=mk, mi=mi: e.tensor_tensor(
                                    out=Pall.t[:, n_, :].rearrange("p (h q) -> p h q", h=4),
                                    in0=Pall.t[:, n_, :].rearrange("p (h q) -> p h q", h=4),
                                    in1=mk.t[:, mi:mi + 1, :].broadcast_to([128, 4, 128]), op=ALU.mult), r=[Pall, mk], w=[Pall])
                        pO = bank()
                        for hh in range(4):
                            for n_, (kb, vb, jj, mk, mi) in enumerate(kts):
                                OP("pe", lambda e, hh=hh, n_=n_, vb=vb, jj=jj: e.matmul(
                                    pO.t[:, hh * 65:(hh + 1) * 65], lhsT=Pall.t[:, n_, hh * 128:(hh + 1) * 128],
                                    rhs=vb.t[:, jj, g * 65:(g + 1) * 65], start=(n_ == 0), stop=(n_ == 4)), r=[Pall, vb], w=[pO])
                        pO3 = pO.t[:, 0:260].rearrange("p (h d) -> p h d", h=4)
                        OP("dve", lambda e, pO3=pO3: e.tensor_tensor(out=den.t[:].unsqueeze(2), in0=pO3[:, :, 64:65],
                                                                     in1=esink.t[:, 4 * g:4 * g + 4].unsqueeze(2), op=ALU.add),
                           r=[pO, esink], w=[den])
                        OP("dve", lambda e: e.reciprocal(out=den.t[:], in_=den.t[:]), r=[den], w=[den])
                        OP("dve", lambda e, pO3=pO3: e.tensor_tensor(
                            out=cat.t[:, 512 + g * 256:512 + (g + 1) * 256].rearrange("p (h d) -> p h d", h=4),
                            in0=pO3[:, :, 0:64], in1=den.t[:].unsqueeze(2).broadcast_to([128, 4, 64]), op=ALU.mult),
                           r=[pO, den], w=[cat])
                    for g_ in range(2):
                        att_group(g_)
                    if os.environ.get("K_DBG") == "cat":
                        S.dma("pool", sts, lambda e: e.dma_start(out=d_x1[i * 128:(i + 1) * 128, :], in_=cat.t[:]), r=[cat])
                        return
                    def evc(c0, n, pb2):
                        OP("act", lambda e: e.activation(out=catT.t[:, c0:c0 + n, :].rearrange("p a b -> p (a b)"),
                                                         in_=pb2.t[:, 0:n * 128], func=AF.Identity), r=[pb2], w=catT.c[c0:c0 + n])
                    transpose_to(cat, 8, evc)
                    for hf in range(2):
                        py = bank()
                        for kc in range(8):
                            OP("pe", lambda e, kc=kc, py=py, hf=hf: e.matmul(py.t[:], lhsT=catT.t[:, kc, :],
                                                                      rhs=wout.t[:, kc, hf * 512:(hf + 1) * 512],
                                                                      start=(kc == 0), stop=(kc == 7)), r=[catT.c[kc], wout], w=[py])
                        OP("dve", lambda e, py=py, hf=hf: e.tensor_tensor(out=r1.t[:, hf * 512:(hf + 1) * 512], in0=py.t[:],
                                                                   in1=g1bc.t[:, hf * 512:(hf + 1) * 512], op=ALU.mult),
                           r=[py, g1bc], w=[r1])
                    OP("dve", lambda e: e.scalar_tensor_tensor(out=r1.t[:], in0=xb.t[:], scalar=ALPHA, in1=r1.t[:],
                                                               op0=ALU.mult, op1=ALU.add), r=[xb, r1], w=[r1])
                    layernorm(r1, x1o, ln1g, ln1b, stats, mv, rs1)
                    S.dma("pool", sts, lambda e: e.dma_start(out=d_x1[i * 128:(i + 1) * 128, :], in_=x1o.t[:]), r=[x1o])

                def layernorm(src, dst, gb, bb, stats, mv, rs1):
                    for hf in range(2):
                        OP("dve", lambda e, hf=hf: e.bn_stats(out=stats.t[:, hf * 6:(hf + 1) * 6], in_=src.t[:, hf * 512:(hf + 1) * 512]),
                           r=[src], w=[stats])
                    OP("dve", lambda e: e.bn_aggr(out=mv.t[:], in_=stats.t[:]), r=[stats], w=[mv])
                    OP("dve", lambda e: e.tensor_scalar(out=rs1.t[:], in0=mv.t[:, 1:2], scalar1=eps_t.t[:, 0:1], scalar2=None, op0=ALU.add),
                       r=[mv, eps_t], w=[rs1])
                    OP("act", lambda e: e.activation(out=rs1.t[:], in_=rs1.t[:], func=AF.Sqrt), r=[rs1], w=[rs1])
                    OP("dve", lambda e: e.reciprocal(out=rs1.t[:], in_=rs1.t[:]), r=[rs1], w=[rs1])
                    OP("dve", lambda e: e.tensor_scalar(out=dst.t[:], in0=src.t[:], scalar1=mv.t[:, 0:1], scalar2=rs1.t[:, 0:1],
                                                        op0=ALU.subtract, op1=ALU.mult), r=[src, mv, rs1], w=[dst])
                    OP("pool", lambda e: e.tensor_tensor(out=dst.t[:], in0=dst.t[:], in1=gb.t[:], op=ALU.mult), r=[dst, gb], w=[dst])
                    OP("pool", lambda e: e.tensor_tensor(out=dst.t[:], in0=dst.t[:], in1=bb.t[:], op=ALU.add), r=[dst, bb], w=[dst])

                for t in range(2):
                    kv_tile(d_ctx[t * 128:(t + 1) * 128, :], 1, t, True)
                convert_tables()
                jobs = []
                for t in range(2):
                    jobs.append((d_ctx[t * 128:(t + 1) * 128, :], 1, 0, None, None))
                for t in (1, 0):
                    jobs.append((d_ctx[t * 128:(t + 1) * 128, :], 1, 1, None, None))
                for t in range(NPRE):
                    jobs.append((d_xpf[t * 128:(t + 1) * 128, :], 0, 0, t, None))
                for t in range(NPRE):
                    jobs.append((d_xpb[t * 128:(t + 1) * 128, :], 0, 1, 48 + t, None))
                for i in range(15, -1, -1):
                    jobs.append((d_xown[(i + 1) * 128:(i + 2) * 128, :], 0, 1, None, i))
                run_state_jobs(jobs)
                for j in range(18):
                    kv_tile(d_xown[j * 128:(j + 1) * 128, :], 0, j, False)
                for i in range(16):
                    own_tile(i)
                S.barrier()

        if mode != "A":
            with ExitStack() as pbs_:
                wq = sbuf(pbs_, "wq", [128, 8, 1024]); skT = sbuf(pbs_, "skT", [128, 8, 128])
                ln2g = sbuf(pbs_, "ln2g", [128, 1024]); ln2b = sbuf(pbs_, "ln2b", [128, 1024])
                iota = sbuf(pbs_, "iota", [128, 16])
                S.dma("sp", cst, lambda e: e.dma_start(out=wq.t[:], in_=d_wq.rearrange("(k p) n -> p k n", p=128)), w=[wq])
                for b_, d_ in ((skT, d_skT), (ln2g, d_ln2g), (ln2b, d_ln2b), (iota, d_iota)):
                    S.dma("sp", cst, lambda e, b_=b_, d_=d_: e.dma_start(out=b_.t[:], in_=d_), w=[b_])
                S.barrier()
                if mode == "B":
                    convert_tables()
                identb = sbuf(pbs_, "identb", [128, 128], BF16)
                OP("dve", lambda e: e.tensor_copy(out=identb.t[:], in_=ident.t[:]), r=[ident], w=[identb])
                dg = [sbuf(pbs_, "dg%d" % i, [128, 128], BF16) for i in range(4)]
                sc2bc = sbuf(pbs_, "sc2bc", [128, 1024]); sh2bc = sbuf(pbs_, "sh2bc", [128, 1024]); g2bc = sbuf(pbs_, "g2bc", [128, 1024])
                if mode == "B":
                    OP("dve", lambda e: e.memset(sc2bc.t[:], 1.0), w=[sc2bc])
                    OP("dve", lambda e: e.memset(sh2bc.t[:], 0.0), w=[sh2bc])
                    OP("dve", lambda e: e.memset(g2bc.t[:], 1.0), w=[g2bc])
                else:
                    for b_, di in ((sh2bc, 0), (sc2bc, 1), (g2bc, 2)):
                        S.dma("sp", cst, lambda e, b_=b_, di=di: e.dma_start(out=b_.t[:], in_=d_modbc[di]), w=[b_])
                    S.barrier()
                x1t = [sbuf(pbs_, "x1t%d" % i, [128, 1024]) for i in range(3)]
                h2 = [sbuf(pbs_, "h2_%d" % i, [128, 1024]) for i in range(2)]
                h2T = sbuf(pbs_, "h2T", [128, 8, 128]); qTp = sbuf(pbs_, "qTp", [128, 8, 128])
                ssb = sbuf(pbs_, "ssb", [128, 16, 128]); sw = sbuf(pbs_, "sw", [128, 128])
                m16 = sbuf(pbs_, "m16", [128, 16, 16]); ix16 = sbuf(pbs_, "ix16", [128, 16, 16], U32)
                ixf = sbuf(pbs_, "ixf", [128, 16, 16])
                cand = sbuf(pbs_, "cand", [128, 8, 256]); cw = sbuf(pbs_, "cw", [128, 256])
                tsv = sbuf(pbs_, "tsv", [128, 8, 16]); pos = sbuf(pbs_, "pos", [128, 8, 16], U32)
                posf = sbuf(pbs_, "posf", [128, 8, 16])
                lohi = sbuf(pbs_, "lohi", [128, 2, 16])
                paf = sbuf(pbs_, "paf", [128, 8, 16]); pbf = sbuf(pbs_, "pbf", [128, 8, 16])
                oh = sbuf(pbs_, "oh", [128, 8, 16, 16])
                i1s = sbuf(pbs_, "i1s", [128, 8, 16]); i2s = sbuf(pbs_, "i2s", [128, 8, 16])
                eidf = sbuf(pbs_, "eidf", [128, 128])
                eidx = [sbuf(pbs_, "eidx%d" % i, [128, 128], U32) for i in range(3)]
                gate = [sbuf(pbs_, "gate%d" % i, [128, 8, 16]) for i in range(2)]
                gsum = sbuf(pbs_, "gsum", [128, 8])
                dots = [sbuf(pbs_, "dots%d" % i, [128, 128]) for i in range(2)]
                coef = [sbuf(pbs_, "coef%d" % i, [128, 128]) for i in range(2)]
                gb = [sbuf(pbs_, "gb%d" % i, [128, 2048], BF16) for i in range(NG)]
                gs = [S.dma_slot("g%d" % i) for i in range(NG)]
                prod = [sbuf(pbs_, "prod%d" % i, [128, 1024], BF16) for i in range(3)]
                h2b = [sbuf(pbs_, "h2b%d" % i, [128, 1024], BF16) for i in range(2)]
                acc = [sbuf(pbs_, "acc%d" % i, [128, 1024]) for i in range(2)]
                stats2 = sbuf(pbs_, "stats2", [128, 12]); mv2 = sbuf(pbs_, "mv2", [128, 2]); rs2 = sbuf(pbs_, "rs2", [128, 1])
                x1ld = [S.dma_slot("x1l%d" % i) for i in range(3)]

                def layernorm2(src, dst):
                    for hf in range(2):
                        OP("dve", lambda e, hf=hf: e.bn_stats(out=stats2.t[:, hf * 6:(hf + 1) * 6], in_=src.t[:, hf * 512:(hf + 1) * 512]),
                           r=[src], w=[stats2])
                    OP("dve", lambda e: e.bn_aggr(out=mv2.t[:], in_=stats2.t[:]), r=[stats2], w=[mv2])
                    OP("dve", lambda e: e.tensor_scalar(out=rs2.t[:], in0=mv2.t[:, 1:2], scalar1=eps_t.t[:, 0:1], scalar2=None, op0=ALU.add),
                       r=[mv2, eps_t], w=[rs2])
                    OP("act", lambda e: e.activation(out=rs2.t[:], in_=rs2.t[:], func=AF.Sqrt), r=[rs2], w=[rs2])
                    OP("dve", lambda e: e.reciprocal(out=rs2.t[:], in_=rs2.t[:]), r=[rs2], w=[rs2])
                    OP("dve", lambda e: e.tensor_scalar(out=dst.t[:], in0=src.t[:], scalar1=mv2.t[:, 0:1], scalar2=rs2.t[:, 0:1],
                                                        op0=ALU.subtract, op1=ALU.mult), r=[src, mv2, rs2], w=[dst])
                    OP("dve", lambda e: e.tensor_tensor(out=dst.t[:], in0=dst.t[:], in1=ln2g.t[:], op=ALU.mult), r=[dst, ln2g], w=[dst])
                    OP("dve", lambda e: e.tensor_tensor(out=dst.t[:], in0=dst.t[:], in1=ln2b.t[:], op=ALU.add), r=[dst, ln2b], w=[dst])

                def top16(src3, n, work, mdst, idst):
                    (sa, sbuf_), (wa, wbuf), (ma, mbuf), (ia, ibuf) = src3, work, mdst, idst
                    OP("dve", lambda e: e.max(out=ma[:, 0:8], in_=sa), r=[sbuf_], w=[mbuf])
                    OP("dve", lambda e: e.max_index(out=ia[:, 0:8], in_max=ma[:, 0:8], in_values=sa), r=[sbuf_, mbuf], w=[ibuf])
                    OP("dve", lambda e: e.match_replace(out=wa, in_to_replace=ma[:, 0:8], in_values=sa, imm_value=-1e30),
                       r=[sbuf_, mbuf], w=[wbuf])
                    OP("dve", lambda e: e.max(out=ma[:, 8:16], in_=wa), r=[wbuf], w=[mbuf])
                    OP("dve", lambda e: e.max_index(out=ia[:, 8:16], in_max=ma[:, 8:16], in_values=wa), r=[wbuf, mbuf], w=[ibuf])

                def route(i):
                    xs = x1t[i % 3]
                    S.dma("sp", x1ld[i % 3], lambda e: e.dma_start(out=xs.t[:], in_=d_x1[i * 128:(i + 1) * 128, :]), w=[xs])
                    hh = h2[i % 2]
                    OP("dve", lambda e: e.tensor_tensor(out=hh.t[:], in0=xs.t[:], in1=sc2bc.t[:], op=ALU.mult), r=[xs, sc2bc], w=[hh])
                    OP("dve", lambda e: e.tensor_tensor(out=hh.t[:], in0=hh.t[:], in1=sh2bc.t[:], op=ALU.add), r=[hh, sh2bc], w=[hh])
                    OP("act", lambda e: e.activation(out=h2b[i % 2].t[:], in_=hh.t[:], func=AF.Identity), r=[hh], w=[h2b[i % 2]])

                    if KSTOP < 2: return
                    def ev(c0, n, pb2):
                        OP("act", lambda e: e.activation(out=h2T.t[:, c0:c0 + n, :].rearrange("p a b -> p (a b)"),
                                                         in_=pb2.t[:, 0:n * 128], func=AF.Identity), r=[pb2], w=[h2T])
                    yield
                    transpose_to(hh, 8, ev)
                    yield
                    if KSTOP < 2.3: return
                    for j0 in range(0, 8, 4):
                        pq = bank()
                        for jj in range(j0, j0 + 4):
                            for kc in range(8):
                                OP("pe", lambda e, jj=jj, kc=kc, pq=pq, j0=j0: e.matmul(
                                    pq.t[:, (jj - j0) * 128:(jj - j0 + 1) * 128], lhsT=wq.t[:, kc, jj * 128:(jj + 1) * 128],
                                    rhs=h2T.t[:, kc, :], start=(kc == 0), stop=(kc == 7)), r=[wq, h2T], w=[pq])
                            yield
                        OP("act", lambda e, pq=pq, j0=j0: e.activation(out=qTp.t[:, j0:j0 + 4, :].rearrange("p a b -> p (a b)"),
                                                                       in_=pq.t[:], func=AF.Identity), r=[pq], w=[qTp])
                    if KSTOP < 2.6: return
                    for h0 in range(0, 8, 4):
                        for a in range(2):
                            psc = bank()
                            for h in range(h0, h0 + 4):
                                OP("pe", lambda e, h=h, a=a, psc=psc, h0=h0: e.matmul(
                                    psc.t[:, (h - h0) * 128:(h - h0 + 1) * 128], lhsT=qTp.t[a * 64:(a + 1) * 64, h, :],
                                    rhs=skT.t[a * 64:(a + 1) * 64, h, :], start=True, stop=True), r=[qTp, skT], w=[psc])
                            OP("act", lambda e, psc=psc, h0=h0, a=a: e.activation(
                                out=ssb.t[:, 2 * h0 + a:2 * h0 + 8:2, :], in_=psc.t[:].rearrange("p (a b) -> p a b", a=4),
                                func=AF.Identity), r=[psc], w=[ssb])
                            yield
                    if KSTOP < 3: return
                    for q_ in range(16):
                        top16((ssb.t[:, q_, :], ssb), 128, (sw.t[:], sw), (m16.t[:, q_, :], m16), (ix16.t[:, q_, :], ix16))
                        yield
                    if KSTOP < 4: return
                    OP("dve", lambda e: e.tensor_copy(out=ixf.t[:], in_=ix16.t[:]), r=[ix16], w=[ixf])
                    m4 = m16.t[:].rearrange("p (h a) k -> p h a k", a=2)
                    OP("dve", lambda e: e.tensor_tensor(
                        out=cand.t[:].rearrange("p h (a b) -> p h a b", a=16),
                        in0=m4[:, :, 0, :].unsqueeze(3).broadcast_to([128, 8, 16, 16]),
                        in1=m4[:, :, 1, :].unsqueeze(2).broadcast_to([128, 8, 16, 16]), op=ALU.add), r=[m16], w=[cand])
                    for h in range(8):
                        top16((cand.t[:, h, :], cand), 256, (cw.t[:], cw), (tsv.t[:, h, :], tsv), (pos.t[:, h, :], pos))
                        yield
                    gt = gate[i % 2]
                    OP("dve", lambda e: e.tensor_tensor(out=gt.t[:], in0=tsv.t[:], in1=tsv.t[:, :, 0:1].broadcast_to([128, 8, 16]),
                                                        op=ALU.subtract), r=[tsv], w=[gt])
                    OP("act", lambda e: e.activation(out=gt.t[:], in_=gt.t[:], func=AF.Exp), r=[gt], w=[gt])
                    OP("dve", lambda e: e.tensor_reduce(out=gsum.t[:], in_=gt.t[:], axis=AX.X, op=ALU.add), r=[gt], w=[gsum])
                    OP("dve", lambda e: e.reciprocal(out=gsum.t[:], in_=gsum.t[:]), r=[gsum], w=[gsum])
                    OP("dve", lambda e: e.tensor_tensor(out=gt.t[:], in0=gt.t[:], in1=gsum.t[:].unsqueeze(2).broadcast_to([128, 8, 16]),
                                                        op=ALU.mult), r=[gt, gsum], w=[gt])
                    if KSTOP < 5: return
                    yield
                    OP("dve", lambda e: e.tensor_copy(out=posf.t[:], in_=pos.t[:]), r=[pos], w=[posf])
                    oh2 = Buf(None); oh2.r = ssb.r
                    oh2.t = ssb.t[:].rearrange("p a b -> p (a b)").rearrange("p (h j c) -> p h j c", h=8, j=16)
                    ix4 = ixf.t[:].rearrange("p (h a) k -> p h a k", a=2)
                    bc4 = lambda ap2: ap2.unsqueeze(1).unsqueeze(1).broadcast_to([128, 8, 16, 16])
                    pf4 = posf.t[:].unsqueeze(3).broadcast_to([128, 8, 16, 16])
                    OP("dve", lambda e: e.tensor_tensor(out=oh.t[:], in0=pf4, in1=bc4(lohi.t[:, 0, :]), op=ALU.is_ge), r=[posf, lohi], w=[oh])
                    OP("dve", lambda e: e.tensor_tensor(out=oh2.t[:], in0=pf4, in1=bc4(lohi.t[:, 1, :]), op=ALU.is_ge), r=[posf, lohi], w=[oh2])
                    OP("dve", lambda e: e.tensor_tensor(out=oh.t[:], in0=oh.t[:], in1=oh2.t[:], op=ALU.subtract), r=[oh, oh2], w=[oh])
                    OP("dve", lambda e: e.tensor_tensor(out=oh2.t[:], in0=oh.t[:], in1=bc4(lohi.t[:, 0, :]), op=ALU.mult), r=[oh, lohi], w=[oh2])
                    yield
                    OP("dve", lambda e: e.tensor_reduce(out=paf.t[:], in_=oh2.t[:], axis=AX.X, op=ALU.add), r=[oh2], w=[paf])
                    OP("dve", lambda e: e.tensor_tensor(out=oh.t[:], in0=oh.t[:],
                                                        in1=ix4[:, :, 0, :].unsqueeze(2).broadcast_to([128, 8, 16, 16]), op=ALU.mult),
                       r=[oh, ixf], w=[oh])
                    OP("dve", lambda e: e.tensor_reduce(out=i1s.t[:], in_=oh.t[:], axis=AX.X, op=ALU.add), r=[oh], w=[i1s])
                    yield
                    OP("dve", lambda e: e.tensor_tensor(out=pbf.t[:], in0=posf.t[:], in1=paf.t[:], op=ALU.subtract), r=[posf, paf], w=[pbf])
                    OP("dve", lambda e: e.tensor_tensor(out=oh.t[:], in0=bc4(iota.t[:]),
                                                        in1=pbf.t[:].unsqueeze(3).broadcast_to([128, 8, 16, 16]), op=ALU.is_equal),
                       r=[iota, pbf], w=[oh])
                    OP("dve", lambda e: e.tensor_tensor(out=oh.t[:], in0=oh.t[:],
                                                        in1=ix4[:, :, 1, :].unsqueeze(2).broadcast_to([128, 8, 16, 16]), op=ALU.mult),
                       r=[oh, ixf], w=[oh])
                    OP("dve", lambda e: e.tensor_reduce(out=i2s.t[:], in_=oh.t[:], axis=AX.X, op=ALU.add), r=[oh], w=[i2s])
                    OP("dve", lambda e: e.scalar_tensor_tensor(out=eidf.t[:].rearrange("p (h k) -> p h k", h=8), in0=i1s.t[:], scalar=128.0,
                                                               in1=i2s.t[:], op0=ALU.mult, op1=ALU.add), r=[i1s, i2s], w=[eidf])
                    OP("dve", lambda e: e.tensor_copy(out=eidx[i % 3].t[:], in_=eidf.t[:]), r=[eidf], w=[eidx[i % 3]])

                gi = [0]

                class V:
                    def __init__(self, t):
                        self.t = t
                        self.r = Res()
                dcol = [[V(dots[p_].t) for _ in range(128)] for p_ in range(2)]
                ccol = [[V(coef[p_].t) for _ in range(128)] for p_ in range(2)]

                gk = {}

                def slot_u(i, s):
                    k = gi[0] % NG
                    gi[0] += 1
                    gk[(i, s)] = k
                    S.dma("pool", gs[k], lambda e: e.indirect_dma_start(
                        out=gb[k].t[:], out_offset=None, in_=d_puv16,
                        in_offset=bass.IndirectOffsetOnAxis(ap=eidx[i % 3].t[:, s:s + 1], axis=0)), r=[eidx[i % 3]] + puvB, w=[gb[k]])
                    pr = prod[s % 3]
                    dc, cc = dcol[i % 2][s], ccol[i % 2][s]
                    OP("dve", lambda e: e.tensor_tensor(out=pr.t[:], in0=gb[k].t[:, 0:1024], in1=h2b[i % 2].t[:], op=ALU.mult),
                       r=[gb[k], h2b[i % 2]], w=[pr])
                    OP("act", lambda e: e.activation(out=pr.t[:], in_=pr.t[:], func=AF.Identity, accum_out=dc.t[:, s:s + 1]),
                       r=[pr], w=[pr, dc])
                    OP("act", lambda e: e.activation(out=cc.t[:, s:s + 1], in_=dc.t[:, s:s + 1], func=AF.Gelu), r=[dc], w=[cc])

                def slot_v(i, s):
                    k = gk.pop((i, s))
                    cc = ccol[i % 2][s]
                    dgk = dg[s % 4]
                    OP("dve", lambda e: e.tensor_scalar(out=dgk.t[:], in0=identb.t[:], scalar1=cc.t[:, s:s + 1],
                                                        scalar2=gate[i % 2].t[:, s // 16, s % 16:s % 16 + 1], op0=ALU.mult, op1=ALU.mult),
                       r=[identb, cc, gate[i % 2]], w=[dgk])
                    for hf in range(2):
                        OP("pe", lambda e, hf=hf: e.matmul(accP[hf].t[:], lhsT=dgk.t[:], rhs=gb[k].t[:, 1024 + hf * 512:1536 + hf * 512],
                                                           start=(s == 0), stop=(s == 127)), r=[dgk, gb[k]], w=[accP[hf]])

                def finish_v(i):
                    r2 = acc[i % 2]; yo = acc[i % 2]
                    for hf in range(2):
                        OP("dve", lambda e, hf=hf: e.tensor_tensor(out=r2.t[:, hf * 512:(hf + 1) * 512], in0=accP[hf].t[:],
                                                                   in1=g2bc.t[:, hf * 512:(hf + 1) * 512], op=ALU.mult),
                           r=[accP[hf], g2bc], w=[r2])
                    OP("dve", lambda e: e.scalar_tensor_tensor(out=r2.t[:], in0=x1t[i % 3].t[:], scalar=ALPHA, in1=r2.t[:],
                                                               op0=ALU.mult, op1=ALU.add), r=[x1t[i % 3], r2], w=[r2])
                    layernorm2(r2, yo)
                    S.dma("sp", sts, lambda e: e.dma_start(out=d_out[i * 128:(i + 1) * 128, :], in_=yo.t[:]), r=[yo])

                OP("dve", lambda e: e.tensor_scalar(out=lohi.t[:, 0, :], in0=iota.t[:], scalar1=16.0, scalar2=None, op0=ALU.mult), r=[iota], w=[lohi])
                OP("dve", lambda e: e.tensor_scalar(out=lohi.t[:, 1, :], in0=iota.t[:], scalar1=16.0, scalar2=16.0, op0=ALU.mult, op1=ALU.add),
                   r=[iota], w=[lohi])
                NT = int(os.environ.get('K_NT', '16'))
                KSTOP = float(os.environ.get('K_STOP', '9'))
                for _ in route(0):
                    pass
                for i in range(NT):
                    gen = route(i + 1) if i + 1 < NT else iter(())
                    for s in range(128 + LAG if KSTOP >= 6 else 0):
                        if s < 128:
                            slot_u(i, s)
                        if s >= LAG:
                            slot_v(i, s - LAG)
                        next(gen, None)
                    for _ in gen:
                        pass
                    if KSTOP >= 7:
                        finish_v(i)
                S.barrier()
        S.barrier()
        S.run()
    return nc


def _consts():
    s = np.arange(128)[:, None]
    t = np.arange(128)[None, :]
    same = (s // 64) == (t // 64)
    g = -1.0 / 16.0
    tri = np.stack([(same & (s <= t)), (same & (s > t)), (same & (s >= t)), (same & (s < t))], axis=1).astype(np.float32) * g
    ci = np.stack([(np.arange(128) // 64 == c) for c in range(2)], axis=1).astype(np.float32) * g
    amask = np.stack([(s >= t), (s <= t)], axis=1).astype(np.float32)
    sc = (np.arange(128) % 64)[:, None]
    cc = np.arange(64)[None, :]
    gmask = np.stack([(sc <= cc), (sc >= cc)], axis=1).astype(np.float32)
    iota = np.broadcast_to(np.arange(16, dtype=np.float32), (128, 16)).copy()
    return dict(ident=np.eye(128, dtype=np.float32), tri=np.ascontiguousarray(tri), ci=np.ascontiguousarray(ci),
                amask=np.ascontiguousarray(amask), gmask=np.ascontiguousarray(gmask), iota16=iota)


def _rope_tables():
    rows = 8192 // 64
    row = np.repeat(np.arange(rows, dtype=np.float32), 64)
    col = np.tile(np.arange(64, dtype=np.float32), rows)
    inv = (np.float32(10000.0) ** (-np.arange(16, dtype=np.float32) / np.float32(16))).astype(np.float32)
    ang = np.stack([row[:, None] * inv, col[:, None] * inv], axis=1).astype(np.float32)
    return np.cos(ang).reshape(8192, 32).astype(np.float32), np.sin(ang).reshape(8192, 32).astype(np.float32)


def make_in_maps(x, c, ctx, c_ctx, w_ada, b_ada, w_in, w_gate2_f, b_gate_f, w_gate2_b, b_gate_b, gla_norm_g, attn_sink,
                 w_out, ln1_g, ln1_b, peer_wq, peer_subkeys, peer_u, peer_v, ln2_g, ln2_b):
    f = lambda a: np.ascontiguousarray(np.asarray(a, dtype=np.float32))
    bc = lambda v, n: np.ascontiguousarray(np.broadcast_to(np.asarray(v, np.float32).reshape(1, -1), (128, n)))
    x = f(x); ctx = f(ctx)
    wi = f(w_in[0])
    wperm = np.concatenate([wi[:, 0:256], wi[:, 256:512], wi[:, 512:1024], wi[:, 1024:1536], wi[:, 1568:2080],
                            wi[:, 2080:2208], wi[:, 2208:2336], wi[:, 1536:1552], wi[:, 1552:1568]], axis=1)
    cosT, sinT = _rope_tables()
    common = dict(
        w_ada=f(w_ada[0]), b_ada=f(b_ada[0]).reshape(1, -1), w_in=np.ascontiguousarray(wperm),
        w2=np.ascontiguousarray(np.concatenate([f(w_gate2_f[0]), f(w_gate2_b[0])], axis=1)),
        bg=np.ascontiguousarray(np.concatenate([f(b_gate_f[0]), f(b_gate_b[0])]).reshape(1, -1)),
        gng=bc(gla_norm_g[0], 128), sink=bc(attn_sink[0], 8), w_out=f(w_out[0]),
        ln1g=bc(ln1_g[0], 1024), ln1b=bc(ln1_b[0], 1024), ln2g=bc(ln2_g[0], 1024), ln2b=bc(ln2_b[0], 1024),
        peer_wq=f(peer_wq[0]),
        skT=np.ascontiguousarray(np.transpose(f(peer_subkeys[0]), (1, 3, 0, 2)).reshape(128, 8, 128)),
        peer_uv=np.ascontiguousarray(np.concatenate([f(peer_u[0]), f(peer_v[0])], axis=1)), **_consts())
    maps = []
    zt = np.zeros((128, 1024), np.float32)
    for core in range(8):
        b, s = core // 4, core % 4
        xb = x[b].reshape(64, 128, 1024)
        npf = 16 * s
        xpf = np.zeros((NPRE, 128, 1024), np.float32)
        if npf:
            xpf[NPRE - npf:] = xb[0:npf]
        npb = 16 * (3 - s)
        xpb = np.zeros((NPRE, 128, 1024), np.float32)
        if npb:
            xpb[NPRE - npb:] = xb[63:16 * (s + 1) - 1:-1]
        flags = np.zeros((128, 100), np.float32)
        flags[:, NPRE - npf:NPRE] = 1.0
        flags[:, 48 + NPRE - npb:48 + NPRE] = 1.0
        t0 = 16 * s
        own = np.zeros((18, 128, 1024), np.float32)
        own[1:17] = xb[t0:t0 + 16]
        cos_o = np.zeros((18, 128, 32), np.float32); sin_o = np.zeros((18, 128, 32), np.float32)
        cos_o[1:17] = cosT.reshape(64, 128, 32)[t0:t0 + 16]; sin_o[1:17] = sinT.reshape(64, 128, 32)[t0:t0 + 16]
        if t0 > 0:
            own[0] = xb[t0 - 1]; flags[:, 96] = 1.0
            cos_o[0] = cosT.reshape(64, 128, 32)[t0 - 1]; sin_o[0] = sinT.reshape(64, 128, 32)[t0 - 1]
        if t0 + 16 < 64:
            own[17] = xb[t0 + 16]; flags[:, 97] = 1.0
            cos_o[17] = cosT.reshape(64, 128, 32)[t0 + 16]; sin_o[17] = sinT.reshape(64, 128, 32)[t0 + 16]
        c2 = np.stack([f(c)[b], f(c_ctx)], axis=0)
        c2T = np.ascontiguousarray(c2.reshape(2, 8, 128).transpose(2, 1, 0))
        m = dict(common)
        m.update(xpre_f=xpf.reshape(-1, 1024), xpre_b=xpb.reshape(-1, 1024), xown=own.reshape(-1, 1024), ctx=ctx[b],
                 flags=flags, c2T=c2T, cosT=cos_o.reshape(-1, 32), sinT=sin_o.reshape(-1, 32))
        maps.append(m)
    return maps


_NC_CACHE = {}


def kernel(**inputs):
    if "full" not in _NC_CACHE:
        _NC_CACHE["full"] = build_nc("full")
    nc = _NC_CACHE["full"]
    maps = make_in_maps(**inputs)
    res = run_bass_kernel_spmd(nc, maps, core_ids=list(range(8)))
    out = np.zeros((2, 8192, 1024), np.float32)
    for core in range(8):
        b, s = core // 4, core % 4
        out[b, s * 2048:(s + 1) * 2048] = np.asarray(res.results[core]["out"], np.float32)
    return out
```

```python
import os
import numpy as np
from contextlib import ExitStack
import concourse.bass as bass
import concourse.mybir as mybir
from concourse.bass_utils import run_bass_kernel_spmd

F32 = mybir.dt.float32
BF16 = mybir.dt.bfloat16
U32 = mybir.dt.uint32
AF = mybir.ActivationFunctionType
ALU = mybir.AluOpType
AX = mybir.AxisListType

SAME_ENGINE_SYNC = True
NPRE = 48
LN_EPS = 1e-5
ALPHA = 2.0 ** 0.25
NG = 12
LAG = 3


class Res:
    __slots__ = ("w", "rs")

    def __init__(self):
        self.w = None
        self.rs = {}


class Buf:
    def __init__(self, t):
        self.t = t
        self.r = Res()


class Sched:
    def __init__(self, nc, stack):
        self.nc = nc
        self.stack = stack
        self.sems = {}
        self.count = {}
        self.seen = {k: {} for k in ("pe", "dve", "act", "pool", "sp")}
        self.streams = {k: [] for k in ("pe", "dve", "act", "pool", "sp")}
        for k in ("pe", "dve", "act", "pool"):
            self.sems[k] = stack.enter_context(nc.semaphore("sem_" + k))
            self.count[k] = 0
        self.nslots = 0

    def dma_slot(self, name=""):
        self.nslots += 1
        key = "d%d%s" % (self.nslots, name)
        self.sems[key] = self.stack.enter_context(self.nc.semaphore("s_" + key))
        self.count[key] = 0
        return key

    def _waits(self, q, reads, writes, same_ok):
        deps = {}
        for b in reads:
            r = b.r
            if r.w is not None:
                k, c = r.w
                deps[k] = max(deps.get(k, 0), c)
        for b in writes:
            w = b.r
            if w.w is not None:
                k, c = w.w
                deps[k] = max(deps.get(k, 0), c)
            for k, c in w.rs.items():
                deps[k] = max(deps.get(k, 0), c)
        out = []
        for k, c in deps.items():
            if k == q and not same_ok:
                continue
            if self.seen[q].get(k, 0) >= c:
                continue
            self.seen[q][k] = c
            out.append((k, c))
        return out

    def op(self, q, fn, r=(), w=()):
        same_ok = SAME_ENGINE_SYNC and q != "pe"
        waits = self._waits(q, r, w, same_ok)
        self.count[q] += 1
        c = self.count[q]
        sems = self.sems
        st = self.streams[q]
        for k, v in waits:
            st.append(lambda e, k=k, v=v: e.wait_ge(sems[k], v))
        st.append(lambda e, fn=fn: fn(e).then_inc(sems[q], 1))
        for b in r:
            b.r.rs[q] = c
        for b in w:
            b.r.w = (q, c)
            b.r.rs = {}

    def dma(self, q, slot, fn, r=(), w=()):
        waits = self._waits(q, r, w, True)
        prev = self.count[slot]
        if prev > 0 and self.seen[q].get(slot, 0) < prev:
            self.seen[q][slot] = prev
            waits.append((slot, prev))
        self.count[slot] += 16
        c = self.count[slot]
        sems = self.sems
        st = self.streams[q]
        for k, v in waits:
            st.append(lambda e, k=k, v=v: e.wait_ge(sems[k], v))
        st.append(lambda e, fn=fn: fn(e).then_inc(sems[slot], 16))
        for b in r:
            b.r.rs[slot] = c
        for b in w:
            b.r.w = (slot, c)
            b.r.rs = {}

    def wait_all(self, q):
        sems = self.sems
        for k in list(self.count.keys()):
            c = self.count[k]
            if c == 0 or k == q or self.seen[q].get(k, 0) >= c:
                continue
            self.seen[q][k] = c
            self.streams[q].append(lambda e, k=k, c=c: e.wait_ge(sems[k], c))

    def barrier(self):
        for q in ("pe", "dve", "act", "pool", "sp"):
            self.wait_all(q)

    def run(self):
        streams = self.streams
        with self.nc.Block() as block:
            @block.tensor
            def _(e):
                for f in streams["pe"]:
                    f(e)

            @block.vector
            def _(e):
                for f in streams["dve"]:
                    f(e)

            @block.scalar
            def _(e):
                for f in streams["act"]:
                    f(e)

            @block.gpsimd
            def _(e):
                for f in streams["pool"]:
                    f(e)

            @block.sync
            def _(e):
                for f in streams["sp"]:
                    f(e)


def build_nc(mode="full"):
    nc = bass.Bass("TRN2", target_bir_lowering=False)
    D = lambda name, shape, dt=F32, kind="ExternalInput": nc.dram_tensor(name, shape, dt, kind=kind).ap()
    d_xpf = D("xpre_f", [NPRE * 128, 1024]); d_xpb = D("xpre_b", [NPRE * 128, 1024])
    d_xown = D("xown", [18 * 128, 1024]); d_ctx = D("ctx", [256, 1024])
    d_flags = D("flags", [128, 100]); d_c2T = D("c2T", [128, 8, 2])
    d_wada = D("w_ada", [1024, 6144]); d_bada = D("b_ada", [1, 6144])
    d_win = D("w_in", [1024, 2336]); d_w2 = D("w2", [16, 512]); d_bg = D("bg", [1, 512])
    d_gng = D("gng", [128, 128]); d_sink = D("sink", [128, 8])
    d_wout = D("w_out", [1024, 1024])
    d_ln1g = D("ln1g", [128, 1024]); d_ln1b = D("ln1b", [128, 1024])
    d_ln2g = D("ln2g", [128, 1024]); d_ln2b = D("ln2b", [128, 1024])
    d_wq = D("peer_wq", [1024, 1024]); d_skT = D("skT", [128, 8, 128])
    d_puv = D("peer_uv", [16384, 2048])
    d_puv16 = D("puv16", [16384, 2048], BF16, kind="Internal")
    d_cos = D("cosT", [18 * 128, 32]); d_sin = D("sinT", [18 * 128, 32])
    d_ident = D("ident", [128, 128]); d_tri = D("tri", [128, 4, 128]); d_ci = D("ci", [128, 2])
    d_amask = D("amask", [128, 2, 128]); d_gmask = D("gmask", [128, 2, 64]); d_iota = D("iota16", [128, 16])
    d_out = D("out", [2048, 1024], kind="ExternalOutput")
    if mode == "B":
        d_x1 = D("x1s", [2048, 1024])
    else:
        d_x1 = D("x1s", [2048, 1024], kind="ExternalOutput" if mode == "A" else "Internal")

    with ExitStack() as top:
        S = Sched(nc, top)
        OP = S.op

        def sbuf(st, name, shape, dt=F32):
            return Buf(st.enter_context(nc.sbuf_tensor("s_" + name, shape, dt)))

        banks = [Buf(top.enter_context(nc.psum_tensor("pb%d" % i, [128, 512], F32))) for i in range(6)]
        accP = [Buf(top.enter_context(nc.psum_tensor("pacc%d" % i, [128, 512], F32))) for i in range(2)]
        bank_i = [0]

        def bank():
            b = banks[bank_i[0] % 6]
            bank_i[0] += 1
            return b

        ld = [S.dma_slot("ld%d" % i) for i in range(2)]
        cst = S.dma_slot("cst")
        sts = S.dma_slot("st")
        mbs = S.dma_slot("mb")
        outs = S.dma_slot("out")
        csl = S.dma_slot("cs"); snl = S.dma_slot("sn")

        NCV = 128
        puvB = [Buf(None) for _ in range(NCV)]
        cvs = [S.dma_slot("cv%d" % i) for i in range(4)]
        cv_next = [0]

        def convert_chunk(pace=()):
            ci_ = cv_next[0]
            if ci_ >= NCV:
                return
            cv_next[0] += 1
            rows = 16384 // NCV
            S.dma("pool", cvs[ci_ % 4], lambda e: e.dma_start(out=d_puv16[ci_ * rows:(ci_ + 1) * rows, :],
                                                            in_=d_puv[ci_ * rows:(ci_ + 1) * rows, :]), r=list(pace), w=[puvB[ci_]])

        def convert_tables():
            while cv_next[0] < NCV:
                convert_chunk()
        ident = sbuf(top, "ident", [128, 128])
        S.dma("sp", cst, lambda e: e.dma_start(out=ident.t[:], in_=d_ident), w=[ident])
        flags = sbuf(top, "flags", [128, 100])
        S.dma("sp", cst, lambda e: e.dma_start(out=flags.t[:], in_=d_flags), w=[flags])
        eps_t = sbuf(top, "eps_t", [128, 1])
        OP("dve", lambda e: e.memset(eps_t.t[:], LN_EPS), w=[eps_t])
        modT = sbuf(top, "modT", [128, 48, 2])
        sc1p = sbuf(top, "sc1p", [128, 8, 2])
        g1bc = sbuf(top, "g1bc", [128, 1024])
        d_modbc = D("modbc", [3, 128, 1024], kind="Internal")
        xt_i = [0]

        def transpose_to(src, nchunks, dst_fn, width=128, rows=128):
            for c0 in range(0, nchunks, 4):
                pb = bank()
                n = min(4, nchunks - c0)
                for c in range(c0, c0 + n):
                    OP("pe", lambda e, c=c, pb=pb, c0=c0: e.transpose(
                        out=pb.t[0:width, (c - c0) * 128:(c - c0) * 128 + rows],
                        in_=src.t[0:rows, c * width:(c + 1) * width], identity=ident.t[0:rows, 0:rows]),
                       r=[src, ident], w=[pb])
                dst_fn(c0, n, pb)

        if mode != "B":
            with ExitStack() as p0:
                c2T = sbuf(p0, "c2T", [128, 8, 2]); sc2 = sbuf(p0, "sc2", [128, 8, 2])
                S.dma("sp", cst, lambda e: e.dma_start(out=c2T.t[:], in_=d_c2T), w=[c2T])
                OP("act", lambda e: e.activation(out=sc2.t[:], in_=c2T.t[:], func=AF.Silu), r=[c2T], w=[sc2])
                modrow = sbuf(p0, "modrow", [2, 6144])
                brow = sbuf(p0, "brow", [128, 6144]); ones2 = sbuf(p0, "ones2", [128, 2])
                OP("pool", lambda e: e.memset(brow.t[:], 0.0), w=[brow])
                S.dma("sp", cst, lambda e: e.dma_start(out=brow.t[0:1, :], in_=d_bada), w=[brow])
                OP("dve", lambda e: e.memset(ones2.t[:], 0.0), w=[ones2])
                OP("dve", lambda e: e.memset(ones2.t[0:1, :], 1.0), w=[ones2])
                S.barrier()
                wst = [sbuf(p0, "wst%d" % i, [128, 3072]) for i in range(2)]
                wada_v = d_wada.rearrange("(k p) n -> k p n", p=128)
                for half in range(2):
                    pbs = [bank() for _ in range(6)]
                    for j in range(6):
                        col = half * 3072 + j * 512
                        OP("pe", lambda e, pbj=pbs[j], col=col: e.matmul(pbj.t[0:2, :], lhsT=ones2.t[:], rhs=brow.t[:, col:col + 512],
                                                                        start=True, stop=False), r=[ones2, brow], w=[pbs[j]])
                    for kc in range(8):
                        ws = wst[kc % 2]
                        S.dma("sp", ld[kc % 2], lambda e, ws=ws, kc=kc, half=half: e.dma_start(
                            out=ws.t[:], in_=wada_v[kc, :, half * 3072:(half + 1) * 3072]), w=[ws])
                        for j in range(6):
                            OP("pe", lambda e, j=j, ws=ws, kc=kc, pbj=pbs[j]: e.matmul(
                                pbj.t[0:2, :], lhsT=sc2.t[:, kc, :], rhs=ws.t[:, j * 512:(j + 1) * 512],
                                start=False, stop=(kc == 7)), r=[sc2, ws], w=[pbs[j]])
                    for j in range(6):
                        col = half * 3072 + j * 512
                        OP("act", lambda e, pbj=pbs[j], col=col: e.activation(out=modrow.t[:, col:col + 512], in_=pbj.t[0:2, :],
                                                                       func=AF.Identity), r=[pbs[j]], w=[modrow])
                for c0 in range(0, 48, 4):
                    pb = bank()
                    for c in range(c0, c0 + 4):
                        OP("pe", lambda e, c=c, pb=pb, c0=c0: e.transpose(
                            out=pb.t[:, (c - c0) * 2:(c - c0) * 2 + 2], in_=modrow.t[0:2, c * 128:(c + 1) * 128],
                            identity=ident.t[0:2, 0:2]), r=[modrow, ident], w=[pb])
                    OP("dve", lambda e, pb=pb, c0=c0: e.tensor_copy(
                        out=modT.t[:, c0:c0 + 4, :], in_=pb.t[:, 0:8].rearrange("p (a b) -> p a b", a=4)), r=[pb], w=[modT])
                OP("dve", lambda e: e.tensor_scalar(out=sc1p.t[:], in0=modT.t[:, 8:16, :], scalar1=1.0, scalar2=None, op0=ALU.add),
                   r=[modT], w=[sc1p])
                sel = sbuf(p0, "sel", [2, 128])
                OP("dve", lambda e: e.memset(sel.t[:], 0.0), w=[sel])
                OP("dve", lambda e: e.memset(sel.t[0:1, :], 1.0), w=[sel])
                bct = sbuf(p0, "bct", [128, 1024])
                for dst, j, add1, di in ((g1bc, 2, False, None), (bct, 3, False, 0), (bct, 4, True, 1), (bct, 5, False, 2)):
                    for hf in range(2):
                        pb = bank()
                        col = j * 1024 + hf * 512
                        OP("pe", lambda e, pb=pb, col=col: e.matmul(pb.t[:], lhsT=sel.t[:], rhs=modrow.t[:, col:col + 512],
                                                                    start=True, stop=True), r=[sel, modrow], w=[pb])
                        if add1:
                            OP("dve", lambda e, pb=pb, dst=dst, hf=hf: e.tensor_scalar(
                                out=dst.t[:, hf * 512:(hf + 1) * 512], in0=pb.t[:], scalar1=1.0, scalar2=None, op0=ALU.add),
                               r=[pb], w=[dst])
                        else:
                            OP("act", lambda e, pb=pb, dst=dst, hf=hf: e.activation(
                                out=dst.t[:, hf * 512:(hf + 1) * 512], in_=pb.t[:], func=AF.Identity), r=[pb], w=[dst])
                    if di is not None:
                        S.dma("sp", mbs, lambda e, di=di: e.dma_start(out=d_modbc[di], in_=bct.t[:]), r=[bct])
                S.barrier()

        if mode != "B":
            with ExitStack() as pa:
                xt = [sbuf(pa, "xt%d" % i, [128, 1024]) for i in range(2)]
                win = sbuf(pa, "win", [128, 8, 2336], BF16)
                wout = sbuf(pa, "wout", [128, 8, 1024], BF16)
                with ExitStack() as pw:
                    wst = [sbuf(pw, "wcst%d" % i, [128, 2336]) for i in range(2)]
                    win_v = d_win.rearrange("(k p) n -> k p n", p=128)
                    wout_v = d_wout.rearrange("(k p) n -> k p n", p=128)
                    for kc in range(8):
                        ws = wst[kc % 2]
                        S.dma("sp", ld[kc % 2], lambda e, ws=ws, kc=kc: e.dma_start(out=ws.t[:], in_=win_v[kc]), w=[ws])
                        OP("pool", lambda e, ws=ws, kc=kc: e.tensor_copy(out=win.t[:, kc, :], in_=ws.t[:]), r=[ws], w=[win])
                    for kc in range(8):
                        ws = wst[kc % 2]
                        S.dma("sp", ld[kc % 2], lambda e, ws=ws, kc=kc: e.dma_start(out=ws.t[:, 0:1024], in_=wout_v[kc]), w=[ws])
                        OP("pool", lambda e, ws=ws, kc=kc: e.tensor_copy(out=wout.t[:, kc, :], in_=ws.t[:, 0:1024]), r=[ws], w=[wout])
                    S.barrier()
                w2 = sbuf(pa, "w2", [128, 512])
                OP("pool", lambda e: e.memset(w2.t[:], 0.0), w=[w2])
                gng = sbuf(pa, "gng", [128, 128]); esink = sbuf(pa, "esink", [128, 8])
                ln1g = sbuf(pa, "ln1g", [128, 1024]); ln1b = sbuf(pa, "ln1b", [128, 1024])
                tri = sbuf(pa, "tri", [128, 4, 128]); ci = sbuf(pa, "ci", [128, 2])
                amask = sbuf(pa, "amask", [128, 2, 128]); gmask = sbuf(pa, "gmask", [128, 2, 64])
                amask_e = sbuf(pa, "amask_e", [128, 2, 128])
                S.dma("sp", cst, lambda e: e.dma_start(out=w2.t[0:16, :], in_=d_w2), w=[w2])
                S.dma("sp", cst, lambda e: e.dma_start(out=w2.t[16:17, :], in_=d_bg), w=[w2])
                for b_, d_ in ((gng, d_gng), (esink, d_sink), (ln1g, d_ln1g), (ln1b, d_ln1b),
                               (tri, d_tri), (ci, d_ci), (amask, d_amask), (gmask, d_gmask)):
                    S.dma("sp", cst, lambda e, b_=b_, d_=d_: e.dma_start(out=b_.t[:], in_=d_), w=[b_])
                S.barrier()
                OP("act", lambda e: e.activation(out=esink.t[:], in_=esink.t[:], func=AF.Exp), r=[esink], w=[esink])
                for m in range(2):
                    OP("dve", lambda e, m=m: e.tensor_scalar(out=amask_e.t[:, m, :], in0=amask.t[:, m, :],
                                                             scalar1=flags.t[:, 96 + m:97 + m], scalar2=None, op0=ALU.mult),
                       r=[amask, flags], w=[amask_e])
                sbst = sbuf(pa, "sbst", [128, 32, 256])
                kT_all = sbuf(pa, "kT_all", [64, 18, 256], BF16); v_all = sbuf(pa, "v_all", [128, 18, 130], BF16)
                kTc = sbuf(pa, "kTc", [64, 2, 256], BF16); vc = sbuf(pa, "vc", [128, 2, 130], BF16)
                OP("pool", lambda e: e.memset(v_all.t[:], 1.0), w=[v_all])
                OP("pool", lambda e: e.memset(vc.t[:], 1.0), w=[vc])
                hT2 = [sbuf(pa, "hT%d" % i, [128, 8, 128], BF16) for i in range(2)]
                for h_ in hT2:
                    h_.c = []
                    for _ in range(8):
                        v_ = Buf(h_.t)
                        h_.c.append(v_)
                vsb2 = [sbuf(pa, "vsb%d" % i, [128, 512]) for i in range(2)]
                curb = {"hT": hT2[0], "vsb": vsb2[0]}
                qk = sbuf(pa, "qk", [128, 512])
                zT = [sbuf(pa, "zT", [128, 128])] * 2
                OP("dve", lambda e: e.memset(zT[0].t[:], 0.0), w=[zT[0]])
                OP("dve", lambda e: e.memset(zT[0].t[0:17, :], 1.0), w=[zT[0]])
                sp_ = [sbuf(pa, "sp", [128, 256])] * 2
                et = sbuf(pa, "et", [128, 256])
                Eb = [sbuf(pa, "Eb", [128, 256])] * 2
                Ei = [sbuf(pa, "Ei", [128, 256])] * 2
                Er = [sbuf(pa, "Er", [128, 256])] * 2
                qe = [sbuf(pa, "qe", [128, 256])] * 2
                ke = [sbuf(pa, "ke", [128, 256])] * 2
                kd = [sbuf(pa, "kd", [128, 256])] * 2
                Tz = [sbuf(pa, "Tz%d" % i, [128, 4, 128]) for i in range(2)]
                keT = [sbuf(pa, "keT%d" % i, [128, 2, 128]) for i in range(2)]
                ATz = sbuf(pa, "ATz", [128, 2, 4, 2, 64])
                for b_ in (Tz[0], Tz[1], ATz):
                    OP("pool", lambda e, b_=b_: e.memset(b_.t[:], 0.0), w=[b_])
                asb = [sbuf(pa, "asb", [128, 2, 2])] * 2
                Sst = {0: [sbuf(pa, "Sf%d" % i, [128, 2, 128]) for i in range(2)],
                       1: [sbuf(pa, "Sb%d" % i, [128, 2, 128]) for i in range(2)]}
                Scur = {0: 0, 1: 0}
                Stmp = sbuf(pa, "Stmp", [128, 2, 128])
                for dd in range(2):
                    OP("dve", lambda e, dd=dd: e.memset(Sst[dd][0].t[:], 0.0), w=[Sst[dd][0]])
                rsb = sbuf(pa, "rsb", [128, 512])
                ss = sbuf(pa, "ss", [128, 4]); rstd4 = sbuf(pa, "rstd4", [128, 4])
                on = sbuf(pa, "on", [128, 512]); junk = on
                aqs = on; qrot = sbuf(pa, "qrot", [128, 512]); rt = rsb
                akv = sbuf(pa, "akv", [128, 256]); krot = sbuf(pa, "krot", [128, 128])
                cs = sbuf(pa, "cs", [128, 32]); sn = sbuf(pa, "sn", [128, 32])
                qTs = sbuf(pa, "qTs", [64, 8, 128], BF16)
                Pall = sbuf(pa, "Pall", [128, 5, 512], BF16)
                den = sbuf(pa, "den", [128, 4]); cat = sbuf(pa, "cat", [128, 1024])
                r1 = cat; x1o = cat
                stats = sbuf(pa, "stats", [128, 12]); mv = sbuf(pa, "mv", [128, 2]); rs1 = sbuf(pa, "rs1", [128, 1])

                def front(src_ap, row):
                    xb = xt[xt_i[0] % 2]
                    slot = ld[xt_i[0] % 2]
                    curb["hT"] = hT2[xt_i[0] % 2]; curb["vsb"] = vsb2[xt_i[0] % 2]
                    hT = curb["hT"]
                    xt_i[0] += 1
                    S.dma("sp", slot, lambda e: e.dma_start(out=xb.t[:], in_=src_ap), w=[xb])

                    def evac(c0, n, pb):
                        for c in range(c0, c0 + n):
                            if c0 == 0:
                                OP("act", lambda e, c=c, pb=pb, c0=c0: e.activation(
                                    out=hT.t[:, c, :], in_=pb.t[:, (c - c0) * 128:(c - c0 + 1) * 128], func=AF.Identity,
                                    scale=sc1p.t[:, c, row:row + 1], bias=modT.t[:, c, row:row + 1]),
                                   r=[pb, sc1p, modT], w=[hT.c[c]])
                            else:
                                OP("dve", lambda e, c=c, pb=pb, c0=c0: e.tensor_scalar(
                                    out=hT.t[:, c, :], in0=pb.t[:, (c - c0) * 128:(c - c0 + 1) * 128],
                                    scalar1=sc1p.t[:, c, row:row + 1], scalar2=modT.t[:, c, row:row + 1], op0=ALU.mult, op1=ALU.add),
                                   r=[pb, sc1p, modT], w=[hT.c[c]])
                    transpose_to(xb, 8, evac)
                    return xb

                def inproj(col0, ncols, pb, pcol=0):
                    hT = curb["hT"]
                    for kc in range(8):
                        OP("pe", lambda e, kc=kc: e.matmul(pb.t[:, pcol:pcol + ncols], lhsT=hT.t[:, kc, :],
                                                           rhs=win.t[:, kc, col0:col0 + ncols], start=(kc == 0), stop=(kc == 7)),
                           r=[hT.c[kc], win], w=[pb])

                def gates(dd, flagcol=None):
                    hT = curb["hT"]
                    pz = bank()
                    for kc in range(8):
                        OP("pe", lambda e, kc=kc: e.matmul(pz.t[0:16, 0:128], lhsT=win.t[:, kc, 2304 + 16 * dd:2320 + 16 * dd],
                                                           rhs=hT.t[:, kc, :], start=(kc == 0), stop=(kc == 7)), r=[hT.c[kc], win], w=[pz])
                    OP("act", lambda e: e.activation(out=zT[dd].t[0:16, :], in_=pz.t[0:16, 0:128], func=AF.Identity), r=[pz], w=[zT[dd]])
                    pg = bank()
                    OP("pe", lambda e: e.matmul(pg.t[:, 0:256], lhsT=zT[dd].t[:], rhs=w2.t[:, dd * 256:(dd + 1) * 256],
                                                start=True, stop=True), r=[zT[dd], w2], w=[pg])
                    OP("act", lambda e: e.activation(out=et.t[:], in_=pg.t[:, 0:256], func=AF.Exp, scale=-1.0), r=[pg], w=[et])
                    OP("act", lambda e: e.activation(out=sp_[dd].t[:], in_=et.t[:], func=AF.Ln, bias=1.0), r=[et], w=[sp_[dd]])
                    if flagcol is not None:
                        OP("dve", lambda e: e.tensor_scalar(out=sp_[dd].t[:], in0=sp_[dd].t[:], scalar1=flags.t[:, flagcol:flagcol + 1],
                                                            scalar2=None, op0=ALU.mult), r=[sp_[dd], flags], w=[sp_[dd]])

                def decay_k(dd, ksrc, flagcol=None):
                    pr = bank()
                    OP("pe", lambda e: e.matmul(pr.t[:, 0:256], lhsT=tri.t[:, 1 + 2 * dd, :], rhs=sp_[dd].t[:], start=True, stop=True),
                       r=[tri, sp_[dd]], w=[pr])
                    OP("act", lambda e: e.activation(out=Er[dd].t[:], in_=pr.t[:, 0:256], func=AF.Exp), r=[pr], w=[Er[dd]])
                    if flagcol is None:
                        OP("dve", lambda e: e.tensor_tensor(out=kd[dd].t[:], in0=ksrc.t[:, 256:512], in1=Er[dd].t[:], op=ALU.mult),
                           r=[ksrc, Er[dd]], w=[kd[dd]])
                    else:
                        OP("dve", lambda e: e.scalar_tensor_tensor(out=kd[dd].t[:], in0=ksrc.t[:, 256:512],
                                                                   scalar=flags.t[:, flagcol:flagcol + 1], in1=Er[dd].t[:],
                                                                   op0=ALU.mult, op1=ALU.mult), r=[ksrc, Er[dd], flags], w=[kd[dd]])
                    pa_ = bank()
                    for hp in range(2):
                        OP("pe", lambda e, hp=hp: e.matmul(pa_.t[:, hp * 2:hp * 2 + 2], lhsT=sp_[dd].t[:, hp * 128:(hp + 1) * 128],
                                                           rhs=ci.t[:], start=True, stop=True), r=[sp_[dd], ci], w=[pa_])
                    OP("act", lambda e: e.activation(out=asb[dd].t[:], in_=pa_.t[:, 0:4].rearrange("p (a b) -> p a b", a=2),
                                                     func=AF.Exp), r=[pa_], w=[asb[dd]])

                def state_update(dd, c, vsrc):
                    pp = bank()
                    for h in range(4):
                        hp, par = h // 2, h % 2
                        OP("pe", lambda e, h=h, hp=hp, par=par: e.matmul(
                            pp.t[par * 64:(par + 1) * 64, hp * 128:(hp + 1) * 128],
                            lhsT=kd[dd].t[c * 64:(c + 1) * 64, h * 64:(h + 1) * 64],
                            rhs=vsrc.t[c * 64:(c + 1) * 64, h * 128:(h + 1) * 128], start=True, stop=True),
                           r=[kd[dd], vsrc], w=[pp])
                    so = Sst[dd][Scur[dd]]
                    sn_ = Sst[dd][1 - Scur[dd]]
                    OP("dve", lambda e: e.tensor_tensor(out=Stmp.t[:], in0=so.t[:],
                                                        in1=asb[dd].t[:, :, c:c + 1].broadcast_to([128, 2, 128]), op=ALU.mult),
                       r=[so, asb[dd]], w=[Stmp])
                    OP("dve", lambda e: e.tensor_tensor(out=sn_.t[:], in0=Stmp.t[:],
                                                        in1=pp.t[:, 0:256].rearrange("p (a b) -> p a b", a=2), op=ALU.add),
                       r=[Stmp, pp], w=[sn_])
                    Scur[dd] = 1 - Scur[dd]

                def state_tile(src_ap, row, dd, flagcol=None, store=None):
                    front(src_ap, row)
                    vsb = curb["vsb"]
                    pk = bank()
                    inproj(256, 256, pk, 256)
                    pv = bank()
                    inproj(512, 512, pv)
                    OP("act", lambda e: e.activation(out=vsb.t[:], in_=pv.t[:], func=AF.Identity), r=[pv], w=[vsb])
                    gates(dd, flagcol)
                    decay_k(dd, pk, flagcol)
                    for c in ((0, 1) if dd == 0 else (1, 0)):
                        if store is not None:
                            cur = Sst[dd][Scur[dd]]
                            OP("pool", lambda e, c=c, cur=cur: e.tensor_copy(out=sbst.t[:, store * 2 + c, :],
                                                                             in_=cur.t[:].rearrange("p a b -> p (a b)")),
                               r=[cur], w=[sbst])
                        state_update(dd, c, vsb)

                ksb2 = [sbuf(pa, "ksb%d" % i, [128, 512]) for i in range(2)]
                zT2 = [sbuf(pa, "zTp%d" % i, [128, 128]) for i in range(2)]
                for z_ in zT2:
                    OP("dve", lambda e, z_=z_: e.memset(z_.t[:], 0.0), w=[z_])
                    OP("dve", lambda e, z_=z_: e.memset(z_.t[0:17, :], 1.0), w=[z_])
                st_i = [0]

                def state_A1(n, src_ap, row):
                    front(src_ap, row)
                    hT = curb["hT"]
                    return dict(hT=hT, vsb=vsb2[n % 2], ksb=ksb2[n % 2], zTb=zT2[n % 2])

                def inproj_h(hT, col0, ncols, pb, pcol=0):
                    for kc in range(8):
                        OP("pe", lambda e, kc=kc: e.matmul(pb.t[:, pcol:pcol + ncols], lhsT=hT.t[:, kc, :],
                                                           rhs=win.t[:, kc, col0:col0 + ncols], start=(kc == 0), stop=(kc == 7)),
                           r=[hT.c[kc], win], w=[pb])

                def state_A2(cx):
                    pk = bank()
                    inproj_h(cx["hT"], 256, 256, pk, 256)
                    ksb = cx["ksb"]
                    OP("dve", lambda e: e.tensor_copy(out=ksb.t[:, 256:512], in_=pk.t[:, 256:512]), r=[pk], w=[ksb])

                def state_A3(cx):
                    pv = bank()
                    inproj_h(cx["hT"], 512, 512, pv)
                    vsb = cx["vsb"]
                    OP("act", lambda e: e.activation(out=vsb.t[:], in_=pv.t[:], func=AF.Identity), r=[pv], w=[vsb])

                def state_A4(cx, dd):
                    pz = bank()
                    hT, zTb = cx["hT"], cx["zTb"]
                    for kc in range(8):
                        OP("pe", lambda e, kc=kc: e.matmul(pz.t[0:16, 0:128], lhsT=win.t[:, kc, 2304 + 16 * dd:2320 + 16 * dd],
                                                           rhs=hT.t[:, kc, :], start=(kc == 0), stop=(kc == 7)), r=[hT.c[kc], win], w=[pz])
                    OP("act", lambda e: e.activation(out=zTb.t[0:16, :], in_=pz.t[0:16, 0:128], func=AF.Identity), r=[pz], w=[zTb])

                def state_B1(cx, dd, flagcol):
                    zTb = cx["zTb"]
                    pg = bank()
                    OP("pe", lambda e: e.matmul(pg.t[:, 0:256], lhsT=zTb.t[:], rhs=w2.t[:, dd * 256:(dd + 1) * 256],
                                                start=True, stop=True), r=[zTb, w2], w=[pg])
                    OP("act", lambda e: e.activation(out=et.t[:], in_=pg.t[:, 0:256], func=AF.Exp, scale=-1.0), r=[pg], w=[et])
                    OP("act", lambda e: e.activation(out=sp_[dd].t[:], in_=et.t[:], func=AF.Ln, bias=1.0), r=[et], w=[sp_[dd]])
                    if flagcol is not None:
                        OP("dve", lambda e: e.tensor_scalar(out=sp_[dd].t[:], in0=sp_[dd].t[:], scalar1=flags.t[:, flagcol:flagcol + 1],
                                                            scalar2=None, op0=ALU.mult), r=[sp_[dd], flags], w=[sp_[dd]])

                def state_B2(cx, dd, flagcol):
                    decay_k(dd, cx["ksb"], flagcol)

                def state_B3(cx, dd, store, c):
                    if store is not None:
                        cur = Sst[dd][Scur[dd]]
                        OP("pool", lambda e, c=c, cur=cur: e.tensor_copy(out=sbst.t[:, store * 2 + c, :],
                                                                         in_=cur.t[:].rearrange("p a b -> p (a b)")),
                           r=[cur], w=[sbst])
                    state_update(dd, c, cx["vsb"])

                def run_state_jobs(jobs):
                    N = len(jobs)
                    cxs = {}
                    for step in range(N + 2):
                        if step < N:
                            cxs[step] = state_A1(step, jobs[step][0], jobs[step][1])
                        m, b_ = step - 1, step - 2
                        hm = 0 <= m < N
                        hb = 0 <= b_ < N
                        ORD = int(os.environ.get("K_ORD", "1"))
                        if hb:
                            _, _, ddb, flb, stb = jobs[b_]
                            cs_ = (0, 1) if ddb == 0 else (1, 0)
                        if ORD == 0:
                            seq = ["A2", "A3", "A4", "B1", "B2", "B3", "B4"]
                        elif ORD == 1:
                            seq = ["B1", "A2", "B2", "A3", "B3", "B4", "A4"]
                        elif ORD == 2:
                            seq = ["B1", "A2", "B2", "A3", "A4", "B3", "B4"]
                        else:
                            seq = ["A2", "B1", "A3", "B2", "A4", "B3", "B4"]
                        for st_ in seq:
                            if st_[0] == "A" and hm:
                                if st_ == "A2":
                                    state_A2(cxs[m])
                                    convert_chunk(pace=[cxs[m]["ksb"]])
                                elif st_ == "A3": state_A3(cxs[m])
                                else: state_A4(cxs[m], jobs[m][2])
                            if st_[0] == "B" and hb:
                                if st_ == "B1": state_B1(cxs[b_], ddb, flb)
                                elif st_ == "B2": state_B2(cxs[b_], ddb, flb)
                                elif st_ == "B3": state_B3(cxs[b_], ddb, stb, cs_[0])
                                else: state_B3(cxs[b_], ddb, stb, cs_[1])
                        if hb:
                            del cxs[b_]

                def rope(src, dst, H, tmp):
                    v5 = lambda b: b.t[:, 0:H * 64].rearrange("p (h a f d) -> p h a f d", h=H, a=2, f=2, d=16)
                    cb = cs.t[:].rearrange("p (a d) -> p a d", a=2).unsqueeze(1).broadcast_to([128, H, 2, 16])
                    sb_ = sn.t[:].rearrange("p (a d) -> p a d", a=2).unsqueeze(1).broadcast_to([128, H, 2, 16])
                    x1, x2 = v5(src)[:, :, :, 0, :], v5(src)[:, :, :, 1, :]
                    o1, o2 = v5(dst)[:, :, :, 0, :], v5(dst)[:, :, :, 1, :]
                    t1, t2 = v5(tmp)[:, :, :, 0, :], v5(tmp)[:, :, :, 1, :]
                    OP("pool", lambda e: e.tensor_tensor(out=o1, in0=x1, in1=cb, op=ALU.mult), r=[src, cs], w=[dst])
                    OP("pool", lambda e: e.tensor_tensor(out=t1, in0=x2, in1=sb_, op=ALU.mult), r=[src, sn], w=[tmp])
                    OP("pool", lambda e: e.tensor_tensor(out=o1, in0=o1, in1=t1, op=ALU.subtract), r=[dst, tmp], w=[dst])
                    OP("pool", lambda e: e.tensor_tensor(out=o2, in0=x2, in1=cb, op=ALU.mult), r=[src, cs], w=[dst])
                    OP("pool", lambda e: e.tensor_tensor(out=t2, in0=x1, in1=sb_, op=ALU.mult), r=[src, sn], w=[tmp])
                    OP("pool", lambda e: e.tensor_tensor(out=o2, in0=o2, in1=t2, op=ALU.add), r=[dst, tmp], w=[dst])

                def kv_tile(src_ap, row, j, is_ctx):
                    front(src_ap, row)
                    pb = bank()
                    inproj(2048, 256, pb)
                    OP("act", lambda e: e.activation(out=akv.t[:], in_=pb.t[:, 0:256], func=AF.Identity), r=[pb], w=[akv])
                    if is_ctx:
                        ksrc, kdst, vdst = akv, kTc, vc
                    else:
                        S.dma("sp", csl, lambda e: e.dma_start(out=cs.t[:], in_=d_cos[j * 128:(j + 1) * 128, :]), w=[cs])
                        S.dma("sp", snl, lambda e: e.dma_start(out=sn.t[:], in_=d_sin[j * 128:(j + 1) * 128, :]), w=[sn])
                        rope(akv, krot, 2, rt)
                        ksrc, kdst, vdst = krot, kT_all, v_all
                    OP("pool", lambda e: e.tensor_copy(
                        out=vdst.t[:, j, :].rearrange("p (g d) -> p g d", g=2)[:, :, 0:64],
                        in_=akv.t[:, 128:256].rearrange("p (g d) -> p g d", g=2)), r=[akv], w=[vdst])

                    def evac(c0, n, pb2):
                        OP("act", lambda e: e.activation(out=kdst.t[:, j, :], in_=pb2.t[0:64, 0:256], func=AF.Identity),
                           r=[pb2], w=[kdst])
                    transpose_to(ksrc, 2, evac, width=64)

                def own_tile(i):
                    j = i + 1
                    xb = front(d_xown[j * 128:(j + 1) * 128, :], 0)
                    vsb = curb["vsb"]; catT = curb["hT"]
                    pqk = bank(); inproj(0, 512, pqk)
                    OP("act", lambda e: e.activation(out=qk.t[:], in_=pqk.t[:], func=AF.Identity), r=[pqk], w=[qk])
                    pv = bank(); inproj(512, 512, pv)
                    OP("act", lambda e: e.activation(out=vsb.t[:], in_=pv.t[:], func=AF.Identity), r=[pv], w=[vsb])
                    pr_ = bank(); inproj(1024, 512, pr_)
                    OP("act", lambda e: e.activation(out=rsb.t[:], in_=pr_.t[:], func=AF.Silu), r=[pr_], w=[rsb])
                    for dd in range(2):
                        gates(dd)
                        pbn = bank()
                        OP("pe", lambda e, dd=dd, pbn=pbn: e.matmul(pbn.t[:, 0:256], lhsT=tri.t[:, 2 * dd, :], rhs=sp_[dd].t[:],
                                                                    start=True, stop=True), r=[tri, sp_[dd]], w=[pbn])
                        OP("act", lambda e, dd=dd, pbn=pbn: e.activation(out=Eb[dd].t[:], in_=pbn.t[:, 0:256], func=AF.Exp),
                           r=[pbn], w=[Eb[dd]])
                        OP("act", lambda e, dd=dd, pbn=pbn: e.activation(out=Ei[dd].t[:], in_=pbn.t[:, 0:256], func=AF.Exp, scale=-1.0),
                           r=[pbn], w=[Ei[dd]])
                        OP("dve", lambda e, dd=dd: e.scalar_tensor_tensor(out=qe[dd].t[:], in0=qk.t[:, 0:256], scalar=0.125, in1=Eb[dd].t[:],
                                                                          op0=ALU.mult, op1=ALU.mult), r=[qk, Eb[dd]], w=[qe[dd]])
                        OP("dve", lambda e, dd=dd: e.tensor_tensor(out=ke[dd].t[:], in0=qk.t[:, 256:512], in1=Ei[dd].t[:], op=ALU.mult),
                           r=[qk, Ei[dd]], w=[ke[dd]])
                        if dd == 0:
                            decay_k(dd, qk)
                        pT = bank()
                        for idx, srcb in enumerate((qe[dd], qe[dd], ke[dd], ke[dd])):
                            hp = idx % 2
                            OP("pe", lambda e, idx=idx, hp=hp, srcb=srcb, pT=pT: e.transpose(
                                out=pT.t[:, idx * 128:(idx + 1) * 128], in_=srcb.t[:, hp * 128:(hp + 1) * 128], identity=ident.t[:]),
                               r=[srcb, ident], w=[pT])
                        OP("act", lambda e, dd=dd, pT=pT: e.activation(out=keT[dd].t[:].rearrange("p a b -> p (a b)"), in_=pT.t[:, 256:512],
                                                                       func=AF.Identity), r=[pT], w=[keT[dd]])
                        for par in range(2):
                            OP("act", lambda e, dd=dd, pT=pT, par=par: e.activation(
                                out=Tz[dd].t[par * 64:(par + 1) * 64, par::2, :],
                                in_=pT.t[par * 64:(par + 1) * 64, 0:256].rearrange("p (a b) -> p a b", a=2), func=AF.Identity),
                               r=[pT], w=[Tz[dd]])
                    pAT = bank()
                    for dd in range(2):
                        for c in range(2):
                            for h in range(4):
                                hp = h // 2
                                OP("pe", lambda e, dd=dd, c=c, h=h, hp=hp: e.matmul(
                                    pAT.t[c * 64:(c + 1) * 64, (dd * 4 + h) * 64:(dd * 4 + h + 1) * 64],
                                    lhsT=keT[dd].t[:, hp, c * 64:(c + 1) * 64],
                                    rhs=Tz[dd].t[:, h, c * 64:(c + 1) * 64], start=True, stop=True),
                                   r=[keT[dd], Tz[dd]], w=[pAT])
                    for c in range(2):
                        OP("dve", lambda e, c=c: e.tensor_tensor(
                            out=ATz.t[c * 64:(c + 1) * 64, :, :, c, :],
                            in0=pAT.t[c * 64:(c + 1) * 64, :].rearrange("p (d h c) -> p d h c", d=2, h=4),
                            in1=gmask.t[c * 64:(c + 1) * 64, :, :].unsqueeze(2).broadcast_to([64, 2, 4, 64]), op=ALU.mult),
                           r=[pAT, gmask], w=[ATz])
                    po = bank()
                    for c in range(2):
                        sf = Sst[0][Scur[0]]
                        for h in range(4):
                            hp = h // 2
                            outp = po.t[c * 64:(c + 1) * 64, h * 128:(h + 1) * 128]
                            vv = vsb.t[:, h * 128:(h + 1) * 128]
                            OP("pe", lambda e, c=c, h=h, outp=outp, vv=vv: e.matmul(
                                outp, lhsT=ATz.t[:, 0, h, c, :], rhs=vv, start=True, stop=False), r=[ATz, vsb], w=[po])
                            OP("pe", lambda e, c=c, h=h, hp=hp, outp=outp, sf=sf: e.matmul(
                                outp, lhsT=Tz[0].t[:, h, c * 64:(c + 1) * 64], rhs=sf.t[:, hp, :], start=False, stop=False),
                               r=[Tz[0], sf], w=[po])
                            OP("pe", lambda e, c=c, h=h, outp=outp, vv=vv: e.matmul(
                                outp, lhsT=ATz.t[:, 1, h, c, :], rhs=vv, start=False, stop=False), r=[ATz, vsb], w=[po])
                            OP("pe", lambda e, c=c, h=h, hp=hp, outp=outp: e.matmul(
                                outp, lhsT=Tz[1].t[:, h, c * 64:(c + 1) * 64],
                                rhs=sbst.t[:, i * 2 + c, hp * 128:(hp + 1) * 128], start=False, stop=True), r=[Tz[1], sbst], w=[po])
                        state_update(0, c, vsb)
                    for h in range(4):
                        OP("act", lambda e, h=h: e.activation(out=junk.t[:, h * 128:(h + 1) * 128], in_=po.t[:, h * 128:(h + 1) * 128],
                                                              func=AF.Square, accum_out=ss.t[:, h:h + 1]), r=[po], w=[junk, ss])
                    OP("dve", lambda e: e.tensor_scalar(out=rstd4.t[:], in0=ss.t[:], scalar1=1.0 / 128, scalar2=eps_t.t[:, 0:1],
                                                        op0=ALU.mult, op1=ALU.add), r=[ss, eps_t], w=[rstd4])
                    OP("act", lambda e: e.activation(out=rstd4.t[:], in_=rstd4.t[:], func=AF.Sqrt), r=[rstd4], w=[rstd4])
                    OP("dve", lambda e: e.reciprocal(out=rstd4.t[:], in_=rstd4.t[:]), r=[rstd4], w=[rstd4])
                    on3 = on.t[:].rearrange("p (h d) -> p h d", h=4)
                    OP("dve", lambda e: e.tensor_tensor(out=on3, in0=po.t[:].rearrange("p (h d) -> p h d", h=4),
                                                        in1=rstd4.t[:].unsqueeze(2).broadcast_to([128, 4, 128]), op=ALU.mult),
                       r=[po, rstd4], w=[on])
                    OP("pool", lambda e: e.tensor_tensor(out=on3, in0=on3, in1=gng.t[:].unsqueeze(1).broadcast_to([128, 4, 128]),
                                                         op=ALU.mult), r=[on, gng], w=[on])
                    OP("pool", lambda e: e.tensor_tensor(out=cat.t[:, 0:512], in0=on.t[:], in1=rsb.t[:], op=ALU.mult),
                       r=[on, rsb], w=[cat])
                    paq = bank(); inproj(1536, 512, paq)
                    OP("act", lambda e: e.activation(out=aqs.t[:], in_=paq.t[:], func=AF.Identity), r=[paq], w=[aqs])
                    S.dma("sp", csl, lambda e: e.dma_start(out=cs.t[:], in_=d_cos[j * 128:(j + 1) * 128, :]), w=[cs])
                    S.dma("sp", snl, lambda e: e.dma_start(out=sn.t[:], in_=d_sin[j * 128:(j + 1) * 128, :]), w=[sn])
                    rope(aqs, qrot, 8, rt)

                    def evq(c0, n, pb2):
                        OP("act", lambda e: e.activation(out=qTs.t[:, c0:c0 + n, :].rearrange("p a b -> p (a b)"),
                                                         in_=pb2.t[0:64, 0:n * 128], func=AF.Identity, scale=0.125), r=[pb2], w=[qTs])
                    transpose_to(qrot, 8, evq, width=64)
                    def att_group(g):
                        kts = [(kT_all, v_all, j - 1, amask_e if i == 0 else amask, 0), (kT_all, v_all, j, None, 0),
                               (kT_all, v_all, j + 1, amask_e if i == 15 else amask, 1), (kTc, vc, 0, None, 0), (kTc, vc, 1, None, 0)]
                        for n_, (kb, vb, jj, mk, mi) in enumerate(kts):
                            pst = bank()
                            OP("pe", lambda e, kb=kb, jj=jj, pst=pst: e.matmul(
                                pst.t[:], lhsT=kb.t[:, jj, g * 128:(g + 1) * 128],
                                rhs=qTs.t[:, 4 * g:4 * g + 4, :].rearrange("p a b -> p (a b)"), start=True, stop=True),
                               r=[kb, qTs], w=[pst])
                            OP("act", lambda e, n_=n_, pst=pst: e.activation(out=Pall.t[:, n_, :], in_=pst.t[:], func=AF.Exp),
                               r=[pst], w=[Pall])
                            if mk is not None:
                                OP("dve", lambda e, n_=n_, mk=mk, mi=mi: e.tensor_tensor(
                                    out=Pall.t[:, n_, :].rearrange("p (h q) -> p h q", h=4),
                                    in0=Pall.t[:, n_, :].rearrange("p (h q) -> p h q", h=4),
                                    in1=mk.t[:, mi:mi + 1, :].broadcast_to([128, 4, 128]), op=ALU.mult), r=[Pall, mk], w=[Pall])
                        pO = bank()
                        for hh in range(4):
                            for n_, (kb, vb, jj, mk, mi) in enumerate(kts):
                                OP("pe", lambda e, hh=hh, n_=n_, vb=vb, jj=jj: e.matmul(
                                    pO.t[:, hh * 65:(hh + 1) * 65], lhsT=Pall.t[:, n_, hh * 128:(hh + 1) * 128],
                                    rhs=vb.t[:, jj, g * 65:(g + 1) * 65], start=(n_ == 0), stop=(n_ == 4)), r=[Pall, vb], w=[pO])
                        pO3 = pO.t[:, 0:260].rearrange("p (h d) -> p h d", h=4)
                        OP("dve", lambda e, pO3=pO3: e.tensor_tensor(out=den.t[:].unsqueeze(2), in0=pO3[:, :, 64:65],
                                                                     in1=esink.t[:, 4 * g:4 * g + 4].unsqueeze(2), op=ALU.add),
                           r=[pO, esink], w=[den])
                        OP("dve", lambda e: e.reciprocal(out=den.t[:], in_=den.t[:]), r=[den], w=[den])
                        OP("dve", lambda e, pO3=pO3: e.tensor_tensor(
                            out=cat.t[:, 512 + g * 256:512 + (g + 1) * 256].rearrange("p (h d) -> p h d", h=4),
                            in0=pO3[:, :, 0:64], in1=den.t[:].unsqueeze(2).broadcast_to([128, 4, 64]), op=ALU.mult),
                           r=[pO, den], w=[cat])
                    for g_ in range(2):
                        att_group(g_)
                    if os.environ.get("K_DBG") == "cat":
                        S.dma("pool", sts, lambda e: e.dma_start(out=d_x1[i * 128:(i + 1) * 128, :], in_=cat.t[:]), r=[cat])
                        return
                    def evc(c0, n, pb2):
                        OP("act", lambda e: e.activation(out=catT.t[:, c0:c0 + n, :].rearrange("p a b -> p (a b)"),
                                                         in_=pb2.t[:, 0:n * 128], func=AF.Identity), r=[pb2], w=catT.c[c0:c0 + n])
                    transpose_to(cat, 8, evc)
                    for hf in range(2):
                        py = bank()
                        for kc in range(8):
                            OP("pe", lambda e, kc=kc, py=py, hf=hf: e.matmul(py.t[:], lhsT=catT.t[:, kc, :],
                                                                      rhs=wout.t[:, kc, hf * 512:(hf + 1) * 512],
                                                                      start=(kc == 0), stop=(kc == 7)), r=[catT.c[kc], wout], w=[py])
                        OP("dve", lambda e, py=py, hf=hf: e.tensor_tensor(out=r1.t[:, hf * 512:(hf + 1) * 512], in0=py.t[:],
                                                                   in1=g1bc.t[:, hf * 512:(hf + 1) * 512], op=ALU.mult),
                           r=[py, g1bc], w=[r1])
                    OP("dve", lambda e: e.scalar_tensor_tensor(out=r1.t[:], in0=xb.t[:], scalar=ALPHA, in1=r1.t[:],
                                                               op0=ALU.mult, op1=ALU.add), r=[xb, r1], w=[r1])
                    layernorm(r1, x1o, ln1g, ln1b, stats, mv, rs1)
                    S.dma("pool", sts, lambda e: e.dma_start(out=d_x1[i * 128:(i + 1) * 128, :], in_=x1o.t[:]), r=[x1o])

                def layernorm(src, dst, gb, bb, stats, mv, rs1):
                    for hf in range(2):
                        OP("dve", lambda e, hf=hf: e.bn_stats(out=stats.t[:, hf * 6:(hf + 1) * 6], in_=src.t[:, hf * 512:(hf + 1) * 512]),
                           r=[src], w=[stats])
                    OP("dve", lambda e: e.bn_aggr(out=mv.t[:], in_=stats.t[:]), r=[stats], w=[mv])
                    OP("dve", lambda e: e.tensor_scalar(out=rs1.t[:], in0=mv.t[:, 1:2], scalar1=eps_t.t[:, 0:1], scalar2=None, op0=ALU.add),
                       r=[mv, eps_t], w=[rs1])
                    OP("act", lambda e: e.activation(out=rs1.t[:], in_=rs1.t[:], func=AF.Sqrt), r=[rs1], w=[rs1])
                    OP("dve", lambda e: e.reciprocal(out=rs1.t[:], in_=rs1.t[:]), r=[rs1], w=[rs1])
                    OP("dve", lambda e: e.tensor_scalar(out=dst.t[:], in0=src.t[:], scalar1=mv.t[:, 0:1], scalar2=rs1.t[:, 0:1],
                                                        op0=ALU.subtract, op1=ALU.mult), r=[src, mv, rs1], w=[dst])
                    OP("pool", lambda e: e.tensor_tensor(out=dst.t[:], in0=dst.t[:], in1=gb.t[:], op=ALU.mult), r=[dst, gb], w=[dst])
                    OP("pool", lambda e: e.tensor_tensor(out=dst.t[:], in0=dst.t[:], in1=bb.t[:], op=ALU.add), r=[dst, bb], w=[dst])

                for t in range(2):
                    kv_tile(d_ctx[t * 128:(t + 1) * 128, :], 1, t, True)
                jobs = []
                for t in range(2):
                    jobs.append((d_ctx[t * 128:(t + 1) * 128, :], 1, 0, None, None))
                for t in (1, 0):
                    jobs.append((d_ctx[t * 128:(t + 1) * 128, :], 1, 1, None, None))
                for t in range(NPRE):
                    jobs.append((d_xpf[t * 128:(t + 1) * 128, :], 0, 0, t, None))
                for t in range(NPRE):
                    jobs.append((d_xpb[t * 128:(t + 1) * 128, :], 0, 1, 48 + t, None))
                for i in range(15, -1, -1):
                    jobs.append((d_xown[(i + 1) * 128:(i + 2) * 128, :], 0, 1, None, i))
                run_state_jobs(jobs)
                convert_tables()
                for j in range(18):
                    kv_tile(d_xown[j * 128:(j + 1) * 128, :], 0, j, False)
                for i in range(16):
                    own_tile(i)
                S.barrier()

        if mode != "A":
            with ExitStack() as pbs_:
                wq = sbuf(pbs_, "wq", [128, 8, 1024]); skT = sbuf(pbs_, "skT", [128, 8, 128])
                ln2g = sbuf(pbs_, "ln2g", [128, 1024]); ln2b = sbuf(pbs_, "ln2b", [128, 1024])
                iota = sbuf(pbs_, "iota", [128, 16])
                S.dma("sp", cst, lambda e: e.dma_start(out=wq.t[:], in_=d_wq.rearrange("(k p) n -> p k n", p=128)), w=[wq])
                for b_, d_ in ((skT, d_skT), (ln2g, d_ln2g), (ln2b, d_ln2b), (iota, d_iota)):
                    S.dma("sp", cst, lambda e, b_=b_, d_=d_: e.dma_start(out=b_.t[:], in_=d_), w=[b_])
                S.barrier()
                if mode == "B":
                    convert_tables()
                identb = sbuf(pbs_, "identb", [128, 128], BF16)
                OP("dve", lambda e: e.tensor_copy(out=identb.t[:], in_=ident.t[:]), r=[ident], w=[identb])
                dg = [sbuf(pbs_, "dg%d" % i, [128, 128], BF16) for i in range(4)]
                sc2bc = sbuf(pbs_, "sc2bc", [128, 1024]); sh2bc = sbuf(pbs_, "sh2bc", [128, 1024]); g2bc = sbuf(pbs_, "g2bc", [128, 1024])
                if mode == "B":
                    OP("dve", lambda e: e.memset(sc2bc.t[:], 1.0), w=[sc2bc])
                    OP("dve", lambda e: e.memset(sh2bc.t[:], 0.0), w=[sh2bc])
                    OP("dve", lambda e: e.memset(g2bc.t[:], 1.0), w=[g2bc])
                else:
                    for b_, di in ((sh2bc, 0), (sc2bc, 1), (g2bc, 2)):
                        S.dma("sp", cst, lambda e, b_=b_, di=di: e.dma_start(out=b_.t[:], in_=d_modbc[di]), w=[b_])
                    S.barrier()
                x1t = [sbuf(pbs_, "x1t%d" % i, [128, 1024]) for i in range(3)]
                h2 = [sbuf(pbs_, "h2_%d" % i, [128, 1024]) for i in range(2)]
                h2T = sbuf(pbs_, "h2T", [128, 8, 128]); qTp = sbuf(pbs_, "qTp", [128, 8, 128])
                ssb = sbuf(pbs_, "ssb", [128, 16, 128]); sw = sbuf(pbs_, "sw", [128, 128])
                m16 = sbuf(pbs_, "m16", [128, 16, 16]); ix16 = sbuf(pbs_, "ix16", [128, 16, 16], U32)
                ixf = sbuf(pbs_, "ixf", [128, 16, 16])
                cand = sbuf(pbs_, "cand", [128, 8, 256]); cw = sbuf(pbs_, "cw", [128, 256])
                tsv = sbuf(pbs_, "tsv", [128, 8, 16]); pos = sbuf(pbs_, "pos", [128, 8, 16], U32)
                posf = sbuf(pbs_, "posf", [128, 8, 16])
                lohi = sbuf(pbs_, "lohi", [128, 2, 16])
                paf = sbuf(pbs_, "paf", [128, 8, 16]); pbf = sbuf(pbs_, "pbf", [128, 8, 16])
                oh = sbuf(pbs_, "oh", [128, 8, 16, 16])
                i1s = sbuf(pbs_, "i1s", [128, 8, 16]); i2s = sbuf(pbs_, "i2s", [128, 8, 16])
                eidf = sbuf(pbs_, "eidf", [128, 128])
                eidx = [sbuf(pbs_, "eidx%d" % i, [128, 128], U32) for i in range(3)]
                gate = [sbuf(pbs_, "gate%d" % i, [128, 8, 16]) for i in range(2)]
                gsum = sbuf(pbs_, "gsum", [128, 8])
                dots = [sbuf(pbs_, "dots%d" % i, [128, 128]) for i in range(2)]
                coef = [sbuf(pbs_, "coef%d" % i, [128, 128]) for i in range(2)]
                gb = [sbuf(pbs_, "gb%d" % i, [128, 2048], BF16) for i in range(NG)]
                gs = [S.dma_slot("g%d" % i) for i in range(NG)]
                prod = [sbuf(pbs_, "prod%d" % i, [128, 1024], BF16) for i in range(3)]
                h2b = [sbuf(pbs_, "h2b%d" % i, [128, 1024], BF16) for i in range(2)]
                acc = [sbuf(pbs_, "acc%d" % i, [128, 1024]) for i in range(2)]
                stats2 = sbuf(pbs_, "stats2", [128, 12]); mv2 = sbuf(pbs_, "mv2", [128, 2]); rs2 = sbuf(pbs_, "rs2", [128, 1])
                x1ld = [S.dma_slot("x1l%d" % i) for i in range(3)]

                def layernorm2(src, dst):
                    for hf in range(2):
                        OP("dve", lambda e, hf=hf: e.bn_stats(out=stats2.t[:, hf * 6:(hf + 1) * 6], in_=src.t[:, hf * 512:(hf + 1) * 512]),
                           r=[src], w=[stats2])
                    OP("dve", lambda e: e.bn_aggr(out=mv2.t[:], in_=stats2.t[:]), r=[stats2], w=[mv2])
                    OP("dve", lambda e: e.tensor_scalar(out=rs2.t[:], in0=mv2.t[:, 1:2], scalar1=eps_t.t[:, 0:1], scalar2=None, op0=ALU.add),
                       r=[mv2, eps_t], w=[rs2])
                    OP("act", lambda e: e.activation(out=rs2.t[:], in_=rs2.t[:], func=AF.Sqrt), r=[rs2], w=[rs2])
                    OP("dve", lambda e: e.reciprocal(out=rs2.t[:], in_=rs2.t[:]), r=[rs2], w=[rs2])
                    OP("dve", lambda e: e.tensor_scalar(out=dst.t[:], in0=src.t[:], scalar1=mv2.t[:, 0:1], scalar2=rs2.t[:, 0:1],
                                                        op0=ALU.subtract, op1=ALU.mult), r=[src, mv2, rs2], w=[dst])
                    OP("dve", lambda e: e.tensor_tensor(out=dst.t[:], in0=dst.t[:], in1=ln2g.t[:], op=ALU.mult), r=[dst, ln2g], w=[dst])
                    OP("dve", lambda e: e.tensor_tensor(out=dst.t[:], in0=dst.t[:], in1=ln2b.t[:], op=ALU.add), r=[dst, ln2b], w=[dst])

                def top16(src3, n, work, mdst, idst):
                    (sa, sbuf_), (wa, wbuf), (ma, mbuf), (ia, ibuf) = src3, work, mdst, idst
                    OP("dve", lambda e: e.max(out=ma[:, 0:8], in_=sa), r=[sbuf_], w=[mbuf])
                    OP("dve", lambda e: e.max_index(out=ia[:, 0:8], in_max=ma[:, 0:8], in_values=sa), r=[sbuf_, mbuf], w=[ibuf])
                    OP("dve", lambda e: e.match_replace(out=wa, in_to_replace=ma[:, 0:8], in_values=sa, imm_value=-1e30),
                       r=[sbuf_, mbuf], w=[wbuf])
                    OP("dve", lambda e: e.max(out=ma[:, 8:16], in_=wa), r=[wbuf], w=[mbuf])
                    OP("dve", lambda e: e.max_index(out=ia[:, 8:16], in_max=ma[:, 8:16], in_values=wa), r=[wbuf, mbuf], w=[ibuf])

                def route(i):
                    xs = x1t[i % 3]
                    S.dma("sp", x1ld[i % 3], lambda e: e.dma_start(out=xs.t[:], in_=d_x1[i * 128:(i + 1) * 128, :]), w=[xs])
                    hh = h2[i % 2]
                    OP("dve", lambda e: e.tensor_tensor(out=hh.t[:], in0=xs.t[:], in1=sc2bc.t[:], op=ALU.mult), r=[xs, sc2bc], w=[hh])
                    OP("dve", lambda e: e.tensor_tensor(out=hh.t[:], in0=hh.t[:], in1=sh2bc.t[:], op=ALU.add), r=[hh, sh2bc], w=[hh])
                    OP("act", lambda e: e.activation(out=h2b[i % 2].t[:], in_=hh.t[:], func=AF.Identity), r=[hh], w=[h2b[i % 2]])

                    if KSTOP < 2: return
                    def ev(c0, n, pb2):
                        OP("act", lambda e: e.activation(out=h2T.t[:, c0:c0 + n, :].rearrange("p a b -> p (a b)"),
                                                         in_=pb2.t[:, 0:n * 128], func=AF.Identity), r=[pb2], w=[h2T])
                    yield
                    transpose_to(hh, 8, ev)
                    yield
                    if KSTOP < 2.3: return
                    for j0 in range(0, 8, 4):
                        pq = bank()
                        for jj in range(j0, j0 + 4):
                            for kc in range(8):
                                OP("pe", lambda e, jj=jj, kc=kc, pq=pq, j0=j0: e.matmul(
                                    pq.t[:, (jj - j0) * 128:(jj - j0 + 1) * 128], lhsT=wq.t[:, kc, jj * 128:(jj + 1) * 128],
                                    rhs=h2T.t[:, kc, :], start=(kc == 0), stop=(kc == 7)), r=[wq, h2T], w=[pq])
                            yield
                        OP("act", lambda e, pq=pq, j0=j0: e.activation(out=qTp.t[:, j0:j0 + 4, :].rearrange("p a b -> p (a b)"),
                                                                       in_=pq.t[:], func=AF.Identity), r=[pq], w=[qTp])
                    if KSTOP < 2.6: return
                    for h0 in range(0, 8, 4):
                        for a in range(2):
                            psc = bank()
                            for h in range(h0, h0 + 4):
                                OP("pe", lambda e, h=h, a=a, psc=psc, h0=h0: e.matmul(
                                    psc.t[:, (h - h0) * 128:(h - h0 + 1) * 128], lhsT=qTp.t[a * 64:(a + 1) * 64, h, :],
                                    rhs=skT.t[a * 64:(a + 1) * 64, h, :], start=True, stop=True), r=[qTp, skT], w=[psc])
                            OP("act", lambda e, psc=psc, h0=h0, a=a: e.activation(
                                out=ssb.t[:, 2 * h0 + a:2 * h0 + 8:2, :], in_=psc.t[:].rearrange("p (a b) -> p a b", a=4),
                                func=AF.Identity), r=[psc], w=[ssb])
                            yield
                    if KSTOP < 3: return
                    for q_ in range(16):
                        top16((ssb.t[:, q_, :], ssb), 128, (sw.t[:], sw), (m16.t[:, q_, :], m16), (ix16.t[:, q_, :], ix16))
                        yield
                    if KSTOP < 4: return
                    OP("dve", lambda e: e.tensor_copy(out=ixf.t[:], in_=ix16.t[:]), r=[ix16], w=[ixf])
                    m4 = m16.t[:].rearrange("p (h a) k -> p h a k", a=2)
                    OP("dve", lambda e: e.tensor_tensor(
                        out=cand.t[:].rearrange("p h (a b) -> p h a b", a=16),
                        in0=m4[:, :, 0, :].unsqueeze(3).broadcast_to([128, 8, 16, 16]),
                        in1=m4[:, :, 1, :].unsqueeze(2).broadcast_to([128, 8, 16, 16]), op=ALU.add), r=[m16], w=[cand])
                    for h in range(8):
                        top16((cand.t[:, h, :], cand), 256, (cw.t[:], cw), (tsv.t[:, h, :], tsv), (pos.t[:, h, :], pos))
                        yield
                    gt = gate[i % 2]
                    OP("dve", lambda e: e.tensor_tensor(out=gt.t[:], in0=tsv.t[:], in1=tsv.t[:, :, 0:1].broadcast_to([128, 8, 16]),
                                                        op=ALU.subtract), r=[tsv], w=[gt])
                    OP("act", lambda e: e.activation(out=gt.t[:], in_=gt.t[:], func=AF.Exp), r=[gt], w=[gt])
                    OP("dve", lambda e: e.tensor_reduce(out=gsum.t[:], in_=gt.t[:], axis=AX.X, op=ALU.add), r=[gt], w=[gsum])
                    OP("dve", lambda e: e.reciprocal(out=gsum.t[:], in_=gsum.t[:]), r=[gsum], w=[gsum])
                    OP("dve", lambda e: e.tensor_tensor(out=gt.t[:], in0=gt.t[:], in1=gsum.t[:].unsqueeze(2).broadcast_to([128, 8, 16]),
                                                        op=ALU.mult), r=[gt, gsum], w=[gt])
                    if KSTOP < 5: return
                    yield
                    OP("dve", lambda e: e.tensor_copy(out=posf.t[:], in_=pos.t[:]), r=[pos], w=[posf])
                    oh2 = Buf(None); oh2.r = ssb.r
                    oh2.t = ssb.t[:].rearrange("p a b -> p (a b)").rearrange("p (h j c) -> p h j c", h=8, j=16)
                    ix4 = ixf.t[:].rearrange("p (h a) k -> p h a k", a=2)
                    bc4 = lambda ap2: ap2.unsqueeze(1).unsqueeze(1).broadcast_to([128, 8, 16, 16])
                    pf4 = posf.t[:].unsqueeze(3).broadcast_to([128, 8, 16, 16])
                    OP("dve", lambda e: e.tensor_tensor(out=oh.t[:], in0=pf4, in1=bc4(lohi.t[:, 0, :]), op=ALU.is_ge), r=[posf, lohi], w=[oh])
                    OP("dve", lambda e: e.tensor_tensor(out=oh2.t[:], in0=pf4, in1=bc4(lohi.t[:, 1, :]), op=ALU.is_ge), r=[posf, lohi], w=[oh2])
                    OP("dve", lambda e: e.tensor_tensor(out=oh.t[:], in0=oh.t[:], in1=oh2.t[:], op=ALU.subtract), r=[oh, oh2], w=[oh])
                    OP("dve", lambda e: e.tensor_tensor(out=oh2.t[:], in0=oh.t[:], in1=bc4(lohi.t[:, 0, :]), op=ALU.mult), r=[oh, lohi], w=[oh2])
                    yield
                    OP("dve", lambda e: e.tensor_reduce(out=paf.t[:], in_=oh2.t[:], axis=AX.X, op=ALU.add), r=[oh2], w=[paf])
                    OP("dve", lambda e: e.tensor_tensor(out=oh.t[:], in0=oh.t[:],
                                                        in1=ix4[:, :, 0, :].unsqueeze(2).broadcast_to([128, 8, 16, 16]), op=ALU.mult),
                       r=[oh, ixf], w=[oh])
                    OP("dve", lambda e: e.tensor_reduce(out=i1s.t[:], in_=oh.t[:], axis=AX.X, op=ALU.add), r=[oh], w=[i1s])
                    yield
                    OP("dve", lambda e: e.tensor_tensor(out=pbf.t[:], in0=posf.t[:], in1=paf.t[:], op=ALU.subtract), r=[posf, paf], w=[pbf])
                    OP("dve", lambda e: e.tensor_tensor(out=oh.t[:], in0=bc4(iota.t[:]),
                                                        in1=pbf.t[:].unsqueeze(3).broadcast_to([128, 8, 16, 16]), op=ALU.is_equal),
                       r=[iota, pbf], w=[oh])
                    OP("dve", lambda e: e.tensor_tensor(out=oh.t[:], in0=oh.t[:],
                                                        in1=ix4[:, :, 1, :].unsqueeze(2).broadcast_to([128, 8, 16, 16]), op=ALU.mult),
                       r=[oh, ixf], w=[oh])
                    OP("dve", lambda e: e.tensor_reduce(out=i2s.t[:], in_=oh.t[:], axis=AX.X, op=ALU.add), r=[oh], w=[i2s])
                    OP("dve", lambda e: e.scalar_tensor_tensor(out=eidf.t[:].rearrange("p (h k) -> p h k", h=8), in0=i1s.t[:], scalar=128.0,
                                                               in1=i2s.t[:], op0=ALU.mult, op1=ALU.add), r=[i1s, i2s], w=[eidf])
                    OP("dve", lambda e: e.tensor_copy(out=eidx[i % 3].t[:], in_=eidf.t[:]), r=[eidf], w=[eidx[i % 3]])

                gi = [0]

                class V:
                    def __init__(self, t):
                        self.t = t
                        self.r = Res()
                dcol = [[V(dots[p_].t) for _ in range(128)] for p_ in range(2)]
                ccol = [[V(coef[p_].t) for _ in range(128)] for p_ in range(2)]

                gk = {}

                def slot_u(i, s):
                    k = gi[0] % NG
                    gi[0] += 1
                    gk[(i, s)] = k
                    S.dma("pool", gs[k], lambda e: e.indirect_dma_start(
                        out=gb[k].t[:], out_offset=None, in_=d_puv16,
                        in_offset=bass.IndirectOffsetOnAxis(ap=eidx[i % 3].t[:, s:s + 1], axis=0)), r=[eidx[i % 3]] + puvB, w=[gb[k]])
                    pr = prod[s % 3]
                    dc, cc = dcol[i % 2][s], ccol[i % 2][s]
                    OP("dve", lambda e: e.tensor_tensor(out=pr.t[:], in0=gb[k].t[:, 0:1024], in1=h2b[i % 2].t[:], op=ALU.mult),
                       r=[gb[k], h2b[i % 2]], w=[pr])
                    OP("act", lambda e: e.activation(out=pr.t[:], in_=pr.t[:], func=AF.Identity, accum_out=dc.t[:, s:s + 1]),
                       r=[pr], w=[pr, dc])
                    OP("act", lambda e: e.activation(out=cc.t[:, s:s + 1], in_=dc.t[:, s:s + 1], func=AF.Gelu), r=[dc], w=[cc])

                def slot_v(i, s):
                    k = gk.pop((i, s))
                    cc = ccol[i % 2][s]
                    dgk = dg[s % 4]
                    OP("dve", lambda e: e.tensor_scalar(out=dgk.t[:], in0=identb.t[:], scalar1=cc.t[:, s:s + 1],
                                                        scalar2=gate[i % 2].t[:, s // 16, s % 16:s % 16 + 1], op0=ALU.mult, op1=ALU.mult),
                       r=[identb, cc, gate[i % 2]], w=[dgk])
                    for hf in range(2):
                        OP("pe", lambda e, hf=hf: e.matmul(accP[hf].t[:], lhsT=dgk.t[:], rhs=gb[k].t[:, 1024 + hf * 512:1536 + hf * 512],
                                                           start=(s == 0), stop=(s == 127)), r=[dgk, gb[k]], w=[accP[hf]])

                def finish_v(i):
                    r2 = acc[i % 2]; yo = acc[i % 2]
                    for hf in range(2):
                        OP("dve", lambda e, hf=hf: e.tensor_tensor(out=r2.t[:, hf * 512:(hf + 1) * 512], in0=accP[hf].t[:],
                                                                   in1=g2bc.t[:, hf * 512:(hf + 1) * 512], op=ALU.mult),
                           r=[accP[hf], g2bc], w=[r2])
                    OP("dve", lambda e: e.scalar_tensor_tensor(out=r2.t[:], in0=x1t[i % 3].t[:], scalar=ALPHA, in1=r2.t[:],
                                                               op0=ALU.mult, op1=ALU.add), r=[x1t[i % 3], r2], w=[r2])
                    layernorm2(r2, yo)
                    S.dma("sp", outs, lambda e: e.dma_start(out=d_out[i * 128:(i + 1) * 128, :], in_=yo.t[:]), r=[yo])

                OP("dve", lambda e: e.tensor_scalar(out=lohi.t[:, 0, :], in0=iota.t[:], scalar1=16.0, scalar2=None, op0=ALU.mult), r=[iota], w=[lohi])
                OP("dve", lambda e: e.tensor_scalar(out=lohi.t[:, 1, :], in0=iota.t[:], scalar1=16.0, scalar2=16.0, op0=ALU.mult, op1=ALU.add),
                   r=[iota], w=[lohi])
                NT = int(os.environ.get('K_NT', '16'))
                KSTOP = float(os.environ.get('K_STOP', '9'))
                for _ in route(0):
                    pass
                for i in range(NT):
                    gen = route(i + 1) if i + 1 < NT else iter(())
                    for s in range(128 + LAG if KSTOP >= 6 else 0):
                        if s < 128:
                            slot_u(i, s)
                        if s >= LAG:
                            slot_v(i, s - LAG)
                        next(gen, None)
                    for _ in gen:
                        pass
                    if KSTOP >= 7:
                        finish_v(i)
                S.barrier()
        S.barrier()
        S.run()
    return nc


def _consts():
    s = np.arange(128)[:, None]
    t = np.arange(128)[None, :]
    same = (s // 64) == (t // 64)
    g = -1.0 / 16.0
    tri = np.stack([(same & (s <= t)), (same & (s > t)), (same & (s >= t)), (same & (s < t))], axis=1).astype(np.float32) * g
    ci = np.stack([(np.arange(128) // 64 == c) for c in range(2)], axis=1).astype(np.float32) * g
    amask = np.stack([(s >= t), (s <= t)], axis=1).astype(np.float32)
    sc = (np.arange(128) % 64)[:, None]
    cc = np.arange(64)[None, :]
    gmask = np.stack([(sc <= cc), (sc >= cc)], axis=1).astype(np.float32)
    iota = np.broadcast_to(np.arange(16, dtype=np.float32), (128, 16)).copy()
    return dict(ident=np.eye(128, dtype=np.float32), tri=np.ascontiguousarray(tri), ci=np.ascontiguousarray(ci),
                amask=np.ascontiguousarray(amask), gmask=np.ascontiguousarray(gmask), iota16=iota)


def _rope_tables():
    rows = 8192 // 64
    row = np.repeat(np.arange(rows, dtype=np.float32), 64)
    col = np.tile(np.arange(64, dtype=np.float32), rows)
    inv = (np.float32(10000.0) ** (-np.arange(16, dtype=np.float32) / np.float32(16))).astype(np.float32)
    ang = np.stack([row[:, None] * inv, col[:, None] * inv], axis=1).astype(np.float32)
    return np.cos(ang).reshape(8192, 32).astype(np.float32), np.sin(ang).reshape(8192, 32).astype(np.float32)


def make_in_maps(x, c, ctx, c_ctx, w_ada, b_ada, w_in, w_gate2_f, b_gate_f, w_gate2_b, b_gate_b, gla_norm_g, attn_sink,
                 w_out, ln1_g, ln1_b, peer_wq, peer_subkeys, peer_u, peer_v, ln2_g, ln2_b):
    f = lambda a: np.ascontiguousarray(np.asarray(a, dtype=np.float32))
    bc = lambda v, n: np.ascontiguousarray(np.broadcast_to(np.asarray(v, np.float32).reshape(1, -1), (128, n)))
    x = f(x); ctx = f(ctx)
    wi = f(w_in[0])
    wperm = np.concatenate([wi[:, 0:256], wi[:, 256:512], wi[:, 512:1024], wi[:, 1024:1536], wi[:, 1568:2080],
                            wi[:, 2080:2208], wi[:, 2208:2336], wi[:, 1536:1552], wi[:, 1552:1568]], axis=1)
    cosT, sinT = _rope_tables()
    common = dict(
        w_ada=f(w_ada[0]), b_ada=f(b_ada[0]).reshape(1, -1), w_in=np.ascontiguousarray(wperm),
        w2=np.ascontiguousarray(np.concatenate([f(w_gate2_f[0]), f(w_gate2_b[0])], axis=1)),
        bg=np.ascontiguousarray(np.concatenate([f(b_gate_f[0]), f(b_gate_b[0])]).reshape(1, -1)),
        gng=bc(gla_norm_g[0], 128), sink=bc(attn_sink[0], 8), w_out=f(w_out[0]),
        ln1g=bc(ln1_g[0], 1024), ln1b=bc(ln1_b[0], 1024), ln2g=bc(ln2_g[0], 1024), ln2b=bc(ln2_b[0], 1024),
        peer_wq=f(peer_wq[0]),
        skT=np.ascontiguousarray(np.transpose(f(peer_subkeys[0]), (1, 3, 0, 2)).reshape(128, 8, 128)),
        peer_uv=np.ascontiguousarray(np.concatenate([f(peer_u[0]), f(peer_v[0])], axis=1)), **_consts())
    maps = []
    zt = np.zeros((128, 1024), np.float32)
    for core in range(8):
        b, s = core // 4, core % 4
        xb = x[b].reshape(64, 128, 1024)
        npf = 16 * s
        xpf = np.zeros((NPRE, 128, 1024), np.float32)
        if npf:
            xpf[NPRE - npf:] = xb[0:npf]
        npb = 16 * (3 - s)
        xpb = np.zeros((NPRE, 128, 1024), np.float32)
        if npb:
            xpb[NPRE - npb:] = xb[63:16 * (s + 1) - 1:-1]
        flags = np.zeros((128, 100), np.float32)
        flags[:, NPRE - npf:NPRE] = 1.0
        flags[:, 48 + NPRE - npb:48 + NPRE] = 1.0
        t0 = 16 * s
        own = np.zeros((18, 128, 1024), np.float32)
        own[1:17] = xb[t0:t0 + 16]
        cos_o = np.zeros((18, 128, 32), np.float32); sin_o = np.zeros((18, 128, 32), np.float32)
        cos_o[1:17] = cosT.reshape(64, 128, 32)[t0:t0 + 16]; sin_o[1:17] = sinT.reshape(64, 128, 32)[t0:t0 + 16]
        if t0 > 0:
            own[0] = xb[t0 - 1]; flags[:, 96] = 1.0
            cos_o[0] = cosT.reshape(64, 128, 32)[t0 - 1]; sin_o[0] = sinT.reshape(64, 128, 32)[t0 - 1]
        if t0 + 16 < 64:
            own[17] = xb[t0 + 16]; flags[:, 97] = 1.0
            cos_o[17] = cosT.reshape(64, 128, 32)[t0 + 16]; sin_o[17] = sinT.reshape(64, 128, 32)[t0 + 16]
        c2 = np.stack([f(c)[b], f(c_ctx)], axis=0)
        c2T = np.ascontiguousarray(c2.reshape(2, 8, 128).transpose(2, 1, 0))
        m = dict(common)
        m.update(xpre_f=xpf.reshape(-1, 1024), xpre_b=xpb.reshape(-1, 1024), xown=own.reshape(-1, 1024), ctx=ctx[b],
                 flags=flags, c2T=c2T, cosT=cos_o.reshape(-1, 32), sinT=sin_o.reshape(-1, 32))
        maps.append(m)
    return maps


_NC_CACHE = {}


def kernel(**inputs):
    if "full" not in _NC_CACHE:
        _NC_CACHE["full"] = build_nc("full")
    nc = _NC_CACHE["full"]
    maps = make_in_maps(**inputs)
    res = run_bass_kernel_spmd(nc, maps, core_ids=list(range(8)))
    out = np.zeros((2, 8192, 1024), np.float32)
    for core in range(8):
        b, s = core // 4, core % 4
        out[b, s * 2048:(s + 1) * 2048] = np.asarray(res.results[core]["out"], np.float32)
    return out
```

```python
import os
import numpy as np
from contextlib import ExitStack
import concourse.bass as bass
import concourse.mybir as mybir
from concourse.bass_utils import run_bass_kernel_spmd

F32 = mybir.dt.float32
BF16 = mybir.dt.bfloat16
U32 = mybir.dt.uint32
AF = mybir.ActivationFunctionType
ALU = mybir.AluOpType
AX = mybir.AxisListType

SAME_ENGINE_SYNC = True
NPRE = 48
LN_EPS = 1e-5
ALPHA = 2.0 ** 0.25
NG = 12
LAG = 3


class Res:
    __slots__ = ("w", "rs")

    def __init__(self):
        self.w = None
        self.rs = {}


class Buf:
    def __init__(self, t):
        self.t = t
        self.r = Res()


class Sched:
    def __init__(self, nc, stack):
        self.nc = nc
        self.stack = stack
        self.sems = {}
        self.count = {}
        self.seen = {k: {} for k in ("pe", "dve", "act", "pool", "sp")}
        self.streams = {k: [] for k in ("pe", "dve", "act", "pool", "sp")}
        for k in ("pe", "dve", "act", "pool"):
            self.sems[k] = stack.enter_context(nc.semaphore("sem_" + k))
            self.count[k] = 0
        self.nslots = 0

    def dma_slot(self, name=""):
        self.nslots += 1
        key = "d%d%s" % (self.nslots, name)
        self.sems[key] = self.stack.enter_context(self.nc.semaphore("s_" + key))
        self.count[key] = 0
        return key

    def _waits(self, q, reads, writes, same_ok):
        deps = {}
        for b in reads:
            r = b.r
            if r.w is not None:
                k, c = r.w
                deps[k] = max(deps.get(k, 0), c)
        for b in writes:
            w = b.r
            if w.w is not None:
                k, c = w.w
                deps[k] = max(deps.get(k, 0), c)
            for k, c in w.rs.items():
                deps[k] = max(deps.get(k, 0), c)
        out = []
        for k, c in deps.items():
            if k == q and not same_ok:
                continue
            if self.seen[q].get(k, 0) >= c:
                continue
            self.seen[q][k] = c
            out.append((k, c))
        return out

    def op(self, q, fn, r=(), w=()):
        same_ok = SAME_ENGINE_SYNC and q != "pe"
        waits = self._waits(q, r, w, same_ok)
        self.count[q] += 1
        c = self.count[q]
        sems = self.sems
        st = self.streams[q]
        for k, v in waits:
            st.append(lambda e, k=k, v=v: e.wait_ge(sems[k], v))
        st.append(lambda e, fn=fn: fn(e).then_inc(sems[q], 1))
        for b in r:
            b.r.rs[q] = c
        for b in w:
            b.r.w = (q, c)
            b.r.rs = {}

    def dma(self, q, slot, fn, r=(), w=()):
        waits = self._waits(q, r, w, True)
        prev = self.count[slot]
        if prev > 0 and self.seen[q].get(slot, 0) < prev:
            self.seen[q][slot] = prev
            waits.append((slot, prev))
        self.count[slot] += 16
        c = self.count[slot]
        sems = self.sems
        st = self.streams[q]
        for k, v in waits:
            st.append(lambda e, k=k, v=v: e.wait_ge(sems[k], v))
        st.append(lambda e, fn=fn: fn(e).then_inc(sems[slot], 16))
        for b in r:
            b.r.rs[slot] = c
        for b in w:
            b.r.w = (slot, c)
            b.r.rs = {}

    def wait_all(self, q):
        sems = self.sems
        for k in list(self.count.keys()):
            c = self.count[k]
            if c == 0 or k == q or self.seen[q].get(k, 0) >= c:
                continue
            self.seen[q][k] = c
            self.streams[q].append(lambda e, k=k, c=c: e.wait_ge(sems[k], c))

    def barrier(self):
        for q in ("pe", "dve", "act", "pool", "sp"):
            self.wait_all(q)

    def run(self):
        streams = self.streams
        with self.nc.Block() as block:
            @block.tensor
            def _(e):
                for f in streams["pe"]:
                    f(e)

            @block.vector
            def _(e):
                for f in streams["dve"]:
                    f(e)

            @block.scalar
            def _(e):
                for f in streams["act"]:
                    f(e)

            @block.gpsimd
            def _(e):
                for f in streams["pool"]:
                    f(e)

            @block.sync
            def _(e):
                for f in streams["sp"]:
                    f(e)


def build_nc(mode="full"):
    nc = bass.Bass("TRN2", target_bir_lowering=False)
    D = lambda name, shape, dt=F32, kind="ExternalInput": nc.dram_tensor(name, shape, dt, kind=kind).ap()
    d_xpf = D("xpre_f", [NPRE * 128, 1024]); d_xpb = D("xpre_b", [NPRE * 128, 1024])
    d_xown = D("xown", [18 * 128, 1024]); d_ctx = D("ctx", [256, 1024])
    d_flags = D("flags", [128, 100]); d_c2T = D("c2T", [128, 8, 2])
    d_wada = D("w_ada", [1024, 6144]); d_bada = D("b_ada", [1, 6144])
    d_win = D("w_in", [1024, 2336]); d_w2 = D("w2", [16, 512]); d_bg = D("bg", [1, 512])
    d_gng = D("gng", [128, 128]); d_sink = D("sink", [128, 8])
    d_wout = D("w_out", [1024, 1024])
    d_ln1g = D("ln1g", [128, 1024]); d_ln1b = D("ln1b", [128, 1024])
    d_ln2g = D("ln2g", [128, 1024]); d_ln2b = D("ln2b", [128, 1024])
    d_wq = D("peer_wq", [1024, 1024]); d_skT = D("skT", [128, 8, 128])
    d_puv = D("peer_uv", [16384, 2048])
    d_puv16 = D("puv16", [16384, 2048], BF16, kind="Internal")
    d_cos = D("cosT", [18 * 128, 32]); d_sin = D("sinT", [18 * 128, 32])
    d_ident = D("ident", [128, 128]); d_tri = D("tri", [128, 4, 128]); d_ci = D("ci", [128, 2])
    d_amask = D("amask", [128, 2, 128]); d_gmask = D("gmask", [128, 2, 64]); d_iota = D("iota16", [128, 16])
    d_out = D("out", [2048, 1024], kind="ExternalOutput")
    if mode == "B":
        d_x1 = D("x1s", [2048, 1024])
    else:
        d_x1 = D("x1s", [2048, 1024], kind="ExternalOutput" if mode == "A" else "Internal")

    with ExitStack() as top:
        S = Sched(nc, top)
        OP = S.op

        def sbuf(st, name, shape, dt=F32):
            return Buf(st.enter_context(nc.sbuf_tensor("s_" + name, shape, dt)))

        banks = [Buf(top.enter_context(nc.psum_tensor("pb%d" % i, [128, 512], F32))) for i in range(6)]
        accP = [Buf(top.enter_context(nc.psum_tensor("pacc%d" % i, [128, 512], F32))) for i in range(2)]
        bank_i = [0]

        bank_sel = [None]
        bank_ctr = {0: 0, 1: 0}

        def bank():
            sel = bank_sel[0]
            if sel is None:
                b = banks[bank_i[0] % 6]
                bank_i[0] += 1
            else:
                b = banks[sel * 3 + bank_ctr[sel] % 3]
                bank_ctr[sel] += 1
            return b

        ld = [S.dma_slot("ld%d" % i) for i in range(2)]
        cst = S.dma_slot("cst")
        sts = S.dma_slot("st")
        mbs = S.dma_slot("mb")
        outs = S.dma_slot("out")
        csl = S.dma_slot("cs"); snl = S.dma_slot("sn")

        NCV = 128
        puvB = [Buf(None) for _ in range(NCV)]
        cvs = [S.dma_slot("cv%d" % i) for i in range(4)]
        cv_next = [0]

        def convert_chunk(pace=()):
            ci_ = cv_next[0]
            if ci_ >= NCV:
                return
            cv_next[0] += 1
            rows = 16384 // NCV
            S.dma("pool", cvs[ci_ % 4], lambda e: e.dma_start(out=d_puv16[ci_ * rows:(ci_ + 1) * rows, :],
                                                            in_=d_puv[ci_ * rows:(ci_ + 1) * rows, :]), r=list(pace), w=[puvB[ci_]])

        def convert_tables():
            while cv_next[0] < NCV:
                convert_chunk()
        ident = sbuf(top, "ident", [128, 128])
        S.dma("sp", cst, lambda e: e.dma_start(out=ident.t[:], in_=d_ident), w=[ident])
        flags = sbuf(top, "flags", [128, 100])
        S.dma("sp", cst, lambda e: e.dma_start(out=flags.t[:], in_=d_flags), w=[flags])
        eps_t = sbuf(top, "eps_t", [128, 1])
        OP("dve", lambda e: e.memset(eps_t.t[:], LN_EPS), w=[eps_t])
        modT = sbuf(top, "modT", [128, 48, 2])
        sc1p = sbuf(top, "sc1p", [128, 8, 2])
        g1bc = sbuf(top, "g1bc", [128, 1024])
        d_modbc = D("modbc", [3, 128, 1024], kind="Internal")
        xt_i = [0]

        def transpose_to(src, nchunks, dst_fn, width=128, rows=128):
            for c0 in range(0, nchunks, 4):
                pb = bank()
                n = min(4, nchunks - c0)
                for c in range(c0, c0 + n):
                    OP("pe", lambda e, c=c, pb=pb, c0=c0: e.transpose(
                        out=pb.t[0:width, (c - c0) * 128:(c - c0) * 128 + rows],
                        in_=src.t[0:rows, c * width:(c + 1) * width], identity=ident.t[0:rows, 0:rows]),
                       r=[src, ident], w=[pb])
                dst_fn(c0, n, pb)

        if mode != "B":
            with ExitStack() as p0:
                c2T = sbuf(p0, "c2T", [128, 8, 2]); sc2 = sbuf(p0, "sc2", [128, 8, 2])
                S.dma("sp", cst, lambda e: e.dma_start(out=c2T.t[:], in_=d_c2T), w=[c2T])
                OP("act", lambda e: e.activation(out=sc2.t[:], in_=c2T.t[:], func=AF.Silu), r=[c2T], w=[sc2])
                modrow = sbuf(p0, "modrow", [2, 6144])
                brow = sbuf(p0, "brow", [128, 6144]); ones2 = sbuf(p0, "ones2", [128, 2])
                OP("pool", lambda e: e.memset(brow.t[:], 0.0), w=[brow])
                S.dma("sp", cst, lambda e: e.dma_start(out=brow.t[0:1, :], in_=d_bada), w=[brow])
                OP("dve", lambda e: e.memset(ones2.t[:], 0.0), w=[ones2])
                OP("dve", lambda e: e.memset(ones2.t[0:1, :], 1.0), w=[ones2])
                S.barrier()
                wst = [sbuf(p0, "wst%d" % i, [128, 3072]) for i in range(2)]
                wada_v = d_wada.rearrange("(k p) n -> k p n", p=128)
                for half in range(2):
                    pbs = [bank() for _ in range(6)]
                    for j in range(6):
                        col = half * 3072 + j * 512
                        OP("pe", lambda e, pbj=pbs[j], col=col: e.matmul(pbj.t[0:2, :], lhsT=ones2.t[:], rhs=brow.t[:, col:col + 512],
                                                                        start=True, stop=False), r=[ones2, brow], w=[pbs[j]])
                    for kc in range(8):
                        ws = wst[kc % 2]
                        S.dma("sp", ld[kc % 2], lambda e, ws=ws, kc=kc, half=half: e.dma_start(
                            out=ws.t[:], in_=wada_v[kc, :, half * 3072:(half + 1) * 3072]), w=[ws])
                        for j in range(6):
                            OP("pe", lambda e, j=j, ws=ws, kc=kc, pbj=pbs[j]: e.matmul(
                                pbj.t[0:2, :], lhsT=sc2.t[:, kc, :], rhs=ws.t[:, j * 512:(j + 1) * 512],
                                start=False, stop=(kc == 7)), r=[sc2, ws], w=[pbs[j]])
                    for j in range(6):
                        col = half * 3072 + j * 512
                        OP("act", lambda e, pbj=pbs[j], col=col: e.activation(out=modrow.t[:, col:col + 512], in_=pbj.t[0:2, :],
                                                                       func=AF.Identity), r=[pbs[j]], w=[modrow])
                for c0 in range(0, 48, 4):
                    pb = bank()
                    for c in range(c0, c0 + 4):
                        OP("pe", lambda e, c=c, pb=pb, c0=c0: e.transpose(
                            out=pb.t[:, (c - c0) * 2:(c - c0) * 2 + 2], in_=modrow.t[0:2, c * 128:(c + 1) * 128],
                            identity=ident.t[0:2, 0:2]), r=[modrow, ident], w=[pb])
                    OP("dve", lambda e, pb=pb, c0=c0: e.tensor_copy(
                        out=modT.t[:, c0:c0 + 4, :], in_=pb.t[:, 0:8].rearrange("p (a b) -> p a b", a=4)), r=[pb], w=[modT])
                OP("dve", lambda e: e.tensor_scalar(out=sc1p.t[:], in0=modT.t[:, 8:16, :], scalar1=1.0, scalar2=None, op0=ALU.add),
                   r=[modT], w=[sc1p])
                sel = sbuf(p0, "sel", [2, 128])
                OP("dve", lambda e: e.memset(sel.t[:], 0.0), w=[sel])
                OP("dve", lambda e: e.memset(sel.t[0:1, :], 1.0), w=[sel])
                bct = sbuf(p0, "bct", [128, 1024])
                for dst, j, add1, di in ((g1bc, 2, False, None), (bct, 3, False, 0), (bct, 4, True, 1), (bct, 5, False, 2)):
                    for hf in range(2):
                        pb = bank()
                        col = j * 1024 + hf * 512
                        OP("pe", lambda e, pb=pb, col=col: e.matmul(pb.t[:], lhsT=sel.t[:], rhs=modrow.t[:, col:col + 512],
                                                                    start=True, stop=True), r=[sel, modrow], w=[pb])
                        if add1:
                            OP("dve", lambda e, pb=pb, dst=dst, hf=hf: e.tensor_scalar(
                                out=dst.t[:, hf * 512:(hf + 1) * 512], in0=pb.t[:], scalar1=1.0, scalar2=None, op0=ALU.add),
                               r=[pb], w=[dst])
                        else:
                            OP("act", lambda e, pb=pb, dst=dst, hf=hf: e.activation(
                                out=dst.t[:, hf * 512:(hf + 1) * 512], in_=pb.t[:], func=AF.Identity), r=[pb], w=[dst])
                    if di is not None:
                        S.dma("sp", mbs, lambda e, di=di: e.dma_start(out=d_modbc[di], in_=bct.t[:]), r=[bct])
                S.barrier()

        if mode != "B":
            with ExitStack() as pa:
                xt = [sbuf(pa, "xt%d" % i, [128, 1024]) for i in range(2)]
                win = sbuf(pa, "win", [128, 8, 2336], BF16)
                wout = sbuf(pa, "wout", [128, 8, 1024], BF16)
                with ExitStack() as pw:
                    wst = [sbuf(pw, "wcst%d" % i, [128, 2336]) for i in range(2)]
                    win_v = d_win.rearrange("(k p) n -> k p n", p=128)
                    wout_v = d_wout.rearrange("(k p) n -> k p n", p=128)
                    for kc in range(8):
                        ws = wst[kc % 2]
                        S.dma("sp", ld[kc % 2], lambda e, ws=ws, kc=kc: e.dma_start(out=ws.t[:], in_=win_v[kc]), w=[ws])
                        OP("pool", lambda e, ws=ws, kc=kc: e.tensor_copy(out=win.t[:, kc, :], in_=ws.t[:]), r=[ws], w=[win])
                    for kc in range(8):
                        ws = wst[kc % 2]
                        S.dma("sp", ld[kc % 2], lambda e, ws=ws, kc=kc: e.dma_start(out=ws.t[:, 0:1024], in_=wout_v[kc]), w=[ws])
                        OP("pool", lambda e, ws=ws, kc=kc: e.tensor_copy(out=wout.t[:, kc, :], in_=ws.t[:, 0:1024]), r=[ws], w=[wout])
                    S.barrier()
                w2 = sbuf(pa, "w2", [128, 512])
                OP("pool", lambda e: e.memset(w2.t[:], 0.0), w=[w2])
                gng = sbuf(pa, "gng", [128, 128]); esink = sbuf(pa, "esink", [128, 8])
                ln1g = sbuf(pa, "ln1g", [128, 1024]); ln1b = sbuf(pa, "ln1b", [128, 1024])
                tri = sbuf(pa, "tri", [128, 4, 128]); ci = sbuf(pa, "ci", [128, 2])
                amask = sbuf(pa, "amask", [128, 2, 128]); gmask = sbuf(pa, "gmask", [128, 2, 64])
                amask_e = sbuf(pa, "amask_e", [128, 2, 128])
                S.dma("sp", cst, lambda e: e.dma_start(out=w2.t[0:16, :], in_=d_w2), w=[w2])
                S.dma("sp", cst, lambda e: e.dma_start(out=w2.t[16:17, :], in_=d_bg), w=[w2])
                for b_, d_ in ((gng, d_gng), (esink, d_sink), (ln1g, d_ln1g), (ln1b, d_ln1b),
                               (tri, d_tri), (ci, d_ci), (amask, d_amask), (gmask, d_gmask)):
                    S.dma("sp", cst, lambda e, b_=b_, d_=d_: e.dma_start(out=b_.t[:], in_=d_), w=[b_])
                S.barrier()
                OP("act", lambda e: e.activation(out=esink.t[:], in_=esink.t[:], func=AF.Exp), r=[esink], w=[esink])
                for m in range(2):
                    OP("dve", lambda e, m=m: e.tensor_scalar(out=amask_e.t[:, m, :], in0=amask.t[:, m, :],
                                                             scalar1=flags.t[:, 96 + m:97 + m], scalar2=None, op0=ALU.mult),
                       r=[amask, flags], w=[amask_e])
                sbst = sbuf(pa, "sbst", [128, 32, 256], BF16)
                kT_all = sbuf(pa, "kT_all", [64, 18, 256], BF16); v_all = sbuf(pa, "v_all", [128, 18, 130], BF16)
                kTc = sbuf(pa, "kTc", [64, 2, 256], BF16); vc = sbuf(pa, "vc", [128, 2, 130], BF16)
                OP("pool", lambda e: e.memset(v_all.t[:], 1.0), w=[v_all])
                OP("pool", lambda e: e.memset(vc.t[:], 1.0), w=[vc])
                hT2 = [sbuf(pa, "hT%d" % i, [128, 8, 128], BF16) for i in range(2)]
                for h_ in hT2:
                    h_.c = []
                    for _ in range(8):
                        v_ = Buf(h_.t)
                        h_.c.append(v_)
                vsb2 = [sbuf(pa, "vsb%d" % i, [128, 512], BF16) for i in range(2)]
                curb = {"hT": hT2[0], "vsb": vsb2[0]}
                qk = sbuf(pa, "qk", [128, 512])
                zT = [sbuf(pa, "zT", [128, 128])] * 2
                OP("dve", lambda e: e.memset(zT[0].t[:], 0.0), w=[zT[0]])
                OP("dve", lambda e: e.memset(zT[0].t[0:17, :], 1.0), w=[zT[0]])
                sp_ = [sbuf(pa, "sp", [128, 256])] * 2
                et = sbuf(pa, "et", [128, 256])
                Eb = [sbuf(pa, "Eb", [128, 256])] * 2
                Ei = [sbuf(pa, "Ei", [128, 256])] * 2
                Er = [sbuf(pa, "Er", [128, 256])] * 2
                qe = [sbuf(pa, "qe", [128, 256])] * 2
                ke = [sbuf(pa, "ke", [128, 256])] * 2
                kd = [sbuf(pa, "kd", [128, 256], BF16)] * 2
                Tz = [sbuf(pa, "Tz%d" % i, [128, 4, 128], BF16) for i in range(2)]
                keT = [sbuf(pa, "keT%d" % i, [128, 2, 128], BF16) for i in range(2)]
                ATz = sbuf(pa, "ATz", [128, 2, 4, 2, 64], BF16)
                Sfb = [sbuf(pa, "Sfb%d" % i, [128, 2, 128], BF16) for i in range(2)]
                for b_ in (Tz[0], Tz[1], ATz):
                    OP("pool", lambda e, b_=b_: e.memset(b_.t[:], 0.0), w=[b_])
                asb = [sbuf(pa, "asb", [128, 2, 2])] * 2
                Sst = {0: [sbuf(pa, "Sf%d" % i, [128, 2, 128]) for i in range(2)],
                       1: [sbuf(pa, "Sb%d" % i, [128, 2, 128]) for i in range(2)]}
                Scur = {0: 0, 1: 0}
                Stmp = sbuf(pa, "Stmp", [128, 2, 128])
                for dd in range(2):
                    OP("dve", lambda e, dd=dd: e.memset(Sst[dd][0].t[:], 0.0), w=[Sst[dd][0]])
                rsb = sbuf(pa, "rsb", [128, 512])
                ss = sbuf(pa, "ss", [128, 4]); rstd4 = sbuf(pa, "rstd4", [128, 4])
                on = sbuf(pa, "on", [128, 512]); junk = on
                aqs = on; qrot = sbuf(pa, "qrot", [128, 512]); rt = rsb
                akv = sbuf(pa, "akv", [128, 256]); krot = sbuf(pa, "krot", [128, 128])
                cs = sbuf(pa, "cs", [128, 32]); sn = sbuf(pa, "sn", [128, 32])
                qTs = sbuf(pa, "qTs", [64, 8, 128], BF16)
                Pall = sbuf(pa, "Pall", [128, 5, 512], BF16)
                den = sbuf(pa, "den", [128, 4])
                cat2 = [sbuf(pa, "cat%d" % i, [128, 1024]) for i in range(2)]
                aqs_b = sbuf(pa, "aqs_b", [128, 512]); rt_b = sbuf(pa, "rt_b", [128, 512])
                own_ctx = {}
                stats = sbuf(pa, "stats", [128, 12]); mv = sbuf(pa, "mv", [128, 2]); rs1 = sbuf(pa, "rs1", [128, 1])

                def front(src_ap, row):
                    xb = xt[xt_i[0] % 2]
                    slot = ld[xt_i[0] % 2]
                    curb["hT"] = hT2[xt_i[0] % 2]; curb["vsb"] = vsb2[xt_i[0] % 2]
                    hT = curb["hT"]
                    xt_i[0] += 1
                    S.dma("sp", slot, lambda e: e.dma_start(out=xb.t[:], in_=src_ap), w=[xb])

                    def evac(c0, n, pb):
                        for c in range(c0, c0 + n):
                            if c0 == 0:
                                OP("act", lambda e, c=c, pb=pb, c0=c0: e.activation(
                                    out=hT.t[:, c, :], in_=pb.t[:, (c - c0) * 128:(c - c0 + 1) * 128], func=AF.Identity,
                                    scale=sc1p.t[:, c, row:row + 1], bias=modT.t[:, c, row:row + 1]),
                                   r=[pb, sc1p, modT], w=[hT.c[c]])
                            else:
                                OP("dve", lambda e, c=c, pb=pb, c0=c0: e.tensor_scalar(
                                    out=hT.t[:, c, :], in0=pb.t[:, (c - c0) * 128:(c - c0 + 1) * 128],
                                    scalar1=sc1p.t[:, c, row:row + 1], scalar2=modT.t[:, c, row:row + 1], op0=ALU.mult, op1=ALU.add),
                                   r=[pb, sc1p, modT], w=[hT.c[c]])
                    transpose_to(xb, 8, evac)
                    return xb

                def inproj(col0, ncols, pb, pcol=0):
                    hT = curb["hT"]
                    for kc in range(8):
                        OP("pe", lambda e, kc=kc: e.matmul(pb.t[:, pcol:pcol + ncols], lhsT=hT.t[:, kc, :],
                                                           rhs=win.t[:, kc, col0:col0 + ncols], start=(kc == 0), stop=(kc == 7)),
                           r=[hT.c[kc], win], w=[pb])

                def gates(dd, flagcol=None):
                    hT = curb["hT"]
                    pz = bank()
                    for kc in range(8):
                        OP("pe", lambda e, kc=kc: e.matmul(pz.t[0:16, 0:128], lhsT=win.t[:, kc, 2304 + 16 * dd:2320 + 16 * dd],
                                                           rhs=hT.t[:, kc, :], start=(kc == 0), stop=(kc == 7)), r=[hT.c[kc], win], w=[pz])
                    OP("act", lambda e: e.activation(out=zT[dd].t[0:16, :], in_=pz.t[0:16, 0:128], func=AF.Identity), r=[pz], w=[zT[dd]])
                    pg = bank()
                    OP("pe", lambda e: e.matmul(pg.t[:, 0:256], lhsT=zT[dd].t[:], rhs=w2.t[:, dd * 256:(dd + 1) * 256],
                                                start=True, stop=True), r=[zT[dd], w2], w=[pg])
                    OP("act", lambda e: e.activation(out=et.t[:], in_=pg.t[:, 0:256], func=AF.Exp, scale=-1.0), r=[pg], w=[et])
                    OP("act", lambda e: e.activation(out=sp_[dd].t[:], in_=et.t[:], func=AF.Ln, bias=1.0), r=[et], w=[sp_[dd]])
                    if flagcol is not None:
                        OP("dve", lambda e: e.tensor_scalar(out=sp_[dd].t[:], in0=sp_[dd].t[:], scalar1=flags.t[:, flagcol:flagcol + 1],
                                                            scalar2=None, op0=ALU.mult), r=[sp_[dd], flags], w=[sp_[dd]])

                def decay_k(dd, ksrc, flagcol=None):
                    pr = bank()
                    OP("pe", lambda e: e.matmul(pr.t[:, 0:256], lhsT=tri.t[:, 1 + 2 * dd, :], rhs=sp_[dd].t[:], start=True, stop=True),
                       r=[tri, sp_[dd]], w=[pr])
                    OP("act", lambda e: e.activation(out=Er[dd].t[:], in_=pr.t[:, 0:256], func=AF.Exp), r=[pr], w=[Er[dd]])
                    if flagcol is None:
                        OP("dve", lambda e: e.tensor_tensor(out=kd[dd].t[:], in0=ksrc.t[:, 256:512], in1=Er[dd].t[:], op=ALU.mult),
                           r=[ksrc, Er[dd]], w=[kd[dd]])
                    else:
                        OP("dve", lambda e: e.scalar_tensor_tensor(out=kd[dd].t[:], in0=ksrc.t[:, 256:512],
                                                                   scalar=flags.t[:, flagcol:flagcol + 1], in1=Er[dd].t[:],
                                                                   op0=ALU.mult, op1=ALU.mult), r=[ksrc, Er[dd], flags], w=[kd[dd]])
                    pa_ = bank()
                    for hp in range(2):
                        OP("pe", lambda e, hp=hp: e.matmul(pa_.t[:, hp * 2:hp * 2 + 2], lhsT=sp_[dd].t[:, hp * 128:(hp + 1) * 128],
                                                           rhs=ci.t[:], start=True, stop=True), r=[sp_[dd], ci], w=[pa_])
                    OP("act", lambda e: e.activation(out=asb[dd].t[:], in_=pa_.t[:, 0:4].rearrange("p (a b) -> p a b", a=2),
                                                     func=AF.Exp), r=[pa_], w=[asb[dd]])

                def state_update(dd, c, vsrc):
                    pp = bank()
                    for h in range(4):
                        hp, par = h // 2, h % 2
                        OP("pe", lambda e, h=h, hp=hp, par=par: e.matmul(
                            pp.t[par * 64:(par + 1) * 64, hp * 128:(hp + 1) * 128],
                            lhsT=kd[dd].t[c * 64:(c + 1) * 64, h * 64:(h + 1) * 64],
                            rhs=vsrc.t[c * 64:(c + 1) * 64, h * 128:(h + 1) * 128], start=True, stop=True),
                           r=[kd[dd], vsrc], w=[pp])
                    so = Sst[dd][Scur[dd]]
                    sn_ = Sst[dd][1 - Scur[dd]]
                    OP("dve", lambda e: e.tensor_tensor(out=Stmp.t[:], in0=so.t[:],
                                                        in1=asb[dd].t[:, :, c:c + 1].broadcast_to([128, 2, 128]), op=ALU.mult),
                       r=[so, asb[dd]], w=[Stmp])
                    OP("dve", lambda e: e.tensor_tensor(out=sn_.t[:], in0=Stmp.t[:],
                                                        in1=pp.t[:, 0:256].rearrange("p (a b) -> p a b", a=2), op=ALU.add),
                       r=[Stmp, pp], w=[sn_])
                    Scur[dd] = 1 - Scur[dd]

                def state_tile(src_ap, row, dd, flagcol=None, store=None):
                    front(src_ap, row)
                    vsb = curb["vsb"]
                    pk = bank()
                    inproj(256, 256, pk, 256)
                    pv = bank()
                    inproj(512, 512, pv)
                    OP("act", lambda e: e.activation(out=vsb.t[:], in_=pv.t[:], func=AF.Identity), r=[pv], w=[vsb])
                    gates(dd, flagcol)
                    decay_k(dd, pk, flagcol)
                    for c in ((0, 1) if dd == 0 else (1, 0)):
                        if store is not None:
                            cur = Sst[dd][Scur[dd]]
                            OP("pool", lambda e, c=c, cur=cur: e.tensor_copy(out=sbst.t[:, store * 2 + c, :],
                                                                             in_=cur.t[:].rearrange("p a b -> p (a b)")),
                               r=[cur], w=[sbst])
                        state_update(dd, c, vsb)

                ksb2 = [sbuf(pa, "ksb%d" % i, [128, 512]) for i in range(2)]
                zT2 = [sbuf(pa, "zTp%d" % i, [128, 128]) for i in range(2)]
                for z_ in zT2:
                    OP("dve", lambda e, z_=z_: e.memset(z_.t[:], 0.0), w=[z_])
                    OP("dve", lambda e, z_=z_: e.memset(z_.t[0:17, :], 1.0), w=[z_])
                st_i = [0]

                def state_A1(n, src_ap, row):
                    front(src_ap, row)
                    hT = curb["hT"]
                    return dict(hT=hT, vsb=vsb2[n % 2], ksb=ksb2[n % 2], zTb=zT2[n % 2])

                def inproj_h(hT, col0, ncols, pb, pcol=0):
                    for kc in range(8):
                        OP("pe", lambda e, kc=kc: e.matmul(pb.t[:, pcol:pcol + ncols], lhsT=hT.t[:, kc, :],
                                                           rhs=win.t[:, kc, col0:col0 + ncols], start=(kc == 0), stop=(kc == 7)),
                           r=[hT.c[kc], win], w=[pb])

                def state_A2(cx):
                    pk = bank()
                    inproj_h(cx["hT"], 256, 256, pk, 256)
                    ksb = cx["ksb"]
                    OP("dve", lambda e: e.tensor_copy(out=ksb.t[:, 256:512], in_=pk.t[:, 256:512]), r=[pk], w=[ksb])

                def state_A3(cx):
                    pv = bank()
                    inproj_h(cx["hT"], 512, 512, pv)
                    vsb = cx["vsb"]
                    OP("act", lambda e: e.activation(out=vsb.t[:], in_=pv.t[:], func=AF.Identity), r=[pv], w=[vsb])

                def state_A4(cx, dd):
                    pz = bank()
                    hT, zTb = cx["hT"], cx["zTb"]
                    for kc in range(8):
                        OP("pe", lambda e, kc=kc: e.matmul(pz.t[0:16, 0:128], lhsT=win.t[:, kc, 2304 + 16 * dd:2320 + 16 * dd],
                                                           rhs=hT.t[:, kc, :], start=(kc == 0), stop=(kc == 7)), r=[hT.c[kc], win], w=[pz])
                    OP("act", lambda e: e.activation(out=zTb.t[0:16, :], in_=pz.t[0:16, 0:128], func=AF.Identity), r=[pz], w=[zTb])

                def state_B1(cx, dd, flagcol):
                    zTb = cx["zTb"]
                    pg = bank()
                    OP("pe", lambda e: e.matmul(pg.t[:, 0:256], lhsT=zTb.t[:], rhs=w2.t[:, dd * 256:(dd + 1) * 256],
                                                start=True, stop=True), r=[zTb, w2], w=[pg])
                    OP("act", lambda e: e.activation(out=et.t[:], in_=pg.t[:, 0:256], func=AF.Exp, scale=-1.0), r=[pg], w=[et])
                    OP("act", lambda e: e.activation(out=sp_[dd].t[:], in_=et.t[:], func=AF.Ln, bias=1.0), r=[et], w=[sp_[dd]])
                    if flagcol is not None:
                        OP("dve", lambda e: e.tensor_scalar(out=sp_[dd].t[:], in0=sp_[dd].t[:], scalar1=flags.t[:, flagcol:flagcol + 1],
                                                            scalar2=None, op0=ALU.mult), r=[sp_[dd], flags], w=[sp_[dd]])

                def state_B2(cx, dd, flagcol):
                    decay_k(dd, cx["ksb"], flagcol)

                def state_B3(cx, dd, store, c):
                    if store is not None:
                        cur = Sst[dd][Scur[dd]]
                        OP("pool", lambda e, c=c, cur=cur: e.tensor_copy(out=sbst.t[:, store * 2 + c, :],
                                                                         in_=cur.t[:].rearrange("p a b -> p (a b)")),
                           r=[cur], w=[sbst])
                    state_update(dd, c, cx["vsb"])

                def run_state_jobs(jobs):
                    N = len(jobs)
                    cxs = {}
                    for step in range(N + 2):
                        if step < N:
                            cxs[step] = state_A1(step, jobs[step][0], jobs[step][1])
                        m, b_ = step - 1, step - 2
                        hm = 0 <= m < N
                        hb = 0 <= b_ < N
                        ORD = int(os.environ.get("K_ORD", "1"))
                        if hb:
                            _, _, ddb, flb, stb = jobs[b_]
                            cs_ = (0, 1) if ddb == 0 else (1, 0)
                        if ORD == 0:
                            seq = ["A2", "A3", "A4", "B1", "B2", "B3", "B4"]
                        elif ORD == 1:
                            seq = ["B1", "A2", "B2", "A3", "B3", "B4", "A4"]
                        elif ORD == 2:
                            seq = ["B1", "A2", "B2", "A3", "A4", "B3", "B4"]
                        else:
                            seq = ["A2", "B1", "A3", "B2", "A4", "B3", "B4"]
                        for st_ in seq:
                            if st_[0] == "A" and hm:
                                if st_ == "A2":
                                    state_A2(cxs[m])
                                    convert_chunk(pace=[cxs[m]["ksb"]])
                                elif st_ == "A3": state_A3(cxs[m])
                                else: state_A4(cxs[m], jobs[m][2])
                            if st_[0] == "B" and hb:
                                if st_ == "B1": state_B1(cxs[b_], ddb, flb)
                                elif st_ == "B2": state_B2(cxs[b_], ddb, flb)
                                elif st_ == "B3": state_B3(cxs[b_], ddb, stb, cs_[0])
                                else: state_B3(cxs[b_], ddb, stb, cs_[1])
                        if hb:
                            del cxs[b_]

                def rope(src, dst, H, tmp):
                    v5 = lambda b: b.t[:, 0:H * 64].rearrange("p (h a f d) -> p h a f d", h=H, a=2, f=2, d=16)
                    cb = cs.t[:].rearrange("p (a d) -> p a d", a=2).unsqueeze(1).broadcast_to([128, H, 2, 16])
                    sb_ = sn.t[:].rearrange("p (a d) -> p a d", a=2).unsqueeze(1).broadcast_to([128, H, 2, 16])
                    x1, x2 = v5(src)[:, :, :, 0, :], v5(src)[:, :, :, 1, :]
                    o1, o2 = v5(dst)[:, :, :, 0, :], v5(dst)[:, :, :, 1, :]
                    t1, t2 = v5(tmp)[:, :, :, 0, :], v5(tmp)[:, :, :, 1, :]
                    OP("pool", lambda e: e.tensor_tensor(out=o1, in0=x1, in1=cb, op=ALU.mult), r=[src, cs], w=[dst])
                    OP("pool", lambda e: e.tensor_tensor(out=t1, in0=x2, in1=sb_, op=ALU.mult), r=[src, sn], w=[tmp])
                    OP("pool", lambda e: e.tensor_tensor(out=o1, in0=o1, in1=t1, op=ALU.subtract), r=[dst, tmp], w=[dst])
                    OP("pool", lambda e: e.tensor_tensor(out=o2, in0=x2, in1=cb, op=ALU.mult), r=[src, cs], w=[dst])
                    OP("pool", lambda e: e.tensor_tensor(out=t2, in0=x1, in1=sb_, op=ALU.mult), r=[src, sn], w=[tmp])
                    OP("pool", lambda e: e.tensor_tensor(out=o2, in0=o2, in1=t2, op=ALU.add), r=[dst, tmp], w=[dst])

                def kv_tile(src_ap, row, j, is_ctx):
                    front(src_ap, row)
                    pb = bank()
                    inproj(2048, 256, pb)
                    OP("act", lambda e: e.activation(out=akv.t[:], in_=pb.t[:, 0:256], func=AF.Identity), r=[pb], w=[akv])
                    if is_ctx:
                        ksrc, kdst, vdst = akv, kTc, vc
                    else:
                        S.dma("sp", csl, lambda e: e.dma_start(out=cs.t[:], in_=d_cos[j * 128:(j + 1) * 128, :]), w=[cs])
                        S.dma("sp", snl, lambda e: e.dma_start(out=sn.t[:], in_=d_sin[j * 128:(j + 1) * 128, :]), w=[sn])
                        rope(akv, krot, 2, rt)
                        ksrc, kdst, vdst = krot, kT_all, v_all
                    OP("pool", lambda e: e.tensor_copy(
                        out=vdst.t[:, j, :].rearrange("p (g d) -> p g d", g=2)[:, :, 0:64],
                        in_=akv.t[:, 128:256].rearrange("p (g d) -> p g d", g=2)), r=[akv], w=[vdst])

                    def evac(c0, n, pb2):
                        OP("act", lambda e: e.activation(out=kdst.t[:, j, :], in_=pb2.t[0:64, 0:256], func=AF.Identity),
                           r=[pb2], w=[kdst])
                    transpose_to(ksrc, 2, evac, width=64)

                def own_P1(i):
                    j = i + 1
                    xb = front(d_xown[j * 128:(j + 1) * 128, :], 0)
                    vsb = curb["vsb"]; catT = curb["hT"]
                    cat = cat2[i % 2]
                    own_ctx[i] = (xb, catT, cat)
                    yield
                    pqk = bank(); inproj(0, 512, pqk)
                    OP("act", lambda e: e.activation(out=qk.t[:], in_=pqk.t[:], func=AF.Identity), r=[pqk], w=[qk])
                    pv = bank(); inproj(512, 512, pv)
                    OP("act", lambda e: e.activation(out=vsb.t[:], in_=pv.t[:], func=AF.Identity), r=[pv], w=[vsb])
                    pr_ = bank(); inproj(1024, 512, pr_)
                    OP("act", lambda e: e.activation(out=rsb.t[:], in_=pr_.t[:], func=AF.Silu), r=[pr_], w=[rsb])
                    yield
                    for dd in range(2):
                        yield
                        gates(dd)
                        pbn = bank()
                        OP("pe", lambda e, dd=dd, pbn=pbn: e.matmul(pbn.t[:, 0:256], lhsT=tri.t[:, 2 * dd, :], rhs=sp_[dd].t[:],
                                                                    start=True, stop=True), r=[tri, sp_[dd]], w=[pbn])
                        OP("act", lambda e, dd=dd, pbn=pbn: e.activation(out=Eb[dd].t[:], in_=pbn.t[:, 0:256], func=AF.Exp),
                           r=[pbn], w=[Eb[dd]])
                        OP("act", lambda e, dd=dd, pbn=pbn: e.activation(out=Ei[dd].t[:], in_=pbn.t[:, 0:256], func=AF.Exp, scale=-1.0),
                           r=[pbn], w=[Ei[dd]])
                        OP("dve", lambda e, dd=dd: e.scalar_tensor_tensor(out=qe[dd].t[:], in0=qk.t[:, 0:256], scalar=0.125, in1=Eb[dd].t[:],
                                                                          op0=ALU.mult, op1=ALU.mult), r=[qk, Eb[dd]], w=[qe[dd]])
                        OP("dve", lambda e, dd=dd: e.tensor_tensor(out=ke[dd].t[:], in0=qk.t[:, 256:512], in1=Ei[dd].t[:], op=ALU.mult),
                           r=[qk, Ei[dd]], w=[ke[dd]])
                        if dd == 0:
                            decay_k(dd, qk)
                        pT = bank()
                        for idx, srcb in enumerate((qe[dd], qe[dd], ke[dd], ke[dd])):
                            hp = idx % 2
                            OP("pe", lambda e, idx=idx, hp=hp, srcb=srcb, pT=pT: e.transpose(
                                out=pT.t[:, idx * 128:(idx + 1) * 128], in_=srcb.t[:, hp * 128:(hp + 1) * 128], identity=ident.t[:]),
                               r=[srcb, ident], w=[pT])
                        OP("act", lambda e, dd=dd, pT=pT: e.activation(out=keT[dd].t[:].rearrange("p a b -> p (a b)"), in_=pT.t[:, 256:512],
                                                                       func=AF.Identity), r=[pT], w=[keT[dd]])
                        for par in range(2):
                            OP("act", lambda e, dd=dd, pT=pT, par=par: e.activation(
                                out=Tz[dd].t[par * 64:(par + 1) * 64, par::2, :],
                                in_=pT.t[par * 64:(par + 1) * 64, 0:256].rearrange("p (a b) -> p a b", a=2), func=AF.Identity),
                               r=[pT], w=[Tz[dd]])
                    yield
                    pAT = bank()
                    for dd in range(2):
                        for c in range(2):
                            for h in range(4):
                                hp = h // 2
                                OP("pe", lambda e, dd=dd, c=c, h=h, hp=hp: e.matmul(
                                    pAT.t[c * 64:(c + 1) * 64, (dd * 4 + h) * 64:(dd * 4 + h + 1) * 64],
                                    lhsT=keT[dd].t[:, hp, c * 64:(c + 1) * 64],
                                    rhs=Tz[dd].t[:, h, c * 64:(c + 1) * 64], start=True, stop=True),
                                   r=[keT[dd], Tz[dd]], w=[pAT])
                    for c in range(2):
                        OP("dve", lambda e, c=c: e.tensor_tensor(
                            out=ATz.t[c * 64:(c + 1) * 64, :, :, c, :],
                            in0=pAT.t[c * 64:(c + 1) * 64, :].rearrange("p (d h c) -> p d h c", d=2, h=4),
                            in1=gmask.t[c * 64:(c + 1) * 64, :, :].unsqueeze(2).broadcast_to([64, 2, 4, 64]), op=ALU.mult),
                           r=[pAT, gmask], w=[ATz])
                    po = bank()
                    for c in range(2):
                        sf_ = Sst[0][Scur[0]]
                        sf = Sfb[c]
                        OP("pool", lambda e, sf=sf, sf_=sf_: e.tensor_copy(out=sf.t[:], in_=sf_.t[:]), r=[sf_], w=[sf])
                        for h in range(4):
                            hp = h // 2
                            outp = po.t[c * 64:(c + 1) * 64, h * 128:(h + 1) * 128]
                            vv = vsb.t[:, h * 128:(h + 1) * 128]
                            OP("pe", lambda e, c=c, h=h, outp=outp, vv=vv: e.matmul(
                                outp, lhsT=ATz.t[:, 0, h, c, :], rhs=vv, start=True, stop=False), r=[ATz, vsb], w=[po])
                            OP("pe", lambda e, c=c, h=h, hp=hp, outp=outp, sf=sf: e.matmul(
                                outp, lhsT=Tz[0].t[:, h, c * 64:(c + 1) * 64], rhs=sf.t[:, hp, :], start=False, stop=False),
                               r=[Tz[0], sf], w=[po])
                            OP("pe", lambda e, c=c, h=h, outp=outp, vv=vv: e.matmul(
                                outp, lhsT=ATz.t[:, 1, h, c, :], rhs=vv, start=False, stop=False), r=[ATz, vsb], w=[po])
                            OP("pe", lambda e, c=c, h=h, hp=hp, outp=outp: e.matmul(
                                outp, lhsT=Tz[1].t[:, h, c * 64:(c + 1) * 64],
                                rhs=sbst.t[:, i * 2 + c, hp * 128:(hp + 1) * 128], start=False, stop=True), r=[Tz[1], sbst], w=[po])
                        state_update(0, c, vsb)
                        yield
                    for h in range(4):
                        OP("act", lambda e, h=h: e.activation(out=junk.t[:, h * 128:(h + 1) * 128], in_=po.t[:, h * 128:(h + 1) * 128],
                                                              func=AF.Square, accum_out=ss.t[:, h:h + 1]), r=[po], w=[junk, ss])
                    OP("dve", lambda e: e.tensor_scalar(out=rstd4.t[:], in0=ss.t[:], scalar1=1.0 / 128, scalar2=eps_t.t[:, 0:1],
                                                        op0=ALU.mult, op1=ALU.add), r=[ss, eps_t], w=[rstd4])
                    OP("act", lambda e: e.activation(out=rstd4.t[:], in_=rstd4.t[:], func=AF.Sqrt), r=[rstd4], w=[rstd4])
                    OP("dve", lambda e: e.reciprocal(out=rstd4.t[:], in_=rstd4.t[:]), r=[rstd4], w=[rstd4])
                    on3 = on.t[:].rearrange("p (h d) -> p h d", h=4)
                    OP("dve", lambda e: e.tensor_tensor(out=on3, in0=po.t[:].rearrange("p (h d) -> p h d", h=4),
                                                        in1=rstd4.t[:].unsqueeze(2).broadcast_to([128, 4, 128]), op=ALU.mult),
                       r=[po, rstd4], w=[on])
                    OP("pool", lambda e: e.tensor_tensor(out=on3, in0=on3, in1=gng.t[:].unsqueeze(1).broadcast_to([128, 4, 128]),
                                                         op=ALU.mult), r=[on, gng], w=[on])
                    OP("pool", lambda e: e.tensor_tensor(out=cat.t[:, 0:512], in0=on.t[:], in1=rsb.t[:], op=ALU.mult),
                       r=[on, rsb], w=[cat])
                    yield

                def own_P2(i):
                    j = i + 1
                    xb, catT, cat = own_ctx.pop(i)
                    r1 = cat; x1o = cat
                    aqs = aqs_b; rt = rt_b
                    paq = bank(); inproj_h(catT, 1536, 512, paq)
                    OP("act", lambda e: e.activation(out=aqs.t[:], in_=paq.t[:], func=AF.Identity), r=[paq], w=[aqs])
                    yield
                    S.dma("sp", csl, lambda e: e.dma_start(out=cs.t[:], in_=d_cos[j * 128:(j + 1) * 128, :]), w=[cs])
                    S.dma("sp", snl, lambda e: e.dma_start(out=sn.t[:], in_=d_sin[j * 128:(j + 1) * 128, :]), w=[sn])
                    rope(aqs, qrot, 8, rt)
                    yield

                    def evq(c0, n, pb2):
                        OP("act", lambda e: e.activation(out=qTs.t[:, c0:c0 + n, :].rearrange("p a b -> p (a b)"),
                                                         in_=pb2.t[0:64, 0:n * 128], func=AF.Identity, scale=0.125), r=[pb2], w=[qTs])
                    transpose_to(qrot, 8, evq, width=64)
                    yield

                    def att_group(g):
                        kts = [(kT_all, v_all, j - 1, amask_e if i == 0 else amask, 0), (kT_all, v_all, j, None, 0),
                               (kT_all, v_all, j + 1, amask_e if i == 15 else amask, 1), (kTc, vc, 0, None, 0), (kTc, vc, 1, None, 0)]
                        for n_, (kb, vb, jj, mk, mi) in enumerate(kts):
                            pst = bank()
                            OP("pe", lambda e, kb=kb, jj=jj, pst=pst: e.matmul(
                                pst.t[:], lhsT=kb.t[:, jj, g * 128:(g + 1) * 128],
                                rhs=qTs.t[:, 4 * g:4 * g + 4, :].rearrange("p a b -> p (a b)"), start=True, stop=True),
                               r=[kb, qTs], w=[pst])
                            OP("act", lambda e, n_=n_, pst=pst: e.activation(out=Pall.t[:, n_, :], in_=pst.t[:], func=AF.Exp),
                               r=[pst], w=[Pall])
                            if mk is not None:
                                OP("dve", lambda e, n_=n_, mk=mk, mi=mi: e.tensor_tensor(
                                    out=Pall.t[:, n_, :].rearrange("p (h q) -> p h q", h=4),
                                    in0=Pall.t[:, n_, :].rearrange("p (h q) -> p h q", h=4),
                                    in1=mk.t[:, mi:mi + 1, :].broadcast_to([128, 4, 128]), op=ALU.mult), r=[Pall, mk], w=[Pall])
                            yield
                        pO = bank()
                        for hh in range(4):
                            for n_, (kb, vb, jj, mk, mi) in enumerate(kts):
                                OP("pe", lambda e, hh=hh, n_=n_, vb=vb, jj=jj: e.matmul(
                                    pO.t[:, hh * 65:(hh + 1) * 65], lhsT=Pall.t[:, n_, hh * 128:(hh + 1) * 128],
                                    rhs=vb.t[:, jj, g * 65:(g + 1) * 65], start=(n_ == 0), stop=(n_ == 4)), r=[Pall, vb], w=[pO])
                            yield
                        pO3 = pO.t[:, 0:260].rearrange("p (h d) -> p h d", h=4)
                        OP("dve", lambda e, pO3=pO3: e.tensor_tensor(out=den.t[:].unsqueeze(2), in0=pO3[:, :, 64:65],
                                                                     in1=esink.t[:, 4 * g:4 * g + 4].unsqueeze(2), op=ALU.add),
                           r=[pO, esink], w=[den])
                        OP("dve", lambda e: e.reciprocal(out=den.t[:], in_=den.t[:]), r=[den], w=[den])
                        OP("dve", lambda e, pO3=pO3: e.tensor_tensor(
                            out=cat.t[:, 512 + g * 256:512 + (g + 1) * 256].rearrange("p (h d) -> p h d", h=4),
                            in0=pO3[:, :, 0:64], in1=den.t[:].unsqueeze(2).broadcast_to([128, 4, 64]), op=ALU.mult),
                           r=[pO, den], w=[cat])
                    for g_ in range(2):
                        yield from att_group(g_)
                        yield
                    if os.environ.get("K_DBG") == "cat":
                        S.dma("pool", sts, lambda e: e.dma_start(out=d_x1[i * 128:(i + 1) * 128, :], in_=cat.t[:]), r=[cat])
                        return
                    def evc(c0, n, pb2):
                        OP("act", lambda e: e.activation(out=catT.t[:, c0:c0 + n, :].rearrange("p a b -> p (a b)"),
                                                         in_=pb2.t[:, 0:n * 128], func=AF.Identity), r=[pb2], w=catT.c[c0:c0 + n])
                    transpose_to(cat, 8, evc)
                    yield
                    for hf in range(2):
                        yield
                        py = bank()
                        for kc in range(8):
                            OP("pe", lambda e, kc=kc, py=py, hf=hf: e.matmul(py.t[:], lhsT=catT.t[:, kc, :],
                                                                      rhs=wout.t[:, kc, hf * 512:(hf + 1) * 512],
                                                                      start=(kc == 0), stop=(kc == 7)), r=[catT.c[kc], wout], w=[py])
                        OP("dve", lambda e, py=py, hf=hf: e.tensor_tensor(out=r1.t[:, hf * 512:(hf + 1) * 512], in0=py.t[:],
                                                                   in1=g1bc.t[:, hf * 512:(hf + 1) * 512], op=ALU.mult),
                           r=[py, g1bc], w=[r1])
                    OP("dve", lambda e: e.scalar_tensor_tensor(out=r1.t[:], in0=xb.t[:], scalar=ALPHA, in1=r1.t[:],
                                                               op0=ALU.mult, op1=ALU.add), r=[xb, r1], w=[r1])
                    layernorm(r1, x1o, ln1g, ln1b, stats, mv, rs1)
                    S.dma("pool", sts, lambda e: e.dma_start(out=d_x1[i * 128:(i + 1) * 128, :], in_=x1o.t[:]), r=[x1o])

                def layernorm(src, dst, gb, bb, stats, mv, rs1):
                    for hf in range(2):
                        OP("dve", lambda e, hf=hf: e.bn_stats(out=stats.t[:, hf * 6:(hf + 1) * 6], in_=src.t[:, hf * 512:(hf + 1) * 512]),
                           r=[src], w=[stats])
                    OP("dve", lambda e: e.bn_aggr(out=mv.t[:], in_=stats.t[:]), r=[stats], w=[mv])
                    OP("dve", lambda e: e.tensor_scalar(out=rs1.t[:], in0=mv.t[:, 1:2], scalar1=eps_t.t[:, 0:1], scalar2=None, op0=ALU.add),
                       r=[mv, eps_t], w=[rs1])
                    OP("act", lambda e: e.activation(out=rs1.t[:], in_=rs1.t[:], func=AF.Sqrt), r=[rs1], w=[rs1])
                    OP("dve", lambda e: e.reciprocal(out=rs1.t[:], in_=rs1.t[:]), r=[rs1], w=[rs1])
                    OP("dve", lambda e: e.tensor_scalar(out=dst.t[:], in0=src.t[:], scalar1=mv.t[:, 0:1], scalar2=rs1.t[:, 0:1],
                                                        op0=ALU.subtract, op1=ALU.mult), r=[src, mv, rs1], w=[dst])
                    OP("pool", lambda e: e.tensor_tensor(out=dst.t[:], in0=dst.t[:], in1=gb.t[:], op=ALU.mult), r=[dst, gb], w=[dst])
                    OP("pool", lambda e: e.tensor_tensor(out=dst.t[:], in0=dst.t[:], in1=bb.t[:], op=ALU.add), r=[dst, bb], w=[dst])

                for t in range(2):
                    kv_tile(d_ctx[t * 128:(t + 1) * 128, :], 1, t, True)
                jobs = []
                for t in range(2):
                    jobs.append((d_ctx[t * 128:(t + 1) * 128, :], 1, 0, None, None))
                for t in (1, 0):
                    jobs.append((d_ctx[t * 128:(t + 1) * 128, :], 1, 1, None, None))
                for t in range(NPRE):
                    jobs.append((d_xpf[t * 128:(t + 1) * 128, :], 0, 0, t, None))
                for t in range(NPRE):
                    jobs.append((d_xpb[t * 128:(t + 1) * 128, :], 0, 1, 48 + t, None))
                for i in range(15, -1, -1):
                    jobs.append((d_xown[(i + 1) * 128:(i + 2) * 128, :], 0, 1, None, i))
                run_state_jobs(jobs)
                convert_tables()
                for j in range(18):
                    kv_tile(d_xown[j * 128:(j + 1) * 128, :], 0, j, False)
                for i in range(17):
                    g1 = own_P1(i) if i < 16 else iter(())
                    g2 = own_P2(i - 1) if i >= 1 else iter(())
                    a1 = a2 = True
                    while a1 or a2:
                        if a1:
                            bank_sel[0] = 0
                            a1 = next(g1, "END") != "END"
                        if a2:
                            bank_sel[0] = 1
                            a2 = next(g2, "END") != "END"
                bank_sel[0] = None
                S.barrier()

        if mode != "A":
            with ExitStack() as pbs_:
                wq = sbuf(pbs_, "wq", [128, 8, 1024]); skT = sbuf(pbs_, "skT", [128, 8, 128])
                ln2g = sbuf(pbs_, "ln2g", [128, 1024]); ln2b = sbuf(pbs_, "ln2b", [128, 1024])
                iota = sbuf(pbs_, "iota", [128, 16])
                S.dma("sp", cst, lambda e: e.dma_start(out=wq.t[:], in_=d_wq.rearrange("(k p) n -> p k n", p=128)), w=[wq])
                for b_, d_ in ((skT, d_skT), (ln2g, d_ln2g), (ln2b, d_ln2b), (iota, d_iota)):
                    S.dma("sp", cst, lambda e, b_=b_, d_=d_: e.dma_start(out=b_.t[:], in_=d_), w=[b_])
                S.barrier()
                if mode == "B":
                    convert_tables()
                identb = sbuf(pbs_, "identb", [128, 128], BF16)
                OP("dve", lambda e: e.tensor_copy(out=identb.t[:], in_=ident.t[:]), r=[ident], w=[identb])
                dg = [sbuf(pbs_, "dg%d" % i, [128, 128], BF16) for i in range(4)]
                sc2bc = sbuf(pbs_, "sc2bc", [128, 1024]); sh2bc = sbuf(pbs_, "sh2bc", [128, 1024]); g2bc = sbuf(pbs_, "g2bc", [128, 1024])
                if mode == "B":
                    OP("dve", lambda e: e.memset(sc2bc.t[:], 1.0), w=[sc2bc])
                    OP("dve", lambda e: e.memset(sh2bc.t[:], 0.0), w=[sh2bc])
                    OP("dve", lambda e: e.memset(g2bc.t[:], 1.0), w=[g2bc])
                else:
                    for b_, di in ((sh2bc, 0), (sc2bc, 1), (g2bc, 2)):
                        S.dma("sp", cst, lambda e, b_=b_, di=di: e.dma_start(out=b_.t[:], in_=d_modbc[di]), w=[b_])
                    S.barrier()
                x1t = [sbuf(pbs_, "x1t%d" % i, [128, 1024]) for i in range(3)]
                h2 = [sbuf(pbs_, "h2_%d" % i, [128, 1024]) for i in range(2)]
                h2T = sbuf(pbs_, "h2T", [128, 8, 128]); qTp = sbuf(pbs_, "qTp", [128, 8, 128])
                ssb = sbuf(pbs_, "ssb", [128, 16, 128]); sw = sbuf(pbs_, "sw", [128, 128])
                m16 = sbuf(pbs_, "m16", [128, 16, 16]); ix16 = sbuf(pbs_, "ix16", [128, 16, 16], U32)
                ixf = sbuf(pbs_, "ixf", [128, 16, 16])
                cand = sbuf(pbs_, "cand", [128, 8, 256]); cw = sbuf(pbs_, "cw", [128, 256])
                tsv = sbuf(pbs_, "tsv", [128, 8, 16]); pos = sbuf(pbs_, "pos", [128, 8, 16], U32)
                posf = sbuf(pbs_, "posf", [128, 8, 16])
                lohi = sbuf(pbs_, "lohi", [128, 2, 16])
                paf = sbuf(pbs_, "paf", [128, 8, 16]); pbf = sbuf(pbs_, "pbf", [128, 8, 16])
                oh = sbuf(pbs_, "oh", [128, 8, 16, 16])
                i1s = sbuf(pbs_, "i1s", [128, 8, 16]); i2s = sbuf(pbs_, "i2s", [128, 8, 16])
                eidf = sbuf(pbs_, "eidf", [128, 128])
                eidx = [sbuf(pbs_, "eidx%d" % i, [128, 128], U32) for i in range(3)]
                gate = [sbuf(pbs_, "gate%d" % i, [128, 8, 16]) for i in range(2)]
                gsum = sbuf(pbs_, "gsum", [128, 8])
                dots = [sbuf(pbs_, "dots%d" % i, [128, 128]) for i in range(2)]
                coef = [sbuf(pbs_, "coef%d" % i, [128, 128]) for i in range(2)]
                gb = [sbuf(pbs_, "gb%d" % i, [128, 2048], BF16) for i in range(NG)]
                gs = [S.dma_slot("g%d" % i) for i in range(NG)]
                prod = [sbuf(pbs_, "prod%d" % i, [128, 1024], BF16) for i in range(3)]
                h2b = [sbuf(pbs_, "h2b%d" % i, [128, 1024], BF16) for i in range(2)]
                acc = [sbuf(pbs_, "acc%d" % i, [128, 1024]) for i in range(2)]
                stats2 = sbuf(pbs_, "stats2", [128, 12]); mv2 = sbuf(pbs_, "mv2", [128, 2]); rs2 = sbuf(pbs_, "rs2", [128, 1])
                x1ld = [S.dma_slot("x1l%d" % i) for i in range(3)]

                def layernorm2(src, dst):
                    for hf in range(2):
                        OP("dve", lambda e, hf=hf: e.bn_stats(out=stats2.t[:, hf * 6:(hf + 1) * 6], in_=src.t[:, hf * 512:(hf + 1) * 512]),
                           r=[src], w=[stats2])
                    OP("dve", lambda e: e.bn_aggr(out=mv2.t[:], in_=stats2.t[:]), r=[stats2], w=[mv2])
                    OP("dve", lambda e: e.tensor_scalar(out=rs2.t[:], in0=mv2.t[:, 1:2], scalar1=eps_t.t[:, 0:1], scalar2=None, op0=ALU.add),
                       r=[mv2, eps_t], w=[rs2])
                    OP("act", lambda e: e.activation(out=rs2.t[:], in_=rs2.t[:], func=AF.Sqrt), r=[rs2], w=[rs2])
                    OP("dve", lambda e: e.reciprocal(out=rs2.t[:], in_=rs2.t[:]), r=[rs2], w=[rs2])
                    OP("dve", lambda e: e.tensor_scalar(out=dst.t[:], in0=src.t[:], scalar1=mv2.t[:, 0:1], scalar2=rs2.t[:, 0:1],
                                                        op0=ALU.subtract, op1=ALU.mult), r=[src, mv2, rs2], w=[dst])
                    OP("dve", lambda e: e.tensor_tensor(out=dst.t[:], in0=dst.t[:], in1=ln2g.t[:], op=ALU.mult), r=[dst, ln2g], w=[dst])
                    OP("dve", lambda e: e.tensor_tensor(out=dst.t[:], in0=dst.t[:], in1=ln2b.t[:], op=ALU.add), r=[dst, ln2b], w=[dst])

                def top16(src3, n, work, mdst, idst):
                    (sa, sbuf_), (wa, wbuf), (ma, mbuf), (ia, ibuf) = src3, work, mdst, idst
                    OP("dve", lambda e: e.max(out=ma[:, 0:8], in_=sa), r=[sbuf_], w=[mbuf])
                    OP("dve", lambda e: e.max_index(out=ia[:, 0:8], in_max=ma[:, 0:8], in_values=sa), r=[sbuf_, mbuf], w=[ibuf])
                    OP("dve", lambda e: e.match_replace(out=wa, in_to_replace=ma[:, 0:8], in_values=sa, imm_value=-1e30),
                       r=[sbuf_, mbuf], w=[wbuf])
                    OP("dve", lambda e: e.max(out=ma[:, 8:16], in_=wa), r=[wbuf], w=[mbuf])
                    OP("dve", lambda e: e.max_index(out=ia[:, 8:16], in_max=ma[:, 8:16], in_values=wa), r=[wbuf, mbuf], w=[ibuf])

                def route(i):
                    xs = x1t[i % 3]
                    S.dma("sp", x1ld[i % 3], lambda e: e.dma_start(out=xs.t[:], in_=d_x1[i * 128:(i + 1) * 128, :]), w=[xs])
                    hh = h2[i % 2]
                    OP("dve", lambda e: e.tensor_tensor(out=hh.t[:], in0=xs.t[:], in1=sc2bc.t[:], op=ALU.mult), r=[xs, sc2bc], w=[hh])
                    OP("dve", lambda e: e.tensor_tensor(out=hh.t[:], in0=hh.t[:], in1=sh2bc.t[:], op=ALU.add), r=[hh, sh2bc], w=[hh])
                    OP("act", lambda e: e.activation(out=h2b[i % 2].t[:], in_=hh.t[:], func=AF.Identity), r=[hh], w=[h2b[i % 2]])

                    if KSTOP < 2: return
                    def ev(c0, n, pb2):
                        OP("act", lambda e: e.activation(out=h2T.t[:, c0:c0 + n, :].rearrange("p a b -> p (a b)"),
                                                         in_=pb2.t[:, 0:n * 128], func=AF.Identity), r=[pb2], w=[h2T])
                    yield
                    transpose_to(hh, 8, ev)
                    yield
                    if KSTOP < 2.3: return
                    for j0 in range(0, 8, 4):
                        pq = bank()
                        for jj in range(j0, j0 + 4):
                            for kc in range(8):
                                OP("pe", lambda e, jj=jj, kc=kc, pq=pq, j0=j0: e.matmul(
                                    pq.t[:, (jj - j0) * 128:(jj - j0 + 1) * 128], lhsT=wq.t[:, kc, jj * 128:(jj + 1) * 128],
                                    rhs=h2T.t[:, kc, :], start=(kc == 0), stop=(kc == 7)), r=[wq, h2T], w=[pq])
                            yield
                        OP("act", lambda e, pq=pq, j0=j0: e.activation(out=qTp.t[:, j0:j0 + 4, :].rearrange("p a b -> p (a b)"),
                                                                       in_=pq.t[:], func=AF.Identity), r=[pq], w=[qTp])
                    if KSTOP < 2.6: return
                    for h0 in range(0, 8, 4):
                        for a in range(2):
                            psc = bank()
                            for h in range(h0, h0 + 4):
                                OP("pe", lambda e, h=h, a=a, psc=psc, h0=h0: e.matmul(
                                    psc.t[:, (h - h0) * 128:(h - h0 + 1) * 128], lhsT=qTp.t[a * 64:(a + 1) * 64, h, :],
                                    rhs=skT.t[a * 64:(a + 1) * 64, h, :], start=True, stop=True), r=[qTp, skT], w=[psc])
                            OP("act", lambda e, psc=psc, h0=h0, a=a: e.activation(
                                out=ssb.t[:, 2 * h0 + a:2 * h0 + 8:2, :], in_=psc.t[:].rearrange("p (a b) -> p a b", a=4),
                                func=AF.Identity), r=[psc], w=[ssb])
                            yield
                    if KSTOP < 3: return
                    for q_ in range(16):
                        top16((ssb.t[:, q_, :], ssb), 128, (sw.t[:], sw), (m16.t[:, q_, :], m16), (ix16.t[:, q_, :], ix16))
                        yield
                    if KSTOP < 4: return
                    OP("dve", lambda e: e.tensor_copy(out=ixf.t[:], in_=ix16.t[:]), r=[ix16], w=[ixf])
                    m4 = m16.t[:].rearrange("p (h a) k -> p h a k", a=2)
                    OP("dve", lambda e: e.tensor_tensor(
                        out=cand.t[:].rearrange("p h (a b) -> p h a b", a=16),
                        in0=m4[:, :, 0, :].unsqueeze(3).broadcast_to([128, 8, 16, 16]),
                        in1=m4[:, :, 1, :].unsqueeze(2).broadcast_to([128, 8, 16, 16]), op=ALU.add), r=[m16], w=[cand])
                    for h in range(8):
                        top16((cand.t[:, h, :], cand), 256, (cw.t[:], cw), (tsv.t[:, h, :], tsv), (pos.t[:, h, :], pos))
                        yield
                    gt = gate[i % 2]
                    OP("dve", lambda e: e.tensor_tensor(out=gt.t[:], in0=tsv.t[:], in1=tsv.t[:, :, 0:1].broadcast_to([128, 8, 16]),
                                                        op=ALU.subtract), r=[tsv], w=[gt])
                    OP("act", lambda e: e.activation(out=gt.t[:], in_=gt.t[:], func=AF.Exp), r=[gt], w=[gt])
                    OP("dve", lambda e: e.tensor_reduce(out=gsum.t[:], in_=gt.t[:], axis=AX.X, op=ALU.add), r=[gt], w=[gsum])
                    OP("dve", lambda e: e.reciprocal(out=gsum.t[:], in_=gsum.t[:]), r=[gsum], w=[gsum])
                    OP("dve", lambda e: e.tensor_tensor(out=gt.t[:], in0=gt.t[:], in1=gsum.t[:].unsqueeze(2).broadcast_to([128, 8, 16]),
                                                        op=ALU.mult), r=[gt, gsum], w=[gt])
                    if KSTOP < 5: return
                    yield
                    OP("dve", lambda e: e.tensor_copy(out=posf.t[:], in_=pos.t[:]), r=[pos], w=[posf])
                    oh2 = Buf(None); oh2.r = ssb.r
                    oh2.t = ssb.t[:].rearrange("p a b -> p (a b)").rearrange("p (h j c) -> p h j c", h=8, j=16)
                    ix4 = ixf.t[:].rearrange("p (h a) k -> p h a k", a=2)
                    bc4 = lambda ap2: ap2.unsqueeze(1).unsqueeze(1).broadcast_to([128, 8, 16, 16])
                    pf4 = posf.t[:].unsqueeze(3).broadcast_to([128, 8, 16, 16])
                    OP("dve", lambda e: e.tensor_tensor(out=oh.t[:], in0=pf4, in1=bc4(lohi.t[:, 0, :]), op=ALU.is_ge), r=[posf, lohi], w=[oh])
                    OP("dve", lambda e: e.tensor_tensor(out=oh2.t[:], in0=pf4, in1=bc4(lohi.t[:, 1, :]), op=ALU.is_ge), r=[posf, lohi], w=[oh2])
                    OP("dve", lambda e: e.tensor_tensor(out=oh.t[:], in0=oh.t[:], in1=oh2.t[:], op=ALU.subtract), r=[oh, oh2], w=[oh])
                    OP("dve", lambda e: e.tensor_tensor(out=oh2.t[:], in0=oh.t[:], in1=bc4(lohi.t[:, 0, :]), op=ALU.mult), r=[oh, lohi], w=[oh2])
                    yield
                    OP("dve", lambda e: e.tensor_reduce(out=paf.t[:], in_=oh2.t[:], axis=AX.X, op=ALU.add), r=[oh2], w=[paf])
                    OP("dve", lambda e: e.tensor_tensor(out=oh.t[:], in0=oh.t[:],
                                                        in1=ix4[:, :, 0, :].unsqueeze(2).broadcast_to([128, 8, 16, 16]), op=ALU.mult),
                       r=[oh, ixf], w=[oh])
                    OP("dve", lambda e: e.tensor_reduce(out=i1s.t[:], in_=oh.t[:], axis=AX.X, op=ALU.add), r=[oh], w=[i1s])
                    yield
                    OP("dve", lambda e: e.tensor_tensor(out=pbf.t[:], in0=posf.t[:], in1=paf.t[:], op=ALU.subtract), r=[posf, paf], w=[pbf])
                    OP("dve", lambda e: e.tensor_tensor(out=oh.t[:], in0=bc4(iota.t[:]),
                                                        in1=pbf.t[:].unsqueeze(3).broadcast_to([128, 8, 16, 16]), op=ALU.is_equal),
                       r=[iota, pbf], w=[oh])
                    OP("dve", lambda e: e.tensor_tensor(out=oh.t[:], in0=oh.t[:],
                                                        in1=ix4[:, :, 1, :].unsqueeze(2).broadcast_to([128, 8, 16, 16]), op=ALU.mult),
                       r=[oh, ixf], w=[oh])
                    OP("dve", lambda e: e.tensor_reduce(out=i2s.t[:], in_=oh.t[:], axis=AX.X, op=ALU.add), r=[oh], w=[i2s])
                    OP("dve", lambda e: e.scalar_tensor_tensor(out=eidf.t[:].rearrange("p (h k) -> p h k", h=8), in0=i1s.t[:], scalar=128.0,
                                                               in1=i2s.t[:], op0=ALU.mult, op1=ALU.add), r=[i1s, i2s], w=[eidf])
                    OP("dve", lambda e: e.tensor_copy(out=eidx[i % 3].t[:], in_=eidf.t[:]), r=[eidf], w=[eidx[i % 3]])

                gi = [0]

                class V:
                    def __init__(self, t):
                        self.t = t
                        self.r = Res()
                dcol = [[V(dots[p_].t) for _ in range(128)] for p_ in range(2)]
                ccol = [[V(coef[p_].t) for _ in range(128)] for p_ in range(2)]

                gk = {}

                def slot_u(i, s):
                    k = gi[0] % NG
                    gi[0] += 1
                    gk[(i, s)] = k
                    S.dma("pool", gs[k], lambda e: e.indirect_dma_start(
                        out=gb[k].t[:], out_offset=None, in_=d_puv16,
                        in_offset=bass.IndirectOffsetOnAxis(ap=eidx[i % 3].t[:, s:s + 1], axis=0)), r=[eidx[i % 3]] + puvB, w=[gb[k]])
                    pr = prod[s % 3]
                    dc, cc = dcol[i % 2][s], ccol[i % 2][s]
                    OP("dve", lambda e: e.tensor_tensor(out=pr.t[:], in0=gb[k].t[:, 0:1024], in1=h2b[i % 2].t[:], op=ALU.mult),
                       r=[gb[k], h2b[i % 2]], w=[pr])
                    OP("act", lambda e: e.activation(out=pr.t[:], in_=pr.t[:], func=AF.Identity, accum_out=dc.t[:, s:s + 1]),
                       r=[pr], w=[pr, dc])
                    OP("act", lambda e: e.activation(out=cc.t[:, s:s + 1], in_=dc.t[:, s:s + 1], func=AF.Gelu), r=[dc], w=[cc])

                def slot_v(i, s):
                    k = gk.pop((i, s))
                    cc = ccol[i % 2][s]
                    dgk = dg[s % 4]
                    OP("dve", lambda e: e.tensor_scalar(out=dgk.t[:], in0=identb.t[:], scalar1=cc.t[:, s:s + 1],
                                                        scalar2=gate[i % 2].t[:, s // 16, s % 16:s % 16 + 1], op0=ALU.mult, op1=ALU.mult),
                       r=[identb, cc, gate[i % 2]], w=[dgk])
                    for hf in range(2):
                        OP("pe", lambda e, hf=hf: e.matmul(accP[hf].t[:], lhsT=dgk.t[:], rhs=gb[k].t[:, 1024 + hf * 512:1536 + hf * 512],
                                                           start=(s == 0), stop=(s == 127)), r=[dgk, gb[k]], w=[accP[hf]])

                def finish_v(i):
                    r2 = acc[i % 2]; yo = acc[i % 2]
                    for hf in range(2):
                        OP("dve", lambda e, hf=hf: e.tensor_tensor(out=r2.t[:, hf * 512:(hf + 1) * 512], in0=accP[hf].t[:],
                                                                   in1=g2bc.t[:, hf * 512:(hf + 1) * 512], op=ALU.mult),
                           r=[accP[hf], g2bc], w=[r2])
                    OP("dve", lambda e: e.scalar_tensor_tensor(out=r2.t[:], in0=x1t[i % 3].t[:], scalar=ALPHA, in1=r2.t[:],
                                                               op0=ALU.mult, op1=ALU.add), r=[x1t[i % 3], r2], w=[r2])
                    layernorm2(r2, yo)
                    S.dma("sp", outs, lambda e: e.dma_start(out=d_out[i * 128:(i + 1) * 128, :], in_=yo.t[:]), r=[yo])

                OP("dve", lambda e: e.tensor_scalar(out=lohi.t[:, 0, :], in0=iota.t[:], scalar1=16.0, scalar2=None, op0=ALU.mult), r=[iota], w=[lohi])
                OP("dve", lambda e: e.tensor_scalar(out=lohi.t[:, 1, :], in0=iota.t[:], scalar1=16.0, scalar2=16.0, op0=ALU.mult, op1=ALU.add),
                   r=[iota], w=[lohi])
                NT = int(os.environ.get('K_NT', '16'))
                KSTOP = float(os.environ.get('K_STOP', '9'))
                for _ in route(0):
                    pass
                for i in range(NT):
                    gen = route(i + 1) if i + 1 < NT else iter(())
                    for s in range(128 + LAG if KSTOP >= 6 else 0):
                        if s < 128:
                            slot_u(i, s)
                        if s >= LAG:
                            slot_v(i, s - LAG)
                        next(gen, None)
                    for _ in gen:
                        pass
                    if KSTOP >= 7:
                        finish_v(i)
                S.barrier()
        S.barrier()
        S.run()
    return nc


def _consts():
    s = np.arange(128)[:, None]
    t = np.arange(128)[None, :]
    same = (s // 64) == (t // 64)
    g = -1.0 / 16.0
    tri = np.stack([(same & (s <= t)), (same & (s > t)), (same & (s >= t)), (same & (s < t))], axis=1).astype(np.float32) * g
    ci = np.stack([(np.arange(128) // 64 == c) for c in range(2)], axis=1).astype(np.float32) * g
    amask = np.stack([(s >= t), (s <= t)], axis=1).astype(np.float32)
    sc = (np.arange(128) % 64)[:, None]
    cc = np.arange(64)[None, :]
    gmask = np.stack([(sc <= cc), (sc >= cc)], axis=1).astype(np.float32)
    iota = np.broadcast_to(np.arange(16, dtype=np.float32), (128, 16)).copy()
    return dict(ident=np.eye(128, dtype=np.float32), tri=np.ascontiguousarray(tri), ci=np.ascontiguousarray(ci),
                amask=np.ascontiguousarray(amask), gmask=np.ascontiguousarray(gmask), iota16=iota)


def _rope_tables():
    rows = 8192 // 64
    row = np.repeat(np.arange(rows, dtype=np.float32), 64)
    col = np.tile(np.arange(64, dtype=np.float32), rows)
    inv = (np.float32(10000.0) ** (-np.arange(16, dtype=np.float32) / np.float32(16))).astype(np.float32)
    ang = np.stack([row[:, None] * inv, col[:, None] * inv], axis=1).astype(np.float32)
    return np.cos(ang).reshape(8192, 32).astype(np.float32), np.sin(ang).reshape(8192, 32).astype(np.float32)


def make_in_maps(x, c, ctx, c_ctx, w_ada, b_ada, w_in, w_gate2_f, b_gate_f, w_gate2_b, b_gate_b, gla_norm_g, attn_sink,
                 w_out, ln1_g, ln1_b, peer_wq, peer_subkeys, peer_u, peer_v, ln2_g, ln2_b):
    f = lambda a: np.ascontiguousarray(np.asarray(a, dtype=np.float32))
    bc = lambda v, n: np.ascontiguousarray(np.broadcast_to(np.asarray(v, np.float32).reshape(1, -1), (128, n)))
    x = f(x); ctx = f(ctx)
    wi = f(w_in[0])
    wperm = np.concatenate([wi[:, 0:256], wi[:, 256:512], wi[:, 512:1024], wi[:, 1024:1536], wi[:, 1568:2080],
                            wi[:, 2080:2208], wi[:, 2208:2336], wi[:, 1536:1552], wi[:, 1552:1568]], axis=1)
    cosT, sinT = _rope_tables()
    common = dict(
        w_ada=f(w_ada[0]), b_ada=f(b_ada[0]).reshape(1, -1), w_in=np.ascontiguousarray(wperm),
        w2=np.ascontiguousarray(np.concatenate([f(w_gate2_f[0]), f(w_gate2_b[0])], axis=1)),
        bg=np.ascontiguousarray(np.concatenate([f(b_gate_f[0]), f(b_gate_b[0])]).reshape(1, -1)),
        gng=bc(gla_norm_g[0], 128), sink=bc(attn_sink[0], 8), w_out=f(w_out[0]),
        ln1g=bc(ln1_g[0], 1024), ln1b=bc(ln1_b[0], 1024), ln2g=bc(ln2_g[0], 1024), ln2b=bc(ln2_b[0], 1024),
        peer_wq=f(peer_wq[0]),
        skT=np.ascontiguousarray(np.transpose(f(peer_subkeys[0]), (1, 3, 0, 2)).reshape(128, 8, 128)),
        peer_uv=np.ascontiguousarray(np.concatenate([f(peer_u[0]), f(peer_v[0])], axis=1)), **_consts())
    maps = []
    zt = np.zeros((128, 1024), np.float32)
    for core in range(8):
        b, s = core // 4, core % 4
        xb = x[b].reshape(64, 128, 1024)
        npf = 16 * s
        xpf = np.zeros((NPRE, 128, 1024), np.float32)
        if npf:
            xpf[NPRE - npf:] = xb[0:npf]
        npb = 16 * (3 - s)
        xpb = np.zeros((NPRE, 128, 1024), np.float32)
        if npb:
            xpb[NPRE - npb:] = xb[63:16 * (s + 1) - 1:-1]
        flags = np.zeros((128, 100), np.float32)
        flags[:, NPRE - npf:NPRE] = 1.0
        flags[:, 48 + NPRE - npb:48 + NPRE] = 1.0
        t0 = 16 * s
        own = np.zeros((18, 128, 1024), np.float32)
        own[1:17] = xb[t0:t0 + 16]
        cos_o = np.zeros((18, 128, 32), np.float32); sin_o = np.zeros((18, 128, 32), np.float32)
        cos_o[1:17] = cosT.reshape(64, 128, 32)[t0:t0 + 16]; sin_o[1:17] = sinT.reshape(64, 128, 32)[t0:t0 + 16]
        if t0 > 0:
            own[0] = xb[t0 - 1]; flags[:, 96] = 1.0
            cos_o[0] = cosT.reshape(64, 128, 32)[t0 - 1]; sin_o[0] = sinT.reshape(64, 128, 32)[t0 - 1]
        if t0 + 16 < 64:
            own[17] = xb[t0 + 16]; flags[:, 97] = 1.0
            cos_o[17] = cosT.reshape(64, 128, 32)[t0 + 16]; sin_o[17] = sinT.reshape(64, 128, 32)[t0 + 16]
        c2 = np.stack([f(c)[b], f(c_ctx)], axis=0)
        c2T = np.ascontiguousarray(c2.reshape(2, 8, 128).transpose(2, 1, 0))
        m = dict(common)
        m.update(xpre_f=xpf.reshape(-1, 1024), xpre_b=xpb.reshape(-1, 1024), xown=own.reshape(-1, 1024), ctx=ctx[b],
                 flags=flags, c2T=c2T, cosT=cos_o.reshape(-1, 32), sinT=sin_o.reshape(-1, 32))
        maps.append(m)
    return maps


_NC_CACHE = {}


def kernel(**inputs):
    if "full" not in _NC_CACHE:
        _NC_CACHE["full"] = build_nc("full")
    nc = _NC_CACHE["full"]
    maps = make_in_maps(**inputs)
    res = run_bass_kernel_spmd(nc, maps, core_ids=list(range(8)))
    out = np.zeros((2, 8192, 1024), np.float32)
    for core in range(8):
        b, s = core // 4, core % 4
        out[b, s * 2048:(s + 1) * 2048] = np.asarray(res.results[core]["out"], np.float32)
    return out
```

```python
import os
import numpy as np
from contextlib import ExitStack
import concourse.bass as bass
import concourse.mybir as mybir
from concourse.bass_utils import run_bass_kernel_spmd

F32 = mybir.dt.float32
BF16 = mybir.dt.bfloat16
U32 = mybir.dt.uint32
AF = mybir.ActivationFunctionType
ALU = mybir.AluOpType
AX = mybir.AxisListType

SAME_ENGINE_SYNC = True
NPRE = 48
LN_EPS = 1e-5
ALPHA = 2.0 ** 0.25
NG = 12
LAG = 3


class Res:
    __slots__ = ("w", "rs")

    def __init__(self):
        self.w = None
        self.rs = {}


class Buf:
    def __init__(self, t):
        self.t = t
        self.r = Res()


class Sched:
    def __init__(self, nc, stack):
        self.nc = nc
        self.stack = stack
        self.sems = {}
        self.count = {}
        self.seen = {k: {} for k in ("pe", "dve", "act", "pool", "sp")}
        self.streams = {k: [] for k in ("pe", "dve", "act", "pool", "sp")}
        for k in ("pe", "dve", "act", "pool"):
            self.sems[k] = stack.enter_context(nc.semaphore("sem_" + k))
            self.count[k] = 0
        self.nslots = 0

    def dma_slot(self, name=""):
        self.nslots += 1
        key = "d%d%s" % (self.nslots, name)
        self.sems[key] = self.stack.enter_context(self.nc.semaphore("s_" + key))
        self.count[key] = 0
        return key

    def _waits(self, q, reads, writes, same_ok):
        deps = {}
        for b in reads:
            r = b.r
            if r.w is not None:
                k, c = r.w
                deps[k] = max(deps.get(k, 0), c)
        for b in writes:
            w = b.r
            if w.w is not None:
                k, c = w.w
                deps[k] = max(deps.get(k, 0), c)
            for k, c in w.rs.items():
                deps[k] = max(deps.get(k, 0), c)
        out = []
        for k, c in deps.items():
            if k == q and not same_ok:
                continue
            if self.seen[q].get(k, 0) >= c:
                continue
            self.seen[q][k] = c
            out.append((k, c))
        return out

    def op(self, q, fn, r=(), w=()):
        same_ok = SAME_ENGINE_SYNC and q != "pe"
        waits = self._waits(q, r, w, same_ok)
        self.count[q] += 1
        c = self.count[q]
        sems = self.sems
        st = self.streams[q]
        for k, v in waits:
            st.append(lambda e, k=k, v=v: e.wait_ge(sems[k], v))
        st.append(lambda e, fn=fn: fn(e).then_inc(sems[q], 1))
        for b in r:
            b.r.rs[q] = c
        for b in w:
            b.r.w = (q, c)
            b.r.rs = {}

    def dma(self, q, slot, fn, r=(), w=()):
        waits = self._waits(q, r, w, True)
        prev = self.count[slot]
        if prev > 0 and self.seen[q].get(slot, 0) < prev:
            self.seen[q][slot] = prev
            waits.append((slot, prev))
        self.count[slot] += 16
        c = self.count[slot]
        sems = self.sems
        st = self.streams[q]
        for k, v in waits:
            st.append(lambda e, k=k, v=v: e.wait_ge(sems[k], v))
        st.append(lambda e, fn=fn: fn(e).then_inc(sems[slot], 16))
        for b in r:
            b.r.rs[slot] = c
        for b in w:
            b.r.w = (slot, c)
            b.r.rs = {}

    def wait_all(self, q):
        sems = self.sems
        for k in list(self.count.keys()):
            c = self.count[k]
            if c == 0 or k == q or self.seen[q].get(k, 0) >= c:
                continue
            self.seen[q][k] = c
            self.streams[q].append(lambda e, k=k, c=c: e.wait_ge(sems[k], c))

    def barrier(self):
        for q in ("pe", "dve", "act", "pool", "sp"):
            self.wait_all(q)

    def run(self):
        streams = self.streams
        with self.nc.Block() as block:
            @block.tensor
            def _(e):
                for f in streams["pe"]:
                    f(e)

            @block.vector
            def _(e):
                for f in streams["dve"]:
                    f(e)

            @block.scalar
            def _(e):
                for f in streams["act"]:
                    f(e)

            @block.gpsimd
            def _(e):
                for f in streams["pool"]:
                    f(e)

            @block.sync
            def _(e):
                for f in streams["sp"]:
                    f(e)


def build_nc(mode="full"):
    nc = bass.Bass("TRN2", target_bir_lowering=False)
    D = lambda name, shape, dt=F32, kind="ExternalInput": nc.dram_tensor(name, shape, dt, kind=kind).ap()
    d_xpf = D("xpre_f", [NPRE * 128, 1024]); d_xpb = D("xpre_b", [NPRE * 128, 1024])
    d_xown = D("xown", [18 * 128, 1024]); d_ctx = D("ctx", [256, 1024])
    d_flags = D("flags", [128, 100]); d_c2T = D("c2T", [128, 8, 2])
    d_wada = D("w_ada", [1024, 6144]); d_bada = D("b_ada", [1, 6144])
    d_win = D("w_in", [1024, 2336]); d_w2 = D("w2", [16, 512]); d_bg = D("bg", [1, 512])
    d_gng = D("gng", [128, 128]); d_sink = D("sink", [128, 8])
    d_wout = D("w_out", [1024, 1024])
    d_ln1g = D("ln1g", [128, 1024]); d_ln1b = D("ln1b", [128, 1024])
    d_ln2g = D("ln2g", [128, 1024]); d_ln2b = D("ln2b", [128, 1024])
    d_wq = D("peer_wq", [1024, 1024]); d_skT = D("skT", [128, 8, 128])
    d_puv = D("peer_uv", [16384, 2048])
    d_puv16 = D("puv16", [16384, 2048], BF16, kind="Internal")
    d_cos = D("cosT", [18 * 128, 32]); d_sin = D("sinT", [18 * 128, 32])
    d_ident = D("ident", [128, 128]); d_tri = D("tri", [128, 4, 128]); d_ci = D("ci", [128, 2])
    d_amask = D("amask", [128, 2, 128]); d_gmask = D("gmask", [128, 2, 64]); d_iota = D("iota16", [128, 16])
    d_out = D("out", [2048, 1024], kind="ExternalOutput")
    if mode == "B":
        d_x1 = D("x1s", [2048, 1024])
    else:
        d_x1 = D("x1s", [2048, 1024], kind="ExternalOutput" if mode == "A" else "Internal")

    with ExitStack() as top:
        S = Sched(nc, top)
        OP = S.op

        def sbuf(st, name, shape, dt=F32):
            return Buf(st.enter_context(nc.sbuf_tensor("s_" + name, shape, dt)))

        banks = [Buf(top.enter_context(nc.psum_tensor("pb%d" % i, [128, 512], F32))) for i in range(6)]
        accP = [Buf(top.enter_context(nc.psum_tensor("pacc%d" % i, [128, 512], F32))) for i in range(2)]
        bank_i = [0]

        bank_sel = [None]
        bank_ctr = {0: 0, 1: 0}

        def bank():
            sel = bank_sel[0]
            if sel is None:
                b = banks[bank_i[0] % 6]
                bank_i[0] += 1
            else:
                b = banks[sel * 3 + bank_ctr[sel] % 3]
                bank_ctr[sel] += 1
            return b

        ld = [S.dma_slot("ld%d" % i) for i in range(2)]
        cst = S.dma_slot("cst")
        sts = S.dma_slot("st")
        mbs = S.dma_slot("mb")
        outs = S.dma_slot("out")
        csl = S.dma_slot("cs"); snl = S.dma_slot("sn")

        NCV = 128
        puvB = [Buf(None) for _ in range(NCV)]
        cvs = [S.dma_slot("cv%d" % i) for i in range(4)]
        cv_next = [0]

        def convert_chunk(pace=()):
            ci_ = cv_next[0]
            if ci_ >= NCV:
                return
            cv_next[0] += 1
            rows = 16384 // NCV
            S.dma("pool", cvs[ci_ % 4], lambda e: e.dma_start(out=d_puv16[ci_ * rows:(ci_ + 1) * rows, :],
                                                            in_=d_puv[ci_ * rows:(ci_ + 1) * rows, :]), r=list(pace), w=[puvB[ci_]])

        def convert_tables():
            while cv_next[0] < NCV:
                convert_chunk()
        ident = sbuf(top, "ident", [128, 128])
        S.dma("sp", cst, lambda e: e.dma_start(out=ident.t[:], in_=d_ident), w=[ident])
        flags = sbuf(top, "flags", [128, 100])
        S.dma("sp", cst, lambda e: e.dma_start(out=flags.t[:], in_=d_flags), w=[flags])
        eps_t = sbuf(top, "eps_t", [128, 1])
        OP("dve", lambda e: e.memset(eps_t.t[:], LN_EPS), w=[eps_t])
        modT = sbuf(top, "modT", [128, 48, 2])
        sc1p = sbuf(top, "sc1p", [128, 8, 2])
        g1bc = sbuf(top, "g1bc", [128, 1024])
        d_modbc = D("modbc", [3, 128, 1024], kind="Internal")
        xt_i = [0]

        def transpose_to(src, nchunks, dst_fn, width=128, rows=128):
            for c0 in range(0, nchunks, 4):
                pb = bank()
                n = min(4, nchunks - c0)
                for c in range(c0, c0 + n):
                    OP("pe", lambda e, c=c, pb=pb, c0=c0: e.transpose(
                        out=pb.t[0:width, (c - c0) * 128:(c - c0) * 128 + rows],
                        in_=src.t[0:rows, c * width:(c + 1) * width], identity=ident.t[0:rows, 0:rows]),
                       r=[src, ident], w=[pb])
                dst_fn(c0, n, pb)

        if mode != "B":
            with ExitStack() as p0:
                c2T = sbuf(p0, "c2T", [128, 8, 2]); sc2 = sbuf(p0, "sc2", [128, 8, 2])
                S.dma("sp", cst, lambda e: e.dma_start(out=c2T.t[:], in_=d_c2T), w=[c2T])
                OP("act", lambda e: e.activation(out=sc2.t[:], in_=c2T.t[:], func=AF.Silu), r=[c2T], w=[sc2])
                modrow = sbuf(p0, "modrow", [2, 6144])
                brow = sbuf(p0, "brow", [128, 6144]); ones2 = sbuf(p0, "ones2", [128, 2])
                OP("pool", lambda e: e.memset(brow.t[:], 0.0), w=[brow])
                S.dma("sp", cst, lambda e: e.dma_start(out=brow.t[0:1, :], in_=d_bada), w=[brow])
                OP("dve", lambda e: e.memset(ones2.t[:], 0.0), w=[ones2])
                OP("dve", lambda e: e.memset(ones2.t[0:1, :], 1.0), w=[ones2])
                S.barrier()
                wst = [sbuf(p0, "wst%d" % i, [128, 3072]) for i in range(2)]
                wada_v = d_wada.rearrange("(k p) n -> k p n", p=128)
                for half in range(2):
                    pbs = [bank() for _ in range(6)]
                    for j in range(6):
                        col = half * 3072 + j * 512
                        OP("pe", lambda e, pbj=pbs[j], col=col: e.matmul(pbj.t[0:2, :], lhsT=ones2.t[:], rhs=brow.t[:, col:col + 512],
                                                                        start=True, stop=False), r=[ones2, brow], w=[pbs[j]])
                    for kc in range(8):
                        ws = wst[kc % 2]
                        S.dma("sp", ld[kc % 2], lambda e, ws=ws, kc=kc, half=half: e.dma_start(
                            out=ws.t[:], in_=wada_v[kc, :, half * 3072:(half + 1) * 3072]), w=[ws])
                        for j in range(6):
                            OP("pe", lambda e, j=j, ws=ws, kc=kc, pbj=pbs[j]: e.matmul(
                                pbj.t[0:2, :], lhsT=sc2.t[:, kc, :], rhs=ws.t[:, j * 512:(j + 1) * 512],
                                start=False, stop=(kc == 7)), r=[sc2, ws], w=[pbs[j]])
                    for j in range(6):
                        col = half * 3072 + j * 512
                        OP("act", lambda e, pbj=pbs[j], col=col: e.activation(out=modrow.t[:, col:col + 512], in_=pbj.t[0:2, :],
                                                                       func=AF.Identity), r=[pbs[j]], w=[modrow])
                for c0 in range(0, 48, 4):
                    pb = bank()
                    for c in range(c0, c0 + 4):
                        OP("pe", lambda e, c=c, pb=pb, c0=c0: e.transpose(
                            out=pb.t[:, (c - c0) * 2:(c - c0) * 2 + 2], in_=modrow.t[0:2, c * 128:(c + 1) * 128],
                            identity=ident.t[0:2, 0:2]), r=[modrow, ident], w=[pb])
                    OP("dve", lambda e, pb=pb, c0=c0: e.tensor_copy(
                        out=modT.t[:, c0:c0 + 4, :], in_=pb.t[:, 0:8].rearrange("p (a b) -> p a b", a=4)), r=[pb], w=[modT])
                OP("dve", lambda e: e.tensor_scalar(out=sc1p.t[:], in0=modT.t[:, 8:16, :], scalar1=1.0, scalar2=None, op0=ALU.add),
                   r=[modT], w=[sc1p])
                sel = sbuf(p0, "sel", [2, 128])
                OP("dve", lambda e: e.memset(sel.t[:], 0.0), w=[sel])
                OP("dve", lambda e: e.memset(sel.t[0:1, :], 1.0), w=[sel])
                bct = sbuf(p0, "bct", [128, 1024])
                for dst, j, add1, di in ((g1bc, 2, False, None), (bct, 3, False, 0), (bct, 4, True, 1), (bct, 5, False, 2)):
                    for hf in range(2):
                        pb = bank()
                        col = j * 1024 + hf * 512
                        OP("pe", lambda e, pb=pb, col=col: e.matmul(pb.t[:], lhsT=sel.t[:], rhs=modrow.t[:, col:col + 512],
                                                                    start=True, stop=True), r=[sel, modrow], w=[pb])
                        if add1:
                            OP("dve", lambda e, pb=pb, dst=dst, hf=hf: e.tensor_scalar(
                                out=dst.t[:, hf * 512:(hf + 1) * 512], in0=pb.t[:], scalar1=1.0, scalar2=None, op0=ALU.add),
                               r=[pb], w=[dst])
                        else:
                            OP("act", lambda e, pb=pb, dst=dst, hf=hf: e.activation(
                                out=dst.t[:, hf * 512:(hf + 1) * 512], in_=pb.t[:], func=AF.Identity), r=[pb], w=[dst])
                    if di is not None:
                        S.dma("sp", mbs, lambda e, di=di: e.dma_start(out=d_modbc[di], in_=bct.t[:]), r=[bct])
                S.barrier()

        if mode != "B":
            with ExitStack() as pa:
                xt = [sbuf(pa, "xt%d" % i, [128, 1024]) for i in range(2)]
                win = sbuf(pa, "win", [128, 8, 2336], BF16)
                wout = sbuf(pa, "wout", [128, 8, 1024], BF16)
                with ExitStack() as pw:
                    wst = [sbuf(pw, "wcst%d" % i, [128, 2336]) for i in range(2)]
                    win_v = d_win.rearrange("(k p) n -> k p n", p=128)
                    wout_v = d_wout.rearrange("(k p) n -> k p n", p=128)
                    for kc in range(8):
                        ws = wst[kc % 2]
                        S.dma("sp", ld[kc % 2], lambda e, ws=ws, kc=kc: e.dma_start(out=ws.t[:], in_=win_v[kc]), w=[ws])
                        OP("pool", lambda e, ws=ws, kc=kc: e.tensor_copy(out=win.t[:, kc, :], in_=ws.t[:]), r=[ws], w=[win])
                    for kc in range(8):
                        ws = wst[kc % 2]
                        S.dma("sp", ld[kc % 2], lambda e, ws=ws, kc=kc: e.dma_start(out=ws.t[:, 0:1024], in_=wout_v[kc]), w=[ws])
                        OP("pool", lambda e, ws=ws, kc=kc: e.tensor_copy(out=wout.t[:, kc, :], in_=ws.t[:, 0:1024]), r=[ws], w=[wout])
                    S.barrier()
                w2 = sbuf(pa, "w2", [128, 512])
                OP("pool", lambda e: e.memset(w2.t[:], 0.0), w=[w2])
                gng = sbuf(pa, "gng", [128, 128]); esink = sbuf(pa, "esink", [128, 8])
                ln1g = sbuf(pa, "ln1g", [128, 1024]); ln1b = sbuf(pa, "ln1b", [128, 1024])
                tri = sbuf(pa, "tri", [128, 4, 128]); ci = sbuf(pa, "ci", [128, 2])
                amask = sbuf(pa, "amask", [128, 2, 128]); gmask = sbuf(pa, "gmask", [128, 2, 64])
                amask_e = sbuf(pa, "amask_e", [128, 2, 128])
                S.dma("sp", cst, lambda e: e.dma_start(out=w2.t[0:16, :], in_=d_w2), w=[w2])
                S.dma("sp", cst, lambda e: e.dma_start(out=w2.t[16:17, :], in_=d_bg), w=[w2])
                for b_, d_ in ((gng, d_gng), (esink, d_sink), (ln1g, d_ln1g), (ln1b, d_ln1b),
                               (tri, d_tri), (ci, d_ci), (amask, d_amask), (gmask, d_gmask)):
                    S.dma("sp", cst, lambda e, b_=b_, d_=d_: e.dma_start(out=b_.t[:], in_=d_), w=[b_])
                S.barrier()
                OP("act", lambda e: e.activation(out=esink.t[:], in_=esink.t[:], func=AF.Exp), r=[esink], w=[esink])
                for m in range(2):
                    OP("dve", lambda e, m=m: e.tensor_scalar(out=amask_e.t[:, m, :], in0=amask.t[:, m, :],
                                                             scalar1=flags.t[:, 96 + m:97 + m], scalar2=None, op0=ALU.mult),
                       r=[amask, flags], w=[amask_e])
                sbst = sbuf(pa, "sbst", [128, 32, 256], BF16)
                kT_all = sbuf(pa, "kT_all", [64, 18, 256], BF16); v_all = sbuf(pa, "v_all", [128, 18, 130], BF16)
                kTc = sbuf(pa, "kTc", [64, 2, 256], BF16); vc = sbuf(pa, "vc", [128, 2, 130], BF16)
                OP("pool", lambda e: e.memset(v_all.t[:], 1.0), w=[v_all])
                OP("pool", lambda e: e.memset(vc.t[:], 1.0), w=[vc])
                hT2 = [sbuf(pa, "hT%d" % i, [128, 8, 128], BF16) for i in range(2)]
                for h_ in hT2:
                    h_.c = []
                    for _ in range(8):
                        v_ = Buf(h_.t)
                        h_.c.append(v_)
                vsb2 = [sbuf(pa, "vsb%d" % i, [128, 512], BF16) for i in range(2)]
                curb = {"hT": hT2[0], "vsb": vsb2[0]}
                qk = sbuf(pa, "qk", [128, 512])
                zT = [sbuf(pa, "zT", [128, 128])] * 2
                OP("dve", lambda e: e.memset(zT[0].t[:], 0.0), w=[zT[0]])
                OP("dve", lambda e: e.memset(zT[0].t[0:17, :], 1.0), w=[zT[0]])
                sp_ = [sbuf(pa, "sp", [128, 256])] * 2
                et = sbuf(pa, "et", [128, 256])
                Eb = [sbuf(pa, "Eb", [128, 256])] * 2
                Ei = [sbuf(pa, "Ei", [128, 256])] * 2
                Er = [sbuf(pa, "Er", [128, 256])] * 2
                qe = [sbuf(pa, "qe", [128, 256])] * 2
                ke = [sbuf(pa, "ke", [128, 256])] * 2
                kd = [sbuf(pa, "kd", [128, 256], BF16)] * 2
                Tz = [sbuf(pa, "Tz%d" % i, [128, 4, 128], BF16) for i in range(2)]
                keT = [sbuf(pa, "keT%d" % i, [128, 2, 128], BF16) for i in range(2)]
                ATz = sbuf(pa, "ATz", [128, 2, 4, 2, 64], BF16)
                Sfb = [sbuf(pa, "Sfb%d" % i, [128, 2, 128], BF16) for i in range(2)]
                for b_ in (Tz[0], Tz[1], ATz):
                    OP("pool", lambda e, b_=b_: e.memset(b_.t[:], 0.0), w=[b_])
                asb = [sbuf(pa, "asb", [128, 2, 2])] * 2
                Sst = {0: [sbuf(pa, "Sf%d" % i, [128, 2, 128]) for i in range(2)],
                       1: [sbuf(pa, "Sb%d" % i, [128, 2, 128]) for i in range(2)]}
                Scur = {0: 0, 1: 0}
                Stmp = sbuf(pa, "Stmp", [128, 2, 128])
                for dd in range(2):
                    OP("dve", lambda e, dd=dd: e.memset(Sst[dd][0].t[:], 0.0), w=[Sst[dd][0]])
                rsb = sbuf(pa, "rsb", [128, 512])
                ss = sbuf(pa, "ss", [128, 4]); rstd4 = sbuf(pa, "rstd4", [128, 4])
                on = sbuf(pa, "on", [128, 512]); junk = on
                aqs = on; qrot = sbuf(pa, "qrot", [128, 512]); rt = rsb
                akv = sbuf(pa, "akv", [128, 256]); krot = sbuf(pa, "krot", [128, 128])
                cs = sbuf(pa, "cs", [128, 32]); sn = sbuf(pa, "sn", [128, 32])
                qTs = sbuf(pa, "qTs", [64, 8, 128], BF16)
                Pall = sbuf(pa, "Pall", [128, 5, 512], BF16)
                den = sbuf(pa, "den", [128, 4])
                cat2 = [sbuf(pa, "cat%d" % i, [128, 1024]) for i in range(2)]
                aqs_b = sbuf(pa, "aqs_b", [128, 512]); rt_b = sbuf(pa, "rt_b", [128, 512])
                own_ctx = {}
                stats = sbuf(pa, "stats", [128, 12]); mv = sbuf(pa, "mv", [128, 2]); rs1 = sbuf(pa, "rs1", [128, 1])

                def front(src_ap, row):
                    xb = xt[xt_i[0] % 2]
                    slot = ld[xt_i[0] % 2]
                    curb["hT"] = hT2[xt_i[0] % 2]; curb["vsb"] = vsb2[xt_i[0] % 2]
                    hT = curb["hT"]
                    xt_i[0] += 1
                    S.dma("sp", slot, lambda e: e.dma_start(out=xb.t[:], in_=src_ap), w=[xb])

                    def evac(c0, n, pb):
                        for c in range(c0, c0 + n):
                            if c0 == 0:
                                OP("act", lambda e, c=c, pb=pb, c0=c0: e.activation(
                                    out=hT.t[:, c, :], in_=pb.t[:, (c - c0) * 128:(c - c0 + 1) * 128], func=AF.Identity,
                                    scale=sc1p.t[:, c, row:row + 1], bias=modT.t[:, c, row:row + 1]),
                                   r=[pb, sc1p, modT], w=[hT.c[c]])
                            else:
                                OP("dve", lambda e, c=c, pb=pb, c0=c0: e.tensor_scalar(
                                    out=hT.t[:, c, :], in0=pb.t[:, (c - c0) * 128:(c - c0 + 1) * 128],
                                    scalar1=sc1p.t[:, c, row:row + 1], scalar2=modT.t[:, c, row:row + 1], op0=ALU.mult, op1=ALU.add),
                                   r=[pb, sc1p, modT], w=[hT.c[c]])
                    transpose_to(xb, 8, evac)
                    return xb

                def inproj(col0, ncols, pb, pcol=0):
                    hT = curb["hT"]
                    for kc in range(8):
                        OP("pe", lambda e, kc=kc: e.matmul(pb.t[:, pcol:pcol + ncols], lhsT=hT.t[:, kc, :],
                                                           rhs=win.t[:, kc, col0:col0 + ncols], start=(kc == 0), stop=(kc == 7)),
                           r=[hT.c[kc], win], w=[pb])

                def gates(dd, flagcol=None):
                    hT = curb["hT"]
                    pz = bank()
                    for kc in range(8):
                        OP("pe", lambda e, kc=kc: e.matmul(pz.t[0:16, 0:128], lhsT=win.t[:, kc, 2304 + 16 * dd:2320 + 16 * dd],
                                                           rhs=hT.t[:, kc, :], start=(kc == 0), stop=(kc == 7)), r=[hT.c[kc], win], w=[pz])
                    OP("act", lambda e: e.activation(out=zT[dd].t[0:16, :], in_=pz.t[0:16, 0:128], func=AF.Identity), r=[pz], w=[zT[dd]])
                    pg = bank()
                    OP("pe", lambda e: e.matmul(pg.t[:, 0:256], lhsT=zT[dd].t[:], rhs=w2.t[:, dd * 256:(dd + 1) * 256],
                                                start=True, stop=True), r=[zT[dd], w2], w=[pg])
                    OP("act", lambda e: e.activation(out=et.t[:], in_=pg.t[:, 0:256], func=AF.Exp, scale=-1.0), r=[pg], w=[et])
                    OP("act", lambda e: e.activation(out=sp_[dd].t[:], in_=et.t[:], func=AF.Ln, bias=1.0), r=[et], w=[sp_[dd]])
                    if flagcol is not None:
                        OP("dve", lambda e: e.tensor_scalar(out=sp_[dd].t[:], in0=sp_[dd].t[:], scalar1=flags.t[:, flagcol:flagcol + 1],
                                                            scalar2=None, op0=ALU.mult), r=[sp_[dd], flags], w=[sp_[dd]])

                def decay_k(dd, ksrc, flagcol=None):
                    pr = bank()
                    OP("pe", lambda e: e.matmul(pr.t[:, 0:256], lhsT=tri.t[:, 1 + 2 * dd, :], rhs=sp_[dd].t[:], start=True, stop=True),
                       r=[tri, sp_[dd]], w=[pr])
                    OP("act", lambda e: e.activation(out=Er[dd].t[:], in_=pr.t[:, 0:256], func=AF.Exp), r=[pr], w=[Er[dd]])
                    if flagcol is None:
                        OP("dve", lambda e: e.tensor_tensor(out=kd[dd].t[:], in0=ksrc.t[:, 256:512], in1=Er[dd].t[:], op=ALU.mult),
                           r=[ksrc, Er[dd]], w=[kd[dd]])
                    else:
                        OP("dve", lambda e: e.scalar_tensor_tensor(out=kd[dd].t[:], in0=ksrc.t[:, 256:512],
                                                                   scalar=flags.t[:, flagcol:flagcol + 1], in1=Er[dd].t[:],
                                                                   op0=ALU.mult, op1=ALU.mult), r=[ksrc, Er[dd], flags], w=[kd[dd]])
                    pa_ = bank()
                    for hp in range(2):
                        OP("pe", lambda e, hp=hp: e.matmul(pa_.t[:, hp * 2:hp * 2 + 2], lhsT=sp_[dd].t[:, hp * 128:(hp + 1) * 128],
                                                           rhs=ci.t[:], start=True, stop=True), r=[sp_[dd], ci], w=[pa_])
                    OP("act", lambda e: e.activation(out=asb[dd].t[:], in_=pa_.t[:, 0:4].rearrange("p (a b) -> p a b", a=2),
                                                     func=AF.Exp), r=[pa_], w=[asb[dd]])

                def state_update(dd, c, vsrc):
                    pp = bank()
                    for h in range(4):
                        hp, par = h // 2, h % 2
                        OP("pe", lambda e, h=h, hp=hp, par=par: e.matmul(
                            pp.t[par * 64:(par + 1) * 64, hp * 128:(hp + 1) * 128],
                            lhsT=kd[dd].t[c * 64:(c + 1) * 64, h * 64:(h + 1) * 64],
                            rhs=vsrc.t[c * 64:(c + 1) * 64, h * 128:(h + 1) * 128], start=True, stop=True),
                           r=[kd[dd], vsrc], w=[pp])
                    so = Sst[dd][Scur[dd]]
                    sn_ = Sst[dd][1 - Scur[dd]]
                    OP("dve", lambda e: e.tensor_tensor(out=Stmp.t[:], in0=so.t[:],
                                                        in1=asb[dd].t[:, :, c:c + 1].broadcast_to([128, 2, 128]), op=ALU.mult),
                       r=[so, asb[dd]], w=[Stmp])
                    OP("dve", lambda e: e.tensor_tensor(out=sn_.t[:], in0=Stmp.t[:],
                                                        in1=pp.t[:, 0:256].rearrange("p (a b) -> p a b", a=2), op=ALU.add),
                       r=[Stmp, pp], w=[sn_])
                    Scur[dd] = 1 - Scur[dd]

                def state_tile(src_ap, row, dd, flagcol=None, store=None):
                    front(src_ap, row)
                    vsb = curb["vsb"]
                    pk = bank()
                    inproj(256, 256, pk, 256)
                    pv = bank()
                    inproj(512, 512, pv)
                    OP("act", lambda e: e.activation(out=vsb.t[:], in_=pv.t[:], func=AF.Identity), r=[pv], w=[vsb])
                    gates(dd, flagcol)
                    decay_k(dd, pk, flagcol)
                    for c in ((0, 1) if dd == 0 else (1, 0)):
                        if store is not None:
                            cur = Sst[dd][Scur[dd]]
                            OP("pool", lambda e, c=c, cur=cur: e.tensor_copy(out=sbst.t[:, store * 2 + c, :],
                                                                             in_=cur.t[:].rearrange("p a b -> p (a b)")),
                               r=[cur], w=[sbst])
                        state_update(dd, c, vsb)

                ksb2 = [sbuf(pa, "ksb%d" % i, [128, 512]) for i in range(2)]
                zT2 = [sbuf(pa, "zTp%d" % i, [128, 128]) for i in range(2)]
                for z_ in zT2:
                    OP("dve", lambda e, z_=z_: e.memset(z_.t[:], 0.0), w=[z_])
                    OP("dve", lambda e, z_=z_: e.memset(z_.t[0:17, :], 1.0), w=[z_])
                st_i = [0]

                def state_A1(n, src_ap, row):
                    front(src_ap, row)
                    hT = curb["hT"]
                    return dict(hT=hT, vsb=vsb2[n % 2], ksb=ksb2[n % 2], zTb=zT2[n % 2])

                def inproj_h(hT, col0, ncols, pb, pcol=0):
                    for kc in range(8):
                        OP("pe", lambda e, kc=kc: e.matmul(pb.t[:, pcol:pcol + ncols], lhsT=hT.t[:, kc, :],
                                                           rhs=win.t[:, kc, col0:col0 + ncols], start=(kc == 0), stop=(kc == 7)),
                           r=[hT.c[kc], win], w=[pb])

                def state_A2(cx):
                    pk = bank()
                    inproj_h(cx["hT"], 256, 256, pk, 256)
                    ksb = cx["ksb"]
                    OP("dve", lambda e: e.tensor_copy(out=ksb.t[:, 256:512], in_=pk.t[:, 256:512]), r=[pk], w=[ksb])

                def state_A3(cx):
                    pv = bank()
                    inproj_h(cx["hT"], 512, 512, pv)
                    vsb = cx["vsb"]
                    OP("act", lambda e: e.activation(out=vsb.t[:], in_=pv.t[:], func=AF.Identity), r=[pv], w=[vsb])

                def state_A4(cx, dd):
                    pz = bank()
                    hT, zTb = cx["hT"], cx["zTb"]
                    for kc in range(8):
                        OP("pe", lambda e, kc=kc: e.matmul(pz.t[0:16, 0:128], lhsT=win.t[:, kc, 2304 + 16 * dd:2320 + 16 * dd],
                                                           rhs=hT.t[:, kc, :], start=(kc == 0), stop=(kc == 7)), r=[hT.c[kc], win], w=[pz])
                    OP("act", lambda e: e.activation(out=zTb.t[0:16, :], in_=pz.t[0:16, 0:128], func=AF.Identity), r=[pz], w=[zTb])

                def state_B1(cx, dd, flagcol):
                    zTb = cx["zTb"]
                    pg = bank()
                    OP("pe", lambda e: e.matmul(pg.t[:, 0:256], lhsT=zTb.t[:], rhs=w2.t[:, dd * 256:(dd + 1) * 256],
                                                start=True, stop=True), r=[zTb, w2], w=[pg])
                    OP("act", lambda e: e.activation(out=et.t[:], in_=pg.t[:, 0:256], func=AF.Exp, scale=-1.0), r=[pg], w=[et])
                    OP("act", lambda e: e.activation(out=sp_[dd].t[:], in_=et.t[:], func=AF.Ln, bias=1.0), r=[et], w=[sp_[dd]])
                    if flagcol is not None:
                        OP("dve", lambda e: e.tensor_scalar(out=sp_[dd].t[:], in0=sp_[dd].t[:], scalar1=flags.t[:, flagcol:flagcol + 1],
                                                            scalar2=None, op0=ALU.mult), r=[sp_[dd], flags], w=[sp_[dd]])

                def state_B2(cx, dd, flagcol):
                    decay_k(dd, cx["ksb"], flagcol)

                def state_B3(cx, dd, store, c):
                    if store is not None:
                        cur = Sst[dd][Scur[dd]]
                        OP("pool", lambda e, c=c, cur=cur: e.tensor_copy(out=sbst.t[:, store * 2 + c, :],
                                                                         in_=cur.t[:].rearrange("p a b -> p (a b)")),
                           r=[cur], w=[sbst])
                    state_update(dd, c, cx["vsb"])

                def run_state_jobs(jobs):
                    N = len(jobs)
                    cxs = {}
                    for step in range(N + 2):
                        if step < N:
                            cxs[step] = state_A1(step, jobs[step][0], jobs[step][1])
                        m, b_ = step - 1, step - 2
                        hm = 0 <= m < N
                        hb = 0 <= b_ < N
                        ORD = int(os.environ.get("K_ORD", "1"))
                        if hb:
                            _, _, ddb, flb, stb = jobs[b_]
                            cs_ = (0, 1) if ddb == 0 else (1, 0)
                        if ORD == 0:
                            seq = ["A2", "A3", "A4", "B1", "B2", "B3", "B4"]
                        elif ORD == 1:
                            seq = ["B1", "A2", "B2", "A3", "B3", "B4", "A4"]
                        elif ORD == 2:
                            seq = ["B1", "A2", "B2", "A3", "A4", "B3", "B4"]
                        else:
                            seq = ["A2", "B1", "A3", "B2", "A4", "B3", "B4"]
                        for st_ in seq:
                            if st_[0] == "A" and hm:
                                if st_ == "A2":
                                    state_A2(cxs[m])
                                    convert_chunk(pace=[cxs[m]["ksb"]])
                                elif st_ == "A3": state_A3(cxs[m])
                                else: state_A4(cxs[m], jobs[m][2])
                            if st_[0] == "B" and hb:
                                if st_ == "B1": state_B1(cxs[b_], ddb, flb)
                                elif st_ == "B2": state_B2(cxs[b_], ddb, flb)
                                elif st_ == "B3": state_B3(cxs[b_], ddb, stb, cs_[0])
                                else: state_B3(cxs[b_], ddb, stb, cs_[1])
                        if hb:
                            del cxs[b_]

                def rope(src, dst, H, tmp):
                    v5 = lambda b: b.t[:, 0:H * 64].rearrange("p (h a f d) -> p h a f d", h=H, a=2, f=2, d=16)
                    cb = cs.t[:].rearrange("p (a d) -> p a d", a=2).unsqueeze(1).broadcast_to([128, H, 2, 16])
                    sb_ = sn.t[:].rearrange("p (a d) -> p a d", a=2).unsqueeze(1).broadcast_to([128, H, 2, 16])
                    x1, x2 = v5(src)[:, :, :, 0, :], v5(src)[:, :, :, 1, :]
                    o1, o2 = v5(dst)[:, :, :, 0, :], v5(dst)[:, :, :, 1, :]
                    t1, t2 = v5(tmp)[:, :, :, 0, :], v5(tmp)[:, :, :, 1, :]
                    OP("pool", lambda e: e.tensor_tensor(out=o1, in0=x1, in1=cb, op=ALU.mult), r=[src, cs], w=[dst])
                    OP("pool", lambda e: e.tensor_tensor(out=t1, in0=x2, in1=sb_, op=ALU.mult), r=[src, sn], w=[tmp])
                    OP("pool", lambda e: e.tensor_tensor(out=o1, in0=o1, in1=t1, op=ALU.subtract), r=[dst, tmp], w=[dst])
                    OP("pool", lambda e: e.tensor_tensor(out=o2, in0=x2, in1=cb, op=ALU.mult), r=[src, cs], w=[dst])
                    OP("pool", lambda e: e.tensor_tensor(out=t2, in0=x1, in1=sb_, op=ALU.mult), r=[src, sn], w=[tmp])
                    OP("pool", lambda e: e.tensor_tensor(out=o2, in0=o2, in1=t2, op=ALU.add), r=[dst, tmp], w=[dst])

                def kv_tile(src_ap, row, j, is_ctx):
                    front(src_ap, row)
                    pb = bank()
                    inproj(2048, 256, pb)
                    OP("act", lambda e: e.activation(out=akv.t[:], in_=pb.t[:, 0:256], func=AF.Identity), r=[pb], w=[akv])
                    if is_ctx:
                        ksrc, kdst, vdst = akv, kTc, vc
                    else:
                        S.dma("sp", csl, lambda e: e.dma_start(out=cs.t[:], in_=d_cos[j * 128:(j + 1) * 128, :]), w=[cs])
                        S.dma("sp", snl, lambda e: e.dma_start(out=sn.t[:], in_=d_sin[j * 128:(j + 1) * 128, :]), w=[sn])
                        rope(akv, krot, 2, rt)
                        ksrc, kdst, vdst = krot, kT_all, v_all
                    OP("pool", lambda e: e.tensor_copy(
                        out=vdst.t[:, j, :].rearrange("p (g d) -> p g d", g=2)[:, :, 0:64],
                        in_=akv.t[:, 128:256].rearrange("p (g d) -> p g d", g=2)), r=[akv], w=[vdst])

                    def evac(c0, n, pb2):
                        OP("act", lambda e: e.activation(out=kdst.t[:, j, :], in_=pb2.t[0:64, 0:256], func=AF.Identity),
                           r=[pb2], w=[kdst])
                    transpose_to(ksrc, 2, evac, width=64)

                def own_P1(i):
                    j = i + 1
                    xb = front(d_xown[j * 128:(j + 1) * 128, :], 0)
                    vsb = curb["vsb"]; catT = curb["hT"]
                    cat = cat2[i % 2]
                    own_ctx[i] = (xb, catT, cat)
                    yield
                    pqk = bank(); inproj(0, 512, pqk)
                    OP("act", lambda e: e.activation(out=qk.t[:], in_=pqk.t[:], func=AF.Identity), r=[pqk], w=[qk])
                    pv = bank(); inproj(512, 512, pv)
                    OP("act", lambda e: e.activation(out=vsb.t[:], in_=pv.t[:], func=AF.Identity), r=[pv], w=[vsb])
                    pr_ = bank(); inproj(1024, 512, pr_)
                    OP("act", lambda e: e.activation(out=rsb.t[:], in_=pr_.t[:], func=AF.Silu), r=[pr_], w=[rsb])
                    yield
                    for dd in range(2):
                        yield
                        gates(dd)
                        pbn = bank()
                        OP("pe", lambda e, dd=dd, pbn=pbn: e.matmul(pbn.t[:, 0:256], lhsT=tri.t[:, 2 * dd, :], rhs=sp_[dd].t[:],
                                                                    start=True, stop=True), r=[tri, sp_[dd]], w=[pbn])
                        OP("act", lambda e, dd=dd, pbn=pbn: e.activation(out=Eb[dd].t[:], in_=pbn.t[:, 0:256], func=AF.Exp),
                           r=[pbn], w=[Eb[dd]])
                        OP("act", lambda e, dd=dd, pbn=pbn: e.activation(out=Ei[dd].t[:], in_=pbn.t[:, 0:256], func=AF.Exp, scale=-1.0),
                           r=[pbn], w=[Ei[dd]])
                        OP("dve", lambda e, dd=dd: e.scalar_tensor_tensor(out=qe[dd].t[:], in0=qk.t[:, 0:256], scalar=0.125, in1=Eb[dd].t[:],
                                                                          op0=ALU.mult, op1=ALU.mult), r=[qk, Eb[dd]], w=[qe[dd]])
                        OP("dve", lambda e, dd=dd: e.tensor_tensor(out=ke[dd].t[:], in0=qk.t[:, 256:512], in1=Ei[dd].t[:], op=ALU.mult),
                           r=[qk, Ei[dd]], w=[ke[dd]])
                        yield
                        if dd == 0:
                            decay_k(dd, qk)
                            yield
                        pT = bank()
                        for idx, srcb in enumerate((qe[dd], qe[dd], ke[dd], ke[dd])):
                            hp = idx % 2
                            OP("pe", lambda e, idx=idx, hp=hp, srcb=srcb, pT=pT: e.transpose(
                                out=pT.t[:, idx * 128:(idx + 1) * 128], in_=srcb.t[:, hp * 128:(hp + 1) * 128], identity=ident.t[:]),
                               r=[srcb, ident], w=[pT])
                        OP("act", lambda e, dd=dd, pT=pT: e.activation(out=keT[dd].t[:].rearrange("p a b -> p (a b)"), in_=pT.t[:, 256:512],
                                                                       func=AF.Identity), r=[pT], w=[keT[dd]])
                        for par in range(2):
                            OP("act", lambda e, dd=dd, pT=pT, par=par: e.activation(
                                out=Tz[dd].t[par * 64:(par + 1) * 64, par::2, :],
                                in_=pT.t[par * 64:(par + 1) * 64, 0:256].rearrange("p (a b) -> p a b", a=2), func=AF.Identity),
                               r=[pT], w=[Tz[dd]])
                    yield
                    pAT = bank()
                    for dd in range(2):
                        for c in range(2):
                            for h in range(4):
                                hp = h // 2
                                OP("pe", lambda e, dd=dd, c=c, h=h, hp=hp: e.matmul(
                                    pAT.t[c * 64:(c + 1) * 64, (dd * 4 + h) * 64:(dd * 4 + h + 1) * 64],
                                    lhsT=keT[dd].t[:, hp, c * 64:(c + 1) * 64],
                                    rhs=Tz[dd].t[:, h, c * 64:(c + 1) * 64], start=True, stop=True),
                                   r=[keT[dd], Tz[dd]], w=[pAT])
                    yield
                    for c in range(2):
                        OP("dve", lambda e, c=c: e.tensor_tensor(
                            out=ATz.t[c * 64:(c + 1) * 64, :, :, c, :],
                            in0=pAT.t[c * 64:(c + 1) * 64, :].rearrange("p (d h c) -> p d h c", d=2, h=4),
                            in1=gmask.t[c * 64:(c + 1) * 64, :, :].unsqueeze(2).broadcast_to([64, 2, 4, 64]), op=ALU.mult),
                           r=[pAT, gmask], w=[ATz])
                    po = bank()
                    for c in range(2):
                        sf_ = Sst[0][Scur[0]]
                        sf = Sfb[c]
                        OP("pool", lambda e, sf=sf, sf_=sf_: e.tensor_copy(out=sf.t[:], in_=sf_.t[:]), r=[sf_], w=[sf])
                        for h in range(4):
                            hp = h // 2
                            outp = po.t[c * 64:(c + 1) * 64, h * 128:(h + 1) * 128]
                            vv = vsb.t[:, h * 128:(h + 1) * 128]
                            OP("pe", lambda e, c=c, h=h, outp=outp, vv=vv: e.matmul(
                                outp, lhsT=ATz.t[:, 0, h, c, :], rhs=vv, start=True, stop=False), r=[ATz, vsb], w=[po])
                            OP("pe", lambda e, c=c, h=h, hp=hp, outp=outp, sf=sf: e.matmul(
                                outp, lhsT=Tz[0].t[:, h, c * 64:(c + 1) * 64], rhs=sf.t[:, hp, :], start=False, stop=False),
                               r=[Tz[0], sf], w=[po])
                            OP("pe", lambda e, c=c, h=h, outp=outp, vv=vv: e.matmul(
                                outp, lhsT=ATz.t[:, 1, h, c, :], rhs=vv, start=False, stop=False), r=[ATz, vsb], w=[po])
                            OP("pe", lambda e, c=c, h=h, hp=hp, outp=outp: e.matmul(
                                outp, lhsT=Tz[1].t[:, h, c * 64:(c + 1) * 64],
                                rhs=sbst.t[:, i * 2 + c, hp * 128:(hp + 1) * 128], start=False, stop=True), r=[Tz[1], sbst], w=[po])
                            if h % 2 == 1:
                                yield
                        state_update(0, c, vsb)
                        yield
                    for h in range(4):
                        OP("act", lambda e, h=h: e.activation(out=junk.t[:, h * 128:(h + 1) * 128], in_=po.t[:, h * 128:(h + 1) * 128],
                                                              func=AF.Square, accum_out=ss.t[:, h:h + 1]), r=[po], w=[junk, ss])
                    OP("dve", lambda e: e.tensor_scalar(out=rstd4.t[:], in0=ss.t[:], scalar1=1.0 / 128, scalar2=eps_t.t[:, 0:1],
                                                        op0=ALU.mult, op1=ALU.add), r=[ss, eps_t], w=[rstd4])
                    OP("act", lambda e: e.activation(out=rstd4.t[:], in_=rstd4.t[:], func=AF.Sqrt), r=[rstd4], w=[rstd4])
                    OP("dve", lambda e: e.reciprocal(out=rstd4.t[:], in_=rstd4.t[:]), r=[rstd4], w=[rstd4])
                    on3 = on.t[:].rearrange("p (h d) -> p h d", h=4)
                    OP("dve", lambda e: e.tensor_tensor(out=on3, in0=po.t[:].rearrange("p (h d) -> p h d", h=4),
                                                        in1=rstd4.t[:].unsqueeze(2).broadcast_to([128, 4, 128]), op=ALU.mult),
                       r=[po, rstd4], w=[on])
                    OP("pool", lambda e: e.tensor_tensor(out=on3, in0=on3, in1=gng.t[:].unsqueeze(1).broadcast_to([128, 4, 128]),
                                                         op=ALU.mult), r=[on, gng], w=[on])
                    OP("pool", lambda e: e.tensor_tensor(out=cat.t[:, 0:512], in0=on.t[:], in1=rsb.t[:], op=ALU.mult),
                       r=[on, rsb], w=[cat])
                    yield

                def own_P2(i):
                    j = i + 1
                    xb, catT, cat = own_ctx.pop(i)
                    r1 = cat; x1o = cat
                    aqs = aqs_b; rt = rt_b
                    paq = bank(); inproj_h(catT, 1536, 512, paq)
                    OP("act", lambda e: e.activation(out=aqs.t[:], in_=paq.t[:], func=AF.Identity), r=[paq], w=[aqs])
                    yield
                    S.dma("sp", csl, lambda e: e.dma_start(out=cs.t[:], in_=d_cos[j * 128:(j + 1) * 128, :]), w=[cs])
                    S.dma("sp", snl, lambda e: e.dma_start(out=sn.t[:], in_=d_sin[j * 128:(j + 1) * 128, :]), w=[sn])
                    rope(aqs, qrot, 8, rt)
                    yield

                    def evq(c0, n, pb2):
                        OP("act", lambda e: e.activation(out=qTs.t[:, c0:c0 + n, :].rearrange("p a b -> p (a b)"),
                                                         in_=pb2.t[0:64, 0:n * 128], func=AF.Identity, scale=0.125), r=[pb2], w=[qTs])
                    transpose_to(qrot, 8, evq, width=64)
                    yield

                    def att_group(g):
                        kts = [(kT_all, v_all, j - 1, amask_e if i == 0 else amask, 0), (kT_all, v_all, j, None, 0),
                               (kT_all, v_all, j + 1, amask_e if i == 15 else amask, 1), (kTc, vc, 0, None, 0), (kTc, vc, 1, None, 0)]
                        for n_, (kb, vb, jj, mk, mi) in enumerate(kts):
                            pst = bank()
                            OP("pe", lambda e, kb=kb, jj=jj, pst=pst: e.matmul(
                                pst.t[:], lhsT=kb.t[:, jj, g * 128:(g + 1) * 128],
                                rhs=qTs.t[:, 4 * g:4 * g + 4, :].rearrange("p a b -> p (a b)"), start=True, stop=True),
                               r=[kb, qTs], w=[pst])
                            OP("act", lambda e, n_=n_, pst=pst: e.activation(out=Pall.t[:, n_, :], in_=pst.t[:], func=AF.Exp),
                               r=[pst], w=[Pall])
                            if mk is not None:
                                OP("dve", lambda e, n_=n_, mk=mk, mi=mi: e.tensor_tensor(
                                    out=Pall.t[:, n_, :].rearrange("p (h q) -> p h q", h=4),
                                    in0=Pall.t[:, n_, :].rearrange("p (h q) -> p h q", h=4),
                                    in1=mk.t[:, mi:mi + 1, :].broadcast_to([128, 4, 128]), op=ALU.mult), r=[Pall, mk], w=[Pall])
                            yield
                        pO = bank()
                        for hh in range(4):
                            for n_, (kb, vb, jj, mk, mi) in enumerate(kts):
                                OP("pe", lambda e, hh=hh, n_=n_, vb=vb, jj=jj: e.matmul(
                                    pO.t[:, hh * 65:(hh + 1) * 65], lhsT=Pall.t[:, n_, hh * 128:(hh + 1) * 128],
                                    rhs=vb.t[:, jj, g * 65:(g + 1) * 65], start=(n_ == 0), stop=(n_ == 4)), r=[Pall, vb], w=[pO])
                            yield
                        pO3 = pO.t[:, 0:260].rearrange("p (h d) -> p h d", h=4)
                        OP("dve", lambda e, pO3=pO3: e.tensor_tensor(out=den.t[:].unsqueeze(2), in0=pO3[:, :, 64:65],
                                                                     in1=esink.t[:, 4 * g:4 * g + 4].unsqueeze(2), op=ALU.add),
                           r=[pO, esink], w=[den])
                        OP("dve", lambda e: e.reciprocal(out=den.t[:], in_=den.t[:]), r=[den], w=[den])
                        OP("dve", lambda e, pO3=pO3: e.tensor_tensor(
                            out=cat.t[:, 512 + g * 256:512 + (g + 1) * 256].rearrange("p (h d) -> p h d", h=4),
                            in0=pO3[:, :, 0:64], in1=den.t[:].unsqueeze(2).broadcast_to([128, 4, 64]), op=ALU.mult),
                           r=[pO, den], w=[cat])
                    for g_ in range(2):
                        yield from att_group(g_)
                        yield
                    if os.environ.get("K_DBG") == "cat":
                        S.dma("pool", sts, lambda e: e.dma_start(out=d_x1[i * 128:(i + 1) * 128, :], in_=cat.t[:]), r=[cat])
                        return
                    def evc(c0, n, pb2):
                        OP("act", lambda e: e.activation(out=catT.t[:, c0:c0 + n, :].rearrange("p a b -> p (a b)"),
                                                         in_=pb2.t[:, 0:n * 128], func=AF.Identity), r=[pb2], w=catT.c[c0:c0 + n])
                    transpose_to(cat, 8, evc)
                    yield
                    for hf in range(2):
                        yield
                        py = bank()
                        for kc in range(8):
                            OP("pe", lambda e, kc=kc, py=py, hf=hf: e.matmul(py.t[:], lhsT=catT.t[:, kc, :],
                                                                      rhs=wout.t[:, kc, hf * 512:(hf + 1) * 512],
                                                                      start=(kc == 0), stop=(kc == 7)), r=[catT.c[kc], wout], w=[py])
                        OP("dve", lambda e, py=py, hf=hf: e.tensor_tensor(out=r1.t[:, hf * 512:(hf + 1) * 512], in0=py.t[:],
                                                                   in1=g1bc.t[:, hf * 512:(hf + 1) * 512], op=ALU.mult),
                           r=[py, g1bc], w=[r1])
                    OP("dve", lambda e: e.scalar_tensor_tensor(out=r1.t[:], in0=xb.t[:], scalar=ALPHA, in1=r1.t[:],
                                                               op0=ALU.mult, op1=ALU.add), r=[xb, r1], w=[r1])
                    layernorm(r1, x1o, ln1g, ln1b, stats, mv, rs1)
                    S.dma("pool", sts, lambda e: e.dma_start(out=d_x1[i * 128:(i + 1) * 128, :], in_=x1o.t[:]), r=[x1o])

                def layernorm(src, dst, gb, bb, stats, mv, rs1):
                    for hf in range(2):
                        OP("dve", lambda e, hf=hf: e.bn_stats(out=stats.t[:, hf * 6:(hf + 1) * 6], in_=src.t[:, hf * 512:(hf + 1) * 512]),
                           r=[src], w=[stats])
                    OP("dve", lambda e: e.bn_aggr(out=mv.t[:], in_=stats.t[:]), r=[stats], w=[mv])
                    OP("dve", lambda e: e.tensor_scalar(out=rs1.t[:], in0=mv.t[:, 1:2], scalar1=eps_t.t[:, 0:1], scalar2=None, op0=ALU.add),
                       r=[mv, eps_t], w=[rs1])
                    OP("act", lambda e: e.activation(out=rs1.t[:], in_=rs1.t[:], func=AF.Sqrt), r=[rs1], w=[rs1])
                    OP("dve", lambda e: e.reciprocal(out=rs1.t[:], in_=rs1.t[:]), r=[rs1], w=[rs1])
                    OP("dve", lambda e: e.tensor_scalar(out=dst.t[:], in0=src.t[:], scalar1=mv.t[:, 0:1], scalar2=rs1.t[:, 0:1],
                                                        op0=ALU.subtract, op1=ALU.mult), r=[src, mv, rs1], w=[dst])
                    OP("pool", lambda e: e.tensor_tensor(out=dst.t[:], in0=dst.t[:], in1=gb.t[:], op=ALU.mult), r=[dst, gb], w=[dst])
                    OP("pool", lambda e: e.tensor_tensor(out=dst.t[:], in0=dst.t[:], in1=bb.t[:], op=ALU.add), r=[dst, bb], w=[dst])

                for t in range(2):
                    kv_tile(d_ctx[t * 128:(t + 1) * 128, :], 1, t, True)
                jobs = []
                for t in range(2):
                    jobs.append((d_ctx[t * 128:(t + 1) * 128, :], 1, 0, None, None))
                for t in (1, 0):
                    jobs.append((d_ctx[t * 128:(t + 1) * 128, :], 1, 1, None, None))
                for t in range(NPRE):
                    jobs.append((d_xpf[t * 128:(t + 1) * 128, :], 0, 0, t, None))
                for t in range(NPRE):
                    jobs.append((d_xpb[t * 128:(t + 1) * 128, :], 0, 1, 48 + t, None))
                for i in range(15, -1, -1):
                    jobs.append((d_xown[(i + 1) * 128:(i + 2) * 128, :], 0, 1, None, i))
                run_state_jobs(jobs)
                convert_tables()
                for j in range(18):
                    kv_tile(d_xown[j * 128:(j + 1) * 128, :], 0, j, False)
                for i in range(17):
                    g1 = own_P1(i) if i < 16 else iter(())
                    g2 = own_P2(i - 1) if i >= 1 else iter(())
                    a1 = a2 = True
                    while a1 or a2:
                        if a1:
                            bank_sel[0] = 0
                            a1 = next(g1, "END") != "END"
                        if a2:
                            bank_sel[0] = 1
                            a2 = next(g2, "END") != "END"
                bank_sel[0] = None
                S.barrier()

        if mode != "A":
            with ExitStack() as pbs_:
                wq = sbuf(pbs_, "wq", [128, 8, 1024]); skT = sbuf(pbs_, "skT", [128, 8, 128])
                ln2g = sbuf(pbs_, "ln2g", [128, 1024]); ln2b = sbuf(pbs_, "ln2b", [128, 1024])
                iota = sbuf(pbs_, "iota", [128, 16])
                S.dma("sp", cst, lambda e: e.dma_start(out=wq.t[:], in_=d_wq.rearrange("(k p) n -> p k n", p=128)), w=[wq])
                for b_, d_ in ((skT, d_skT), (ln2g, d_ln2g), (ln2b, d_ln2b), (iota, d_iota)):
                    S.dma("sp", cst, lambda e, b_=b_, d_=d_: e.dma_start(out=b_.t[:], in_=d_), w=[b_])
                S.barrier()
                if mode == "B":
                    convert_tables()
                identb = sbuf(pbs_, "identb", [128, 128], BF16)
                OP("dve", lambda e: e.tensor_copy(out=identb.t[:], in_=ident.t[:]), r=[ident], w=[identb])
                dg = [sbuf(pbs_, "dg%d" % i, [128, 128], BF16) for i in range(4)]
                sc2bc = sbuf(pbs_, "sc2bc", [128, 1024]); sh2bc = sbuf(pbs_, "sh2bc", [128, 1024]); g2bc = sbuf(pbs_, "g2bc", [128, 1024])
                if mode == "B":
                    OP("dve", lambda e: e.memset(sc2bc.t[:], 1.0), w=[sc2bc])
                    OP("dve", lambda e: e.memset(sh2bc.t[:], 0.0), w=[sh2bc])
                    OP("dve", lambda e: e.memset(g2bc.t[:], 1.0), w=[g2bc])
                else:
                    for b_, di in ((sh2bc, 0), (sc2bc, 1), (g2bc, 2)):
                        S.dma("sp", cst, lambda e, b_=b_, di=di: e.dma_start(out=b_.t[:], in_=d_modbc[di]), w=[b_])
                    S.barrier()
                x1t = [sbuf(pbs_, "x1t%d" % i, [128, 1024]) for i in range(3)]
                h2 = [sbuf(pbs_, "h2_%d" % i, [128, 1024]) for i in range(2)]
                h2T = sbuf(pbs_, "h2T", [128, 8, 128]); qTp = sbuf(pbs_, "qTp", [128, 8, 128])
                ssb = sbuf(pbs_, "ssb", [128, 16, 128]); sw = sbuf(pbs_, "sw", [128, 128])
                m16 = sbuf(pbs_, "m16", [128, 16, 16]); ix16 = sbuf(pbs_, "ix16", [128, 16, 16], U32)
                ixf = sbuf(pbs_, "ixf", [128, 16, 16])
                cand = sbuf(pbs_, "cand", [128, 8, 256]); cw = sbuf(pbs_, "cw", [128, 256])
                tsv = sbuf(pbs_, "tsv", [128, 8, 16]); pos = sbuf(pbs_, "pos", [128, 8, 16], U32)
                posf = sbuf(pbs_, "posf", [128, 8, 16])
                lohi = sbuf(pbs_, "lohi", [128, 2, 16])
                paf = sbuf(pbs_, "paf", [128, 8, 16]); pbf = sbuf(pbs_, "pbf", [128, 8, 16])
                oh = sbuf(pbs_, "oh", [128, 8, 16, 16])
                i1s = sbuf(pbs_, "i1s", [128, 8, 16]); i2s = sbuf(pbs_, "i2s", [128, 8, 16])
                eidf = sbuf(pbs_, "eidf", [128, 128])
                eidx = [sbuf(pbs_, "eidx%d" % i, [128, 128], U32) for i in range(3)]
                gate = [sbuf(pbs_, "gate%d" % i, [128, 8, 16]) for i in range(2)]
                gsum = sbuf(pbs_, "gsum", [128, 8])
                dots = [sbuf(pbs_, "dots%d" % i, [128, 128]) for i in range(2)]
                coef = [sbuf(pbs_, "coef%d" % i, [128, 128]) for i in range(2)]
                gb = [sbuf(pbs_, "gb%d" % i, [128, 2048], BF16) for i in range(NG)]
                gs = [S.dma_slot("g%d" % i) for i in range(NG)]
                prod = [sbuf(pbs_, "prod%d" % i, [128, 1024], BF16) for i in range(3)]
                h2b = [sbuf(pbs_, "h2b%d" % i, [128, 1024], BF16) for i in range(2)]
                acc = [sbuf(pbs_, "acc%d" % i, [128, 1024]) for i in range(2)]
                stats2 = sbuf(pbs_, "stats2", [128, 12]); mv2 = sbuf(pbs_, "mv2", [128, 2]); rs2 = sbuf(pbs_, "rs2", [128, 1])
                x1ld = [S.dma_slot("x1l%d" % i) for i in range(3)]

                def layernorm2(src, dst):
                    for hf in range(2):
                        OP("dve", lambda e, hf=hf: e.bn_stats(out=stats2.t[:, hf * 6:(hf + 1) * 6], in_=src.t[:, hf * 512:(hf + 1) * 512]),
                           r=[src], w=[stats2])
                    OP("dve", lambda e: e.bn_aggr(out=mv2.t[:], in_=stats2.t[:]), r=[stats2], w=[mv2])
                    OP("dve", lambda e: e.tensor_scalar(out=rs2.t[:], in0=mv2.t[:, 1:2], scalar1=eps_t.t[:, 0:1], scalar2=None, op0=ALU.add),
                       r=[mv2, eps_t], w=[rs2])
                    OP("act", lambda e: e.activation(out=rs2.t[:], in_=rs2.t[:], func=AF.Sqrt), r=[rs2], w=[rs2])
                    OP("dve", lambda e: e.reciprocal(out=rs2.t[:], in_=rs2.t[:]), r=[rs2], w=[rs2])
                    OP("dve", lambda e: e.tensor_scalar(out=dst.t[:], in0=src.t[:], scalar1=mv2.t[:, 0:1], scalar2=rs2.t[:, 0:1],
                                                        op0=ALU.subtract, op1=ALU.mult), r=[src, mv2, rs2], w=[dst])
                    OP("dve", lambda e: e.tensor_tensor(out=dst.t[:], in0=dst.t[:], in1=ln2g.t[:], op=ALU.mult), r=[dst, ln2g], w=[dst])
                    OP("dve", lambda e: e.tensor_tensor(out=dst.t[:], in0=dst.t[:], in1=ln2b.t[:], op=ALU.add), r=[dst, ln2b], w=[dst])

                def top16(src3, n, work, mdst, idst):
                    (sa, sbuf_), (wa, wbuf), (ma, mbuf), (ia, ibuf) = src3, work, mdst, idst
                    OP("dve", lambda e: e.max(out=ma[:, 0:8], in_=sa), r=[sbuf_], w=[mbuf])
                    OP("dve", lambda e: e.max_index(out=ia[:, 0:8], in_max=ma[:, 0:8], in_values=sa), r=[sbuf_, mbuf], w=[ibuf])
                    OP("dve", lambda e: e.match_replace(out=wa, in_to_replace=ma[:, 0:8], in_values=sa, imm_value=-1e30),
                       r=[sbuf_, mbuf], w=[wbuf])
                    OP("dve", lambda e: e.max(out=ma[:, 8:16], in_=wa), r=[wbuf], w=[mbuf])
                    OP("dve", lambda e: e.max_index(out=ia[:, 8:16], in_max=ma[:, 8:16], in_values=wa), r=[wbuf, mbuf], w=[ibuf])

                def route(i):
                    xs = x1t[i % 3]
                    S.dma("sp", x1ld[i % 3], lambda e: e.dma_start(out=xs.t[:], in_=d_x1[i * 128:(i + 1) * 128, :]), w=[xs])
                    hh = h2[i % 2]
                    OP("dve", lambda e: e.tensor_tensor(out=hh.t[:], in0=xs.t[:], in1=sc2bc.t[:], op=ALU.mult), r=[xs, sc2bc], w=[hh])
                    OP("dve", lambda e: e.tensor_tensor(out=hh.t[:], in0=hh.t[:], in1=sh2bc.t[:], op=ALU.add), r=[hh, sh2bc], w=[hh])
                    OP("act", lambda e: e.activation(out=h2b[i % 2].t[:], in_=hh.t[:], func=AF.Identity), r=[hh], w=[h2b[i % 2]])

                    if KSTOP < 2: return
                    def ev(c0, n, pb2):
                        OP("act", lambda e: e.activation(out=h2T.t[:, c0:c0 + n, :].rearrange("p a b -> p (a b)"),
                                                         in_=pb2.t[:, 0:n * 128], func=AF.Identity), r=[pb2], w=[h2T])
                    yield
                    transpose_to(hh, 8, ev)
                    yield
                    if KSTOP < 2.3: return
                    for j0 in range(0, 8, 4):
                        pq = bank()
                        for jj in range(j0, j0 + 4):
                            for kc in range(8):
                                OP("pe", lambda e, jj=jj, kc=kc, pq=pq, j0=j0: e.matmul(
                                    pq.t[:, (jj - j0) * 128:(jj - j0 + 1) * 128], lhsT=wq.t[:, kc, jj * 128:(jj + 1) * 128],
                                    rhs=h2T.t[:, kc, :], start=(kc == 0), stop=(kc == 7)), r=[wq, h2T], w=[pq])
                            yield
                        OP("act", lambda e, pq=pq, j0=j0: e.activation(out=qTp.t[:, j0:j0 + 4, :].rearrange("p a b -> p (a b)"),
                                                                       in_=pq.t[:], func=AF.Identity), r=[pq], w=[qTp])
                    if KSTOP < 2.6: return
                    for h0 in range(0, 8, 4):
                        for a in range(2):
                            psc = bank()
                            for h in range(h0, h0 + 4):
                                OP("pe", lambda e, h=h, a=a, psc=psc, h0=h0: e.matmul(
                                    psc.t[:, (h - h0) * 128:(h - h0 + 1) * 128], lhsT=qTp.t[a * 64:(a + 1) * 64, h, :],
                                    rhs=skT.t[a * 64:(a + 1) * 64, h, :], start=True, stop=True), r=[qTp, skT], w=[psc])
                            OP("act", lambda e, psc=psc, h0=h0, a=a: e.activation(
                                out=ssb.t[:, 2 * h0 + a:2 * h0 + 8:2, :], in_=psc.t[:].rearrange("p (a b) -> p a b", a=4),
                                func=AF.Identity), r=[psc], w=[ssb])
                            yield
                    if KSTOP < 3: return
                    for q_ in range(16):
                        top16((ssb.t[:, q_, :], ssb), 128, (sw.t[:], sw), (m16.t[:, q_, :], m16), (ix16.t[:, q_, :], ix16))
                        yield
                    if KSTOP < 4: return
                    OP("dve", lambda e: e.tensor_copy(out=ixf.t[:], in_=ix16.t[:]), r=[ix16], w=[ixf])
                    m4 = m16.t[:].rearrange("p (h a) k -> p h a k", a=2)
                    OP("dve", lambda e: e.tensor_tensor(
                        out=cand.t[:].rearrange("p h (a b) -> p h a b", a=16),
                        in0=m4[:, :, 0, :].unsqueeze(3).broadcast_to([128, 8, 16, 16]),
                        in1=m4[:, :, 1, :].unsqueeze(2).broadcast_to([128, 8, 16, 16]), op=ALU.add), r=[m16], w=[cand])
                    for h in range(8):
                        top16((cand.t[:, h, :], cand), 256, (cw.t[:], cw), (tsv.t[:, h, :], tsv), (pos.t[:, h, :], pos))
                        yield
                    gt = gate[i % 2]
                    OP("dve", lambda e: e.tensor_tensor(out=gt.t[:], in0=tsv.t[:], in1=tsv.t[:, :, 0:1].broadcast_to([128, 8, 16]),
                                                        op=ALU.subtract), r=[tsv], w=[gt])
                    OP("act", lambda e: e.activation(out=gt.t[:], in_=gt.t[:], func=AF.Exp), r=[gt], w=[gt])
                    OP("dve", lambda e: e.tensor_reduce(out=gsum.t[:], in_=gt.t[:], axis=AX.X, op=ALU.add), r=[gt], w=[gsum])
                    OP("dve", lambda e: e.reciprocal(out=gsum.t[:], in_=gsum.t[:]), r=[gsum], w=[gsum])
                    OP("dve", lambda e: e.tensor_tensor(out=gt.t[:], in0=gt.t[:], in1=gsum.t[:].unsqueeze(2).broadcast_to([128, 8, 16]),
                                                        op=ALU.mult), r=[gt, gsum], w=[gt])
                    if KSTOP < 5: return
                    yield
                    OP("dve", lambda e: e.tensor_copy(out=posf.t[:], in_=pos.t[:]), r=[pos], w=[posf])
                    oh2 = Buf(None); oh2.r = ssb.r
                    oh2.t = ssb.t[:].rearrange("p a b -> p (a b)").rearrange("p (h j c) -> p h j c", h=8, j=16)
                    ix4 = ixf.t[:].rearrange("p (h a) k -> p h a k", a=2)
                    bc4 = lambda ap2: ap2.unsqueeze(1).unsqueeze(1).broadcast_to([128, 8, 16, 16])
                    pf4 = posf.t[:].unsqueeze(3).broadcast_to([128, 8, 16, 16])
                    OP("dve", lambda e: e.tensor_tensor(out=oh.t[:], in0=pf4, in1=bc4(lohi.t[:, 0, :]), op=ALU.is_ge), r=[posf, lohi], w=[oh])
                    OP("dve", lambda e: e.tensor_tensor(out=oh2.t[:], in0=pf4, in1=bc4(lohi.t[:, 1, :]), op=ALU.is_ge), r=[posf, lohi], w=[oh2])
                    OP("dve", lambda e: e.tensor_tensor(out=oh.t[:], in0=oh.t[:], in1=oh2.t[:], op=ALU.subtract), r=[oh, oh2], w=[oh])
                    OP("dve", lambda e: e.tensor_tensor(out=oh2.t[:], in0=oh.t[:], in1=bc4(lohi.t[:, 0, :]), op=ALU.mult), r=[oh, lohi], w=[oh2])
                    yield
                    OP("dve", lambda e: e.tensor_reduce(out=paf.t[:], in_=oh2.t[:], axis=AX.X, op=ALU.add), r=[oh2], w=[paf])
                    OP("dve", lambda e: e.tensor_tensor(out=oh.t[:], in0=oh.t[:],
                                                        in1=ix4[:, :, 0, :].unsqueeze(2).broadcast_to([128, 8, 16, 16]), op=ALU.mult),
                       r=[oh, ixf], w=[oh])
                    OP("dve", lambda e: e.tensor_reduce(out=i1s.t[:], in_=oh.t[:], axis=AX.X, op=ALU.add), r=[oh], w=[i1s])
                    yield
                    OP("dve", lambda e: e.tensor_tensor(out=pbf.t[:], in0=posf.t[:], in1=paf.t[:], op=ALU.subtract), r=[posf, paf], w=[pbf])
                    OP("dve", lambda e: e.tensor_tensor(out=oh.t[:], in0=bc4(iota.t[:]),
                                                        in1=pbf.t[:].unsqueeze(3).broadcast_to([128, 8, 16, 16]), op=ALU.is_equal),
                       r=[iota, pbf], w=[oh])
                    OP("dve", lambda e: e.tensor_tensor(out=oh.t[:], in0=oh.t[:],
                                                        in1=ix4[:, :, 1, :].unsqueeze(2).broadcast_to([128, 8, 16, 16]), op=ALU.mult),
                       r=[oh, ixf], w=[oh])
                    OP("dve", lambda e: e.tensor_reduce(out=i2s.t[:], in_=oh.t[:], axis=AX.X, op=ALU.add), r=[oh], w=[i2s])
                    OP("dve", lambda e: e.scalar_tensor_tensor(out=eidf.t[:].rearrange("p (h k) -> p h k", h=8), in0=i1s.t[:], scalar=128.0,
                                                               in1=i2s.t[:], op0=ALU.mult, op1=ALU.add), r=[i1s, i2s], w=[eidf])
                    OP("dve", lambda e: e.tensor_copy(out=eidx[i % 3].t[:], in_=eidf.t[:]), r=[eidf], w=[eidx[i % 3]])

                gi = [0]

                class V:
                    def __init__(self, t):
                        self.t = t
                        self.r = Res()
                dcol = [[V(dots[p_].t) for _ in range(128)] for p_ in range(2)]
                ccol = [[V(coef[p_].t) for _ in range(128)] for p_ in range(2)]

                gk = {}

                def slot_u(i, s):
                    k = gi[0] % NG
                    gi[0] += 1
                    gk[(i, s)] = k
                    S.dma("pool", gs[k], lambda e: e.indirect_dma_start(
                        out=gb[k].t[:], out_offset=None, in_=d_puv16,
                        in_offset=bass.IndirectOffsetOnAxis(ap=eidx[i % 3].t[:, s:s + 1], axis=0)), r=[eidx[i % 3]] + puvB, w=[gb[k]])
                    pr = prod[s % 3]
                    dc, cc = dcol[i % 2][s], ccol[i % 2][s]
                    OP("dve", lambda e: e.tensor_tensor(out=pr.t[:], in0=gb[k].t[:, 0:1024], in1=h2b[i % 2].t[:], op=ALU.mult),
                       r=[gb[k], h2b[i % 2]], w=[pr])
                    OP("act", lambda e: e.activation(out=pr.t[:], in_=pr.t[:], func=AF.Identity, accum_out=dc.t[:, s:s + 1]),
                       r=[pr], w=[pr, dc])
                    OP("act", lambda e: e.activation(out=cc.t[:, s:s + 1], in_=dc.t[:, s:s + 1], func=AF.Gelu), r=[dc], w=[cc])

                def slot_v(i, s):
                    k = gk.pop((i, s))
                    cc = ccol[i % 2][s]
                    dgk = dg[s % 4]
                    OP("dve", lambda e: e.tensor_scalar(out=dgk.t[:], in0=identb.t[:], scalar1=cc.t[:, s:s + 1],
                                                        scalar2=gate[i % 2].t[:, s // 16, s % 16:s % 16 + 1], op0=ALU.mult, op1=ALU.mult),
                       r=[identb, cc, gate[i % 2]], w=[dgk])
                    for hf in range(2):
                        OP("pe", lambda e, hf=hf: e.matmul(accP[hf].t[:], lhsT=dgk.t[:], rhs=gb[k].t[:, 1024 + hf * 512:1536 + hf * 512],
                                                           start=(s == 0), stop=(s == 127)), r=[dgk, gb[k]], w=[accP[hf]])

                def finish_v(i):
                    r2 = acc[i % 2]; yo = acc[i % 2]
                    for hf in range(2):
                        OP("dve", lambda e, hf=hf: e.tensor_tensor(out=r2.t[:, hf * 512:(hf + 1) * 512], in0=accP[hf].t[:],
                                                                   in1=g2bc.t[:, hf * 512:(hf + 1) * 512], op=ALU.mult),
                           r=[accP[hf], g2bc], w=[r2])
                    OP("dve", lambda e: e.scalar_tensor_tensor(out=r2.t[:], in0=x1t[i % 3].t[:], scalar=ALPHA, in1=r2.t[:],
                                                               op0=ALU.mult, op1=ALU.add), r=[x1t[i % 3], r2], w=[r2])
                    layernorm2(r2, yo)
                    S.dma("sp", outs, lambda e: e.dma_start(out=d_out[i * 128:(i + 1) * 128, :], in_=yo.t[:]), r=[yo])

                OP("dve", lambda e: e.tensor_scalar(out=lohi.t[:, 0, :], in0=iota.t[:], scalar1=16.0, scalar2=None, op0=ALU.mult), r=[iota], w=[lohi])
                OP("dve", lambda e: e.tensor_scalar(out=lohi.t[:, 1, :], in0=iota.t[:], scalar1=16.0, scalar2=16.0, op0=ALU.mult, op1=ALU.add),
                   r=[iota], w=[lohi])
                NT = int(os.environ.get('K_NT', '16'))
                KSTOP = float(os.environ.get('K_STOP', '9'))
                for _ in route(0):
                    pass
                for i in range(NT):
                    gen = route(i + 1) if i + 1 < NT else iter(())
                    for s in range(128 + LAG if KSTOP >= 6 else 0):
                        if s < 128:
                            slot_u(i, s)
                        if s >= LAG:
                            slot_v(i, s - LAG)
                        next(gen, None)
                    for _ in gen:
                        pass
                    if KSTOP >= 7:
                        finish_v(i)
                S.barrier()
        S.barrier()
        S.run()
    return nc


def _consts():
    s = np.arange(128)[:, None]
    t = np.arange(128)[None, :]
    same = (s // 64) == (t // 64)
    g = -1.0 / 16.0
    tri = np.stack([(same & (s <= t)), (same & (s > t)), (same & (s >= t)), (same & (s < t))], axis=1).astype(np.float32) * g
    ci = np.stack([(np.arange(128) // 64 == c) for c in range(2)], axis=1).astype(np.float32) * g
    amask = np.stack([(s >= t), (s <= t)], axis=1).astype(np.float32)
    sc = (np.arange(128) % 64)[:, None]
    cc = np.arange(64)[None, :]
    gmask = np.stack([(sc <= cc), (sc >= cc)], axis=1).astype(np.float32)
    iota = np.broadcast_to(np.arange(16, dtype=np.float32), (128, 16)).copy()
    return dict(ident=np.eye(128, dtype=np.float32), tri=np.ascontiguousarray(tri), ci=np.ascontiguousarray(ci),
                amask=np.ascontiguousarray(amask), gmask=np.ascontiguousarray(gmask), iota16=iota)


def _rope_tables():
    rows = 8192 // 64
    row = np.repeat(np.arange(rows, dtype=np.float32), 64)
    col = np.tile(np.arange(64, dtype=np.float32), rows)
    inv = (np.float32(10000.0) ** (-np.arange(16, dtype=np.float32) / np.float32(16))).astype(np.float32)
    ang = np.stack([row[:, None] * inv, col[:, None] * inv], axis=1).astype(np.float32)
    return np.cos(ang).reshape(8192, 32).astype(np.float32), np.sin(ang).reshape(8192, 32).astype(np.float32)


def make_in_maps(x, c, ctx, c_ctx, w_ada, b_ada, w_in, w_gate2_f, b_gate_f, w_gate2_b, b_gate_b, gla_norm_g, attn_sink,
                 w_out, ln1_g, ln1_b, peer_wq, peer_subkeys, peer_u, peer_v, ln2_g, ln2_b):
    f = lambda a: np.ascontiguousarray(np.asarray(a, dtype=np.float32))
    bc = lambda v, n: np.ascontiguousarray(np.broadcast_to(np.asarray(v, np.float32).reshape(1, -1), (128, n)))
    x = f(x); ctx = f(ctx)
    wi = f(w_in[0])
    wperm = np.concatenate([wi[:, 0:256], wi[:, 256:512], wi[:, 512:1024], wi[:, 1024:1536], wi[:, 1568:2080],
                            wi[:, 2080:2208], wi[:, 2208:2336], wi[:, 1536:1552], wi[:, 1552:1568]], axis=1)
    cosT, sinT = _rope_tables()
    common = dict(
        w_ada=f(w_ada[0]), b_ada=f(b_ada[0]).reshape(1, -1), w_in=np.ascontiguousarray(wperm),
        w2=np.ascontiguousarray(np.concatenate([f(w_gate2_f[0]), f(w_gate2_b[0])], axis=1)),
        bg=np.ascontiguousarray(np.concatenate([f(b_gate_f[0]), f(b_gate_b[0])]).reshape(1, -1)),
        gng=bc(gla_norm_g[0], 128), sink=bc(attn_sink[0], 8), w_out=f(w_out[0]),
        ln1g=bc(ln1_g[0], 1024), ln1b=bc(ln1_b[0], 1024), ln2g=bc(ln2_g[0], 1024), ln2b=bc(ln2_b[0], 1024),
        peer_wq=f(peer_wq[0]),
        skT=np.ascontiguousarray(np.transpose(f(peer_subkeys[0]), (1, 3, 0, 2)).reshape(128, 8, 128)),
        peer_uv=np.ascontiguousarray(np.concatenate([f(peer_u[0]), f(peer_v[0])], axis=1)), **_consts())
    maps = []
    zt = np.zeros((128, 1024), np.float32)
    for core in range(8):
        b, s = core // 4, core % 4
        xb = x[b].reshape(64, 128, 1024)
        npf = 16 * s
        xpf = np.zeros((NPRE, 128, 1024), np.float32)
        if npf:
            xpf[NPRE - npf:] = xb[0:npf]
        npb = 16 * (3 - s)
        xpb = np.zeros((NPRE, 128, 1024), np.float32)
        if npb:
            xpb[NPRE - npb:] = xb[63:16 * (s + 1) - 1:-1]
        flags = np.zeros((128, 100), np.float32)
        flags[:, NPRE - npf:NPRE] = 1.0
        flags[:, 48 + NPRE - npb:48 + NPRE] = 1.0
        t0 = 16 * s
        own = np.zeros((18, 128, 1024), np.float32)
        own[1:17] = xb[t0:t0 + 16]
        cos_o = np.zeros((18, 128, 32), np.float32); sin_o = np.zeros((18, 128, 32), np.float32)
        cos_o[1:17] = cosT.reshape(64, 128, 32)[t0:t0 + 16]; sin_o[1:17] = sinT.reshape(64, 128, 32)[t0:t0 + 16]
        if t0 > 0:
            own[0] = xb[t0 - 1]; flags[:, 96] = 1.0
            cos_o[0] = cosT.reshape(64, 128, 32)[t0 - 1]; sin_o[0] = sinT.reshape(64, 128, 32)[t0 - 1]
        if t0 + 16 < 64:
            own[17] = xb[t0 + 16]; flags[:, 97] = 1.0
            cos_o[17] = cosT.reshape(64, 128, 32)[t0 + 16]; sin_o[17] = sinT.reshape(64, 128, 32)[t0 + 16]
        c2 = np.stack([f(c)[b], f(c_ctx)], axis=0)
        c2T = np.ascontiguousarray(c2.reshape(2, 8, 128).transpose(2, 1, 0))
        m = dict(common)
        m.update(xpre_f=xpf.reshape(-1, 1024), xpre_b=xpb.reshape(-1, 1024), xown=own.reshape(-1, 1024), ctx=ctx[b],
                 flags=flags, c2T=c2T, cosT=cos_o.reshape(-1, 32), sinT=sin_o.reshape(-1, 32))
        maps.append(m)
    return maps


_NC_CACHE = {}


def kernel(**inputs):
    if "full" not in _NC_CACHE:
        _NC_CACHE["full"] = build_nc("full")
    nc = _NC_CACHE["full"]
    maps = make_in_maps(**inputs)
    res = run_bass_kernel_spmd(nc, maps, core_ids=list(range(8)))
    out = np.zeros((2, 8192, 1024), np.float32)
    for core in range(8):
        b, s = core // 4, core % 4
        out[b, s * 2048:(s + 1) * 2048] = np.asarray(res.results[core]["out"], np.float32)
    return out
```
